# Optimizing a Trainium2 kernel written in Bass

```python
import math
import jax, jax.numpy as jnp
from jax import lax
import numpy as np


D_MODEL = 1024
BATCH = 4
SEQ = 4096
DEPTH = 2

GRID_W = 64
CTX_LEN = 256
N_MIXERS = 2
EPS = 1e-6
D_RNN = 1024
RG_BLOCKS = 8
RG_BLOCK = D_RNN // RG_BLOCKS
CONV_W = 4
CONV_LEFT = 2
RG_C = 8.0
DA_HEADS = 8
DA_HEAD_DIM = D_MODEL // DA_HEADS // 2
ROPE_THETA = 10000.0
Q_BLOCK = 128
N_EXPERTS = 32
N_GROUPS = 4
EXPERTS_PER_GROUP = N_EXPERTS // N_GROUPS
TOP_K = 2
D_EXPERT = 512
MOE_BLOCK = 128

kernel_name = 'hybrid_rglru_diffattn_groupmoe_block'


def rms_norm(x, g):
    xf = x.astype(jnp.float32)
    y = xf * lax.rsqrt(jnp.mean(xf * xf, axis=-1, keepdims=True) + EPS)
    return y.astype(x.dtype) * g


def modulate(h, shift, scale):
    return h * (1.0 + scale) + shift


def adaln_chunks(cond, w_mod, b_mod):
    return jnp.split(jax.nn.silu(cond) @ w_mod + b_mod, 6, axis=-1)


def centred_dwconv(x, w, b):
    T = x.shape[1]
    xp = jnp.pad(x, ((0, 0), (CONV_LEFT, CONV_W - 1 - CONV_LEFT), (0, 0)))
    y = xp[:, 0:T] * w[0]
    for k in range(1, CONV_W):
        y = y + xp[:, k:k + T] * w[k]
    return y + b


def rglru_coeffs(u, wa, ba, wi, bi, lam):
    B_, T, _ = u.shape
    ur = u.reshape(B_, T, RG_BLOCKS, RG_BLOCK)
    r = jax.nn.sigmoid(jnp.einsum('btnk,nkj->btnj', ur, wa).reshape(B_, T, D_RNN) + ba)
    i = jax.nn.sigmoid(jnp.einsum('btnk,nkj->btnj', ur, wi).reshape(B_, T, D_RNN) + bi)
    log_a = -RG_C * r.astype(jnp.float32) * jax.nn.softplus(-lam.astype(jnp.float32))
    a = jnp.exp(log_a)
    b = jnp.sqrt(-jnp.expm1(2.0 * log_a)) * (i * u).astype(jnp.float32)
    return a, b


def _combine(left, right):
    a1, b1 = left
    a2, b2 = right
    return a1 * a2, a2 * b1 + b2


def linear_scan(a, b, h0, reverse):
    A, H = lax.associative_scan(_combine, (a, b), axis=1, reverse=reverse)
    if h0 is None:
        return H
    return H + A * h0[:, None, :]


def rglru_mixer(hx, hc, w_in, conv_w, conv_b, ga_w, ga_b, gx_w, gx_b, lam, w_out, need_ctx):
    def branches(h):
        y, u = jnp.split(h @ w_in, 2, axis=-1)
        return jax.nn.gelu(y), centred_dwconv(u, conv_w, conv_b)
    yx, ux = branches(hx)
    yc, uc = branches(hc)
    hs_x = None
    hs_c = None
    for d in range(2):
        rev = d == 1
        a_c, b_c = rglru_coeffs(uc, ga_w[d], ga_b[d], gx_w[d], gx_b[d], lam[d])
        h_c = linear_scan(a_c, b_c, None, rev)
        h0 = h_c[:, 0] if rev else h_c[:, -1]
        a_x, b_x = rglru_coeffs(ux, ga_w[d], ga_b[d], gx_w[d], gx_b[d], lam[d])
        h_x = linear_scan(a_x, b_x, h0, rev)
        hs_x = h_x if hs_x is None else hs_x + h_x
        hs_c = h_c if hs_c is None else hs_c + h_c
    out_x = (hs_x.astype(hx.dtype) * yx) @ w_out
    out_c = (hs_c.astype(hc.dtype) * yc) @ w_out if need_ctx else None
    return out_x, out_c


def rope_tables(n_tokens):
    n_rows = n_tokens // GRID_W
    row = jnp.repeat(jnp.arange(n_rows), GRID_W).astype(jnp.float32)
    col = jnp.tile(jnp.arange(GRID_W), n_rows).astype(jnp.float32)
    n_freq = DA_HEAD_DIM // 4
    inv = 1.0 / (ROPE_THETA ** (jnp.arange(n_freq, dtype=jnp.float32) / n_freq))
    ang = jnp.stack([row, col], axis=-1)[:, :, None] * inv
    ang = jnp.broadcast_to(ang[:, :, None, :], (n_tokens, 2, 2, n_freq)).reshape(n_tokens, DA_HEAD_DIM)
    return jnp.cos(ang), jnp.sin(ang)


def apply_rope(x, cos, sin):
    xr = x.reshape(x.shape[:-1] + (2, 2, DA_HEAD_DIM // 4))
    rot = jnp.concatenate([-xr[..., 1:2, :], xr[..., 0:1, :]], axis=-2).reshape(x.shape)
    c = cos[:, None, None, :].astype(x.dtype)
    s = sin[:, None, None, :].astype(x.dtype)
    return x * c + rot * s


def diff_softmax_attend(q, k, v, lam, subln_g, lambda_init):
    s = jnp.einsum('bhiqd,bhikd->bhiqk', q, k).astype(jnp.float32)
    p = jax.nn.softmax(s, axis=-1)
    w = p[:, :, 0] - lam * p[:, :, 1]
    o = jnp.einsum('bhqk,bhkv->bhqv', w.astype(v.dtype), v)
    return rms_norm(o, subln_g) * (1.0 - lambda_init)


def diff_attn_mixer(hx, hc, w_qkv, lam_vecs, subln_g, w_o, lambda_init, need_ctx):
    B_, S, _ = hx.shape
    C = hc.shape[1]

    def project(h):
        T = h.shape[1]
        q, k, v = jnp.split(h @ w_qkv, 3, axis=-1)
        return (q.reshape(B_, T, DA_HEADS, 2, DA_HEAD_DIM),
                k.reshape(B_, T, DA_HEADS, 2, DA_HEAD_DIM),
                v.reshape(B_, T, DA_HEADS, 2 * DA_HEAD_DIM))

    qx, kx, vx = project(hx)
    qc, kc, vc = project(hc)
    cos, sin = rope_tables(S)
    qx = apply_rope(qx, cos, sin)
    kx = apply_rope(kx, cos, sin)
    scale = DA_HEAD_DIM ** -0.5
    qk_heads = lambda t: jnp.transpose(t, (0, 2, 3, 1, 4))
    v_heads = lambda t: jnp.transpose(t, (0, 2, 1, 3))
    lf = lam_vecs.astype(jnp.float32)
    lam = jnp.exp(jnp.sum(lf[0] * lf[1])) - jnp.exp(jnp.sum(lf[2] * lf[3])) + lambda_init
    kc_h = qk_heads(kc)
    vc_h = v_heads(vc)
    k_all = jnp.concatenate([qk_heads(kx), kc_h], axis=3)
    v_all = jnp.concatenate([v_heads(vx), vc_h], axis=2)
    qx_h = qk_heads(qx) * scale
    nb = S // Q_BLOCK
    q_blocks = jnp.moveaxis(qx_h.reshape(B_, DA_HEADS, 2, nb, Q_BLOCK, DA_HEAD_DIM), 3, 0)
    o = lax.map(lambda qb: diff_softmax_attend(qb, k_all, v_all, lam, subln_g, lambda_init), q_blocks)
    o = jnp.transpose(o, (1, 0, 3, 2, 4)).reshape(B_, S, D_MODEL)
    out_x = o @ w_o
    out_c = None
    if need_ctx:
        oc = diff_softmax_attend(qk_heads(qc) * scale, kc_h, vc_h, lam, subln_g, lambda_init)
        out_c = jnp.transpose(oc, (0, 2, 1, 3)).reshape(B_, C, D_MODEL) @ w_o
    return out_x, out_c


def moe_ffn(h, router_w, router_bias, w_gate, w_up, w_down):
    N, D = h.shape
    s = jax.nn.sigmoid((h @ router_w).astype(jnp.float32))
    s_sel = s + router_bias.astype(jnp.float32)
    grp_score = lax.top_k(s_sel.reshape(N, N_GROUPS, EXPERTS_PER_GROUP), 2)[0].sum(-1)
    grp = jnp.argmax(grp_score, axis=-1)
    in_grp = (jnp.arange(N_EXPERTS) // EXPERTS_PER_GROUP)[None, :] == grp[:, None]
    _, idx = lax.top_k(jnp.where(in_grp, s_sel, -jnp.inf), TOP_K)
    wts = jnp.take_along_axis(s, idx, axis=-1)
    wts = wts / jnp.sum(wts, axis=-1, keepdims=True)
    e_flat = idx.reshape(-1)
    t_flat = jnp.repeat(jnp.arange(N), TOP_K)
    w_flat = wts.reshape(-1)
    order = jnp.argsort(e_flat)
    e_s, t_s, w_s = e_flat[order], t_flat[order], w_flat[order]
    counts = jnp.bincount(e_flat, length=N_EXPERTS)
    starts = jnp.cumsum(counts) - counts
    padded = ((counts + MOE_BLOCK - 1) // MOE_BLOCK) * MOE_BLOCK
    p_ends = jnp.cumsum(padded)
    p_starts = p_ends - padded
    dest = p_starts[e_s] + jnp.arange(N * TOP_K) - starts[e_s]
    n_blocks = -(-(N * TOP_K) // MOE_BLOCK) + N_EXPERTS
    P = n_blocks * MOE_BLOCK
    buf_tok = jnp.zeros((P,), jnp.int32).at[dest].set(t_s.astype(jnp.int32))
    buf_w = jnp.zeros((P,), jnp.float32).at[dest].set(w_s)
    blk_exp = jnp.minimum(jnp.searchsorted(p_ends, jnp.arange(n_blocks) * MOE_BLOCK, side='right'), N_EXPERTS - 1)
    xb = h[buf_tok].reshape(n_blocks, MOE_BLOCK, D)

    def expert_block(args):
        xblk, e = args
        return (jax.nn.silu(xblk @ w_gate[e]) * (xblk @ w_up[e])) @ w_down[e]

    yb = lax.map(expert_block, (xb, blk_exp)).reshape(P, D)
    return jnp.zeros_like(h).at[buf_tok].add(yb * buf_w[:, None].astype(yb.dtype))


def setup_inputs(seed: int = 0) -> dict:
    key = jax.random.key(seed)
    ks = iter(jax.random.split(key, 40))
    f32 = jnp.float32
    nrm = lambda shape, sc: jax.random.normal(next(ks), shape, f32) * sc
    n_rg = (DEPTH + N_MIXERS - 1) // N_MIXERS
    n_da = DEPTH // N_MIXERS
    D = D_MODEL
    a8 = jax.random.uniform(next(ks), (n_rg, 2, D_RNN), f32, 0.9, 0.999)
    p = a8 ** (1.0 / RG_C)
    rg_lambda = jnp.log(p) - jnp.log1p(-p)
    return {
        'x': nrm((BATCH, SEQ, D), 1.0),
        'c': nrm((BATCH, D), 1.0),
        'ctx': nrm((BATCH, CTX_LEN, D), 1.0),
        'c_ctx': nrm((D,), 1.0),
        'w_mod': nrm((DEPTH, D, 6 * D), 0.5 * D ** -0.5),
        'b_mod': nrm((DEPTH, 6 * D), 0.02),
        'norm1_g': 1.0 + nrm((DEPTH, D), 0.05),
        'norm2_g': 1.0 + nrm((DEPTH, D), 0.05),
        'rg_w_in': nrm((n_rg, D, 2 * D_RNN), D ** -0.5),
        'rg_conv_w': nrm((n_rg, CONV_W, D_RNN), CONV_W ** -0.5),
        'rg_conv_b': nrm((n_rg, D_RNN), 0.02),
        'rg_gate_a_w': nrm((n_rg, 2, RG_BLOCKS, RG_BLOCK, RG_BLOCK), RG_BLOCK ** -0.5),
        'rg_gate_a_b': nrm((n_rg, 2, D_RNN), 0.02),
        'rg_gate_x_w': nrm((n_rg, 2, RG_BLOCKS, RG_BLOCK, RG_BLOCK), RG_BLOCK ** -0.5),
        'rg_gate_x_b': nrm((n_rg, 2, D_RNN), 0.02),
        'rg_lambda': rg_lambda,
        'rg_w_out': nrm((n_rg, D_RNN, D), D_RNN ** -0.5),
        'da_w_qkv': nrm((n_da, D, 3 * D), D ** -0.5),
        'da_lambda': nrm((n_da, 4, DA_HEAD_DIM), 0.1),
        'da_subln_g': 1.0 + nrm((n_da, 2 * DA_HEAD_DIM), 0.05),
        'da_w_o': nrm((n_da, D, D), D ** -0.5),
        'router_w': nrm((D, N_EXPERTS), D ** -0.5),
        'router_bias': nrm((N_EXPERTS,), 0.01),
        'moe_w_gate': nrm((DEPTH, N_EXPERTS, D, D_EXPERT), D ** -0.5),
        'moe_w_up': nrm((DEPTH, N_EXPERTS, D, D_EXPERT), D ** -0.5),
        'moe_w_down': nrm((DEPTH, N_EXPERTS, D_EXPERT, D), D_EXPERT ** -0.5),
        'final_g': 1.0 + nrm((D,), 0.05),
    }


def reference(x, c, ctx, c_ctx, w_mod, b_mod, norm1_g, norm2_g, rg_w_in, rg_conv_w, rg_conv_b,
              rg_gate_a_w, rg_gate_a_b, rg_gate_x_w, rg_gate_x_b, rg_lambda, rg_w_out,
              da_w_qkv, da_lambda, da_subln_g, da_w_o, router_w, router_bias,
              moe_w_gate, moe_w_up, moe_w_down, final_g):
    B_, S, D = x.shape
    C = ctx.shape[1]
    cond_x = c[:, None, :]
    cond_c = c_ctx[None, None, :]
    i_rg = 0
    i_da = 0
    for layer in range(DEPTH):
        need_ctx = layer < DEPTH - 1
        shx1, scx1, gx1, shx2, scx2, gx2 = adaln_chunks(cond_x, w_mod[layer], b_mod[layer])
        shc1, scc1, gc1, shc2, scc2, gc2 = adaln_chunks(cond_c, w_mod[layer], b_mod[layer])
        hx = modulate(rms_norm(x, norm1_g[layer]), shx1, scx1)
        hc = modulate(rms_norm(ctx, norm1_g[layer]), shc1, scc1)
        if layer % N_MIXERS == 0:
            ox, oc = rglru_mixer(hx, hc, rg_w_in[i_rg], rg_conv_w[i_rg], rg_conv_b[i_rg],
                                 rg_gate_a_w[i_rg], rg_gate_a_b[i_rg], rg_gate_x_w[i_rg],
                                 rg_gate_x_b[i_rg], rg_lambda[i_rg], rg_w_out[i_rg], need_ctx)
            i_rg += 1
        else:
            lambda_init = 0.8 - 0.6 * math.exp(-0.3 * layer)
            ox, oc = diff_attn_mixer(hx, hc, da_w_qkv[i_da], da_lambda[i_da], da_subln_g[i_da],
                                     da_w_o[i_da], lambda_init, need_ctx)
            i_da += 1
        x = x + gx1 * ox
        hx2 = modulate(rms_norm(x, norm2_g[layer]), shx2, scx2)
        if need_ctx:
            ctx = ctx + gc1 * oc
            hc2 = modulate(rms_norm(ctx, norm2_g[layer]), shc2, scc2)
            tokens = jnp.concatenate([hx2.reshape(-1, D), hc2.reshape(-1, D)], axis=0)
            y = moe_ffn(tokens, router_w, router_bias, moe_w_gate[layer], moe_w_up[layer], moe_w_down[layer])
            x = x + gx2 * y[:B_ * S].reshape(B_, S, D)
            ctx = ctx + gc2 * y[B_ * S:].reshape(B_, C, D)
        else:
            y = moe_ffn(hx2.reshape(-1, D), router_w, router_bias, moe_w_gate[layer], moe_w_up[layer], moe_w_down[layer])
            x = x + gx2 * y.reshape(B_, S, D)
    return rms_norm(x, final_g)
```

```python
import contextlib
import math
import numpy as np
import concourse.bass as bass
import concourse.mybir as mybir
from concourse.bass_utils import run_bass_kernel_spmd

F32 = mybir.dt.float32
BF16 = mybir.dt.bfloat16
ALU = mybir.AluOpType
AF = mybir.ActivationFunctionType
AX = mybir.AxisListType

EPS = 1e-6
NT = 4352
NX = 4096
NCTX = 256
TILES = [(i * 512, 512, 2 + i * 512, i * 512, 0) for i in range(8)] + [(4096, 256, 4102, 4100, 1)]
UW = 4360
UCW = 4356
NQ = 2048
LAMBDA_INIT = 0.8 - 0.6 * math.exp(-0.3 * 1)
DEBUG = False


class Sched:
    ROT = 30000

    def __init__(self, nc, stack):
        self.nc = nc
        self.stack = stack
        self.engs = {'pe': nc.tensor, 'act': nc.scalar, 'dve': nc.vector,
                     'pool': nc.gpsimd, 'sp': nc.sync}
        self.cur = {}
        self.nsem = 0
        for e in self.engs:
            self.cur[e] = [self._newsem(e), 0]
        self.lastw = {}
        self.readers = {}
        self.waited = {e: {} for e in self.engs}
        self.dsem = {}
        self.alltok = {}

    def _newsem(self, nm):
        self.nsem += 1
        return self.stack.enter_context(self.nc.semaphore(f"s_{nm}_{self.nsem}"))

    def _wait(self, eng, toks):
        best = {}
        for (s, v) in toks:
            k = id(s)
            if k not in best or best[k][1] < v:
                best[k] = (s, v)
        for k, (s, v) in best.items():
            if self.waited[eng].get(k, 0) >= v:
                continue
            self.engs[eng].wait_ge(s, v)
            self.waited[eng][k] = v

    def _deps(self, eng, reads, writes, skip_same_pe=True):
        toks = []
        for k in reads:
            if k in self.lastw:
                toks.append(self.lastw[k])
        for k in writes:
            if k in self.lastw:
                toks.append(self.lastw[k])
            toks.extend(self.readers.get(k, ()))
        if eng == 'pe' and skip_same_pe:
            toks = [t for t in toks if t[0] is not self.cur['pe'][0]]
        return toks

    def _commit(self, tok, reads, writes):
        for k in writes:
            self.lastw[k] = tok
            self.readers[k] = []
        for k in reads:
            if k in writes:
                continue
            self.readers.setdefault(k, []).append(tok)
        self.alltok[id(tok[0])] = tok

    def op(self, eng, fn, reads=(), writes=()):
        self._wait(eng, self._deps(eng, reads, writes))
        c = self.cur[eng]
        if c[1] >= self.ROT:
            c[0] = self._newsem(eng)
            c[1] = 0
        ins = fn(self.engs[eng])
        c[1] += 1
        ins.then_inc(c[0], 1)
        self._commit((c[0], c[1]), reads, writes)

    def dma(self, eng, out, in_, reads=(), writes=(), slot=None, **kw):
        if slot is None:
            slot = ('auto',) + tuple(writes)
        self._wait(eng, self._deps(eng, reads, writes, skip_same_pe=False))
        if slot not in self.dsem:
            self.dsem[slot] = [self._newsem('d'), 0]
        d = self.dsem[slot]
        ins = self.engs[eng].dma_start(out=out, in_=in_, **kw)
        d[1] += 16
        ins.then_inc(d[0], 16)
        self._commit((d[0], d[1]), reads, writes)

    def barrier(self):
        toks = list(self.alltok.values())
        for e in self.engs:
            self._wait(e, toks)


def bc_last(a, n):
    return bass.AP(a.tensor, a.offset, [list(x) for x in a.ap] + [[0, n]])


def bc_mid(a, n):
    l = [list(x) for x in a.ap]
    return bass.AP(a.tensor, a.offset, [l[0], [0, n]] + l[1:])


def build(dbg=False):
    nc = bass.Bass("TRN2", target_bir_lowering=False)

    def din(name, shape, dt=F32):
        return nc.dram_tensor(name, list(shape), dt, kind="ExternalInput").ap()

    xc = din("xc", [8, 128, NT])
    cond_in = din("cond", [128, 8, 2])
    w_mod = din("w_mod", [2, 1024, 6144])
    bmod_in = din("bmod", [128, 2, 48])
    n1g_in = din("n1g", [128, 2, 8])
    n2g_in = din("n2g", [128, 2, 8])
    fing_in = din("fing", [128, 8])
    w_in = din("w_in", [1024, 2048])
    convw_in = din("convw", [128, 8, 5])
    convb_in = din("convb", [128, 8])
    gaw = din("gaw", [2, 8, 128, 128])
    gxw = din("gxw", [2, 8, 128, 128])
    gab_in = din("gab", [128, 2, 8])
    gxb_in = din("gxb", [128, 2, 8])
    lam_in = din("lam", [128, 2, 8])
    w_out = din("w_out", [1024, 1024])
    w_qkv = din("w_qkv", [1024, 3072])
    dalam_in = din("dalam", [128, 4, 64])
    subg_in = din("subg", [128, 1])
    w_o = din("w_o", [1024, 1024])
    rw_in = din("rw", [128, 8, 32])
    rb_in = din("rb", [128, 32])
    wg = din("wg", [2, 32, 1024, 512])
    wu = din("wu", [2, 32, 1024, 512])
    wd = din("wd", [2, 32, 512, 1024])
    rmat_in = din("rmat", [128, 128])
    cos_in = din("cos", [128, NT])
    sin_in = din("sin", [128, NT])
    outT = nc.dram_tensor("outT", [8, 128, NQ], F32, kind="ExternalOutput").ap()
    xr = nc.dram_tensor("xr", [8, 128, NT], F32, kind="Internal").ap()
    zscr = nc.dram_tensor("zscr", [8, 128, NT], BF16, kind="Internal").ap()
    h2tm = nc.dram_tensor("h2tm", [NT, 1024], BF16, kind="Internal").ap()
    Xs = nc.dram_tensor("Xs", [100 * 128, 1024], BF16, kind="Internal").ap()
    Ys = nc.dram_tensor("Ys", [100 * 128, 1024], F32, kind="Internal").ap()
    wgb = nc.dram_tensor("wgb", [2 * 8192, 2048], BF16, kind="Internal").ap()
    wub = nc.dram_tensor("wub", [2 * 8192, 2048], BF16, kind="Internal").ap()
    wdb = nc.dram_tensor("wdb", [2 * 8192, 2048], BF16, kind="Internal").ap()
    pcol_in = din("pcol", [128, 1])
    blkoff_in = din("blkoff", [128, 100])
    dbgo = {}
    if dbg:
        for nm, shp in [("d_mod", [128, 2 * 48 * 2]), ("d_h", [128, 8, NT]), ("d_z", [8, 128, NT]),
                        ("d_x1", [8, 128, NT]), ("d_x2", [8, 128, NT]),
                        ("d_ao", [8, 128, NT])]:
            dbgo[nm] = nc.dram_tensor(nm, shp, F32, kind="ExternalOutput").ap()

    with contextlib.ExitStack() as st:
        S = Sched(nc, st)

        uid = [0]

        def sb(stack, name, shape, dt):
            uid[0] += 1
            return stack.enter_context(nc.sbuf_tensor(f"sb{uid[0]}_{name}", list(shape), dt))

        psbig = st.enter_context(nc.psum_tensor("psbig", [128, 8 * 512], F32))
        PS = [psbig[:, i * 512:(i + 1) * 512] for i in range(8)]
        pk = lambda i: ('ps', i)
        wgv_all = wg.rearrange("l e (q r) f -> (l e q) (r f)", r=4)
        wuv_all = wu.rearrange("l e (q r) f -> (l e q) (r f)", r=4)
        wdv_all = wd.rearrange("l e (q r) d -> (l e q) (r d)", r=2)

        def convert_experts(l, e0, e1):
            for e_ in range(e0, e1):
                r0 = (l * 32 + e_) * 256
                for dst, src in [(wgb, wgv_all), (wub, wuv_all), (wdb, wdv_all)]:
                    S.dma('pool', dst[r0:r0 + 256, :], src[r0:r0 + 256, :], writes=[('wcv', l)])

        ones_bf = sb(st, "ones_bf", [128, 128], BF16)
        ones32 = sb(st, "ones32", [128, 128], F32)
        ident = sb(st, "ident", [128, 128], F32)
        identb = sb(st, "identb", [128, 128], BF16)
        modT = sb(st, "modT", [128, 2, 48, 2], F32)
        gmT = sb(st, "gmT", [128, 2, 2, 8, 2], F32)
        n1g = sb(st, "n1g", [128, 2, 8], F32)
        n2g = sb(st, "n2g", [128, 2, 8], F32)
        fing = sb(st, "fing", [128, 8], F32)
        rw = sb(st, "rw", [128, 8, 32], F32)
        rb = sb(st, "rb", [128, 32], F32)
        S.op('dve', lambda e: e.memset(ones_bf[:], 1.0), writes=['ones_bf'])
        S.op('dve', lambda e: e.memset(ones32[:], 1.0), writes=['ones32'])
        S.op('pool', lambda e: e.memset(ident[:], 1.0), writes=['ident'])
        S.op('pool', lambda e: e.affine_select(out=ident[:], in_=ident[:], pattern=[[-1, 128]],
                                               compare_op=ALU.is_equal, fill=0.0, base=0, channel_multiplier=1),
             reads=['ident'], writes=['ident'])
        S.op('act', lambda e: e.activation(out=identb[:], in_=ident[:], func=AF.Identity), reads=['ident'], writes=['identb'])
        for t, src, k in [(n1g, n1g_in, 'n1g'), (n2g, n2g_in, 'n2g'), (fing, fing_in, 'fing'),
                          (rw, rw_in, 'rw'), (rb, rb_in, 'rb')]:
            S.dma('sp', t[:], src, writes=[k])

        with contextlib.ExitStack() as ph:
            condt = sb(ph, "condt", [128, 8, 2], F32)
            scond = sb(ph, "scond", [128, 8, 2], F32)
            bm = sb(ph, "bm", [128, 2, 48], F32)
            wm = [sb(ph, f"wm{i}", [128, 8, 1024], F32) for i in range(2)]
            S.dma('sp', condt[:], cond_in, writes=['cond'])
            S.dma('sp', bm[:], bmod_in, writes=['bm'])
            S.op('act', lambda e: e.activation(out=scond[:], in_=condt[:], func=AF.Silu),
                 reads=['cond'], writes=['scond'])
            it = 0
            for l in range(2):
                for gi in range(6):
                    buf = wm[it % 2]
                    key = ('wm', it % 2)
                    S.dma('sp', buf[:], w_mod[l, :, gi * 1024:(gi + 1) * 1024].rearrange("(k p) f -> p k f", p=128),
                          writes=[key])
                    pst = PS[it % 2]

                    def mm(e, buf=buf, pst=pst):
                        for j in range(8):
                            for kc in range(8):
                                ins = e.matmul(pst[:, j * 2:(j + 1) * 2], lhsT=buf[:, kc, j * 128:(j + 1) * 128],
                                               rhs=scond[:, kc, :], start=(kc == 0), stop=(kc == 7))
                        return ins
                    S.op('pe', mm, reads=[key, 'scond'], writes=[pk(it % 2)])
                    S.op('dve', lambda e: e.tensor_tensor(
                        out=modT[:, l, gi * 8:(gi + 1) * 8, :],
                        in0=pst[:, 0:16].rearrange("p (j r) -> p j r", r=2),
                        in1=bc_last(bm[:, l, gi * 8:(gi + 1) * 8], 2), op=ALU.add),
                        reads=[pk(it % 2), 'bm'], writes=['modT'])
                    it += 1
            for l in range(2):
                for w_, (gt, gk, sidx) in enumerate([(n1g, 'n1g', 1), (n2g, 'n2g', 4)]):
                    S.op('dve', lambda e: e.tensor_scalar(out=gmT[:, l, w_], in0=modT[:, l, sidx * 8:(sidx + 1) * 8, :],
                                                          scalar1=1.0, scalar2=None, op0=ALU.add),
                         reads=['modT'], writes=['gmT'])
                    S.op('dve', lambda e: e.tensor_tensor(out=gmT[:, l, w_], in0=gmT[:, l, w_],
                                                          in1=bc_last(gt[:, l, :], 2), op=ALU.mult),
                         reads=['gmT', gk], writes=['gmT'])
            if dbg:
                S.dma('sp', dbgo["d_mod"], modT[:].rearrange("p a b c -> p (a b c)"), reads=['modT'], writes=['d_mod'])
            S.barrier()

        def mod_ap(l, idx, j, r):
            return modT[:, l, idx * 8 + j, r:r + 1]

        def norm_mod(tmp, xt, n, kx, out, kout, gm_of_j, sh_of_j, psi, extra_reads=(), tag=''):
            sq, rt, rstd, xn = tmp
            kxl = list(kx) if isinstance(kx, list) else [kx]
            S.op('act', lambda e: e.activation(out=sq[:, :, :n], in_=xt[:, :, :n], func=AF.Square),
                 reads=kxl, writes=['nm_sq' + tag])

            def mm(e):
                for j in range(8):
                    ins = e.matmul(PS[psi][:, :n], lhsT=ones_bf[:], rhs=sq[:, j, :n], start=(j == 0), stop=(j == 7))
                return ins
            S.op('pe', mm, reads=['nm_sq' + tag, 'ones_bf'], writes=[pk(psi)])
            S.op('act', lambda e: e.activation(out=rt[:, :n], in_=PS[psi][:, :n], func=AF.Ln,
                                               bias=eps_t[:, 0:1], scale=1.0 / 1024.0),
                 reads=[pk(psi), 'eps'], writes=['nm_rt' + tag])
            S.op('act', lambda e: e.activation(out=rstd[:, :n], in_=rt[:, :n], func=AF.Exp, scale=-0.5),
                 reads=['nm_rt' + tag], writes=['nm_rstd' + tag])
            S.op('dve', lambda e: e.tensor_tensor(out=xn[:, :, :n], in0=xt[:, :, :n], in1=bc_mid(rstd[:, :n], 8),
                                                  op=ALU.mult), reads=kxl + ['nm_rstd' + tag], writes=['nm_xn' + tag])
            for j in range(8):
                if sh_of_j is None:
                    S.op('dve', lambda e: e.tensor_scalar(out=out(j), in0=xn[:, j, :n], scalar1=gm_of_j(j),
                                                          scalar2=None, op0=ALU.mult),
                         reads=['nm_xn' + tag] + list(extra_reads), writes=[kout])
                elif j % 2 == 0:
                    S.op('dve', lambda e: e.tensor_scalar(out=out(j), in0=xn[:, j, :n], scalar1=gm_of_j(j),
                                                          scalar2=sh_of_j(j), op0=ALU.mult, op1=ALU.add),
                         reads=['nm_xn' + tag] + list(extra_reads), writes=[kout])
                else:
                    S.op('act', lambda e: e.activation(out=out(j), in_=xn[:, j, :n], func=AF.Identity,
                                                       bias=sh_of_j(j), scale=gm_of_j(j)),
                         reads=['nm_xn' + tag] + list(extra_reads), writes=[kout])

        def norm_tmp(ph):
            return (sb(ph, "nm_sq", [128, 8, 512], BF16), sb(ph, "nm_rt", [128, 512], F32),
                    sb(ph, "nm_rstd", [128, 512], F32), sb(ph, "nm_xn", [128, 8, 512], F32))

        eps_t = sb(st, "eps_t", [128, 1], F32)
        S.op('dve', lambda e: e.memset(eps_t[:], EPS), writes=['eps'])
        one_t = sb(st, "one_t", [128, 1], F32)
        S.op('dve', lambda e: e.memset(one_t[:], 1.0), writes=['one'])


        def phase_A(l, src, hbuf):
            with contextlib.ExitStack() as ph:
                xts = [sb(ph, f"xt{i}", [128, 8, 512], F32) for i in range(2)]
                tmp = norm_tmp(ph)
                for ti, (off, cnt, _, _, r) in enumerate(TILES):
                    xt = xts[ti % 2]
                    kx = ('xt', ti % 2)
                    S.dma('sp', xt[:, :, :cnt], src[:, :, off:off + cnt].rearrange("j p t -> p j t"),
                          reads=[('xr', ti)], writes=[kx])
                    norm_mod(tmp, xt, cnt, kx, lambda j: hbuf[:, j, off:off + cnt], ('h', ti),
                             lambda j: gmT[:, l, 0, j, r:r + 1], lambda j: mod_ap(l, 0, j, r), 7,
                             extra_reads=['gmT', 'modT'])
                S.barrier()

        hst = contextlib.ExitStack()
        hbuf = sb(hst, "hbuf", [128, 8, NT], BF16)
        phase_A(0, xc, hbuf)
        if dbg:
            with contextlib.ExitStack() as ph:
                t32 = sb(ph, "dbg32", [128, 8, 512], F32)
                for ti, (off, cnt, _, _, r) in enumerate(TILES):
                    S.op('dve', lambda e: e.tensor_copy(out=t32[:, :, :cnt], in_=hbuf[:, :, off:off + cnt]),
                         reads=[('h', ti)], writes=['dbg32'])
                    S.dma('sp', dbgo["d_h"][:, :, off:off + cnt], t32[:, :, :cnt], reads=['dbg32'], writes=['d_h'])
                S.barrier()

        with contextlib.ExitStack() as ph:
            ub = sb(ph, "ub", [128, UW], F32)
            uc = sb(ph, "uc", [128, UCW], F32)
            ucb = sb(ph, "ucb", [128, UCW], BF16)
            gy = sb(ph, "gy", [128, NT], BF16)
            wyu = [sb(ph, f"wyu{i}", [128, 8, 256], BF16) for i in range(2)]
            gw = [sb(ph, f"gw{i}", [128, 4, 128], BF16) for i in range(2)]
            convw = sb(ph, "convw", [128, 8, 5], F32)
            convb = sb(ph, "convb", [128, 8], F32)
            gab = sb(ph, "gab", [128, 2, 8], F32)
            gxb = sb(ph, "gxb", [128, 2, 8], F32)
            lamt = sb(ph, "lamt", [128, 2, 8], F32)
            cneg = sb(ph, "cneg", [128, 2, 8], F32)
            cneg2 = sb(ph, "cneg2", [128, 2, 8], F32)
            rbuf = [sb(ph, f"rbuf{i}", [128, 512], F32) for i in range(2)]
            ibuf = [sb(ph, f"ibuf{i}", [128, 512], F32) for i in range(2)]
            sbuf_ = [sb(ph, f"sbuf{i}", [128, 512], F32) for i in range(2)]
            hbt = [sb(ph, f"hbt{i}", [128, 512], F32) for i in range(2)]
            gt1 = [sb(ph, f"gt1{i}", [128, 512], F32) for i in range(2)]
            zt = [sb(ph, f"zt{i}", [128, 512], BF16) for i in range(2)]
            for t, src, k in [(convw, convw_in, 'convw'), (convb, convb_in, 'convb'), (gab, gab_in, 'gab'),
                              (gxb, gxb_in, 'gxb'), (lamt, lam_in, 'lamt')]:
                S.dma('sp', t[:], src, writes=[k])
            S.op('act', lambda e: e.activation(out=cneg[:], in_=lamt[:], func=AF.Exp, scale=-1.0),
                 reads=['lamt'], writes=['cneg'])
            S.op('act', lambda e: e.activation(out=cneg[:], in_=cneg[:], func=AF.Ln, bias=one_t[:, 0:1], scale=1.0),
                 reads=['cneg', 'one'], writes=['cneg'])
            S.op('dve', lambda e: e.tensor_scalar(out=cneg2[:], in0=cneg[:], scalar1=-16.0, scalar2=None, op0=ALU.mult),
                 reads=['cneg'], writes=['cneg2'])
            S.op('dve', lambda e: e.tensor_scalar(out=cneg[:], in0=cneg[:], scalar1=-8.0, scalar2=None, op0=ALU.mult),
                 reads=['cneg', 'cneg2'], writes=['cneg'])
            zer = sb(ph, "zer", [128, 4, 1024], BF16)
            S.op('pool', lambda e: e.memset(zer[:], 0.0), writes=['zer'])
            ngab = sb(ph, "ngab", [128, 2, 8], F32)
            ngxb = sb(ph, "ngxb", [128, 2, 8], F32)
            S.op('dve', lambda e: e.tensor_scalar(out=ngab[:], in0=gab[:], scalar1=-1.0, scalar2=None, op0=ALU.mult),
                 reads=['gab'], writes=['ngab'])
            S.op('dve', lambda e: e.tensor_scalar(out=ngxb[:], in0=gxb[:], scalar1=-1.0, scalar2=None, op0=ALU.mult),
                 reads=['gxb'], writes=['ngxb'])
            S.op('dve', lambda e: e.memset(ub[:], 0.0), writes=[('ub', ti) for ti in range(9)])
            ubkeys = [('ub', ti) for ti in range(9)]
            cnt_sc = 0
            for n in range(8):
                w = wyu[n % 2]
                kw_ = ('wyu', n % 2)
                S.dma('pool', w[:, :, 0:128], w_in[:, n * 128:(n + 1) * 128].rearrange("(k p) f -> p k f", p=128),
                      writes=[kw_])
                S.dma('pool', w[:, :, 128:256],
                      w_in[:, 1024 + n * 128:1024 + (n + 1) * 128].rearrange("(k p) f -> p k f", p=128), writes=[kw_])
                g = gw[n % 2]
                kg = ('gw', n % 2)
                for d in range(2):
                    S.dma('pool', g[:, 2 * d, :], gaw[d, n], writes=[kg])
                    S.dma('pool', g[:, 2 * d + 1, :], gxw[d, n], writes=[kg])
                convert_experts(0, 4 * n, 4 * n + 4)
                for b4 in range(4 * n, min(4 * n + 4, 25)):
                    S.dma('pool', Xs[b4 * 512:(b4 + 1) * 512, :].rearrange("(a p) f -> p a f", p=128), zer[:],
                          reads=['zer'], writes=['Xs'])
                for ti, (off, cnt, uoff, ucoff, r) in enumerate(TILES):
                    pu, py = (ti % 2) * 2, (ti % 2) * 2 + 1

                    def mmu(e, c0=128, p=pu):
                        for kc in range(8):
                            ins = e.matmul(PS[p][:, :cnt], lhsT=w[:, kc, c0:c0 + 128], rhs=hbuf[:, kc, off:off + cnt],
                                           start=(kc == 0), stop=(kc == 7))
                        return ins
                    S.op('pe', mmu, reads=[kw_, ('h', ti)], writes=[pk(pu)])
                    S.op('act', lambda e: e.activation(out=ub[:, uoff:uoff + cnt], in_=PS[pu][:, :cnt], func=AF.Identity),
                         reads=[pk(pu)], writes=[('ub', ti)])
                    S.op('pe', lambda e: mmu(e, 0, py), reads=[kw_, ('h', ti)], writes=[pk(py)])
                    t1 = gt1[ti % 2]
                    k1 = ('gt1', ti % 2)
                    S.op('act', lambda e: e.activation(out=t1[:, :cnt], in_=PS[py][:, :cnt], func=AF.Square),
                         reads=[pk(py)], writes=[k1])
                    S.op('dve', lambda e: e.tensor_scalar(out=t1[:, :cnt], in0=t1[:, :cnt], scalar1=0.044715, scalar2=1.0,
                                                          op0=ALU.mult, op1=ALU.add), reads=[k1], writes=[k1])
                    S.op('dve', lambda e: e.tensor_tensor(out=t1[:, :cnt], in0=t1[:, :cnt], in1=PS[py][:, :cnt], op=ALU.mult),
                         reads=[k1, pk(py)], writes=[k1])
                    S.op('act', lambda e: e.activation(out=t1[:, :cnt], in_=t1[:, :cnt], func=AF.Sigmoid,
                                                       scale=1.5957691216057308), reads=[k1], writes=[k1])
                    S.op('dve', lambda e: e.tensor_tensor(out=gy[:, off:off + cnt], in0=t1[:, :cnt], in1=PS[py][:, :cnt],
                                                          op=ALU.mult), reads=[k1, pk(py)], writes=[('gy', ti)])
                S.op('dve', lambda e: e.tensor_scalar(out=uc[:], in0=ub[:, 0:UCW], scalar1=convw[:, n, 0:1],
                                                      scalar2=convb[:, n:n + 1], op0=ALU.mult, op1=ALU.add),
                     reads=ubkeys + ['convw', 'convb'], writes=['uc'])
                for k in range(1, 5):
                    S.op('dve', lambda e: e.scalar_tensor_tensor(out=uc[:], in0=ub[:, k:k + UCW], scalar=convw[:, n, k:k + 1],
                                                                 in1=uc[:], op0=ALU.mult, op1=ALU.add),
                         reads=ubkeys + ['convw', 'uc'], writes=['uc'])
                S.op('act', lambda e: e.activation(out=ucb[:], in_=uc[:], func=AF.Identity), reads=['uc'], writes=['ucb'])
                for d in range(2):
                    order = [8] + (list(range(8)) if d == 0 else list(range(7, -1, -1)))
                    prev = None
                    for oi, ti in enumerate(order):
                        off, cnt, uoff, ucoff, r = TILES[ti]
                        b = cnt_sc % 2
                        cnt_sc += 1
                        pr, pi = 4 + 2 * b, 5 + 2 * b
                        S.op('pe', lambda e: e.matmul(PS[pr][:, :cnt], lhsT=g[:, 2 * d, :], rhs=ucb[:, ucoff:ucoff + cnt],
                                                      start=True, stop=True), reads=[kg, 'ucb'], writes=[pk(pr)])
                        S.op('pe', lambda e: e.matmul(PS[pi][:, :cnt], lhsT=g[:, 2 * d + 1, :], rhs=ucb[:, ucoff:ucoff + cnt],
                                                      start=True, stop=True), reads=[kg, 'ucb'], writes=[pk(pi)])
                        rb_, ib_, sb_, hb_ = rbuf[b], ibuf[b], sbuf_[b], hbt[b]
                        kr, ki, ks, kh = ('rbuf', b), ('ibuf', b), ('sbuf', b), ('hbt', b)
                        S.op('act', lambda e: e.activation(out=rb_[:, :cnt], in_=PS[pr][:, :cnt], func=AF.Exp,
                                                           bias=ngab[:, d, n:n + 1], scale=-1.0),
                             reads=[pk(pr), 'ngab'], writes=[kr])
                        S.op('act', lambda e: e.activation(out=ib_[:, :cnt], in_=PS[pi][:, :cnt], func=AF.Exp,
                                                           bias=ngxb[:, d, n:n + 1], scale=-1.0),
                             reads=[pk(pi), 'ngxb'], writes=[ki])
                        for t_, k_ in ((rb_, kr), (ib_, ki)):
                            S.op('act', lambda e: e.activation(out=t_[:, :cnt], in_=t_[:, :cnt], func=AF.Ln,
                                                               bias=one_t[:, 0:1], scale=1.0), reads=[k_, 'one'], writes=[k_])
                            S.op('act', lambda e: e.activation(out=t_[:, :cnt], in_=t_[:, :cnt], func=AF.Exp, scale=-1.0),
                                 reads=[k_], writes=[k_])
                        S.op('act', lambda e: e.activation(out=sb_[:, :cnt], in_=rb_[:, :cnt], func=AF.Exp,
                                                           scale=cneg2[:, d, n:n + 1]), reads=[kr, 'cneg2'], writes=[ks])
                        S.op('act', lambda e: e.activation(out=rb_[:, :cnt], in_=rb_[:, :cnt], func=AF.Exp,
                                                           scale=cneg[:, d, n:n + 1]), reads=[kr, 'cneg'], writes=[kr])
                        S.op('act', lambda e: e.activation(out=sb_[:, :cnt], in_=sb_[:, :cnt], func=AF.Ln,
                                                           bias=one_t[:, 0:1], scale=-1.0), reads=[ks, 'one'], writes=[ks])
                        S.op('act', lambda e: e.activation(out=sb_[:, :cnt], in_=sb_[:, :cnt], func=AF.Exp, scale=0.5),
                             reads=[ks], writes=[ks])
                        S.op('dve', lambda e: e.tensor_tensor(out=ib_[:, :cnt], in0=ib_[:, :cnt],
                                                              in1=uc[:, ucoff:ucoff + cnt], op=ALU.mult),
                             reads=[ki, 'uc'], writes=[ki])
                        S.op('dve', lambda e: e.tensor_tensor(out=ib_[:, :cnt], in0=ib_[:, :cnt], in1=sb_[:, :cnt],
                                                              op=ALU.mult), reads=[ki, ks], writes=[ki])
                        if d == 0:
                            if prev is None:
                                init, kin = 0.0, []
                            else:
                                po, pc, puo, _, _ = TILES[prev]
                                init, kin = ub[:, puo + pc - 1:puo + pc], [('ub', prev)]
                            S.op('dve', lambda e: e.tensor_tensor_scan(out=ub[:, uoff:uoff + cnt], data0=rb_[:, :cnt],
                                                                       data1=ib_[:, :cnt], initial=init,
                                                                       op0=ALU.mult, op1=ALU.add),
                                 reads=[kr, ki] + kin, writes=[('ub', ti)])
                        else:
                            if prev is None:
                                init, kin = 0.0, []
                            else:
                                init, kin = hbt[1 - b][:, 0:1], [('hbt', 1 - b)]
                            S.op('dve', lambda e: e.tensor_tensor_scan(out=hb_[:, :cnt][:, ::-1], data0=rb_[:, :cnt][:, ::-1],
                                                                       data1=ib_[:, :cnt][:, ::-1], initial=init,
                                                                       op0=ALU.mult, op1=ALU.add),
                                 reads=[kr, ki] + kin, writes=[kh])
                            S.op('dve', lambda e: e.tensor_tensor(out=sb_[:, :cnt], in0=hb_[:, :cnt],
                                                                  in1=ub[:, uoff:uoff + cnt], op=ALU.add),
                                 reads=[kh, ('ub', ti)], writes=[ks])
                            z_ = zt[b]
                            S.op('dve', lambda e: e.tensor_tensor(out=z_[:, :cnt], in0=sb_[:, :cnt],
                                                                  in1=gy[:, off:off + cnt], op=ALU.mult),
                                 reads=[ks, ('gy', ti)], writes=[('zt', b)])
                            S.dma('sp', zscr[n, :, off:off + cnt], z_[:, :cnt], reads=[('zt', b)], writes=[('zs', ti)])
                        prev = ti
            S.barrier()
        hst.close()

        I32 = mybir.dt.int32
        MAXSUB = 34
        NBMAX = 100
        BIGW = 2 * 32 * 256 + 64
        utri = sb(st, "utri", [128, 128], F32)
        S.op('pool', lambda e: e.memset(utri[:], 1.0), writes=['utri'])
        S.op('pool', lambda e: e.affine_select(out=utri[:], in_=utri[:], pattern=[[1, 128]],
                                               compare_op=ALU.is_gt, fill=0.0, base=0, channel_multiplier=-1),
             reads=['utri'], writes=['utri'])
        pcol = sb(st, "pcol", [128, 1], F32)
        blkoff = sb(st, "blkoff", [128, NBMAX], F32)
        ones_row = sb(st, "ones_row", [128, 32], F32)
        S.dma('sp', pcol[:], pcol_in, writes=['pcol'])
        S.dma('sp', blkoff[:], blkoff_in, writes=['blkoff'])
        S.op('dve', lambda e: e.memset(ones_row[:], 1.0), writes=['ones_row'])
        onesb = sb(st, "onesb", [128, 4, 32], F32)
        c12 = sb(st, "c12", [128, 2, 4, 32], F32)
        S.op('dve', lambda e: e.memset(onesb[:], 1.0), writes=['onesb'])
        for k2_ in range(2):
            for s_ in range(4):
                S.op('dve', lambda e: e.memset(c12[:, k2_, s_, :], float(2 * s_ + 1 + k2_)), writes=['c12'])
        FM = sb(st, "FM", [128, MAXSUB, 2, 32], F32)
        RK = sb(st, "RK", [128, MAXSUB, 2], F32)
        WK = sb(st, "WK", [128, MAXSUB, 2], F32)
        DI = sb(st, "DI", [128, MAXSUB, 2], I32)
        WI = sb(st, "WI", [128, NBMAX, 2], I32)
        cntm = sb(st, "cntm", [128, 32], F32)

        bregs = {}

        def idma(out, out_off, in_, in_off, bounds, reads, writes, slot):
            S._wait('pool', S._deps('pool', reads, writes, skip_same_pe=False))
            if slot not in S.dsem:
                S.dsem[slot] = [S._newsem('d'), 0]
            d = S.dsem[slot]
            if bounds not in bregs:
                bregs[bounds] = nc.gpsimd.to_reg(bounds)
            ins = nc.gpsimd.indirect_dma_start(out=out, out_offset=out_off, in_=in_, in_offset=in_off,
                                               bounds_check=bregs[bounds], oob_is_err=False)
            d[1] += 16
            ins.then_inc(d[0], 16)
            S._commit((d[0], d[1]), reads, writes)

        def phase_C(l, wmat, tiles, xsrc):
            nsub_tot = sum(TILES[ti][1] // 128 for ti in tiles)
            NB = 2 * nsub_tot + 32
            with contextlib.ExitStack() as ph:
                wsb = sb(ph, "wsb", [128, 8, 1024], BF16)
                zts = [sb(ph, f"zts{i}", [128, 8, 512], BF16) for i in range(2)]
                xts = [sb(ph, f"xtc{i}", [128, 8, 512], F32) for i in range(2)]
                h2fs = [sb(ph, f"h2f{i}", [128, 8, 512], F32) for i in range(2)]
                h2t = [sb(ph, f"h2t{i}", [128, 1024], BF16) for i in range(2)]
                tmps = [norm_tmp(ph), norm_tmp(ph)]
                mk3 = lambda nm: [sb(ph, f"{nm}{i}", [128, 4, 32], F32) for i in range(2)]
                ssel, sg, em, mk, cmv, rk, t3 = mk3("ssel"), mk3("sg"), mk3("em"), mk3("mk"), mk3("cmv"), mk3("rk"), mk3("t3")
                mk16 = lambda nm: [sb(ph, f"{nm}{i}", [128, 16], F32) for i in range(2)]
                m1, m2, gs, gm_ = mk16("m1"), mk16("m2"), mk16("gs"), mk16("gmk")
                sm = [sb(ph, f"sm{i}", [128, 4, 4], F32) for i in range(2)]
                t32 = [sb(ph, f"t32{i}", [128, 32], F32) for i in range(2)]
                S.dma('pool', wsb[:], wmat.rearrange("(k p) f -> p k f", p=128), writes=['wsb'])
                S.op('dve', lambda e: e.memset(cntm[:], 0.0), writes=['cntm'])
                nsub = 0
                for ti in tiles:
                    off, cnt, _, _, r = TILES[ti]
                    b = ti % 2
                    z_, x_ = zts[b], xts[b]
                    h2f, tmp, kh2 = h2fs[b], tmps[b], ('h2f', b)
                    kz, kx = ('zts', b), ('xtc', b)
                    S.dma('sp', z_[:, :, :cnt], zscr[:, :, off:off + cnt].rearrange("j p t -> p j t"),
                          reads=[('zs', ti)], writes=[kz])
                    S.dma('sp', x_[:, :, :cnt], xsrc[:, :, off:off + cnt].rearrange("j p t -> p j t"),
                          reads=[('xr', ti)], writes=[kx])
                    for j in range(8):
                        p = j % 3

                        def mm(e):
                            for kc in range(8):
                                ins = e.matmul(PS[p][:, :cnt], lhsT=wsb[:, kc, j * 128:(j + 1) * 128], rhs=z_[:, kc, :cnt],
                                               start=(kc == 0), stop=(kc == 7))
                            return ins
                        S.op('pe', mm, reads=['wsb', kz], writes=[pk(p)])
                        S.op('dve', lambda e: e.scalar_tensor_tensor(out=x_[:, j, :cnt], in0=PS[p][:, :cnt],
                                                                     scalar=mod_ap(l, 2, j, r), in1=x_[:, j, :cnt],
                                                                     op0=ALU.mult, op1=ALU.add),
                             reads=[pk(p), kx, 'modT'], writes=[kx])
                    S.dma('sp', xr[:, :, off:off + cnt].rearrange("j p t -> p j t"), x_[:, :, :cnt],
                          reads=[kx], writes=[('xr', ti)])
                    if dbg and l == 0:
                        S.dma('sp', dbgo["d_x1"][:, :, off:off + cnt].rearrange("j p t -> p j t"), x_[:, :, :cnt],
                              reads=[kx], writes=['d_x1'])
                    norm_mod(tmp, x_, cnt, kx, lambda j: h2f[:, j, :cnt], kh2,
                             lambda j: gmT[:, l, 1, j, r:r + 1], lambda j: mod_ap(l, 3, j, r), 3,
                             extra_reads=['gmT', 'modT'], tag=str(b))
                    nsb = cnt // 128
                    gs0 = nsub
                    nsub += nsb
                    q = ti % 2
                    pl = 4 + q
                    W_ = nsb * 32
                    for s in range(nsb):
                        gsi = gs0 + s
                        qq = gsi % 2
                        for half in range(2):
                            pt = 6 + half

                            def mmt(e):
                                for jj in range(4):
                                    j = half * 4 + jj
                                    ins = e.transpose(out=PS[pt][:, jj * 128:(jj + 1) * 128],
                                                      in_=h2f[:, j, s * 128:(s + 1) * 128], identity=ident[:])
                                return ins
                            S.op('pe', mmt, reads=[kh2, 'ident'], writes=[pk(pt)])
                            if half == 0:
                                S.op('act', lambda e: e.activation(out=h2t[qq][:, 0:512], in_=PS[pt][:, :], func=AF.Identity),
                                     reads=[pk(pt)], writes=[('h2t', qq)])
                            else:
                                S.op('pool' if False else 'dve', lambda e: e.tensor_copy(out=h2t[qq][:, 512:1024], in_=PS[pt][:, :]),
                                     reads=[pk(pt)], writes=[('h2t', qq)])
                        S.dma('sp', h2tm[gsi * 128:(gsi + 1) * 128, :], h2t[qq][:], reads=[('h2t', qq)], writes=[('h2tm', gsi % 4)])

                        def mmr(e):
                            for kc in range(8):
                                ins = e.matmul(PS[pl][:, s * 32:(s + 1) * 32], lhsT=h2f[:, kc, s * 128:(s + 1) * 128], rhs=rw[:, kc, :],
                                               start=(kc == 0), stop=(kc == 7))
                            return ins
                        S.op('pe', mmr, reads=[kh2, 'rw'], writes=[pk(pl)])
                    kq = ('rt', q)
                    v3 = lambda t: t[:, :nsb, :]
                    v8 = lambda t: t[:, :nsb, :].rearrange("p s (g e) -> p (s g) e", e=8)
                    f2 = lambda t: t[:, :nsb, :].rearrange("p s e -> p (s e)")
                    g4 = lambda t: t[:, :nsb * 4]
                    g43 = lambda t: t[:, :nsb * 4].rearrange("p (s g) -> p s g", g=4)
                    S.op('act', lambda e: e.activation(out=f2(sg[q]), in_=PS[pl][:, :W_], func=AF.Sigmoid),
                         reads=[pk(pl)], writes=[kq])
                    S.op('dve', lambda e: e.tensor_tensor(out=v3(ssel[q]), in0=v3(sg[q]), in1=bc_mid(rb[:], nsb), op=ALU.add),
                         reads=[kq, 'rb'], writes=[kq])
                    S.op('dve', lambda e: e.tensor_reduce(out=g4(m1[q]), in_=v8(ssel[q]), axis=AX.X, op=ALU.max), reads=[kq], writes=[kq])
                    S.op('dve', lambda e: e.tensor_tensor(out=v8(t3[q]), in0=v8(ssel[q]), in1=bc_last(g4(m1[q]), 8), op=ALU.is_equal),
                         reads=[kq], writes=[kq])
                    S.op('dve', lambda e: e.scalar_tensor_tensor(out=f2(t3[q]), in0=f2(t3[q]), scalar=-1.0e9, in1=f2(ssel[q]),
                                                                 op0=ALU.mult, op1=ALU.add), reads=[kq], writes=[kq])
                    S.op('dve', lambda e: e.tensor_reduce(out=g4(m2[q]), in_=v8(t3[q]), axis=AX.X, op=ALU.max), reads=[kq], writes=[kq])
                    S.op('dve', lambda e: e.tensor_tensor(out=g4(gs[q]), in0=g4(m1[q]), in1=g4(m2[q]), op=ALU.add), reads=[kq], writes=[kq])
                    S.op('dve', lambda e: e.tensor_reduce(out=sm[q][:, 0, :nsb], in_=g43(gs[q]), axis=AX.X, op=ALU.max),
                         reads=[kq], writes=[kq])
                    S.op('dve', lambda e: e.tensor_tensor(out=g43(gm_[q]), in0=g43(gs[q]), in1=bc_last(sm[q][:, 0, :nsb], 4),
                                                          op=ALU.is_equal), reads=[kq], writes=[kq])
                    S.op('dve', lambda e: e.tensor_tensor(out=g4(gs[q]), in0=g4(gm_[q]), in1=g4(m2[q]), op=ALU.mult), reads=[kq], writes=[kq])
                    S.op('dve', lambda e: e.tensor_reduce(out=sm[q][:, 1, :nsb], in_=g43(gs[q]), axis=AX.X, op=ALU.add),
                         reads=[kq], writes=[kq])
                    S.op('dve', lambda e: e.tensor_tensor(out=v3(mk[q]), in0=v3(ssel[q]), in1=bc_last(sm[q][:, 1, :nsb], 32),
                                                          op=ALU.is_ge), reads=[kq], writes=[kq])
                    S.op('dve', lambda e: e.tensor_tensor(out=v8(mk[q]), in0=v8(mk[q]), in1=bc_last(g4(gm_[q]), 8), op=ALU.mult),
                         reads=[kq], writes=[kq])
                    S.op('dve', lambda e: e.tensor_tensor(out=f2(em[q]), in0=f2(mk[q]), in1=f2(sg[q]), op=ALU.mult),
                         reads=[kq], writes=[kq])
                    S.op('dve', lambda e: e.tensor_reduce(out=sm[q][:, 2, :nsb], in_=v3(em[q]), axis=AX.X, op=ALU.add),
                         reads=[kq], writes=[kq])
                    S.op('dve', lambda e: e.reciprocal(out=sm[q][:, 3, :nsb], in_=sm[q][:, 2, :nsb]), reads=[kq], writes=[kq])
                    S.op('dve', lambda e: e.tensor_tensor(out=v3(em[q]), in0=v3(em[q]), in1=bc_last(sm[q][:, 3, :nsb], 32), op=ALU.mult),
                         reads=[kq], writes=[kq])

                    def mmk(e):
                        for s in range(nsb):
                            o_ = PS[pl][:, 128 + s * 32:128 + (s + 1) * 32]
                            e.matmul(o_, lhsT=utri[:], rhs=mk[q][:, s, :], start=True, stop=False)
                            for s2 in range(s):
                                e.matmul(o_, lhsT=ones32[:], rhs=mk[q][:, s2, :], start=False, stop=False)
                            ins = e.matmul(o_, lhsT=ones32[:], rhs=cntm[:], start=False, stop=True)
                        return ins
                    S.op('pe', mmk, reads=[kq, 'utri', 'ones32', 'cntm'], writes=[pk(pl)])
                    S.op('dve', lambda e: e.tensor_copy(out=f2(rk[q]), in_=PS[pl][:, 128:128 + W_]), reads=[pk(pl)], writes=[kq])
                    S.op('dve', lambda e: e.tensor_reduce(out=t32[q][:], in_=mk[q][:, :nsb, :].rearrange("p s e -> p e s"), axis=AX.X,
                                                          op=ALU.add), reads=[kq], writes=[kq])
                    S.op('dve', lambda e: e.tensor_tensor(out=cntm[:], in0=cntm[:], in1=t32[q][:], op=ALU.add),
                         reads=[kq, 'cntm'], writes=['cntm'])
                    S.op('dve', lambda e: e.tensor_tensor_scan(out=f2(cmv[q]), data0=f2(onesb), data1=f2(mk[q]), initial=0.0,
                                                               op0=ALU.mult, op1=ALU.add), reads=[kq, 'onesb'], writes=[kq])
                    for k2 in range(2):
                        fm_ = FM[:, gs0:gs0 + nsb, k2, :]
                        S.op('dve', lambda e: e.tensor_tensor(out=v3(t3[q]), in0=v3(cmv[q]), in1=c12[:, k2, :nsb, :], op=ALU.is_equal),
                             reads=[kq, 'c12'], writes=[kq])
                        S.op('dve', lambda e: e.tensor_tensor(out=fm_, in0=v3(t3[q]), in1=v3(mk[q]), op=ALU.mult),
                             reads=[kq], writes=['FM'])
                        S.op('dve', lambda e: e.tensor_tensor(out=v3(t3[q]), in0=fm_, in1=v3(rk[q]), op=ALU.mult),
                             reads=[kq, 'FM'], writes=[kq])
                        S.op('dve', lambda e: e.tensor_reduce(out=RK[:, gs0:gs0 + nsb, k2], in_=v3(t3[q]), axis=AX.X, op=ALU.add),
                             reads=[kq], writes=['RK'])
                        S.op('dve', lambda e: e.tensor_tensor(out=v3(t3[q]), in0=fm_, in1=v3(em[q]), op=ALU.mult),
                             reads=[kq, 'FM'], writes=[kq])
                        S.op('dve', lambda e: e.tensor_reduce(out=WK[:, gs0:gs0 + nsb, k2], in_=v3(t3[q]), axis=AX.X, op=ALU.add),
                             reads=[kq], writes=['WK'])
                S.barrier()
            with contextlib.ExitStack() as ph:
                J = nsub_tot
                cb = sb(ph, "cb", [128, 32], F32)
                nblk = sb(ph, "nblk", [128, 32], F32)
                pend = sb(ph, "pend", [128, 32], F32)
                pst = sb(ph, "pst", [128, 32], F32)
                big = sb(ph, "bigc", [128, 32, NBMAX], F32)
                eb = sb(ph, "eb", [128, NBMAX], F32)
                chg = sb(ph, "chg", [128, NBMAX], F32)
                wif = sb(ph, "wif", [128, NBMAX, 2], F32)
                dtmp2 = sb(ph, "dtmp2", [128, MAXSUB, 32], F32)
                dif = sb(ph, "dif", [128, MAXSUB, 2], F32)
                rows = [sb(ph, f"rows{i}", [128, 1024], BF16) for i in range(2)]
                S.op('pe', lambda e: e.matmul(PS[0][:, 0:32], lhsT=ones32[:], rhs=cntm[:], start=True, stop=True),
                     reads=['ones32', 'cntm'], writes=[pk(0)])
                S.op('dve', lambda e: e.tensor_copy(out=cb[:], in_=PS[0][:, 0:32]), reads=[pk(0)], writes=['cb'])
                S.op('dve', lambda e: e.tensor_tensor(out=big[:, :, :J], in0=bc_last(cb[:], J), in1=bc_mid(blkoff[:, :J], 32),
                                                      op=ALU.is_gt), reads=['cb', 'blkoff'], writes=['big'])
                S.op('dve', lambda e: e.tensor_reduce(out=nblk[:], in_=big[:, :, :J], axis=AX.X, op=ALU.add),
                     reads=['big'], writes=['nblk'])
                S.op('dve', lambda e: e.tensor_tensor_scan(out=pend[:], data0=ones_row[:], data1=nblk[:], initial=0.0,
                                                           op0=ALU.mult, op1=ALU.add), reads=['nblk', 'ones_row'], writes=['pend'])
                S.op('dve', lambda e: e.tensor_tensor(out=pst[:], in0=pend[:], in1=nblk[:], op=ALU.subtract),
                     reads=['pend', 'nblk'], writes=['pst'])
                S.op('dve', lambda e: e.tensor_scalar(out=pst[:], in0=pst[:], scalar1=128.0, scalar2=None, op0=ALU.mult),
                     reads=['pst'], writes=['pst'])
                S.op('dve', lambda e: e.tensor_scalar(out=pend[:], in0=pend[:], scalar1=128.0, scalar2=None, op0=ALU.mult),
                     reads=['pend'], writes=['pend'])
                S.op('dve', lambda e: e.tensor_tensor(out=big[:, :, :NB].rearrange("p e b -> p b e"),
                                                      in0=bc_mid(pend[:], NB), in1=bc_last(blkoff[:, :NB], 32),
                                                      op=ALU.is_le), reads=['pend', 'blkoff', 'big'], writes=['big'])
                S.op('dve', lambda e: e.tensor_reduce(out=eb[:, :NB], in_=big[:, :, :NB].rearrange("p e b -> p b e"),
                                                      axis=AX.X, op=ALU.add), reads=['big'], writes=['eb'])
                S.op('dve', lambda e: e.tensor_scalar(out=eb[:, :NB], in0=eb[:, :NB], scalar1=31.0, scalar2=None, op0=ALU.min),
                     reads=['eb'], writes=['eb'])
                S.op('dve', lambda e: e.memset(chg[:], 1.0), writes=['chg'])
                S.op('dve', lambda e: e.tensor_tensor(out=chg[:, 1:NB], in0=eb[:, 1:NB], in1=eb[:, 0:NB - 1], op=ALU.not_equal),
                     reads=['eb', 'chg'], writes=['chg'])
                for k4 in range(1, 4):
                    S.op('dve', lambda e: e.memset(chg[:, k4 * (NB // 4):k4 * (NB // 4) + 1], 1.0), reads=['chg'], writes=['chg'])
                for h in range(1):
                    S.op('dve', lambda e: e.tensor_scalar(out=wif[:, :NB, h], in0=eb[:, :NB], scalar1=128.0,
                                                          scalar2=float(l * 4096 - BIGW), op0=ALU.mult, op1=ALU.add),
                         reads=['eb', 'wif'], writes=['wif'])
                    S.op('dve', lambda e: e.tensor_scalar(out=wif[:, :NB, h], in0=wif[:, :NB, h], scalar1=pcol[:, 0:1],
                                                          scalar2=None, op0=ALU.add), reads=['wif', 'pcol'], writes=['wif'])
                    S.op('dve', lambda e: e.tensor_tensor(out=wif[:, :NB, h], in0=wif[:, :NB, h], in1=chg[:, :NB], op=ALU.mult),
                         reads=['wif', 'chg'], writes=['wif'])
                    S.op('dve', lambda e: e.tensor_scalar(out=wif[:, :NB, h], in0=wif[:, :NB, h], scalar1=float(BIGW),
                                                          scalar2=None, op0=ALU.add), reads=['wif'], writes=['wif'])
                S.op('dve', lambda e: e.tensor_copy(out=WI[:, :NB, 0:1], in_=wif[:, :NB, 0:1]), reads=['wif'], writes=['WI'])
                for k2 in range(2):
                    S.op('dve', lambda e: e.tensor_tensor(out=dtmp2[:, :J, :], in0=FM[:, :J, k2, :], in1=bc_mid(pst[:], J),
                                                          op=ALU.mult), reads=['FM', 'pst', 'dtmp2'], writes=['dtmp2'])
                    S.op('dve', lambda e: e.tensor_reduce(out=dif[:, :J, k2], in_=dtmp2[:, :J, :], axis=AX.X, op=ALU.add),
                         reads=['dtmp2', 'dif'], writes=['dif'])
                S.op('dve', lambda e: e.tensor_tensor(out=dif[:, :J, :], in0=dif[:, :J, :], in1=RK[:, :J, :], op=ALU.add),
                     reads=['dif', 'RK'], writes=['dif'])
                S.op('dve', lambda e: e.tensor_copy(out=DI[:, :J, :], in_=dif[:, :J, :]), reads=['dif'], writes=['DI'])
                for gsi in range(J):
                    q = gsi % 2
                    S.dma('sp', rows[q][:], h2tm[gsi * 128:(gsi + 1) * 128, :], reads=[('h2tm', gsi % 4)], writes=[('rows', q)])
                    for k2 in range(2):
                        idma(Xs[:, :], bass.IndirectOffsetOnAxis(ap=DI[:, gsi, k2:k2 + 1], axis=0), rows[q][:, :], None,
                             NB * 128 - 1, reads=[('rows', q), 'DI', 'Xs'], writes=[('Xsc', q)], slot=('Xsc', q))
                S.barrier()

        def phase_D(l, tiles, final):
            nsub_tot = sum(TILES[ti][1] // 128 for ti in tiles)
            NB = 2 * nsub_tot + 32
            wgv = wgb.rearrange("(a b) f -> a (b f)", b=2)
            wuv = wub.rearrange("(a b) f -> a (b f)", b=2)
            wdv = wdb.rearrange("(a b) f -> a (b f)", b=2)
            with contextlib.ExitStack() as ph:
                wgs = [sb(ph, f"wgs{i}", [128, 4096], BF16) for i in range(4)]
                wus = [sb(ph, f"wus{i}", [128, 4096], BF16) for i in range(4)]
                wds = [sb(ph, f"wds{i}", [128, 4096], BF16) for i in range(4)]
                blk = lambda p_: (p_ % 4) * (NB // 4) + p_ // 4
                xbs = [sb(ph, f"xbs{i}", [128, 1024], BF16) for i in range(2)]
                XTs = [sb(ph, f"XTs{i}", [128, 8, 128], BF16) for i in range(2)]
                s1s = [sb(ph, f"s1s{i}", [128, 512], F32) for i in range(2)]
                ATs = [sb(ph, f"ATs{i}", [128, 4, 128], BF16) for i in range(2)]
                Yts = [sb(ph, f"Yts{i}", [128, 1024], F32) for i in range(2)]

                def loadw(p_):
                    w_ = p_ % 4
                    for nm, view, tile_ in [('wgs', wgv, wgs[w_]), ('wus', wuv, wus[w_]), ('wds', wdv, wds[w_])]:
                        idma(tile_[:, :], None, view[:, :], bass.IndirectOffsetOnAxis(ap=WI[:, blk(p_), 0:1], axis=0),
                             (l + 1) * 4096 - 1, reads=['WI', ('wcv', l)], writes=[(nm, w_)], slot=(nm, w_))
                for p_ in range(3):
                    loadw(p_)
                def xbload(p_):
                    bn = blk(p_)
                    S.dma('sp', xbs[p_ % 2][:], Xs[bn * 128:(bn + 1) * 128, :], reads=[('Xsc', 0), ('Xsc', 1), 'Xs'],
                          writes=[('xbs', p_ % 2)])

                def stage_T(p_):
                    q = p_ % 2
                    xb, XT = xbs[q], XTs[q]
                    ptb = PS[q].bitcast(BF16)

                    def mmt(e):
                        for c in range(8):
                            ins = e.transpose(out=ptb[:, c * 128:(c + 1) * 128], in_=xb[:, c:1024:8], identity=identb[:])
                        return ins
                    S.op('pe', mmt, reads=[('xbs', q), 'identb'], writes=[pk(q)])
                    S.op('act', lambda e: e.activation(out=XT[:].rearrange("p c s -> p (c s)"), in_=ptb[:, :], func=AF.Identity),
                         reads=[pk(q)], writes=[('XTs', q)])

                def stage_G(p_):
                    q = p_ % 2
                    ws_ = p_ % 4
                    XT, AT = XTs[q], ATs[q]
                    wg_, wu_ = wgs[ws_], wus[ws_]
                    p1, p2 = 2 + 2 * q, 3 + 2 * q

                    def mmg(e, wt, p):
                        for fo in range(4):
                            for c in range(8):
                                c0 = c * 512 + fo
                                ins = e.matmul(PS[p][:, fo * 128:(fo + 1) * 128], lhsT=wt[:, c0:c0 + 509:4], rhs=XT[:, c, :],
                                               start=(c == 0), stop=(c == 7))
                        return ins
                    S.op('pe', lambda e: mmg(e, wg_, p1), reads=[('wgs', ws_), ('XTs', q)], writes=[pk(p1)])
                    S.op('pe', lambda e: mmg(e, wu_, p2), reads=[('wus', ws_), ('XTs', q)], writes=[pk(p2)])
                    S.op('act', lambda e: e.activation(out=s1s[q][:], in_=PS[p1][:, :], func=AF.Silu),
                         reads=[pk(p1)], writes=[('s1s', q)])
                    S.op('dve', lambda e: e.tensor_tensor(out=AT[:].rearrange("p c s -> p (c s)"), in0=s1s[q][:], in1=PS[p2][:, :],
                                                          op=ALU.mult), reads=[('s1s', q), pk(p2)], writes=[('ATs', q)])

                def stage_D(p_):
                    q = p_ % 2
                    ws_ = p_ % 4
                    b = blk(p_)
                    AT, Yt, wd_ = ATs[q], Yts[q], wds[ws_]
                    for dh in range(2):
                        py = 6 + dh

                        def mmd(e):
                            for fo in range(4):
                                ins = e.matmul(PS[py][:, :], lhsT=AT[:, fo, :],
                                               rhs=wd_[:, fo * 1024 + dh * 512:fo * 1024 + (dh + 1) * 512],
                                               start=(fo == 0), stop=(fo == 3))
                            return ins
                        S.op('pe', mmd, reads=[('wds', ws_), ('ATs', q)], writes=[pk(py)])
                        if dh == 0:
                            S.op('act', lambda e: e.activation(out=Yt[:, 0:512], in_=PS[py][:, :], func=AF.Identity),
                                 reads=[pk(py)], writes=[('Yts', q)])
                        else:
                            S.op('dve', lambda e: e.tensor_copy(out=Yt[:, 512:1024], in_=PS[py][:, :]),
                                 reads=[pk(py)], writes=[('Yts', q)])
                    S.dma('sp', Ys[b * 128:(b + 1) * 128, :], Yt[:], reads=[('Yts', q)], writes=[('Ys', q)])

                xbload(0)
                xbload(1)
                stage_T(0)
                for pos in range(NB):
                    if pos + 3 < NB:
                        loadw(pos + 3)
                    stage_G(pos)
                    if pos + 1 < NB:
                        stage_T(pos + 1)
                    if pos + 2 < NB:
                        xbload(pos + 2)
                    stage_D(pos)
                S.barrier()
            with contextlib.ExitStack() as ph3:
                xts = [sb(ph3, f"xtd{i}", [128, 8, 512], F32) for i in range(2)]
                g1 = [sb(ph3, f"g1{i}", [128, 1024], F32) for i in range(4)]
                g2 = [sb(ph3, f"g2{i}", [128, 1024], F32) for i in range(4)]
                tmp = norm_tmp(ph3) if final else None
                ots = [sb(ph3, f"otd{i}", [128, 8, 512], F32) for i in range(2)] if final else None
                subs = []
                for li, ti in enumerate(tiles):
                    for s_ in range(TILES[ti][1] // 128):
                        subs.append((li, ti, s_, len(subs)))

                def stage_a(li, ti, s, gsi):
                    off, cnt, _, _, r = TILES[ti]
                    b = li % 2
                    if s == 0:
                        S.dma('sp', xts[b][:, :, :cnt], xr[:, :, off:off + cnt].rearrange("j p t -> p j t"),
                              reads=[('xr', ti)], writes=[('xtd', b, j) for j in range(8)])
                    q = gsi % 4
                    idma(g1[q][:, :], None, Ys[:, :], bass.IndirectOffsetOnAxis(ap=DI[:, gsi, 0:1], axis=0),
                         NB * 128 - 1, reads=['DI', ('Ys', 0), ('Ys', 1)], writes=[('g1', q)], slot=('g1', q))
                    idma(g2[q][:, :], None, Ys[:, :], bass.IndirectOffsetOnAxis(ap=DI[:, gsi, 1:2], axis=0),
                         NB * 128 - 1, reads=['DI', ('Ys', 0), ('Ys', 1)], writes=[('g2', q)], slot=('g2', q))
                    S.op('dve', lambda e: e.tensor_scalar(out=g1[q][:], in0=g1[q][:], scalar1=WK[:, gsi, 0:1], scalar2=None,
                                                          op0=ALU.mult), reads=[('g1', q), 'WK'], writes=[('g1', q)])
                    S.op('dve', lambda e: e.scalar_tensor_tensor(out=g1[q][:], in0=g2[q][:], scalar=WK[:, gsi, 1:2],
                                                                 in1=g1[q][:], op0=ALU.mult, op1=ALU.add),
                         reads=[('g1', q), ('g2', q), 'WK'], writes=[('g1', q)])
                    for half in range(2):
                        pt = 2 * (q % 2) + half

                        def mmt2(e):
                            for jj in range(4):
                                j = half * 4 + jj
                                ins = e.transpose(out=PS[pt][:, jj * 128:(jj + 1) * 128], in_=g1[q][:, j * 128:(j + 1) * 128],
                                                  identity=ident[:])
                            return ins
                        S.op('pe', mmt2, reads=[('g1', q), 'ident'], writes=[pk(pt)])

                def stage_b(li, ti, s, gsi):
                    off, cnt, _, _, r = TILES[ti]
                    b = li % 2
                    x_ = xts[b]
                    q = gsi % 4
                    kxs = [('xtd', b, j) for j in range(8)]
                    for half in range(2):
                        pt = 2 * (q % 2) + half
                        for jj in range(4):
                            j = half * 4 + jj
                            S.op('dve', lambda e: e.scalar_tensor_tensor(out=x_[:, j, s * 128:(s + 1) * 128],
                                                                         in0=PS[pt][:, jj * 128:(jj + 1) * 128],
                                                                         scalar=mod_ap(l, 5, j, r),
                                                                         in1=x_[:, j, s * 128:(s + 1) * 128],
                                                                         op0=ALU.mult, op1=ALU.add),
                                 reads=[pk(pt), kxs[j], ('modT', l)], writes=[kxs[j]])
                    if s == cnt // 128 - 1:
                        if not final:
                            S.dma('sp', xr[:, :, off:off + cnt].rearrange("j p t -> p j t"), x_[:, :, :cnt],
                                  reads=kxs, writes=[('xr', ti)])
                            if dbg:
                                S.dma('sp', dbgo["d_x2"][:, :, off:off + cnt].rearrange("j p t -> p j t"), x_[:, :, :cnt],
                                      reads=kxs, writes=['d_x2'])
                        else:
                            o_ = ots[b]
                            norm_mod(tmp, x_, cnt, kxs, lambda j: o_[:, j, :cnt], ('otd', b),
                                     lambda j: fing[:, j:j + 1], None, 7, extra_reads=['fing'])
                            S.dma('sp', outT[:, :, off:off + cnt].rearrange("j p t -> p j t"), o_[:, :, :cnt],
                                  reads=[('otd', b)], writes=[('out', b)])

                for idx in range(len(subs) + 1):
                    if idx < len(subs):
                        stage_a(*subs[idx])
                    if idx >= 1:
                        stage_b(*subs[idx - 1])
                S.barrier()

        phase_C(0, w_out, list(range(9)), xc)
        phase_D(0, list(range(9)), final=False)

        hst = contextlib.ExitStack()
        hbuf = sb(hst, "hbuf1", [128, 8, NT], BF16)
        phase_A(1, xr, hbuf)
        with contextlib.ExitStack() as ph:
            cost = sb(ph, "cost", [128, NT], F32)
            sint = sb(ph, "sint", [128, NT], F32)
            dal = sb(ph, "dal", [128, 4, 64], F32)
            dtmp = sb(ph, "dtmp", [128, 64], F32)
            lamv = sb(ph, "lamv", [128, 4], F32)
            subg = sb(ph, "subg", [128, 1], F32)
            wq = sb(ph, "wq", [128, 8, 128], BF16)
            wk = sb(ph, "wk", [128, 8, 128], BF16)
            wv = sb(ph, "wv", [128, 8, 128], BF16)
            qbt = [sb(ph, f"qbt{i}", [128, 512], BF16) for i in range(2)]
            rmb = sb(ph, "rmb", [128, 128], BF16)
            QT = sb(ph, "QT", [128, NQ], BF16)
            vtb = [sb(ph, f"vtb{i}", [128, 512], BF16) for i in range(2)]
            KT = sb(ph, "KT", [128, NT], BF16)
            Vt = sb(ph, "Vt", [128, 34, 128], BF16)
            rt1 = [sb(ph, f"rt1{i}", [128, 512], F32) for i in range(2)]
            rt2 = [sb(ph, f"rt2{i}", [128, 512], F32) for i in range(2)]
            Eb2 = [sb(ph, f"Eb{i}", [128, 2, 512], BF16) for i in range(2)]
            acc2 = sb(ph, "acc2", [128, 2, 512], F32)
            obw = sb(ph, "obw", [128, 2, 512], F32)
            ob = [obw[:, 0, :], obw[:, 1, :]]
            rlw = sb(ph, "rlw", [128, 2, 512], F32)
            rl = sb(ph, "rl", [128, 512], F32)
            accD = sb(ph, "accD", [128, 512], F32)
            accP = sb(ph, "accP", [128, 512], F32)
            osq = sb(ph, "osq", [128, 512], F32)
            aot = [sb(ph, f"aot{i}", [128, 512], BF16) for i in range(2)]
            S.dma('sp', cost[:], cos_in, writes=['cos'])
            S.dma('pool', rmb[:], rmat_in, writes=['rmb'])
            S.dma('sp', sint[:], sin_in, writes=['sin'])
            S.dma('sp', dal[:], dalam_in, writes=['dal'])
            S.dma('sp', subg[:], subg_in, writes=['subg'])
            for i2 in range(2):
                S.op('dve', lambda e: e.tensor_tensor(out=dtmp[:], in0=dal[:, 2 * i2, :], in1=dal[:, 2 * i2 + 1, :], op=ALU.mult),
                     reads=['dal'], writes=['dtmp'])
                S.op('dve', lambda e: e.tensor_reduce(out=lamv[:, i2:i2 + 1], in_=dtmp[:], axis=AX.X, op=ALU.add),
                     reads=['dtmp'], writes=['lamv'])
            S.op('act', lambda e: e.activation(out=lamv[:, 0:2], in_=lamv[:, 0:2], func=AF.Exp), reads=['lamv'], writes=['lamv'])
            S.op('dve', lambda e: e.tensor_tensor(out=lamv[:, 2:3], in0=lamv[:, 1:2], in1=lamv[:, 0:1], op=ALU.subtract),
                 reads=['lamv'], writes=['lamv'])
            S.op('dve', lambda e: e.tensor_scalar(out=lamv[:, 2:3], in0=lamv[:, 2:3], scalar1=-LAMBDA_INIT, scalar2=None,
                                                  op0=ALU.add), reads=['lamv'], writes=['lamv'])
            S.op('dve', lambda e: e.tensor_scalar(out=lamv[:, 3:4], in0=subg[:], scalar1=1.0 - LAMBDA_INIT, scalar2=None,
                                                  op0=ALU.mult), reads=['lamv', 'subg'], writes=['lamv'])
            nkc = 34
            acnt = 0
            for hh in range(8):
                for t_, c0, k_ in [(wq, hh * 128, 'wq'), (wk, 1024 + hh * 128, 'wk'), (wv, 2048 + hh * 128, 'wv')]:
                    S.dma('pool', t_[:], w_qkv[:, c0:c0 + 128].rearrange("(k p) f -> p k f", p=128), writes=[k_])
                convert_experts(1, 4 * hh, 4 * hh + 4)
                for ti, (off, cnt, _, _, r) in enumerate(TILES):
                    for which in ([0, 1] if ti < 4 else [1]):
                        wa, ka, dst, kdst = (wq, 'wq', QT, 'QT') if which == 0 else (wk, 'wk', KT, 'KT')
                        q = (ti + which) % 2

                        def mmp(e, wt, p):
                            for kc in range(8):
                                ins = e.matmul(PS[p][:, :cnt], lhsT=wt[:, kc, :], rhs=hbuf[:, kc, off:off + cnt],
                                               start=(kc == 0), stop=(kc == 7))
                            return ins
                        S.op('pe', lambda e: mmp(e, wa, 0), reads=[ka, ('h', ti)], writes=[pk(0)])
                        S.op('act', lambda e: e.activation(out=qbt[q][:, :cnt], in_=PS[0][:, :cnt], func=AF.Identity),
                             reads=[pk(0)], writes=[('qbt', q)])
                        S.op('pe', lambda e: e.matmul(PS[1][:, :cnt], lhsT=rmb[:], rhs=qbt[q][:, :cnt], start=True, stop=True),
                             reads=['rmb', ('qbt', q)], writes=[pk(1)])
                        S.op('dve', lambda e: e.tensor_tensor(out=rt1[q][:, :cnt], in0=PS[0][:, :cnt], in1=cost[:, off:off + cnt],
                                                              op=ALU.mult), reads=[pk(0), 'cos', ('qbt', q)], writes=[('rt1', q)])
                        S.op('dve', lambda e: e.tensor_tensor(out=rt2[q][:, :cnt], in0=PS[1][:, :cnt], in1=sint[:, off:off + cnt],
                                                              op=ALU.mult), reads=[pk(1), 'sin'], writes=[('rt2', q)])
                        S.op('dve', lambda e: e.tensor_tensor(out=dst[:, off:off + cnt], in0=rt1[q][:, :cnt], in1=rt2[q][:, :cnt],
                                                              op=ALU.add), reads=[('rt1', q), ('rt2', q)], writes=[kdst])
                    nsb = cnt // 128
                    qv = ti % 2
                    S.op('pe', lambda e: mmp(e, wv, 5), reads=['wv', ('h', ti)], writes=[pk(5)])
                    S.op('act', lambda e: e.activation(out=vtb[qv][:, :cnt], in_=PS[5][:, :cnt], func=AF.Identity),
                         reads=[pk(5)], writes=[('vtb', qv)])
                    p6b = PS[6].bitcast(BF16)

                    def mmvt(e):
                        for s in range(nsb):
                            ins = e.transpose(out=p6b[:, s * 128:(s + 1) * 128], in_=vtb[qv][:, s * 128:(s + 1) * 128],
                                              identity=identb[:])
                        return ins
                    S.op('pe', mmvt, reads=[('vtb', qv), 'identb'], writes=[pk(6)])
                    si0 = off // 128
                    S.op('dve', lambda e: e.tensor_copy(out=Vt[:, si0:si0 + nsb, :].rearrange("p s v -> p (s v)"),
                                                        in_=p6b[:, :nsb * 128]), reads=[pk(6)], writes=['Vt'])
                for qt in range(4):
                    q0 = qt * 512
                    pend_ = None
                    for kc in range(nkc + 1):
                        if kc < nkc:
                            sidx = acnt % 2
                            acnt += 1
                            sb0 = 2 * sidx
                            for mi in range(2):
                                lo_, hi_ = mi * 64, (mi + 1) * 64
                                S.op('pe', lambda e: e.matmul(PS[sb0 + mi][:, :], lhsT=KT[lo_:hi_, kc * 128:(kc + 1) * 128],
                                                              rhs=QT[lo_:hi_, q0:q0 + 512], start=True, stop=True),
                                     reads=['KT', 'QT'], writes=[pk(sb0 + mi)])
                            ke = ('Eb', sidx)
                            S.op('act', lambda e: e.activation(out=Eb2[sidx][:].rearrange("p m q -> p (m q)"),
                                                               in_=psbig[:, sb0 * 512:(sb0 + 2) * 512], func=AF.Exp, scale=0.125),
                                 reads=[pk(sb0), pk(sb0 + 1)], writes=[ke])
                            cur = (Eb2[sidx], ke)
                        if pend_ is not None:
                            kp, (ebp, kep) = pend_
                            for mi in range(2):
                                S.op('pe', lambda e: e.matmul(PS[4 + mi][:, :], lhsT=Vt[:, kp, :], rhs=ebp[:, mi, :], start=(kp == 0),
                                                              stop=(kp == nkc - 1)), reads=['Vt', kep], writes=[pk(4 + mi)])
                            accv = psbig[:, 6 * 512:8 * 512]
                            ebf = ebp[:].rearrange("p m q -> p (m q)")
                            if kp == 0:
                                S.op('dve', lambda e: e.tensor_copy(out=accv, in_=ebf), reads=[kep], writes=[pk(6), pk(7)])
                            else:
                                S.op('dve', lambda e: e.tensor_tensor(out=accv, in0=accv, in1=ebf, op=ALU.add),
                                     reads=[kep, pk(6), pk(7)], writes=[pk(6), pk(7)])
                        pend_ = (kc, cur) if kc < nkc else None
                    S.op('act', lambda e: e.activation(out=acc2[:].rearrange("p m q -> p (m q)"), in_=psbig[:, 6 * 512:8 * 512],
                                                       func=AF.Identity), reads=[pk(6), pk(7)], writes=['acc2'])
                    for mi in range(2):
                        S.op('pe', lambda e: e.matmul(PS[mi][:, :], lhsT=ones32[:], rhs=acc2[:, mi, :], start=True, stop=True),
                             reads=['ones32', 'acc2'], writes=[pk(mi)])
                    rlf = rlw[:].rearrange("p m q -> p (m q)")
                    S.op('act', lambda e: e.activation(out=rlf, in_=psbig[:, 0:1024], func=AF.Ln), reads=[pk(0), pk(1)], writes=['rlw'])
                    S.op('act', lambda e: e.activation(out=rlf, in_=rlf, func=AF.Exp, scale=-1.0), reads=['rlw'], writes=['rlw'])
                    S.op('dve', lambda e: e.tensor_tensor(out=obw[:].rearrange("p m q -> p (m q)"), in0=psbig[:, 4 * 512:6 * 512],
                                                          in1=rlf, op=ALU.mult),
                         reads=[pk(4), pk(5), 'rlw'], writes=[('ob', 0), ('ob', 1)])
                    S.op('dve', lambda e: e.scalar_tensor_tensor(out=ob[0][:], in0=ob[1][:], scalar=lamv[:, 2:3], in1=ob[0][:],
                                                                 op0=ALU.mult, op1=ALU.add),
                         reads=[('ob', 0), ('ob', 1), 'lamv'], writes=[('ob', 0)])
                    S.op('act', lambda e: e.activation(out=osq[:], in_=ob[0][:], func=AF.Square), reads=[('ob', 0)], writes=['osq'])
                    S.op('pe', lambda e: e.matmul(PS[2][:, :], lhsT=ones32[:], rhs=osq[:], start=True, stop=True),
                         reads=['ones32', 'osq'], writes=[pk(2)])
                    S.op('act', lambda e: e.activation(out=osq[:], in_=PS[2][:, :], func=AF.Ln, bias=eps_t[:, 0:1],
                                                       scale=1.0 / 128.0), reads=[pk(2), 'eps'], writes=['osq'])
                    S.op('act', lambda e: e.activation(out=osq[:], in_=osq[:], func=AF.Exp, scale=-0.5), reads=['osq'], writes=['osq'])
                    a_ = aot[qt % 2]
                    S.op('dve', lambda e: e.scalar_tensor_tensor(out=a_[:], in0=ob[0][:], scalar=lamv[:, 3:4], in1=osq[:],
                                                                 op0=ALU.mult, op1=ALU.mult),
                         reads=[('ob', 0), 'osq', 'lamv'], writes=[('aot', qt % 2)])
                    S.dma('sp', zscr[hh, :, q0:q0 + 512], a_[:], reads=[('aot', qt % 2)], writes=[('zs', qt)])
            S.barrier()
        hst.close()

        phase_C(1, w_o, list(range(4)), xr)
        phase_D(1, list(range(4)), final=True)
        S.barrier()
    return nc


def _prep_shared(inp, rev):
    dsl = slice(None, None, -1) if rev else slice(None)
    f = lambda a: np.ascontiguousarray(np.asarray(a, dtype=np.float32))
    pj = lambda v: f(np.asarray(v).reshape(8, 128).T)
    sh = {}
    sh["w_mod"] = f(inp["w_mod"])
    sh["bmod"] = f(np.asarray(inp["b_mod"]).reshape(2, 48, 128).transpose(2, 0, 1))
    sh["n1g"] = f(np.asarray(inp["norm1_g"]).reshape(2, 8, 128).transpose(2, 0, 1))
    sh["n2g"] = f(np.asarray(inp["norm2_g"]).reshape(2, 8, 128).transpose(2, 0, 1))
    sh["fing"] = pj(inp["final_g"])
    sh["w_in"] = f(inp["rg_w_in"][0])
    cw = np.asarray(inp["rg_conv_w"][0])
    z1 = np.zeros((1, 1024), np.float32)
    cw5 = np.concatenate([cw, z1], 0) if not rev else np.concatenate([z1, cw[::-1]], 0)
    sh["convw"] = f(cw5.reshape(5, 8, 128).transpose(2, 1, 0))
    sh["convb"] = pj(inp["rg_conv_b"][0])
    sh["gaw"] = f(np.asarray(inp["rg_gate_a_w"][0])[dsl])
    sh["gxw"] = f(np.asarray(inp["rg_gate_x_w"][0])[dsl])
    sh["gab"] = f(np.asarray(inp["rg_gate_a_b"][0])[dsl].reshape(2, 8, 128).transpose(2, 0, 1))
    sh["gxb"] = f(np.asarray(inp["rg_gate_x_b"][0])[dsl].reshape(2, 8, 128).transpose(2, 0, 1))
    sh["lam"] = f(np.asarray(inp["rg_lambda"][0])[dsl].reshape(2, 8, 128).transpose(2, 0, 1))
    sh["w_out"] = f(inp["rg_w_out"][0])
    sh["w_qkv"] = f(inp["da_w_qkv"][0])
    sh["dalam"] = f(np.broadcast_to(np.asarray(inp["da_lambda"][0])[None], (128, 4, 64)))
    sh["subg"] = f(np.asarray(inp["da_subln_g"][0]).reshape(128, 1))
    sh["w_o"] = f(inp["da_w_o"][0])
    sh["rw"] = f(np.asarray(inp["router_w"]).reshape(8, 128, 32).transpose(1, 0, 2))
    sh["rb"] = f(np.broadcast_to(np.asarray(inp["router_bias"])[None], (128, 32)))
    sh["wg"] = f(inp["moe_w_gate"])
    sh["wu"] = f(inp["moe_w_up"])
    sh["wd"] = f(inp["moe_w_down"])
    t = np.arange(NX)
    row = (t // 64).astype(np.float32)
    col = (t % 64).astype(np.float32)
    inv = (1.0 / (10000.0 ** (np.arange(16, dtype=np.float32) / 16))).astype(np.float32)
    ang = np.stack([row, col], -1)[:, :, None] * inv
    ang = np.broadcast_to(ang[:, :, None, :], (NX, 2, 2, 16)).reshape(NX, 64).astype(np.float32)
    cos = np.ones((128, NT), np.float32)
    sin = np.zeros((128, NT), np.float32)
    if rev:
        ang = ang[::-1]
    cos[:, :NX] = np.tile(np.cos(ang).T, (2, 1))
    sin[:, :NX] = np.tile(np.sin(ang).T, (2, 1))
    sh["pcol"] = np.arange(128, dtype=np.float32).reshape(128, 1)
    sh["blkoff"] = np.ascontiguousarray(np.broadcast_to((128.0 * np.arange(100, dtype=np.float32))[None], (128, 100)))
    rm = np.zeros((128, 128), np.float32)
    for m in range(128):
        if (m % 32) < 16:
            rm[m + 16, m] = -1.0
        else:
            rm[m - 16, m] = 1.0
    sh["rmat"] = rm
    sh["cos"] = cos
    sh["sin"] = sin
    return sh


def _prep_core(inp, b, rev):
    f = lambda a: np.ascontiguousarray(np.asarray(a, dtype=np.float32))
    x = np.asarray(inp["x"][b])
    ctx = np.asarray(inp["ctx"][b])
    if rev:
        x = x[::-1]
        ctx = ctx[::-1]
    tok = np.concatenate([x, ctx], axis=0)
    d = {"xc": f(tok.T.reshape(8, 128, NT))}
    cond = np.stack([np.asarray(inp["c"][b]), np.asarray(inp["c_ctx"])], -1)
    d["cond"] = f(cond.reshape(8, 128, 2).transpose(1, 0, 2))
    return d


def kernel(**inputs):
    nc = build(DEBUG)
    shs = [_prep_shared(inputs, False), _prep_shared(inputs, True)]
    in_maps = []
    for core in range(8):
        b, rev = core // 2, core % 2
        m = dict(shs[rev])
        m.update(_prep_core(inputs, b, bool(rev)))
        in_maps.append(m)
    res = run_bass_kernel_spmd(nc, in_maps, core_ids=list(range(8)))
    out = np.empty((4, NX, 1024), np.float32)
    for core in range(8):
        b, rev = core // 2, core % 2
        o = np.asarray(res.results[core]["outT"]).reshape(1024, NQ).T
        if rev:
            out[b, NX - NQ:] = o[::-1]
        else:
            out[b, :NQ] = o
    return out
```

```python
import contextlib
import math
import numpy as np
import concourse.bass as bass
import concourse.mybir as mybir
from concourse.bass_utils import run_bass_kernel_spmd

F32 = mybir.dt.float32
BF16 = mybir.dt.bfloat16
ALU = mybir.AluOpType
AF = mybir.ActivationFunctionType
AX = mybir.AxisListType

EPS = 1e-6
NT = 4352
NX = 4096
NCTX = 256
TILES = [(i * 512, 512, 2 + i * 512, i * 512, 0) for i in range(8)] + [(4096, 256, 4102, 4100, 1)]
UW = 4360
UCW = 4356
NQ = 2048
LAMBDA_INIT = 0.8 - 0.6 * math.exp(-0.3 * 1)
DEBUG = False


class Sched:
    ROT = 30000

    def __init__(self, nc, stack):
        self.nc = nc
        self.stack = stack
        self.engs = {'pe': nc.tensor, 'act': nc.scalar, 'dve': nc.vector,
                     'pool': nc.gpsimd, 'sp': nc.sync}
        self.cur = {}
        self.nsem = 0
        for e in self.engs:
            self.cur[e] = [self._newsem(e), 0]
        self.lastw = {}
        self.readers = {}
        self.waited = {e: {} for e in self.engs}
        self.dsem = {}
        self.alltok = {}

    def _newsem(self, nm):
        self.nsem += 1
        return self.stack.enter_context(self.nc.semaphore(f"s_{nm}_{self.nsem}"))

    def _wait(self, eng, toks):
        best = {}
        for (s, v) in toks:
            k = id(s)
            if k not in best or best[k][1] < v:
                best[k] = (s, v)
        for k, (s, v) in best.items():
            if self.waited[eng].get(k, 0) >= v:
                continue
            self.engs[eng].wait_ge(s, v)
            self.waited[eng][k] = v

    def _deps(self, eng, reads, writes, skip_same_pe=True):
        toks = []
        for k in reads:
            if k in self.lastw:
                toks.append(self.lastw[k])
        for k in writes:
            if k in self.lastw:
                toks.append(self.lastw[k])
            toks.extend(self.readers.get(k, ()))
        if eng == 'pe' and skip_same_pe:
            toks = [t for t in toks if t[0] is not self.cur['pe'][0]]
        return toks

    def _commit(self, tok, reads, writes):
        for k in writes:
            self.lastw[k] = tok
            self.readers[k] = []
        for k in reads:
            if k in writes:
                continue
            self.readers.setdefault(k, []).append(tok)
        self.alltok[id(tok[0])] = tok

    def op(self, eng, fn, reads=(), writes=()):
        self._wait(eng, self._deps(eng, reads, writes))
        c = self.cur[eng]
        if c[1] >= self.ROT:
            c[0] = self._newsem(eng)
            c[1] = 0
        ins = fn(self.engs[eng])
        c[1] += 1
        ins.then_inc(c[0], 1)
        self._commit((c[0], c[1]), reads, writes)

    def dma(self, eng, out, in_, reads=(), writes=(), slot=None, **kw):
        if slot is None:
            slot = ('auto',) + tuple(writes)
        self._wait(eng, self._deps(eng, reads, writes, skip_same_pe=False))
        if slot not in self.dsem:
            self.dsem[slot] = [self._newsem('d'), 0]
        d = self.dsem[slot]
        ins = self.engs[eng].dma_start(out=out, in_=in_, **kw)
        d[1] += 16
        ins.then_inc(d[0], 16)
        self._commit((d[0], d[1]), reads, writes)

    def barrier(self):
        toks = list(self.alltok.values())
        for e in self.engs:
            self._wait(e, toks)


def bc_last(a, n):
    return bass.AP(a.tensor, a.offset, [list(x) for x in a.ap] + [[0, n]])


def bc_mid(a, n):
    l = [list(x) for x in a.ap]
    return bass.AP(a.tensor, a.offset, [l[0], [0, n]] + l[1:])


def build(dbg=False):
    nc = bass.Bass("TRN2", target_bir_lowering=False)

    def din(name, shape, dt=F32):
        return nc.dram_tensor(name, list(shape), dt, kind="ExternalInput").ap()

    xc = din("xc", [8, 128, NT])
    cond_in = din("cond", [128, 8, 2])
    w_mod = din("w_mod", [2, 1024, 6144])
    bmod_in = din("bmod", [128, 2, 48])
    n1g_in = din("n1g", [128, 2, 8])
    n2g_in = din("n2g", [128, 2, 8])
    fing_in = din("fing", [128, 8])
    w_in = din("w_in", [1024, 2048])
    convw_in = din("convw", [128, 8, 5])
    convb_in = din("convb", [128, 8])
    gaw = din("gaw", [2, 8, 128, 128])
    gxw = din("gxw", [2, 8, 128, 128])
    gab_in = din("gab", [128, 2, 8])
    gxb_in = din("gxb", [128, 2, 8])
    lam_in = din("lam", [128, 2, 8])
    w_out = din("w_out", [1024, 1024])
    w_qkv = din("w_qkv", [1024, 3072])
    dalam_in = din("dalam", [128, 4, 64])
    subg_in = din("subg", [128, 1])
    w_o = din("w_o", [1024, 1024])
    rw_in = din("rw", [128, 8, 32])
    rb_in = din("rb", [128, 32])
    wg = din("wg", [2, 32, 1024, 512])
    wu = din("wu", [2, 32, 1024, 512])
    wd = din("wd", [2, 32, 512, 1024])
    rmat_in = din("rmat", [128, 128])
    cos_in = din("cos", [128, NT])
    sin_in = din("sin", [128, NT])
    outT = nc.dram_tensor("outT", [8, 128, NQ], F32, kind="ExternalOutput").ap()
    xr = nc.dram_tensor("xr", [8, 128, NT], F32, kind="Internal").ap()
    zscr = nc.dram_tensor("zscr", [8, 128, NT], BF16, kind="Internal").ap()
    h2tm = nc.dram_tensor("h2tm", [NT, 1024], BF16, kind="Internal").ap()
    Xs = nc.dram_tensor("Xs", [100 * 128, 1024], BF16, kind="Internal").ap()
    Ys = nc.dram_tensor("Ys", [100 * 128, 1024], F32, kind="Internal").ap()
    wgb = nc.dram_tensor("wgb", [2 * 8192, 2048], BF16, kind="Internal").ap()
    wub = nc.dram_tensor("wub", [2 * 8192, 2048], BF16, kind="Internal").ap()
    wdb = nc.dram_tensor("wdb", [2 * 8192, 2048], BF16, kind="Internal").ap()
    pcol_in = din("pcol", [128, 1])
    blkoff_in = din("blkoff", [128, 100])
    dbgo = {}
    if dbg:
        for nm, shp in [("d_mod", [128, 2 * 48 * 2]), ("d_h", [128, 8, NT]), ("d_z", [8, 128, NT]),
                        ("d_x1", [8, 128, NT]), ("d_x2", [8, 128, NT]),
                        ("d_ao", [8, 128, NT])]:
            dbgo[nm] = nc.dram_tensor(nm, shp, F32, kind="ExternalOutput").ap()

    with contextlib.ExitStack() as st:
        S = Sched(nc, st)

        uid = [0]

        def sb(stack, name, shape, dt):
            uid[0] += 1
            return stack.enter_context(nc.sbuf_tensor(f"sb{uid[0]}_{name}", list(shape), dt))

        psbig = st.enter_context(nc.psum_tensor("psbig", [128, 8 * 512], F32))
        PS = [psbig[:, i * 512:(i + 1) * 512] for i in range(8)]
        pk = lambda i: ('ps', i)
        wgv_all = wg.rearrange("l e (q r) f -> (l e q) (r f)", r=4)
        wuv_all = wu.rearrange("l e (q r) f -> (l e q) (r f)", r=4)
        wdv_all = wd.rearrange("l e (q r) d -> (l e q) (r d)", r=2)

        def convert_experts(l, e0, e1):
            for e_ in range(e0, e1):
                r0 = (l * 32 + e_) * 256
                for dst, src in [(wgb, wgv_all), (wub, wuv_all), (wdb, wdv_all)]:
                    S.dma('pool', dst[r0:r0 + 256, :], src[r0:r0 + 256, :], writes=[('wcv', l)])

        ones_bf = sb(st, "ones_bf", [128, 128], BF16)
        ones32 = sb(st, "ones32", [128, 128], F32)
        ident = sb(st, "ident", [128, 128], F32)
        identb = sb(st, "identb", [128, 128], BF16)
        modT = sb(st, "modT", [128, 2, 48, 2], F32)
        gmT = sb(st, "gmT", [128, 2, 2, 8, 2], F32)
        n1g = sb(st, "n1g", [128, 2, 8], F32)
        n2g = sb(st, "n2g", [128, 2, 8], F32)
        fing = sb(st, "fing", [128, 8], F32)
        rw = sb(st, "rw", [128, 8, 32], F32)
        rb = sb(st, "rb", [128, 32], F32)
        S.op('dve', lambda e: e.memset(ones_bf[:], 1.0), writes=['ones_bf'])
        S.op('dve', lambda e: e.memset(ones32[:], 1.0), writes=['ones32'])
        S.op('pool', lambda e: e.memset(ident[:], 1.0), writes=['ident'])
        S.op('pool', lambda e: e.affine_select(out=ident[:], in_=ident[:], pattern=[[-1, 128]],
                                               compare_op=ALU.is_equal, fill=0.0, base=0, channel_multiplier=1),
             reads=['ident'], writes=['ident'])
        S.op('act', lambda e: e.activation(out=identb[:], in_=ident[:], func=AF.Identity), reads=['ident'], writes=['identb'])
        for t, src, k in [(n1g, n1g_in, 'n1g'), (n2g, n2g_in, 'n2g'), (fing, fing_in, 'fing'),
                          (rw, rw_in, 'rw'), (rb, rb_in, 'rb')]:
            S.dma('sp', t[:], src, writes=[k])

        with contextlib.ExitStack() as ph:
            condt = sb(ph, "condt", [128, 8, 2], F32)
            scond = sb(ph, "scond", [128, 8, 2], F32)
            bm = sb(ph, "bm", [128, 2, 48], F32)
            wm = [sb(ph, f"wm{i}", [128, 8, 1024], F32) for i in range(2)]
            S.dma('sp', condt[:], cond_in, writes=['cond'])
            S.dma('sp', bm[:], bmod_in, writes=['bm'])
            S.op('act', lambda e: e.activation(out=scond[:], in_=condt[:], func=AF.Silu),
                 reads=['cond'], writes=['scond'])
            it = 0
            for l in range(2):
                for gi in range(6):
                    buf = wm[it % 2]
                    key = ('wm', it % 2)
                    S.dma('sp', buf[:], w_mod[l, :, gi * 1024:(gi + 1) * 1024].rearrange("(k p) f -> p k f", p=128),
                          writes=[key])
                    pst = PS[it % 2]

                    def mm(e, buf=buf, pst=pst):
                        for j in range(8):
                            for kc in range(8):
                                ins = e.matmul(pst[:, j * 2:(j + 1) * 2], lhsT=buf[:, kc, j * 128:(j + 1) * 128],
                                               rhs=scond[:, kc, :], start=(kc == 0), stop=(kc == 7))
                        return ins
                    S.op('pe', mm, reads=[key, 'scond'], writes=[pk(it % 2)])
                    S.op('dve', lambda e: e.tensor_tensor(
                        out=modT[:, l, gi * 8:(gi + 1) * 8, :],
                        in0=pst[:, 0:16].rearrange("p (j r) -> p j r", r=2),
                        in1=bc_last(bm[:, l, gi * 8:(gi + 1) * 8], 2), op=ALU.add),
                        reads=[pk(it % 2), 'bm'], writes=['modT'])
                    it += 1
            for l in range(2):
                for w_, (gt, gk, sidx) in enumerate([(n1g, 'n1g', 1), (n2g, 'n2g', 4)]):
                    S.op('dve', lambda e: e.tensor_scalar(out=gmT[:, l, w_], in0=modT[:, l, sidx * 8:(sidx + 1) * 8, :],
                                                          scalar1=1.0, scalar2=None, op0=ALU.add),
                         reads=['modT'], writes=['gmT'])
                    S.op('dve', lambda e: e.tensor_tensor(out=gmT[:, l, w_], in0=gmT[:, l, w_],
                                                          in1=bc_last(gt[:, l, :], 2), op=ALU.mult),
                         reads=['gmT', gk], writes=['gmT'])
            if dbg:
                S.dma('sp', dbgo["d_mod"], modT[:].rearrange("p a b c -> p (a b c)"), reads=['modT'], writes=['d_mod'])
            S.barrier()

        def mod_ap(l, idx, j, r):
            return modT[:, l, idx * 8 + j, r:r + 1]

        def norm_mod(tmp, xt, n, kx, out, kout, gm_of_j, sh_of_j, psi, extra_reads=(), tag=''):
            sq, rt, rstd, xn = tmp
            kxl = list(kx) if isinstance(kx, list) else [kx]
            S.op('act', lambda e: e.activation(out=sq[:, :, :n], in_=xt[:, :, :n], func=AF.Square),
                 reads=kxl, writes=['nm_sq' + tag])

            def mm(e):
                for j in range(8):
                    ins = e.matmul(PS[psi][:, :n], lhsT=ones_bf[:], rhs=sq[:, j, :n], start=(j == 0), stop=(j == 7))
                return ins
            S.op('pe', mm, reads=['nm_sq' + tag, 'ones_bf'], writes=[pk(psi)])
            S.op('act', lambda e: e.activation(out=rt[:, :n], in_=PS[psi][:, :n], func=AF.Ln,
                                               bias=eps_t[:, 0:1], scale=1.0 / 1024.0),
                 reads=[pk(psi), 'eps'], writes=['nm_rt' + tag])
            S.op('act', lambda e: e.activation(out=rstd[:, :n], in_=rt[:, :n], func=AF.Exp, scale=-0.5),
                 reads=['nm_rt' + tag], writes=['nm_rstd' + tag])
            S.op('dve', lambda e: e.tensor_tensor(out=xn[:, :, :n], in0=xt[:, :, :n], in1=bc_mid(rstd[:, :n], 8),
                                                  op=ALU.mult), reads=kxl + ['nm_rstd' + tag], writes=['nm_xn' + tag])
            for j in range(8):
                if sh_of_j is None:
                    S.op('dve', lambda e: e.tensor_scalar(out=out(j), in0=xn[:, j, :n], scalar1=gm_of_j(j),
                                                          scalar2=None, op0=ALU.mult),
                         reads=['nm_xn' + tag] + list(extra_reads), writes=[kout])
                elif j % 2 == 0:
                    S.op('dve', lambda e: e.tensor_scalar(out=out(j), in0=xn[:, j, :n], scalar1=gm_of_j(j),
                                                          scalar2=sh_of_j(j), op0=ALU.mult, op1=ALU.add),
                         reads=['nm_xn' + tag] + list(extra_reads), writes=[kout])
                else:
                    S.op('act', lambda e: e.activation(out=out(j), in_=xn[:, j, :n], func=AF.Identity,
                                                       bias=sh_of_j(j), scale=gm_of_j(j)),
                         reads=['nm_xn' + tag] + list(extra_reads), writes=[kout])

        def norm_tmp(ph):
            return (sb(ph, "nm_sq", [128, 8, 512], BF16), sb(ph, "nm_rt", [128, 512], F32),
                    sb(ph, "nm_rstd", [128, 512], F32), sb(ph, "nm_xn", [128, 8, 512], F32))

        eps_t = sb(st, "eps_t", [128, 1], F32)
        S.op('dve', lambda e: e.memset(eps_t[:], EPS), writes=['eps'])
        one_t = sb(st, "one_t", [128, 1], F32)
        S.op('dve', lambda e: e.memset(one_t[:], 1.0), writes=['one'])


        def phase_A(l, src, hbuf):
            with contextlib.ExitStack() as ph:
                xts = [sb(ph, f"xt{i}", [128, 8, 512], F32) for i in range(2)]
                tmp = norm_tmp(ph)
                for ti, (off, cnt, _, _, r) in enumerate(TILES):
                    xt = xts[ti % 2]
                    kx = ('xt', ti % 2)
                    S.dma('sp', xt[:, :, :cnt], src[:, :, off:off + cnt].rearrange("j p t -> p j t"),
                          reads=[('xr', ti)], writes=[kx])
                    norm_mod(tmp, xt, cnt, kx, lambda j: hbuf[:, j, off:off + cnt], ('h', ti),
                             lambda j: gmT[:, l, 0, j, r:r + 1], lambda j: mod_ap(l, 0, j, r), 7,
                             extra_reads=['gmT', 'modT'])
                S.barrier()

        hst = contextlib.ExitStack()
        hbuf = sb(hst, "hbuf", [128, 8, NT], BF16)
        phase_A(0, xc, hbuf)
        if dbg:
            with contextlib.ExitStack() as ph:
                t32 = sb(ph, "dbg32", [128, 8, 512], F32)
                for ti, (off, cnt, _, _, r) in enumerate(TILES):
                    S.op('dve', lambda e: e.tensor_copy(out=t32[:, :, :cnt], in_=hbuf[:, :, off:off + cnt]),
                         reads=[('h', ti)], writes=['dbg32'])
                    S.dma('sp', dbgo["d_h"][:, :, off:off + cnt], t32[:, :, :cnt], reads=['dbg32'], writes=['d_h'])
                S.barrier()

        with contextlib.ExitStack() as ph:
            ub = sb(ph, "ub", [128, UW], F32)
            uc = sb(ph, "uc", [128, UCW], F32)
            ucb = sb(ph, "ucb", [128, UCW], BF16)
            gy = sb(ph, "gy", [128, NT], BF16)
            wyu = [sb(ph, f"wyu{i}", [128, 8, 256], BF16) for i in range(2)]
            gw = [sb(ph, f"gw{i}", [128, 4, 128], BF16) for i in range(2)]
            convw = sb(ph, "convw", [128, 8, 5], F32)
            convb = sb(ph, "convb", [128, 8], F32)
            gab = sb(ph, "gab", [128, 2, 8], F32)
            gxb = sb(ph, "gxb", [128, 2, 8], F32)
            lamt = sb(ph, "lamt", [128, 2, 8], F32)
            cneg = sb(ph, "cneg", [128, 2, 8], F32)
            cneg2 = sb(ph, "cneg2", [128, 2, 8], F32)
            rbuf = [sb(ph, f"rbuf{i}", [128, 512], F32) for i in range(2)]
            ibuf = [sb(ph, f"ibuf{i}", [128, 512], F32) for i in range(2)]
            sbuf_ = [sb(ph, f"sbuf{i}", [128, 512], F32) for i in range(2)]
            hbt = [sb(ph, f"hbt{i}", [128, 512], F32) for i in range(2)]
            gt1 = [sb(ph, f"gt1{i}", [128, 512], F32) for i in range(2)]
            zt = [sb(ph, f"zt{i}", [128, 512], BF16) for i in range(2)]
            for t, src, k in [(convw, convw_in, 'convw'), (convb, convb_in, 'convb'), (gab, gab_in, 'gab'),
                              (gxb, gxb_in, 'gxb'), (lamt, lam_in, 'lamt')]:
                S.dma('sp', t[:], src, writes=[k])
            S.op('act', lambda e: e.activation(out=cneg[:], in_=lamt[:], func=AF.Exp, scale=-1.0),
                 reads=['lamt'], writes=['cneg'])
            S.op('act', lambda e: e.activation(out=cneg[:], in_=cneg[:], func=AF.Ln, bias=one_t[:, 0:1], scale=1.0),
                 reads=['cneg', 'one'], writes=['cneg'])
            S.op('dve', lambda e: e.tensor_scalar(out=cneg2[:], in0=cneg[:], scalar1=-16.0, scalar2=None, op0=ALU.mult),
                 reads=['cneg'], writes=['cneg2'])
            S.op('dve', lambda e: e.tensor_scalar(out=cneg[:], in0=cneg[:], scalar1=-8.0, scalar2=None, op0=ALU.mult),
                 reads=['cneg', 'cneg2'], writes=['cneg'])
            zer = sb(ph, "zer", [128, 4, 1024], BF16)
            S.op('pool', lambda e: e.memset(zer[:], 0.0), writes=['zer'])
            ngab = sb(ph, "ngab", [128, 2, 8], F32)
            ngxb = sb(ph, "ngxb", [128, 2, 8], F32)
            S.op('dve', lambda e: e.tensor_scalar(out=ngab[:], in0=gab[:], scalar1=-1.0, scalar2=None, op0=ALU.mult),
                 reads=['gab'], writes=['ngab'])
            S.op('dve', lambda e: e.tensor_scalar(out=ngxb[:], in0=gxb[:], scalar1=-1.0, scalar2=None, op0=ALU.mult),
                 reads=['gxb'], writes=['ngxb'])
            S.op('dve', lambda e: e.memset(ub[:], 0.0), writes=[('ub', ti) for ti in range(9)])
            ubkeys = [('ub', ti) for ti in range(9)]
            cnt_sc = 0
            for n in range(8):
                w = wyu[n % 2]
                kw_ = ('wyu', n % 2)
                S.dma('pool', w[:, :, 0:128], w_in[:, n * 128:(n + 1) * 128].rearrange("(k p) f -> p k f", p=128),
                      writes=[kw_])
                S.dma('pool', w[:, :, 128:256],
                      w_in[:, 1024 + n * 128:1024 + (n + 1) * 128].rearrange("(k p) f -> p k f", p=128), writes=[kw_])
                g = gw[n % 2]
                kg = ('gw', n % 2)
                for d in range(2):
                    S.dma('pool', g[:, 2 * d, :], gaw[d, n], writes=[kg])
                    S.dma('pool', g[:, 2 * d + 1, :], gxw[d, n], writes=[kg])
                convert_experts(0, 4 * n, 4 * n + 4)
                for b4 in range(4 * n, min(4 * n + 4, 25)):
                    S.dma('pool', Xs[b4 * 512:(b4 + 1) * 512, :].rearrange("(a p) f -> p a f", p=128), zer[:],
                          reads=['zer'], writes=['Xs'])
                for ti, (off, cnt, uoff, ucoff, r) in enumerate(TILES):
                    pu, py = (ti % 2) * 2, (ti % 2) * 2 + 1

                    def mmu(e, c0=128, p=pu):
                        for kc in range(8):
                            ins = e.matmul(PS[p][:, :cnt], lhsT=w[:, kc, c0:c0 + 128], rhs=hbuf[:, kc, off:off + cnt],
                                           start=(kc == 0), stop=(kc == 7))
                        return ins
                    S.op('pe', mmu, reads=[kw_, ('h', ti)], writes=[pk(pu)])
                    S.op('act', lambda e: e.activation(out=ub[:, uoff:uoff + cnt], in_=PS[pu][:, :cnt], func=AF.Identity),
                         reads=[pk(pu)], writes=[('ub', ti)])
                    S.op('pe', lambda e: mmu(e, 0, py), reads=[kw_, ('h', ti)], writes=[pk(py)])
                    t1 = gt1[ti % 2]
                    k1 = ('gt1', ti % 2)
                    S.op('act', lambda e: e.activation(out=t1[:, :cnt], in_=PS[py][:, :cnt], func=AF.Square),
                         reads=[pk(py)], writes=[k1])
                    S.op('dve', lambda e: e.tensor_scalar(out=t1[:, :cnt], in0=t1[:, :cnt], scalar1=0.044715, scalar2=1.0,
                                                          op0=ALU.mult, op1=ALU.add), reads=[k1], writes=[k1])
                    S.op('dve', lambda e: e.tensor_tensor(out=t1[:, :cnt], in0=t1[:, :cnt], in1=PS[py][:, :cnt], op=ALU.mult),
                         reads=[k1, pk(py)], writes=[k1])
                    S.op('act', lambda e: e.activation(out=t1[:, :cnt], in_=t1[:, :cnt], func=AF.Sigmoid,
                                                       scale=1.5957691216057308), reads=[k1], writes=[k1])
                    S.op('dve', lambda e: e.tensor_tensor(out=gy[:, off:off + cnt], in0=t1[:, :cnt], in1=PS[py][:, :cnt],
                                                          op=ALU.mult), reads=[k1, pk(py)], writes=[('gy', ti)])
                S.op('dve', lambda e: e.tensor_scalar(out=uc[:], in0=ub[:, 0:UCW], scalar1=convw[:, n, 0:1],
                                                      scalar2=convb[:, n:n + 1], op0=ALU.mult, op1=ALU.add),
                     reads=ubkeys + ['convw', 'convb'], writes=['uc'])
                for k in range(1, 5):
                    S.op('dve', lambda e: e.scalar_tensor_tensor(out=uc[:], in0=ub[:, k:k + UCW], scalar=convw[:, n, k:k + 1],
                                                                 in1=uc[:], op0=ALU.mult, op1=ALU.add),
                         reads=ubkeys + ['convw', 'uc'], writes=['uc'])
                S.op('act', lambda e: e.activation(out=ucb[:], in_=uc[:], func=AF.Identity), reads=['uc'], writes=['ucb'])
                for d in range(2):
                    order = [8] + (list(range(8)) if d == 0 else list(range(7, -1, -1)))
                    prev = None
                    for oi, ti in enumerate(order):
                        off, cnt, uoff, ucoff, r = TILES[ti]
                        b = cnt_sc % 2
                        cnt_sc += 1
                        pr, pi = 4 + 2 * b, 5 + 2 * b
                        S.op('pe', lambda e: e.matmul(PS[pr][:, :cnt], lhsT=g[:, 2 * d, :], rhs=ucb[:, ucoff:ucoff + cnt],
                                                      start=True, stop=True), reads=[kg, 'ucb'], writes=[pk(pr)])
                        S.op('pe', lambda e: e.matmul(PS[pi][:, :cnt], lhsT=g[:, 2 * d + 1, :], rhs=ucb[:, ucoff:ucoff + cnt],
                                                      start=True, stop=True), reads=[kg, 'ucb'], writes=[pk(pi)])
                        rb_, ib_, sb_, hb_ = rbuf[b], ibuf[b], sbuf_[b], hbt[b]
                        kr, ki, ks, kh = ('rbuf', b), ('ibuf', b), ('sbuf', b), ('hbt', b)
                        S.op('act', lambda e: e.activation(out=rb_[:, :cnt], in_=PS[pr][:, :cnt], func=AF.Exp,
                                                           bias=ngab[:, d, n:n + 1], scale=-1.0),
                             reads=[pk(pr), 'ngab'], writes=[kr])
                        S.op('act', lambda e: e.activation(out=ib_[:, :cnt], in_=PS[pi][:, :cnt], func=AF.Exp,
                                                           bias=ngxb[:, d, n:n + 1], scale=-1.0),
                             reads=[pk(pi), 'ngxb'], writes=[ki])
                        for t_, k_ in ((rb_, kr), (ib_, ki)):
                            S.op('act', lambda e: e.activation(out=t_[:, :cnt], in_=t_[:, :cnt], func=AF.Ln,
                                                               bias=one_t[:, 0:1], scale=1.0), reads=[k_, 'one'], writes=[k_])
                            S.op('act', lambda e: e.activation(out=t_[:, :cnt], in_=t_[:, :cnt], func=AF.Exp, scale=-1.0),
                                 reads=[k_], writes=[k_])
                        S.op('act', lambda e: e.activation(out=sb_[:, :cnt], in_=rb_[:, :cnt], func=AF.Exp,
                                                           scale=cneg2[:, d, n:n + 1]), reads=[kr, 'cneg2'], writes=[ks])
                        S.op('act', lambda e: e.activation(out=rb_[:, :cnt], in_=rb_[:, :cnt], func=AF.Exp,
                                                           scale=cneg[:, d, n:n + 1]), reads=[kr, 'cneg'], writes=[kr])
                        S.op('act', lambda e: e.activation(out=sb_[:, :cnt], in_=sb_[:, :cnt], func=AF.Ln,
                                                           bias=one_t[:, 0:1], scale=-1.0), reads=[ks, 'one'], writes=[ks])
                        S.op('act', lambda e: e.activation(out=sb_[:, :cnt], in_=sb_[:, :cnt], func=AF.Exp, scale=0.5),
                             reads=[ks], writes=[ks])
                        S.op('dve', lambda e: e.tensor_tensor(out=ib_[:, :cnt], in0=ib_[:, :cnt],
                                                              in1=uc[:, ucoff:ucoff + cnt], op=ALU.mult),
                             reads=[ki, 'uc'], writes=[ki])
                        S.op('dve', lambda e: e.tensor_tensor(out=ib_[:, :cnt], in0=ib_[:, :cnt], in1=sb_[:, :cnt],
                                                              op=ALU.mult), reads=[ki, ks], writes=[ki])
                        if d == 0:
                            if prev is None:
                                init, kin = 0.0, []
                            else:
                                po, pc, puo, _, _ = TILES[prev]
                                init, kin = ub[:, puo + pc - 1:puo + pc], [('ub', prev)]
                            S.op('dve', lambda e: e.tensor_tensor_scan(out=ub[:, uoff:uoff + cnt], data0=rb_[:, :cnt],
                                                                       data1=ib_[:, :cnt], initial=init,
                                                                       op0=ALU.mult, op1=ALU.add),
                                 reads=[kr, ki] + kin, writes=[('ub', ti)])
                        else:
                            if prev is None:
                                init, kin = 0.0, []
                            else:
                                init, kin = hbt[1 - b][:, 0:1], [('hbt', 1 - b)]
                            S.op('dve', lambda e: e.tensor_tensor_scan(out=hb_[:, :cnt][:, ::-1], data0=rb_[:, :cnt][:, ::-1],
                                                                       data1=ib_[:, :cnt][:, ::-1], initial=init,
                                                                       op0=ALU.mult, op1=ALU.add),
                                 reads=[kr, ki] + kin, writes=[kh])
                            S.op('dve', lambda e: e.tensor_tensor(out=sb_[:, :cnt], in0=hb_[:, :cnt],
                                                                  in1=ub[:, uoff:uoff + cnt], op=ALU.add),
                                 reads=[kh, ('ub', ti)], writes=[ks])
                            z_ = zt[b]
                            S.op('dve', lambda e: e.tensor_tensor(out=z_[:, :cnt], in0=sb_[:, :cnt],
                                                                  in1=gy[:, off:off + cnt], op=ALU.mult),
                                 reads=[ks, ('gy', ti)], writes=[('zt', b)])
                            S.dma('sp', zscr[n, :, off:off + cnt], z_[:, :cnt], reads=[('zt', b)], writes=[('zs', ti)])
                        prev = ti
            S.barrier()
        hst.close()

        I32 = mybir.dt.int32
        MAXSUB = 34
        NBMAX = 100
        BIGW = 2 * 32 * 256 + 64
        utri = sb(st, "utri", [128, 128], F32)
        S.op('pool', lambda e: e.memset(utri[:], 1.0), writes=['utri'])
        S.op('pool', lambda e: e.affine_select(out=utri[:], in_=utri[:], pattern=[[1, 128]],
                                               compare_op=ALU.is_gt, fill=0.0, base=0, channel_multiplier=-1),
             reads=['utri'], writes=['utri'])
        pcol = sb(st, "pcol", [128, 1], F32)
        blkoff = sb(st, "blkoff", [128, NBMAX], F32)
        ones_row = sb(st, "ones_row", [128, 32], F32)
        S.dma('sp', pcol[:], pcol_in, writes=['pcol'])
        S.dma('sp', blkoff[:], blkoff_in, writes=['blkoff'])
        S.op('dve', lambda e: e.memset(ones_row[:], 1.0), writes=['ones_row'])
        onesb = sb(st, "onesb", [128, 4, 32], F32)
        c12 = sb(st, "c12", [128, 2, 4, 32], F32)
        S.op('dve', lambda e: e.memset(onesb[:], 1.0), writes=['onesb'])
        for k2_ in range(2):
            for s_ in range(4):
                S.op('dve', lambda e: e.memset(c12[:, k2_, s_, :], float(2 * s_ + 1 + k2_)), writes=['c12'])
        FM = sb(st, "FM", [128, MAXSUB, 2, 32], F32)
        RK = sb(st, "RK", [128, MAXSUB, 2], F32)
        WK = sb(st, "WK", [128, MAXSUB, 2], F32)
        DI = sb(st, "DI", [128, MAXSUB, 2], I32)
        WI = sb(st, "WI", [128, NBMAX, 2], I32)
        cntm = sb(st, "cntm", [128, 32], F32)

        bregs = {}

        def idma(out, out_off, in_, in_off, bounds, reads, writes, slot):
            S._wait('pool', S._deps('pool', reads, writes, skip_same_pe=False))
            if slot not in S.dsem:
                S.dsem[slot] = [S._newsem('d'), 0]
            d = S.dsem[slot]
            if bounds not in bregs:
                bregs[bounds] = nc.gpsimd.to_reg(bounds)
            ins = nc.gpsimd.indirect_dma_start(out=out, out_offset=out_off, in_=in_, in_offset=in_off,
                                               bounds_check=bregs[bounds], oob_is_err=False)
            d[1] += 16
            ins.then_inc(d[0], 16)
            S._commit((d[0], d[1]), reads, writes)

        def phase_C(l, wmat, tiles, xsrc):
            nsub_tot = sum(TILES[ti][1] // 128 for ti in tiles)
            NB = 2 * nsub_tot + 32
            with contextlib.ExitStack() as ph:
                wsb = sb(ph, "wsb", [128, 8, 1024], BF16)
                zts = [sb(ph, f"zts{i}", [128, 8, 512], BF16) for i in range(2)]
                xts = [sb(ph, f"xtc{i}", [128, 8, 512], F32) for i in range(2)]
                h2fs = [sb(ph, f"h2f{i}", [128, 8, 512], F32) for i in range(2)]
                h2t = [sb(ph, f"h2t{i}", [128, 1024], BF16) for i in range(2)]
                tmps = [norm_tmp(ph), norm_tmp(ph)]
                mk3 = lambda nm: [sb(ph, f"{nm}{i}", [128, 4, 32], F32) for i in range(2)]
                ssel, sg, em, mk, cmv, rk, t3 = mk3("ssel"), mk3("sg"), mk3("em"), mk3("mk"), mk3("cmv"), mk3("rk"), mk3("t3")
                mk16 = lambda nm: [sb(ph, f"{nm}{i}", [128, 16], F32) for i in range(2)]
                m1, m2, gs, gm_ = mk16("m1"), mk16("m2"), mk16("gs"), mk16("gmk")
                sm = [sb(ph, f"sm{i}", [128, 4, 4], F32) for i in range(2)]
                t32 = [sb(ph, f"t32{i}", [128, 32], F32) for i in range(2)]
                S.dma('pool', wsb[:], wmat.rearrange("(k p) f -> p k f", p=128), writes=['wsb'])
                S.op('dve', lambda e: e.memset(cntm[:], 0.0), writes=['cntm'])
                nsub_box = [0]

                def stage_W(ti):
                    off, cnt, _, _, r = TILES[ti]
                    b = ti % 2
                    z_, x_ = zts[b], xts[b]
                    h2f, tmp, kh2 = h2fs[b], tmps[b], ('h2f', b)
                    kz, kx = ('zts', b), ('xtc', b)
                    S.dma('sp', z_[:, :, :cnt], zscr[:, :, off:off + cnt].rearrange("j p t -> p j t"),
                          reads=[('zs', ti)], writes=[kz])
                    S.dma('sp', x_[:, :, :cnt], xsrc[:, :, off:off + cnt].rearrange("j p t -> p j t"),
                          reads=[('xr', ti)], writes=[kx])
                    for j in range(8):
                        p = j % 3

                        def mm(e):
                            for kc in range(8):
                                ins = e.matmul(PS[p][:, :cnt], lhsT=wsb[:, kc, j * 128:(j + 1) * 128], rhs=z_[:, kc, :cnt],
                                               start=(kc == 0), stop=(kc == 7))
                            return ins
                        S.op('pe', mm, reads=['wsb', kz], writes=[pk(p)])
                        S.op('dve', lambda e: e.scalar_tensor_tensor(out=x_[:, j, :cnt], in0=PS[p][:, :cnt],
                                                                     scalar=mod_ap(l, 2, j, r), in1=x_[:, j, :cnt],
                                                                     op0=ALU.mult, op1=ALU.add),
                             reads=[pk(p), kx, 'modT'], writes=[kx])
                    S.dma('sp', xr[:, :, off:off + cnt].rearrange("j p t -> p j t"), x_[:, :, :cnt],
                          reads=[kx], writes=[('xr', ti)])
                    if dbg and l == 0:
                        S.dma('sp', dbgo["d_x1"][:, :, off:off + cnt].rearrange("j p t -> p j t"), x_[:, :, :cnt],
                              reads=[kx], writes=['d_x1'])

                def stage_N(ti):
                    off, cnt, _, _, r = TILES[ti]
                    b = ti % 2
                    z_, x_ = zts[b], xts[b]
                    h2f, tmp, kh2 = h2fs[b], tmps[b], ('h2f', b)
                    kz, kx = ('zts', b), ('xtc', b)
                    norm_mod(tmp, x_, cnt, kx, lambda j: h2f[:, j, :cnt], kh2,
                             lambda j: gmT[:, l, 1, j, r:r + 1], lambda j: mod_ap(l, 3, j, r), 3,
                             extra_reads=['gmT', 'modT'], tag=str(b))

                def stage_R(ti):
                    off, cnt, _, _, r = TILES[ti]
                    b = ti % 2
                    z_, x_ = zts[b], xts[b]
                    h2f, tmp, kh2 = h2fs[b], tmps[b], ('h2f', b)
                    kz, kx = ('zts', b), ('xtc', b)
                    nsb = cnt // 128
                    gs0 = nsub_box[0]
                    nsub_box[0] += nsb
                    q = ti % 2
                    pl = 4 + q
                    W_ = nsb * 32
                    for s in range(nsb):
                        gsi = gs0 + s
                        qq = gsi % 2
                        for half in range(2):
                            pt = 6 + half

                            def mmt(e):
                                for jj in range(4):
                                    j = half * 4 + jj
                                    ins = e.transpose(out=PS[pt][:, jj * 128:(jj + 1) * 128],
                                                      in_=h2f[:, j, s * 128:(s + 1) * 128], identity=ident[:])
                                return ins
                            S.op('pe', mmt, reads=[kh2, 'ident'], writes=[pk(pt)])
                            if half == 0:
                                S.op('act', lambda e: e.activation(out=h2t[qq][:, 0:512], in_=PS[pt][:, :], func=AF.Identity),
                                     reads=[pk(pt)], writes=[('h2t', qq)])
                            else:
                                S.op('pool' if False else 'dve', lambda e: e.tensor_copy(out=h2t[qq][:, 512:1024], in_=PS[pt][:, :]),
                                     reads=[pk(pt)], writes=[('h2t', qq)])
                        S.dma('sp', h2tm[gsi * 128:(gsi + 1) * 128, :], h2t[qq][:], reads=[('h2t', qq)], writes=[('h2tm', gsi % 4)])

                        def mmr(e):
                            for kc in range(8):
                                ins = e.matmul(PS[pl][:, s * 32:(s + 1) * 32], lhsT=h2f[:, kc, s * 128:(s + 1) * 128], rhs=rw[:, kc, :],
                                               start=(kc == 0), stop=(kc == 7))
                            return ins
                        S.op('pe', mmr, reads=[kh2, 'rw'], writes=[pk(pl)])
                    kq = ('rt', q)
                    v3 = lambda t: t[:, :nsb, :]
                    v8 = lambda t: t[:, :nsb, :].rearrange("p s (g e) -> p (s g) e", e=8)
                    f2 = lambda t: t[:, :nsb, :].rearrange("p s e -> p (s e)")
                    g4 = lambda t: t[:, :nsb * 4]
                    g43 = lambda t: t[:, :nsb * 4].rearrange("p (s g) -> p s g", g=4)
                    S.op('act', lambda e: e.activation(out=f2(sg[q]), in_=PS[pl][:, :W_], func=AF.Sigmoid),
                         reads=[pk(pl)], writes=[kq])
                    S.op('dve', lambda e: e.tensor_tensor(out=v3(ssel[q]), in0=v3(sg[q]), in1=bc_mid(rb[:], nsb), op=ALU.add),
                         reads=[kq, 'rb'], writes=[kq])
                    S.op('dve', lambda e: e.tensor_reduce(out=g4(m1[q]), in_=v8(ssel[q]), axis=AX.X, op=ALU.max), reads=[kq], writes=[kq])
                    S.op('dve', lambda e: e.tensor_tensor(out=v8(t3[q]), in0=v8(ssel[q]), in1=bc_last(g4(m1[q]), 8), op=ALU.is_equal),
                         reads=[kq], writes=[kq])
                    S.op('dve', lambda e: e.scalar_tensor_tensor(out=f2(t3[q]), in0=f2(t3[q]), scalar=-1.0e9, in1=f2(ssel[q]),
                                                                 op0=ALU.mult, op1=ALU.add), reads=[kq], writes=[kq])
                    S.op('dve', lambda e: e.tensor_reduce(out=g4(m2[q]), in_=v8(t3[q]), axis=AX.X, op=ALU.max), reads=[kq], writes=[kq])
                    S.op('dve', lambda e: e.tensor_tensor(out=g4(gs[q]), in0=g4(m1[q]), in1=g4(m2[q]), op=ALU.add), reads=[kq], writes=[kq])
                    S.op('dve', lambda e: e.tensor_reduce(out=sm[q][:, 0, :nsb], in_=g43(gs[q]), axis=AX.X, op=ALU.max),
                         reads=[kq], writes=[kq])
                    S.op('dve', lambda e: e.tensor_tensor(out=g43(gm_[q]), in0=g43(gs[q]), in1=bc_last(sm[q][:, 0, :nsb], 4),
                                                          op=ALU.is_equal), reads=[kq], writes=[kq])
                    S.op('dve', lambda e: e.tensor_tensor(out=g4(gs[q]), in0=g4(gm_[q]), in1=g4(m2[q]), op=ALU.mult), reads=[kq], writes=[kq])
                    S.op('dve', lambda e: e.tensor_reduce(out=sm[q][:, 1, :nsb], in_=g43(gs[q]), axis=AX.X, op=ALU.add),
                         reads=[kq], writes=[kq])
                    S.op('dve', lambda e: e.tensor_tensor(out=v3(mk[q]), in0=v3(ssel[q]), in1=bc_last(sm[q][:, 1, :nsb], 32),
                                                          op=ALU.is_ge), reads=[kq], writes=[kq])
                    S.op('dve', lambda e: e.tensor_tensor(out=v8(mk[q]), in0=v8(mk[q]), in1=bc_last(g4(gm_[q]), 8), op=ALU.mult),
                         reads=[kq], writes=[kq])
                    S.op('dve', lambda e: e.tensor_tensor(out=f2(em[q]), in0=f2(mk[q]), in1=f2(sg[q]), op=ALU.mult),
                         reads=[kq], writes=[kq])
                    S.op('dve', lambda e: e.tensor_reduce(out=sm[q][:, 2, :nsb], in_=v3(em[q]), axis=AX.X, op=ALU.add),
                         reads=[kq], writes=[kq])
                    S.op('dve', lambda e: e.reciprocal(out=sm[q][:, 3, :nsb], in_=sm[q][:, 2, :nsb]), reads=[kq], writes=[kq])
                    S.op('dve', lambda e: e.tensor_tensor(out=v3(em[q]), in0=v3(em[q]), in1=bc_last(sm[q][:, 3, :nsb], 32), op=ALU.mult),
                         reads=[kq], writes=[kq])

                    def mmk(e):
                        for s in range(nsb):
                            o_ = PS[pl][:, 128 + s * 32:128 + (s + 1) * 32]
                            e.matmul(o_, lhsT=utri[:], rhs=mk[q][:, s, :], start=True, stop=False)
                            for s2 in range(s):
                                e.matmul(o_, lhsT=ones32[:], rhs=mk[q][:, s2, :], start=False, stop=False)
                            ins = e.matmul(o_, lhsT=ones32[:], rhs=cntm[:], start=False, stop=True)
                        return ins
                    S.op('pe', mmk, reads=[kq, 'utri', 'ones32', 'cntm'], writes=[pk(pl)])
                    S.op('dve', lambda e: e.tensor_copy(out=f2(rk[q]), in_=PS[pl][:, 128:128 + W_]), reads=[pk(pl)], writes=[kq])
                    S.op('dve', lambda e: e.tensor_reduce(out=t32[q][:], in_=mk[q][:, :nsb, :].rearrange("p s e -> p e s"), axis=AX.X,
                                                          op=ALU.add), reads=[kq], writes=[kq])
                    S.op('dve', lambda e: e.tensor_tensor(out=cntm[:], in0=cntm[:], in1=t32[q][:], op=ALU.add),
                         reads=[kq, 'cntm'], writes=['cntm'])
                    S.op('dve', lambda e: e.tensor_tensor_scan(out=f2(cmv[q]), data0=f2(onesb), data1=f2(mk[q]), initial=0.0,
                                                               op0=ALU.mult, op1=ALU.add), reads=[kq, 'onesb'], writes=[kq])
                    for k2 in range(2):
                        fm_ = FM[:, gs0:gs0 + nsb, k2, :]
                        S.op('dve', lambda e: e.tensor_tensor(out=v3(t3[q]), in0=v3(cmv[q]), in1=c12[:, k2, :nsb, :], op=ALU.is_equal),
                             reads=[kq, 'c12'], writes=[kq])
                        S.op('dve', lambda e: e.tensor_tensor(out=fm_, in0=v3(t3[q]), in1=v3(mk[q]), op=ALU.mult),
                             reads=[kq], writes=['FM'])
                        S.op('dve', lambda e: e.tensor_tensor(out=v3(t3[q]), in0=fm_, in1=v3(rk[q]), op=ALU.mult),
                             reads=[kq, 'FM'], writes=[kq])
                        S.op('dve', lambda e: e.tensor_reduce(out=RK[:, gs0:gs0 + nsb, k2], in_=v3(t3[q]), axis=AX.X, op=ALU.add),
                             reads=[kq], writes=['RK'])
                        S.op('dve', lambda e: e.tensor_tensor(out=v3(t3[q]), in0=fm_, in1=v3(em[q]), op=ALU.mult),
                             reads=[kq, 'FM'], writes=[kq])
                        S.op('dve', lambda e: e.tensor_reduce(out=WK[:, gs0:gs0 + nsb, k2], in_=v3(t3[q]), axis=AX.X, op=ALU.add),
                             reads=[kq], writes=['WK'])

                stage_W(tiles[0])
                for i_, ti in enumerate(tiles):
                    stage_N(ti)
                    if i_ + 1 < len(tiles):
                        stage_W(tiles[i_ + 1])
                    stage_R(ti)
                S.barrier()
            with contextlib.ExitStack() as ph:
                J = nsub_tot
                cb = sb(ph, "cb", [128, 32], F32)
                nblk = sb(ph, "nblk", [128, 32], F32)
                pend = sb(ph, "pend", [128, 32], F32)
                pst = sb(ph, "pst", [128, 32], F32)
                big = sb(ph, "bigc", [128, 32, NBMAX], F32)
                eb = sb(ph, "eb", [128, NBMAX], F32)
                chg = sb(ph, "chg", [128, NBMAX], F32)
                wif = sb(ph, "wif", [128, NBMAX, 2], F32)
                dtmp2 = sb(ph, "dtmp2", [128, MAXSUB, 32], F32)
                dif = sb(ph, "dif", [128, MAXSUB, 2], F32)
                rows = [sb(ph, f"rows{i}", [128, 1024], BF16) for i in range(2)]
                S.op('pe', lambda e: e.matmul(PS[0][:, 0:32], lhsT=ones32[:], rhs=cntm[:], start=True, stop=True),
                     reads=['ones32', 'cntm'], writes=[pk(0)])
                S.op('dve', lambda e: e.tensor_copy(out=cb[:], in_=PS[0][:, 0:32]), reads=[pk(0)], writes=['cb'])
                S.op('dve', lambda e: e.tensor_tensor(out=big[:, :, :J], in0=bc_last(cb[:], J), in1=bc_mid(blkoff[:, :J], 32),
                                                      op=ALU.is_gt), reads=['cb', 'blkoff'], writes=['big'])
                S.op('dve', lambda e: e.tensor_reduce(out=nblk[:], in_=big[:, :, :J], axis=AX.X, op=ALU.add),
                     reads=['big'], writes=['nblk'])
                S.op('dve', lambda e: e.tensor_tensor_scan(out=pend[:], data0=ones_row[:], data1=nblk[:], initial=0.0,
                                                           op0=ALU.mult, op1=ALU.add), reads=['nblk', 'ones_row'], writes=['pend'])
                S.op('dve', lambda e: e.tensor_tensor(out=pst[:], in0=pend[:], in1=nblk[:], op=ALU.subtract),
                     reads=['pend', 'nblk'], writes=['pst'])
                S.op('dve', lambda e: e.tensor_scalar(out=pst[:], in0=pst[:], scalar1=128.0, scalar2=None, op0=ALU.mult),
                     reads=['pst'], writes=['pst'])
                S.op('dve', lambda e: e.tensor_scalar(out=pend[:], in0=pend[:], scalar1=128.0, scalar2=None, op0=ALU.mult),
                     reads=['pend'], writes=['pend'])
                S.op('dve', lambda e: e.tensor_tensor(out=big[:, :, :NB].rearrange("p e b -> p b e"),
                                                      in0=bc_mid(pend[:], NB), in1=bc_last(blkoff[:, :NB], 32),
                                                      op=ALU.is_le), reads=['pend', 'blkoff', 'big'], writes=['big'])
                S.op('dve', lambda e: e.tensor_reduce(out=eb[:, :NB], in_=big[:, :, :NB].rearrange("p e b -> p b e"),
                                                      axis=AX.X, op=ALU.add), reads=['big'], writes=['eb'])
                S.op('dve', lambda e: e.tensor_scalar(out=eb[:, :NB], in0=eb[:, :NB], scalar1=31.0, scalar2=None, op0=ALU.min),
                     reads=['eb'], writes=['eb'])
                S.op('dve', lambda e: e.memset(chg[:], 1.0), writes=['chg'])
                S.op('dve', lambda e: e.tensor_tensor(out=chg[:, 1:NB], in0=eb[:, 1:NB], in1=eb[:, 0:NB - 1], op=ALU.not_equal),
                     reads=['eb', 'chg'], writes=['chg'])
                for k4 in range(1, 4):
                    S.op('dve', lambda e: e.memset(chg[:, k4 * (NB // 4):k4 * (NB // 4) + 1], 1.0), reads=['chg'], writes=['chg'])
                for h in range(1):
                    S.op('dve', lambda e: e.tensor_scalar(out=wif[:, :NB, h], in0=eb[:, :NB], scalar1=128.0,
                                                          scalar2=float(l * 4096 - BIGW), op0=ALU.mult, op1=ALU.add),
                         reads=['eb', 'wif'], writes=['wif'])
                    S.op('dve', lambda e: e.tensor_scalar(out=wif[:, :NB, h], in0=wif[:, :NB, h], scalar1=pcol[:, 0:1],
                                                          scalar2=None, op0=ALU.add), reads=['wif', 'pcol'], writes=['wif'])
                    S.op('dve', lambda e: e.tensor_tensor(out=wif[:, :NB, h], in0=wif[:, :NB, h], in1=chg[:, :NB], op=ALU.mult),
                         reads=['wif', 'chg'], writes=['wif'])
                    S.op('dve', lambda e: e.tensor_scalar(out=wif[:, :NB, h], in0=wif[:, :NB, h], scalar1=float(BIGW),
                                                          scalar2=None, op0=ALU.add), reads=['wif'], writes=['wif'])
                S.op('dve', lambda e: e.tensor_copy(out=WI[:, :NB, 0:1], in_=wif[:, :NB, 0:1]), reads=['wif'], writes=['WI'])
                for k2 in range(2):
                    S.op('dve', lambda e: e.tensor_tensor(out=dtmp2[:, :J, :], in0=FM[:, :J, k2, :], in1=bc_mid(pst[:], J),
                                                          op=ALU.mult), reads=['FM', 'pst', 'dtmp2'], writes=['dtmp2'])
                    S.op('dve', lambda e: e.tensor_reduce(out=dif[:, :J, k2], in_=dtmp2[:, :J, :], axis=AX.X, op=ALU.add),
                         reads=['dtmp2', 'dif'], writes=['dif'])
                S.op('dve', lambda e: e.tensor_tensor(out=dif[:, :J, :], in0=dif[:, :J, :], in1=RK[:, :J, :], op=ALU.add),
                     reads=['dif', 'RK'], writes=['dif'])
                S.op('dve', lambda e: e.tensor_copy(out=DI[:, :J, :], in_=dif[:, :J, :]), reads=['dif'], writes=['DI'])
                for gsi in range(J):
                    q = gsi % 2
                    S.dma('sp', rows[q][:], h2tm[gsi * 128:(gsi + 1) * 128, :], reads=[('h2tm', gsi % 4)], writes=[('rows', q)])
                    for k2 in range(2):
                        idma(Xs[:, :], bass.IndirectOffsetOnAxis(ap=DI[:, gsi, k2:k2 + 1], axis=0), rows[q][:, :], None,
                             NB * 128 - 1, reads=[('rows', q), 'DI', 'Xs'], writes=[('Xsc', q)], slot=('Xsc', q))
                S.barrier()

        def phase_D(l, tiles, final):
            nsub_tot = sum(TILES[ti][1] // 128 for ti in tiles)
            NB = 2 * nsub_tot + 32
            wgv = wgb.rearrange("(a b) f -> a (b f)", b=2)
            wuv = wub.rearrange("(a b) f -> a (b f)", b=2)
            wdv = wdb.rearrange("(a b) f -> a (b f)", b=2)
            with contextlib.ExitStack() as ph:
                wgs = [sb(ph, f"wgs{i}", [128, 4096], BF16) for i in range(4)]
                wus = [sb(ph, f"wus{i}", [128, 4096], BF16) for i in range(4)]
                wds = [sb(ph, f"wds{i}", [128, 4096], BF16) for i in range(4)]
                blk = lambda p_: (p_ % 4) * (NB // 4) + p_ // 4
                xbs = [sb(ph, f"xbs{i}", [128, 1024], BF16) for i in range(2)]
                XTs = [sb(ph, f"XTs{i}", [128, 8, 128], BF16) for i in range(2)]
                s1s = [sb(ph, f"s1s{i}", [128, 512], F32) for i in range(2)]
                ATs = [sb(ph, f"ATs{i}", [128, 4, 128], BF16) for i in range(2)]
                Yts = [sb(ph, f"Yts{i}", [128, 1024], F32) for i in range(2)]

                def loadw(p_):
                    w_ = p_ % 4
                    for nm, view, tile_ in [('wgs', wgv, wgs[w_]), ('wus', wuv, wus[w_]), ('wds', wdv, wds[w_])]:
                        idma(tile_[:, :], None, view[:, :], bass.IndirectOffsetOnAxis(ap=WI[:, blk(p_), 0:1], axis=0),
                             (l + 1) * 4096 - 1, reads=['WI', ('wcv', l)], writes=[(nm, w_)], slot=(nm, w_))
                for p_ in range(3):
                    loadw(p_)
                def xbload(p_):
                    bn = blk(p_)
                    S.dma('sp', xbs[p_ % 2][:], Xs[bn * 128:(bn + 1) * 128, :], reads=[('Xsc', 0), ('Xsc', 1), 'Xs'],
                          writes=[('xbs', p_ % 2)])

                def stage_T(p_):
                    q = p_ % 2
                    xb, XT = xbs[q], XTs[q]
                    ptb = PS[q].bitcast(BF16)

                    def mmt(e):
                        for c in range(8):
                            ins = e.transpose(out=ptb[:, c * 128:(c + 1) * 128], in_=xb[:, c:1024:8], identity=identb[:])
                        return ins
                    S.op('pe', mmt, reads=[('xbs', q), 'identb'], writes=[pk(q)])
                    S.op('act', lambda e: e.activation(out=XT[:].rearrange("p c s -> p (c s)"), in_=ptb[:, :], func=AF.Identity),
                         reads=[pk(q)], writes=[('XTs', q)])

                def stage_G(p_):
                    q = p_ % 2
                    ws_ = p_ % 4
                    XT, AT = XTs[q], ATs[q]
                    wg_, wu_ = wgs[ws_], wus[ws_]
                    p1, p2 = 2 + 2 * q, 3 + 2 * q

                    def mmg(e, wt, p):
                        for fo in range(4):
                            for c in range(8):
                                c0 = c * 512 + fo
                                ins = e.matmul(PS[p][:, fo * 128:(fo + 1) * 128], lhsT=wt[:, c0:c0 + 509:4], rhs=XT[:, c, :],
                                               start=(c == 0), stop=(c == 7))
                        return ins
                    S.op('pe', lambda e: mmg(e, wg_, p1), reads=[('wgs', ws_), ('XTs', q)], writes=[pk(p1)])
                    S.op('pe', lambda e: mmg(e, wu_, p2), reads=[('wus', ws_), ('XTs', q)], writes=[pk(p2)])
                    S.op('act', lambda e: e.activation(out=s1s[q][:], in_=PS[p1][:, :], func=AF.Silu),
                         reads=[pk(p1)], writes=[('s1s', q)])
                    S.op('dve', lambda e: e.tensor_tensor(out=AT[:].rearrange("p c s -> p (c s)"), in0=s1s[q][:], in1=PS[p2][:, :],
                                                          op=ALU.mult), reads=[('s1s', q), pk(p2)], writes=[('ATs', q)])

                def stage_D(p_):
                    q = p_ % 2
                    ws_ = p_ % 4
                    b = blk(p_)
                    AT, Yt, wd_ = ATs[q], Yts[q], wds[ws_]
                    for dh in range(2):
                        py = 6 + dh

                        def mmd(e):
                            for fo in range(4):
                                ins = e.matmul(PS[py][:, :], lhsT=AT[:, fo, :],
                                               rhs=wd_[:, fo * 1024 + dh * 512:fo * 1024 + (dh + 1) * 512],
                                               start=(fo == 0), stop=(fo == 3))
                            return ins
                        S.op('pe', mmd, reads=[('wds', ws_), ('ATs', q)], writes=[pk(py)])
                        if dh == 0:
                            S.op('act', lambda e: e.activation(out=Yt[:, 0:512], in_=PS[py][:, :], func=AF.Identity),
                                 reads=[pk(py)], writes=[('Yts', q)])
                        else:
                            S.op('dve', lambda e: e.tensor_copy(out=Yt[:, 512:1024], in_=PS[py][:, :]),
                                 reads=[pk(py)], writes=[('Yts', q)])
                    S.dma('sp', Ys[b * 128:(b + 1) * 128, :], Yt[:], reads=[('Yts', q)], writes=[('Ys', q)])

                xbload(0)
                xbload(1)
                stage_T(0)
                for pos in range(NB):
                    if pos + 3 < NB:
                        loadw(pos + 3)
                    stage_G(pos)
                    if pos + 1 < NB:
                        stage_T(pos + 1)
                    if pos + 2 < NB:
                        xbload(pos + 2)
                    stage_D(pos)
                S.barrier()
            with contextlib.ExitStack() as ph3:
                xts = [sb(ph3, f"xtd{i}", [128, 8, 512], F32) for i in range(2)]
                g1 = [sb(ph3, f"g1{i}", [128, 1024], F32) for i in range(4)]
                g2 = [sb(ph3, f"g2{i}", [128, 1024], F32) for i in range(4)]
                tmp = norm_tmp(ph3) if final else None
                ots = [sb(ph3, f"otd{i}", [128, 8, 512], F32) for i in range(2)] if final else None
                subs = []
                for li, ti in enumerate(tiles):
                    for s_ in range(TILES[ti][1] // 128):
                        subs.append((li, ti, s_, len(subs)))

                def stage_a(li, ti, s, gsi):
                    off, cnt, _, _, r = TILES[ti]
                    b = li % 2
                    if s == 0:
                        S.dma('sp', xts[b][:, :, :cnt], xr[:, :, off:off + cnt].rearrange("j p t -> p j t"),
                              reads=[('xr', ti)], writes=[('xtd', b, j) for j in range(8)])
                    q = gsi % 4
                    idma(g1[q][:, :], None, Ys[:, :], bass.IndirectOffsetOnAxis(ap=DI[:, gsi, 0:1], axis=0),
                         NB * 128 - 1, reads=['DI', ('Ys', 0), ('Ys', 1)], writes=[('g1', q)], slot=('g1', q))
                    idma(g2[q][:, :], None, Ys[:, :], bass.IndirectOffsetOnAxis(ap=DI[:, gsi, 1:2], axis=0),
                         NB * 128 - 1, reads=['DI', ('Ys', 0), ('Ys', 1)], writes=[('g2', q)], slot=('g2', q))
                    S.op('dve', lambda e: e.tensor_scalar(out=g1[q][:], in0=g1[q][:], scalar1=WK[:, gsi, 0:1], scalar2=None,
                                                          op0=ALU.mult), reads=[('g1', q), 'WK'], writes=[('g1', q)])
                    S.op('dve', lambda e: e.scalar_tensor_tensor(out=g1[q][:], in0=g2[q][:], scalar=WK[:, gsi, 1:2],
                                                                 in1=g1[q][:], op0=ALU.mult, op1=ALU.add),
                         reads=[('g1', q), ('g2', q), 'WK'], writes=[('g1', q)])
                    for half in range(2):
                        pt = 2 * (q % 2) + half

                        def mmt2(e):
                            for jj in range(4):
                                j = half * 4 + jj
                                ins = e.transpose(out=PS[pt][:, jj * 128:(jj + 1) * 128], in_=g1[q][:, j * 128:(j + 1) * 128],
                                                  identity=ident[:])
                            return ins
                        S.op('pe', mmt2, reads=[('g1', q), 'ident'], writes=[pk(pt)])

                def stage_b(li, ti, s, gsi):
                    off, cnt, _, _, r = TILES[ti]
                    b = li % 2
                    x_ = xts[b]
                    q = gsi % 4
                    kxs = [('xtd', b, j) for j in range(8)]
                    for half in range(2):
                        pt = 2 * (q % 2) + half
                        for jj in range(4):
                            j = half * 4 + jj
                            S.op('dve', lambda e: e.scalar_tensor_tensor(out=x_[:, j, s * 128:(s + 1) * 128],
                                                                         in0=PS[pt][:, jj * 128:(jj + 1) * 128],
                                                                         scalar=mod_ap(l, 5, j, r),
                                                                         in1=x_[:, j, s * 128:(s + 1) * 128],
                                                                         op0=ALU.mult, op1=ALU.add),
                                 reads=[pk(pt), kxs[j], ('modT', l)], writes=[kxs[j]])
                    if s == cnt // 128 - 1:
                        if not final:
                            S.dma('sp', xr[:, :, off:off + cnt].rearrange("j p t -> p j t"), x_[:, :, :cnt],
                                  reads=kxs, writes=[('xr', ti)])
                            if dbg:
                                S.dma('sp', dbgo["d_x2"][:, :, off:off + cnt].rearrange("j p t -> p j t"), x_[:, :, :cnt],
                                      reads=kxs, writes=['d_x2'])
                        else:
                            o_ = ots[b]
                            norm_mod(tmp, x_, cnt, kxs, lambda j: o_[:, j, :cnt], ('otd', b),
                                     lambda j: fing[:, j:j + 1], None, 7, extra_reads=['fing'])
                            S.dma('sp', outT[:, :, off:off + cnt].rearrange("j p t -> p j t"), o_[:, :, :cnt],
                                  reads=[('otd', b)], writes=[('out', b)])

                for idx in range(len(subs) + 1):
                    if idx < len(subs):
                        stage_a(*subs[idx])
                    if idx >= 1:
                        stage_b(*subs[idx - 1])
                S.barrier()

        phase_C(0, w_out, list(range(9)), xc)
        phase_D(0, list(range(9)), final=False)

        hst = contextlib.ExitStack()
        hbuf = sb(hst, "hbuf1", [128, 8, NT], BF16)
        phase_A(1, xr, hbuf)
        with contextlib.ExitStack() as ph:
            cost = sb(ph, "cost", [128, NT], F32)
            sint = sb(ph, "sint", [128, NT], F32)
            dal = sb(ph, "dal", [128, 4, 64], F32)
            dtmp = sb(ph, "dtmp", [128, 64], F32)
            lamv = sb(ph, "lamv", [128, 4], F32)
            subg = sb(ph, "subg", [128, 1], F32)
            wq = sb(ph, "wq", [128, 8, 128], BF16)
            wk = sb(ph, "wk", [128, 8, 128], BF16)
            wv = sb(ph, "wv", [128, 8, 128], BF16)
            qbt = [sb(ph, f"qbt{i}", [128, 512], BF16) for i in range(2)]
            rmb = sb(ph, "rmb", [128, 128], BF16)
            QT = sb(ph, "QT", [128, NQ], BF16)
            vtb = [sb(ph, f"vtb{i}", [128, 512], BF16) for i in range(2)]
            KT = sb(ph, "KT", [128, NT], BF16)
            Vt = sb(ph, "Vt", [128, 34, 128], BF16)
            rt1 = [sb(ph, f"rt1{i}", [128, 512], F32) for i in range(2)]
            rt2 = [sb(ph, f"rt2{i}", [128, 512], F32) for i in range(2)]
            Eb2 = [sb(ph, f"Eb{i}", [128, 2, 512], BF16) for i in range(2)]
            acc2 = sb(ph, "acc2", [128, 2, 512], F32)
            obw = sb(ph, "obw", [128, 2, 512], F32)
            ob = [obw[:, 0, :], obw[:, 1, :]]
            rlw = sb(ph, "rlw", [128, 2, 512], F32)
            rl = sb(ph, "rl", [128, 512], F32)
            accD = sb(ph, "accD", [128, 512], F32)
            accP = sb(ph, "accP", [128, 512], F32)
            osq = sb(ph, "osq", [128, 512], F32)
            aot = [sb(ph, f"aot{i}", [128, 512], BF16) for i in range(2)]
            S.dma('sp', cost[:], cos_in, writes=['cos'])
            S.dma('pool', rmb[:], rmat_in, writes=['rmb'])
            S.dma('sp', sint[:], sin_in, writes=['sin'])
            S.dma('sp', dal[:], dalam_in, writes=['dal'])
            S.dma('sp', subg[:], subg_in, writes=['subg'])
            for i2 in range(2):
                S.op('dve', lambda e: e.tensor_tensor(out=dtmp[:], in0=dal[:, 2 * i2, :], in1=dal[:, 2 * i2 + 1, :], op=ALU.mult),
                     reads=['dal'], writes=['dtmp'])
                S.op('dve', lambda e: e.tensor_reduce(out=lamv[:, i2:i2 + 1], in_=dtmp[:], axis=AX.X, op=ALU.add),
                     reads=['dtmp'], writes=['lamv'])
            S.op('act', lambda e: e.activation(out=lamv[:, 0:2], in_=lamv[:, 0:2], func=AF.Exp), reads=['lamv'], writes=['lamv'])
            S.op('dve', lambda e: e.tensor_tensor(out=lamv[:, 2:3], in0=lamv[:, 1:2], in1=lamv[:, 0:1], op=ALU.subtract),
                 reads=['lamv'], writes=['lamv'])
            S.op('dve', lambda e: e.tensor_scalar(out=lamv[:, 2:3], in0=lamv[:, 2:3], scalar1=-LAMBDA_INIT, scalar2=None,
                                                  op0=ALU.add), reads=['lamv'], writes=['lamv'])
            S.op('dve', lambda e: e.tensor_scalar(out=lamv[:, 3:4], in0=subg[:], scalar1=1.0 - LAMBDA_INIT, scalar2=None,
                                                  op0=ALU.mult), reads=['lamv', 'subg'], writes=['lamv'])
            nkc = 34
            acnt = 0
            for hh in range(8):
                for t_, c0, k_ in [(wq, hh * 128, 'wq'), (wk, 1024 + hh * 128, 'wk'), (wv, 2048 + hh * 128, 'wv')]:
                    S.dma('pool', t_[:], w_qkv[:, c0:c0 + 128].rearrange("(k p) f -> p k f", p=128), writes=[k_])
                convert_experts(1, 4 * hh, 4 * hh + 4)
                for ti, (off, cnt, _, _, r) in enumerate(TILES):
                    for which in ([0, 1] if ti < 4 else [1]):
                        wa, ka, dst, kdst = (wq, 'wq', QT, 'QT') if which == 0 else (wk, 'wk', KT, 'KT')
                        q = (ti + which) % 2

                        def mmp(e, wt, p):
                            for kc in range(8):
                                ins = e.matmul(PS[p][:, :cnt], lhsT=wt[:, kc, :], rhs=hbuf[:, kc, off:off + cnt],
                                               start=(kc == 0), stop=(kc == 7))
                            return ins
                        S.op('pe', lambda e: mmp(e, wa, 0), reads=[ka, ('h', ti)], writes=[pk(0)])
                        S.op('act', lambda e: e.activation(out=qbt[q][:, :cnt], in_=PS[0][:, :cnt], func=AF.Identity),
                             reads=[pk(0)], writes=[('qbt', q)])
                        S.op('pe', lambda e: e.matmul(PS[1][:, :cnt], lhsT=rmb[:], rhs=qbt[q][:, :cnt], start=True, stop=True),
                             reads=['rmb', ('qbt', q)], writes=[pk(1)])
                        S.op('dve', lambda e: e.tensor_tensor(out=rt1[q][:, :cnt], in0=PS[0][:, :cnt], in1=cost[:, off:off + cnt],
                                                              op=ALU.mult), reads=[pk(0), 'cos', ('qbt', q)], writes=[('rt1', q)])
                        S.op('dve', lambda e: e.tensor_tensor(out=rt2[q][:, :cnt], in0=PS[1][:, :cnt], in1=sint[:, off:off + cnt],
                                                              op=ALU.mult), reads=[pk(1), 'sin'], writes=[('rt2', q)])
                        S.op('dve', lambda e: e.tensor_tensor(out=dst[:, off:off + cnt], in0=rt1[q][:, :cnt], in1=rt2[q][:, :cnt],
                                                              op=ALU.add), reads=[('rt1', q), ('rt2', q)], writes=[kdst])
                    nsb = cnt // 128
                    qv = ti % 2
                    S.op('pe', lambda e: mmp(e, wv, 5), reads=['wv', ('h', ti)], writes=[pk(5)])
                    S.op('act', lambda e: e.activation(out=vtb[qv][:, :cnt], in_=PS[5][:, :cnt], func=AF.Identity),
                         reads=[pk(5)], writes=[('vtb', qv)])
                    p6b = PS[6].bitcast(BF16)

                    def mmvt(e):
                        for s in range(nsb):
                            ins = e.transpose(out=p6b[:, s * 128:(s + 1) * 128], in_=vtb[qv][:, s * 128:(s + 1) * 128],
                                              identity=identb[:])
                        return ins
                    S.op('pe', mmvt, reads=[('vtb', qv), 'identb'], writes=[pk(6)])
                    si0 = off // 128
                    S.op('dve', lambda e: e.tensor_copy(out=Vt[:, si0:si0 + nsb, :].rearrange("p s v -> p (s v)"),
                                                        in_=p6b[:, :nsb * 128]), reads=[pk(6)], writes=['Vt'])
                for qt in range(4):
                    q0 = qt * 512
                    pend_ = None
                    for kc in range(nkc + 1):
                        if kc < nkc:
                            sidx = acnt % 2
                            acnt += 1
                            sb0 = 2 * sidx
                            for mi in range(2):
                                lo_, hi_ = mi * 64, (mi + 1) * 64
                                S.op('pe', lambda e: e.matmul(PS[sb0 + mi][:, :], lhsT=KT[lo_:hi_, kc * 128:(kc + 1) * 128],
                                                              rhs=QT[lo_:hi_, q0:q0 + 512], start=True, stop=True),
                                     reads=['KT', 'QT'], writes=[pk(sb0 + mi)])
                            ke = ('Eb', sidx)
                            S.op('act', lambda e: e.activation(out=Eb2[sidx][:].rearrange("p m q -> p (m q)"),
                                                               in_=psbig[:, sb0 * 512:(sb0 + 2) * 512], func=AF.Exp, scale=0.125),
                                 reads=[pk(sb0), pk(sb0 + 1)], writes=[ke])
                            cur = (Eb2[sidx], ke)
                        if pend_ is not None:
                            kp, (ebp, kep) = pend_
                            for mi in range(2):
                                S.op('pe', lambda e: e.matmul(PS[4 + mi][:, :], lhsT=Vt[:, kp, :], rhs=ebp[:, mi, :], start=(kp == 0),
                                                              stop=(kp == nkc - 1)), reads=['Vt', kep], writes=[pk(4 + mi)])
                            accv = psbig[:, 6 * 512:8 * 512]
                            ebf = ebp[:].rearrange("p m q -> p (m q)")
                            if kp == 0:
                                S.op('dve', lambda e: e.tensor_copy(out=accv, in_=ebf), reads=[kep], writes=[pk(6), pk(7)])
                            else:
                                S.op('dve', lambda e: e.tensor_tensor(out=accv, in0=accv, in1=ebf, op=ALU.add),
                                     reads=[kep, pk(6), pk(7)], writes=[pk(6), pk(7)])
                        pend_ = (kc, cur) if kc < nkc else None
                    S.op('act', lambda e: e.activation(out=acc2[:].rearrange("p m q -> p (m q)"), in_=psbig[:, 6 * 512:8 * 512],
                                                       func=AF.Identity), reads=[pk(6), pk(7)], writes=['acc2'])
                    for mi in range(2):
                        S.op('pe', lambda e: e.matmul(PS[mi][:, :], lhsT=ones32[:], rhs=acc2[:, mi, :], start=True, stop=True),
                             reads=['ones32', 'acc2'], writes=[pk(mi)])
                    rlf = rlw[:].rearrange("p m q -> p (m q)")
                    S.op('act', lambda e: e.activation(out=rlf, in_=psbig[:, 0:1024], func=AF.Ln), reads=[pk(0), pk(1)], writes=['rlw'])
                    S.op('act', lambda e: e.activation(out=rlf, in_=rlf, func=AF.Exp, scale=-1.0), reads=['rlw'], writes=['rlw'])
                    S.op('dve', lambda e: e.tensor_tensor(out=obw[:].rearrange("p m q -> p (m q)"), in0=psbig[:, 4 * 512:6 * 512],
                                                          in1=rlf, op=ALU.mult),
                         reads=[pk(4), pk(5), 'rlw'], writes=[('ob', 0), ('ob', 1)])
                    S.op('dve', lambda e: e.scalar_tensor_tensor(out=ob[0][:], in0=ob[1][:], scalar=lamv[:, 2:3], in1=ob[0][:],
                                                                 op0=ALU.mult, op1=ALU.add),
                         reads=[('ob', 0), ('ob', 1), 'lamv'], writes=[('ob', 0)])
                    S.op('act', lambda e: e.activation(out=osq[:], in_=ob[0][:], func=AF.Square), reads=[('ob', 0)], writes=['osq'])
                    S.op('pe', lambda e: e.matmul(PS[2][:, :], lhsT=ones32[:], rhs=osq[:], start=True, stop=True),
                         reads=['ones32', 'osq'], writes=[pk(2)])
                    S.op('act', lambda e: e.activation(out=osq[:], in_=PS[2][:, :], func=AF.Ln, bias=eps_t[:, 0:1],
                                                       scale=1.0 / 128.0), reads=[pk(2), 'eps'], writes=['osq'])
                    S.op('act', lambda e: e.activation(out=osq[:], in_=osq[:], func=AF.Exp, scale=-0.5), reads=['osq'], writes=['osq'])
                    a_ = aot[qt % 2]
                    S.op('dve', lambda e: e.scalar_tensor_tensor(out=a_[:], in0=ob[0][:], scalar=lamv[:, 3:4], in1=osq[:],
                                                                 op0=ALU.mult, op1=ALU.mult),
                         reads=[('ob', 0), 'osq', 'lamv'], writes=[('aot', qt % 2)])
                    S.dma('sp', zscr[hh, :, q0:q0 + 512], a_[:], reads=[('aot', qt % 2)], writes=[('zs', qt)])
            S.barrier()
        hst.close()

        phase_C(1, w_o, list(range(4)), xr)
        phase_D(1, list(range(4)), final=True)
        S.barrier()
    return nc


def _prep_shared(inp, rev):
    dsl = slice(None, None, -1) if rev else slice(None)
    f = lambda a: np.ascontiguousarray(np.asarray(a, dtype=np.float32))
    pj = lambda v: f(np.asarray(v).reshape(8, 128).T)
    sh = {}
    sh["w_mod"] = f(inp["w_mod"])
    sh["bmod"] = f(np.asarray(inp["b_mod"]).reshape(2, 48, 128).transpose(2, 0, 1))
    sh["n1g"] = f(np.asarray(inp["norm1_g"]).reshape(2, 8, 128).transpose(2, 0, 1))
    sh["n2g"] = f(np.asarray(inp["norm2_g"]).reshape(2, 8, 128).transpose(2, 0, 1))
    sh["fing"] = pj(inp["final_g"])
    sh["w_in"] = f(inp["rg_w_in"][0])
    cw = np.asarray(inp["rg_conv_w"][0])
    z1 = np.zeros((1, 1024), np.float32)
    cw5 = np.concatenate([cw, z1], 0) if not rev else np.concatenate([z1, cw[::-1]], 0)
    sh["convw"] = f(cw5.reshape(5, 8, 128).transpose(2, 1, 0))
    sh["convb"] = pj(inp["rg_conv_b"][0])
    sh["gaw"] = f(np.asarray(inp["rg_gate_a_w"][0])[dsl])
    sh["gxw"] = f(np.asarray(inp["rg_gate_x_w"][0])[dsl])
    sh["gab"] = f(np.asarray(inp["rg_gate_a_b"][0])[dsl].reshape(2, 8, 128).transpose(2, 0, 1))
    sh["gxb"] = f(np.asarray(inp["rg_gate_x_b"][0])[dsl].reshape(2, 8, 128).transpose(2, 0, 1))
    sh["lam"] = f(np.asarray(inp["rg_lambda"][0])[dsl].reshape(2, 8, 128).transpose(2, 0, 1))
    sh["w_out"] = f(inp["rg_w_out"][0])
    sh["w_qkv"] = f(inp["da_w_qkv"][0])
    sh["dalam"] = f(np.broadcast_to(np.asarray(inp["da_lambda"][0])[None], (128, 4, 64)))
    sh["subg"] = f(np.asarray(inp["da_subln_g"][0]).reshape(128, 1))
    sh["w_o"] = f(inp["da_w_o"][0])
    sh["rw"] = f(np.asarray(inp["router_w"]).reshape(8, 128, 32).transpose(1, 0, 2))
    sh["rb"] = f(np.broadcast_to(np.asarray(inp["router_bias"])[None], (128, 32)))
    sh["wg"] = f(inp["moe_w_gate"])
    sh["wu"] = f(inp["moe_w_up"])
    sh["wd"] = f(inp["moe_w_down"])
    t = np.arange(NX)
    row = (t // 64).astype(np.float32)
    col = (t % 64).astype(np.float32)
    inv = (1.0 / (10000.0 ** (np.arange(16, dtype=np.float32) / 16))).astype(np.float32)
    ang = np.stack([row, col], -1)[:, :, None] * inv
    ang = np.broadcast_to(ang[:, :, None, :], (NX, 2, 2, 16)).reshape(NX, 64).astype(np.float32)
    cos = np.ones((128, NT), np.float32)
    sin = np.zeros((128, NT), np.float32)
    if rev:
        ang = ang[::-1]
    cos[:, :NX] = np.tile(np.cos(ang).T, (2, 1))
    sin[:, :NX] = np.tile(np.sin(ang).T, (2, 1))
    sh["pcol"] = np.arange(128, dtype=np.float32).reshape(128, 1)
    sh["blkoff"] = np.ascontiguousarray(np.broadcast_to((128.0 * np.arange(100, dtype=np.float32))[None], (128, 100)))
    rm = np.zeros((128, 128), np.float32)
    for m in range(128):
        if (m % 32) < 16:
            rm[m + 16, m] = -1.0
        else:
            rm[m - 16, m] = 1.0
    sh["rmat"] = rm
    sh["cos"] = cos
    sh["sin"] = sin
    return sh


def _prep_core(inp, b, rev):
    f = lambda a: np.ascontiguousarray(np.asarray(a, dtype=np.float32))
    x = np.asarray(inp["x"][b])
    ctx = np.asarray(inp["ctx"][b])
    if rev:
        x = x[::-1]
        ctx = ctx[::-1]
    tok = np.concatenate([x, ctx], axis=0)
    d = {"xc": f(tok.T.reshape(8, 128, NT))}
    cond = np.stack([np.asarray(inp["c"][b]), np.asarray(inp["c_ctx"])], -1)
    d["cond"] = f(cond.reshape(8, 128, 2).transpose(1, 0, 2))
    return d


def kernel(**inputs):
    nc = build(DEBUG)
    shs = [_prep_shared(inputs, False), _prep_shared(inputs, True)]
    in_maps = []
    for core in range(8):
        b, rev = core // 2, core % 2
        m = dict(shs[rev])
        m.update(_prep_core(inputs, b, bool(rev)))
        in_maps.append(m)
    res = run_bass_kernel_spmd(nc, in_maps, core_ids=list(range(8)))
    out = np.empty((4, NX, 1024), np.float32)
    for core in range(8):
        b, rev = core // 2, core % 2
        o = np.asarray(res.results[core]["outT"]).reshape(1024, NQ).T
        if rev:
            out[b, NX - NQ:] = o[::-1]
        else:
            out[b, :NQ] = o
    return out
```

```python
import contextlib
import math
import numpy as np
import concourse.bass as bass
import concourse.mybir as mybir
from concourse.bass_utils import run_bass_kernel_spmd

F32 = mybir.dt.float32
BF16 = mybir.dt.bfloat16
ALU = mybir.AluOpType
AF = mybir.ActivationFunctionType
AX = mybir.AxisListType

EPS = 1e-6
NT = 4352
NX = 4096
NCTX = 256
TILES = [(i * 512, 512, 2 + i * 512, i * 512, 0) for i in range(8)] + [(4096, 256, 4102, 4100, 1)]
UW = 4360
UCW = 4356
NQ = 2048
LAMBDA_INIT = 0.8 - 0.6 * math.exp(-0.3 * 1)
DEBUG = False


class Sched:
    ROT = 30000

    def __init__(self, nc, stack):
        self.nc = nc
        self.stack = stack
        self.engs = {'pe': nc.tensor, 'act': nc.scalar, 'dve': nc.vector,
                     'pool': nc.gpsimd, 'sp': nc.sync}
        self.cur = {}
        self.nsem = 0
        for e in self.engs:
            self.cur[e] = [self._newsem(e), 0]
        self.lastw = {}
        self.readers = {}
        self.waited = {e: {} for e in self.engs}
        self.dsem = {}
        self.alltok = {}

    def _newsem(self, nm):
        self.nsem += 1
        return self.stack.enter_context(self.nc.semaphore(f"s_{nm}_{self.nsem}"))

    def _wait(self, eng, toks):
        best = {}
        for (s, v) in toks:
            k = id(s)
            if k not in best or best[k][1] < v:
                best[k] = (s, v)
        for k, (s, v) in best.items():
            if self.waited[eng].get(k, 0) >= v:
                continue
            self.engs[eng].wait_ge(s, v)
            self.waited[eng][k] = v

    def _deps(self, eng, reads, writes, skip_same_pe=True):
        toks = []
        for k in reads:
            if k in self.lastw:
                toks.append(self.lastw[k])
        for k in writes:
            if k in self.lastw:
                toks.append(self.lastw[k])
            toks.extend(self.readers.get(k, ()))
        if eng == 'pe' and skip_same_pe:
            toks = [t for t in toks if t[0] is not self.cur['pe'][0]]
        return toks

    def _commit(self, tok, reads, writes):
        for k in writes:
            self.lastw[k] = tok
            self.readers[k] = []
        for k in reads:
            if k in writes:
                continue
            self.readers.setdefault(k, []).append(tok)
        self.alltok[id(tok[0])] = tok

    def op(self, eng, fn, reads=(), writes=()):
        self._wait(eng, self._deps(eng, reads, writes))
        c = self.cur[eng]
        if c[1] >= self.ROT:
            c[0] = self._newsem(eng)
            c[1] = 0
        ins = fn(self.engs[eng])
        c[1] += 1
        ins.then_inc(c[0], 1)
        self._commit((c[0], c[1]), reads, writes)

    def dma(self, eng, out, in_, reads=(), writes=(), slot=None, **kw):
        if slot is None:
            slot = ('auto',) + tuple(writes)
        self._wait(eng, self._deps(eng, reads, writes, skip_same_pe=False))
        if slot not in self.dsem:
            self.dsem[slot] = [self._newsem('d'), 0]
        d = self.dsem[slot]
        ins = self.engs[eng].dma_start(out=out, in_=in_, **kw)
        d[1] += 16
        ins.then_inc(d[0], 16)
        self._commit((d[0], d[1]), reads, writes)

    def barrier(self):
        toks = list(self.alltok.values())
        for e in self.engs:
            self._wait(e, toks)


def bc_last(a, n):
    return bass.AP(a.tensor, a.offset, [list(x) for x in a.ap] + [[0, n]])


def bc_mid(a, n):
    l = [list(x) for x in a.ap]
    return bass.AP(a.tensor, a.offset, [l[0], [0, n]] + l[1:])


def build(dbg=False):
    nc = bass.Bass("TRN2", target_bir_lowering=False)

    def din(name, shape, dt=F32):
        return nc.dram_tensor(name, list(shape), dt, kind="ExternalInput").ap()

    xc = din("xc", [8, 128, NT])
    cond_in = din("cond", [128, 8, 2])
    w_mod = din("w_mod", [2, 1024, 6144])
    bmod_in = din("bmod", [128, 2, 48])
    n1g_in = din("n1g", [128, 2, 8])
    n2g_in = din("n2g", [128, 2, 8])
    fing_in = din("fing", [128, 8])
    w_in = din("w_in", [1024, 2048])
    convw_in = din("convw", [128, 8, 5])
    convb_in = din("convb", [128, 8])
    gaw = din("gaw", [2, 8, 128, 128])
    gxw = din("gxw", [2, 8, 128, 128])
    gab_in = din("gab", [128, 2, 8])
    gxb_in = din("gxb", [128, 2, 8])
    lam_in = din("lam", [128, 2, 8])
    w_out = din("w_out", [1024, 1024])
    w_qkv = din("w_qkv", [1024, 3072])
    dalam_in = din("dalam", [128, 4, 64])
    subg_in = din("subg", [128, 1])
    w_o = din("w_o", [1024, 1024])
    rw_in = din("rw", [128, 8, 32])
    rb_in = din("rb", [128, 32])
    wg = din("wg", [2, 32, 1024, 512])
    wu = din("wu", [2, 32, 1024, 512])
    wd = din("wd", [2, 32, 512, 1024])
    rmat_in = din("rmat", [128, 128])
    cos_in = din("cos", [128, NT])
    sin_in = din("sin", [128, NT])
    outT = nc.dram_tensor("outT", [8, 128, NQ], F32, kind="ExternalOutput").ap()
    xr = nc.dram_tensor("xr", [8, 128, NT], F32, kind="Internal").ap()
    zscr = nc.dram_tensor("zscr", [8, 128, NT], BF16, kind="Internal").ap()
    h2tm = nc.dram_tensor("h2tm", [NT, 1024], BF16, kind="Internal").ap()
    Xs = nc.dram_tensor("Xs", [100 * 128, 1024], BF16, kind="Internal").ap()
    Ys = nc.dram_tensor("Ys", [100 * 128, 1024], F32, kind="Internal").ap()
    wgb = nc.dram_tensor("wgb", [2 * 8192, 2048], BF16, kind="Internal").ap()
    wub = nc.dram_tensor("wub", [2 * 8192, 2048], BF16, kind="Internal").ap()
    wdb = nc.dram_tensor("wdb", [2 * 8192, 2048], BF16, kind="Internal").ap()
    pcol_in = din("pcol", [128, 1])
    blkoff_in = din("blkoff", [128, 100])
    dbgo = {}
    if dbg:
        for nm, shp in [("d_mod", [128, 2 * 48 * 2]), ("d_h", [128, 8, NT]), ("d_z", [8, 128, NT]),
                        ("d_x1", [8, 128, NT]), ("d_x2", [8, 128, NT]),
                        ("d_ao", [8, 128, NT])]:
            dbgo[nm] = nc.dram_tensor(nm, shp, F32, kind="ExternalOutput").ap()

    with contextlib.ExitStack() as st:
        S = Sched(nc, st)

        uid = [0]

        def sb(stack, name, shape, dt):
            uid[0] += 1
            return stack.enter_context(nc.sbuf_tensor(f"sb{uid[0]}_{name}", list(shape), dt))

        psbig = st.enter_context(nc.psum_tensor("psbig", [128, 8 * 512], F32))
        PS = [psbig[:, i * 512:(i + 1) * 512] for i in range(8)]
        pk = lambda i: ('ps', i)
        wgv_all = wg.rearrange("l e (q r) f -> (l e q) (r f)", r=4)
        wuv_all = wu.rearrange("l e (q r) f -> (l e q) (r f)", r=4)
        wdv_all = wd.rearrange("l e (q r) d -> (l e q) (r d)", r=2)

        def convert_experts(l, e0, e1):
            for e_ in range(e0, e1):
                r0 = (l * 32 + e_) * 256
                for dst, src in [(wgb, wgv_all), (wub, wuv_all), (wdb, wdv_all)]:
                    S.dma('pool', dst[r0:r0 + 256, :], src[r0:r0 + 256, :], writes=[('wcv', l)])

        ones_bf = sb(st, "ones_bf", [128, 128], BF16)
        ones32 = sb(st, "ones32", [128, 128], F32)
        ident = sb(st, "ident", [128, 128], F32)
        identb = sb(st, "identb", [128, 128], BF16)
        modT = sb(st, "modT", [128, 2, 48, 2], F32)
        gmT = sb(st, "gmT", [128, 2, 2, 8, 2], F32)
        n1g = sb(st, "n1g", [128, 2, 8], F32)
        n2g = sb(st, "n2g", [128, 2, 8], F32)
        fing = sb(st, "fing", [128, 8], F32)
        rw = sb(st, "rw", [128, 8, 32], F32)
        rb = sb(st, "rb", [128, 32], F32)
        S.op('dve', lambda e: e.memset(ones_bf[:], 1.0), writes=['ones_bf'])
        S.op('dve', lambda e: e.memset(ones32[:], 1.0), writes=['ones32'])
        S.op('pool', lambda e: e.memset(ident[:], 1.0), writes=['ident'])
        S.op('pool', lambda e: e.affine_select(out=ident[:], in_=ident[:], pattern=[[-1, 128]],
                                               compare_op=ALU.is_equal, fill=0.0, base=0, channel_multiplier=1),
             reads=['ident'], writes=['ident'])
        S.op('act', lambda e: e.activation(out=identb[:], in_=ident[:], func=AF.Identity), reads=['ident'], writes=['identb'])
        for t, src, k in [(n1g, n1g_in, 'n1g'), (n2g, n2g_in, 'n2g'), (fing, fing_in, 'fing'),
                          (rw, rw_in, 'rw'), (rb, rb_in, 'rb')]:
            S.dma('sp', t[:], src, writes=[k])

        with contextlib.ExitStack() as ph:
            condt = sb(ph, "condt", [128, 8, 2], F32)
            scond = sb(ph, "scond", [128, 8, 2], F32)
            bm = sb(ph, "bm", [128, 2, 48], F32)
            wm = [sb(ph, f"wm{i}", [128, 8, 1024], F32) for i in range(2)]
            S.dma('sp', condt[:], cond_in, writes=['cond'])
            S.dma('sp', bm[:], bmod_in, writes=['bm'])
            S.op('act', lambda e: e.activation(out=scond[:], in_=condt[:], func=AF.Silu),
                 reads=['cond'], writes=['scond'])
            it = 0
            for l in range(2):
                for gi in range(6):
                    buf = wm[it % 2]
                    key = ('wm', it % 2)
                    S.dma('sp', buf[:], w_mod[l, :, gi * 1024:(gi + 1) * 1024].rearrange("(k p) f -> p k f", p=128),
                          writes=[key])
                    pst = PS[it % 2]

                    def mm(e, buf=buf, pst=pst):
                        for j in range(8):
                            for kc in range(8):
                                ins = e.matmul(pst[:, j * 2:(j + 1) * 2], lhsT=buf[:, kc, j * 128:(j + 1) * 128],
                                               rhs=scond[:, kc, :], start=(kc == 0), stop=(kc == 7))
                        return ins
                    S.op('pe', mm, reads=[key, 'scond'], writes=[pk(it % 2)])
                    S.op('dve', lambda e: e.tensor_tensor(
                        out=modT[:, l, gi * 8:(gi + 1) * 8, :],
                        in0=pst[:, 0:16].rearrange("p (j r) -> p j r", r=2),
                        in1=bc_last(bm[:, l, gi * 8:(gi + 1) * 8], 2), op=ALU.add),
                        reads=[pk(it % 2), 'bm'], writes=['modT'])
                    it += 1
            for l in range(2):
                for w_, (gt, gk, sidx) in enumerate([(n1g, 'n1g', 1), (n2g, 'n2g', 4)]):
                    S.op('dve', lambda e: e.tensor_scalar(out=gmT[:, l, w_], in0=modT[:, l, sidx * 8:(sidx + 1) * 8, :],
                                                          scalar1=1.0, scalar2=None, op0=ALU.add),
                         reads=['modT'], writes=['gmT'])
                    S.op('dve', lambda e: e.tensor_tensor(out=gmT[:, l, w_], in0=gmT[:, l, w_],
                                                          in1=bc_last(gt[:, l, :], 2), op=ALU.mult),
                         reads=['gmT', gk], writes=['gmT'])
            if dbg:
                S.dma('sp', dbgo["d_mod"], modT[:].rearrange("p a b c -> p (a b c)"), reads=['modT'], writes=['d_mod'])
            S.barrier()

        def mod_ap(l, idx, j, r):
            return modT[:, l, idx * 8 + j, r:r + 1]

        def norm_mod(tmp, xt, n, kx, out, kout, gm_of_j, sh_of_j, psi, extra_reads=(), tag=''):
            sq, rt, rstd, xn = tmp
            kxl = list(kx) if isinstance(kx, list) else [kx]
            S.op('act', lambda e: e.activation(out=sq[:, :, :n], in_=xt[:, :, :n], func=AF.Square),
                 reads=kxl, writes=['nm_sq' + tag])

            def mm(e):
                for j in range(8):
                    ins = e.matmul(PS[psi][:, :n], lhsT=ones_bf[:], rhs=sq[:, j, :n], start=(j == 0), stop=(j == 7))
                return ins
            S.op('pe', mm, reads=['nm_sq' + tag, 'ones_bf'], writes=[pk(psi)])
            S.op('act', lambda e: e.activation(out=rt[:, :n], in_=PS[psi][:, :n], func=AF.Ln,
                                               bias=eps_t[:, 0:1], scale=1.0 / 1024.0),
                 reads=[pk(psi), 'eps'], writes=['nm_rt' + tag])
            S.op('act', lambda e: e.activation(out=rstd[:, :n], in_=rt[:, :n], func=AF.Exp, scale=-0.5),
                 reads=['nm_rt' + tag], writes=['nm_rstd' + tag])
            S.op('dve', lambda e: e.tensor_tensor(out=xn[:, :, :n], in0=xt[:, :, :n], in1=bc_mid(rstd[:, :n], 8),
                                                  op=ALU.mult), reads=kxl + ['nm_rstd' + tag], writes=['nm_xn' + tag])
            for j in range(8):
                if sh_of_j is None:
                    S.op('dve', lambda e: e.tensor_scalar(out=out(j), in0=xn[:, j, :n], scalar1=gm_of_j(j),
                                                          scalar2=None, op0=ALU.mult),
                         reads=['nm_xn' + tag] + list(extra_reads), writes=[kout])
                elif j % 2 == 0:
                    S.op('dve', lambda e: e.tensor_scalar(out=out(j), in0=xn[:, j, :n], scalar1=gm_of_j(j),
                                                          scalar2=sh_of_j(j), op0=ALU.mult, op1=ALU.add),
                         reads=['nm_xn' + tag] + list(extra_reads), writes=[kout])
                else:
                    S.op('act', lambda e: e.activation(out=out(j), in_=xn[:, j, :n], func=AF.Identity,
                                                       bias=sh_of_j(j), scale=gm_of_j(j)),
                         reads=['nm_xn' + tag] + list(extra_reads), writes=[kout])

        def norm_tmp(ph):
            return (sb(ph, "nm_sq", [128, 8, 512], BF16), sb(ph, "nm_rt", [128, 512], F32),
                    sb(ph, "nm_rstd", [128, 512], F32), sb(ph, "nm_xn", [128, 8, 512], F32))

        eps_t = sb(st, "eps_t", [128, 1], F32)
        S.op('dve', lambda e: e.memset(eps_t[:], EPS), writes=['eps'])
        one_t = sb(st, "one_t", [128, 1], F32)
        S.op('dve', lambda e: e.memset(one_t[:], 1.0), writes=['one'])


        def phase_A(l, src, hbuf):
            with contextlib.ExitStack() as ph:
                xts = [sb(ph, f"xt{i}", [128, 8, 512], F32) for i in range(2)]
                tmp = norm_tmp(ph)
                for ti, (off, cnt, _, _, r) in enumerate(TILES):
                    xt = xts[ti % 2]
                    kx = ('xt', ti % 2)
                    S.dma('sp', xt[:, :, :cnt], src[:, :, off:off + cnt].rearrange("j p t -> p j t"),
                          reads=[('xr', ti)], writes=[kx])
                    norm_mod(tmp, xt, cnt, kx, lambda j: hbuf[:, j, off:off + cnt], ('h', ti),
                             lambda j: gmT[:, l, 0, j, r:r + 1], lambda j: mod_ap(l, 0, j, r), 7,
                             extra_reads=['gmT', 'modT'])
                S.barrier()

        hst = contextlib.ExitStack()
        hbuf = sb(hst, "hbuf", [128, 8, NT], BF16)
        phase_A(0, xc, hbuf)
        if dbg:
            with contextlib.ExitStack() as ph:
                t32 = sb(ph, "dbg32", [128, 8, 512], F32)
                for ti, (off, cnt, _, _, r) in enumerate(TILES):
                    S.op('dve', lambda e: e.tensor_copy(out=t32[:, :, :cnt], in_=hbuf[:, :, off:off + cnt]),
                         reads=[('h', ti)], writes=['dbg32'])
                    S.dma('sp', dbgo["d_h"][:, :, off:off + cnt], t32[:, :, :cnt], reads=['dbg32'], writes=['d_h'])
                S.barrier()

        with contextlib.ExitStack() as ph:
            ub = sb(ph, "ub", [128, UW], F32)
            uc = sb(ph, "uc", [128, UCW], F32)
            ucb = sb(ph, "ucb", [128, UCW], BF16)
            gy = sb(ph, "gy", [128, NT], BF16)
            wyu = [sb(ph, f"wyu{i}", [128, 8, 256], BF16) for i in range(2)]
            gw = [sb(ph, f"gw{i}", [128, 4, 128], BF16) for i in range(2)]
            convw = sb(ph, "convw", [128, 8, 5], F32)
            convb = sb(ph, "convb", [128, 8], F32)
            gab = sb(ph, "gab", [128, 2, 8], F32)
            gxb = sb(ph, "gxb", [128, 2, 8], F32)
            lamt = sb(ph, "lamt", [128, 2, 8], F32)
            cneg = sb(ph, "cneg", [128, 2, 8], F32)
            cneg2 = sb(ph, "cneg2", [128, 2, 8], F32)
            rbuf = [sb(ph, f"rbuf{i}", [128, 512], F32) for i in range(2)]
            ibuf = [sb(ph, f"ibuf{i}", [128, 512], F32) for i in range(2)]
            sbuf_ = [sb(ph, f"sbuf{i}", [128, 512], F32) for i in range(2)]
            hbt = [sb(ph, f"hbt{i}", [128, 512], F32) for i in range(2)]
            gt1 = [sb(ph, f"gt1{i}", [128, 512], F32) for i in range(2)]
            zt = [sb(ph, f"zt{i}", [128, 512], BF16) for i in range(2)]
            for t, src, k in [(convw, convw_in, 'convw'), (convb, convb_in, 'convb'), (gab, gab_in, 'gab'),
                              (gxb, gxb_in, 'gxb'), (lamt, lam_in, 'lamt')]:
                S.dma('sp', t[:], src, writes=[k])
            S.op('act', lambda e: e.activation(out=cneg[:], in_=lamt[:], func=AF.Exp, scale=-1.0),
                 reads=['lamt'], writes=['cneg'])
            S.op('act', lambda e: e.activation(out=cneg[:], in_=cneg[:], func=AF.Ln, bias=one_t[:, 0:1], scale=1.0),
                 reads=['cneg', 'one'], writes=['cneg'])
            S.op('dve', lambda e: e.tensor_scalar(out=cneg2[:], in0=cneg[:], scalar1=-16.0, scalar2=None, op0=ALU.mult),
                 reads=['cneg'], writes=['cneg2'])
            S.op('dve', lambda e: e.tensor_scalar(out=cneg[:], in0=cneg[:], scalar1=-8.0, scalar2=None, op0=ALU.mult),
                 reads=['cneg', 'cneg2'], writes=['cneg'])
            zer = sb(ph, "zer", [128, 4, 1024], BF16)
            S.op('pool', lambda e: e.memset(zer[:], 0.0), writes=['zer'])
            ngab = sb(ph, "ngab", [128, 2, 8], F32)
            ngxb = sb(ph, "ngxb", [128, 2, 8], F32)
            S.op('dve', lambda e: e.tensor_scalar(out=ngab[:], in0=gab[:], scalar1=-1.0, scalar2=None, op0=ALU.mult),
                 reads=['gab'], writes=['ngab'])
            S.op('dve', lambda e: e.tensor_scalar(out=ngxb[:], in0=gxb[:], scalar1=-1.0, scalar2=None, op0=ALU.mult),
                 reads=['gxb'], writes=['ngxb'])
            S.op('dve', lambda e: e.memset(ub[:], 0.0), writes=[('ub', ti) for ti in range(9)])
            ubkeys = [('ub', ti) for ti in range(9)]
            cnt_sc = 0
            for n in range(8):
                w = wyu[n % 2]
                kw_ = ('wyu', n % 2)
                S.dma('pool', w[:, :, 0:128], w_in[:, n * 128:(n + 1) * 128].rearrange("(k p) f -> p k f", p=128),
                      writes=[kw_])
                S.dma('pool', w[:, :, 128:256],
                      w_in[:, 1024 + n * 128:1024 + (n + 1) * 128].rearrange("(k p) f -> p k f", p=128), writes=[kw_])
                g = gw[n % 2]
                kg = ('gw', n % 2)
                for d in range(2):
                    S.dma('pool', g[:, 2 * d, :], gaw[d, n], writes=[kg])
                    S.dma('pool', g[:, 2 * d + 1, :], gxw[d, n], writes=[kg])
                convert_experts(0, 4 * n, 4 * n + 4)
                for b4 in range(4 * n, min(4 * n + 4, 25)):
                    S.dma('pool', Xs[b4 * 512:(b4 + 1) * 512, :].rearrange("(a p) f -> p a f", p=128), zer[:],
                          reads=['zer'], writes=['Xs'])
                for ti, (off, cnt, uoff, ucoff, r) in enumerate(TILES):
                    pu, py = (ti % 2) * 2, (ti % 2) * 2 + 1

                    def mmu(e, c0=128, p=pu):
                        for kc in range(8):
                            ins = e.matmul(PS[p][:, :cnt], lhsT=w[:, kc, c0:c0 + 128], rhs=hbuf[:, kc, off:off + cnt],
                                           start=(kc == 0), stop=(kc == 7))
                        return ins
                    S.op('pe', mmu, reads=[kw_, ('h', ti)], writes=[pk(pu)])
                    S.op('act', lambda e: e.activation(out=ub[:, uoff:uoff + cnt], in_=PS[pu][:, :cnt], func=AF.Identity),
                         reads=[pk(pu)], writes=[('ub', ti)])
                    S.op('pe', lambda e: mmu(e, 0, py), reads=[kw_, ('h', ti)], writes=[pk(py)])
                    t1 = gt1[ti % 2]
                    k1 = ('gt1', ti % 2)
                    S.op('act', lambda e: e.activation(out=t1[:, :cnt], in_=PS[py][:, :cnt], func=AF.Square),
                         reads=[pk(py)], writes=[k1])
                    S.op('dve', lambda e: e.tensor_scalar(out=t1[:, :cnt], in0=t1[:, :cnt], scalar1=0.044715, scalar2=1.0,
                                                          op0=ALU.mult, op1=ALU.add), reads=[k1], writes=[k1])
                    S.op('dve', lambda e: e.tensor_tensor(out=t1[:, :cnt], in0=t1[:, :cnt], in1=PS[py][:, :cnt], op=ALU.mult),
                         reads=[k1, pk(py)], writes=[k1])
                    S.op('act', lambda e: e.activation(out=t1[:, :cnt], in_=t1[:, :cnt], func=AF.Sigmoid,
                                                       scale=1.5957691216057308), reads=[k1], writes=[k1])
                    S.op('dve', lambda e: e.tensor_tensor(out=gy[:, off:off + cnt], in0=t1[:, :cnt], in1=PS[py][:, :cnt],
                                                          op=ALU.mult), reads=[k1, pk(py)], writes=[('gy', ti)])
                S.op('dve', lambda e: e.tensor_scalar(out=uc[:], in0=ub[:, 0:UCW], scalar1=convw[:, n, 0:1],
                                                      scalar2=convb[:, n:n + 1], op0=ALU.mult, op1=ALU.add),
                     reads=ubkeys + ['convw', 'convb'], writes=['uc'])
                for k in range(1, 5):
                    S.op('dve', lambda e: e.scalar_tensor_tensor(out=uc[:], in0=ub[:, k:k + UCW], scalar=convw[:, n, k:k + 1],
                                                                 in1=uc[:], op0=ALU.mult, op1=ALU.add),
                         reads=ubkeys + ['convw', 'uc'], writes=['uc'])
                S.op('act', lambda e: e.activation(out=ucb[:], in_=uc[:], func=AF.Identity), reads=['uc'], writes=['ucb'])
                for d in range(2):
                    order = [8] + (list(range(8)) if d == 0 else list(range(7, -1, -1)))
                    prev = None
                    for oi, ti in enumerate(order):
                        off, cnt, uoff, ucoff, r = TILES[ti]
                        b = cnt_sc % 2
                        cnt_sc += 1
                        pr, pi = 4 + 2 * b, 5 + 2 * b
                        S.op('pe', lambda e: e.matmul(PS[pr][:, :cnt], lhsT=g[:, 2 * d, :], rhs=ucb[:, ucoff:ucoff + cnt],
                                                      start=True, stop=True), reads=[kg, 'ucb'], writes=[pk(pr)])
                        S.op('pe', lambda e: e.matmul(PS[pi][:, :cnt], lhsT=g[:, 2 * d + 1, :], rhs=ucb[:, ucoff:ucoff + cnt],
                                                      start=True, stop=True), reads=[kg, 'ucb'], writes=[pk(pi)])
                        rb_, ib_, sb_, hb_ = rbuf[b], ibuf[b], sbuf_[b], hbt[b]
                        kr, ki, ks, kh = ('rbuf', b), ('ibuf', b), ('sbuf', b), ('hbt', b)
                        S.op('act', lambda e: e.activation(out=rb_[:, :cnt], in_=PS[pr][:, :cnt], func=AF.Exp,
                                                           bias=ngab[:, d, n:n + 1], scale=-1.0),
                             reads=[pk(pr), 'ngab'], writes=[kr])
                        S.op('act', lambda e: e.activation(out=ib_[:, :cnt], in_=PS[pi][:, :cnt], func=AF.Exp,
                                                           bias=ngxb[:, d, n:n + 1], scale=-1.0),
                             reads=[pk(pi), 'ngxb'], writes=[ki])
                        for t_, k_ in ((rb_, kr), (ib_, ki)):
                            S.op('act', lambda e: e.activation(out=t_[:, :cnt], in_=t_[:, :cnt], func=AF.Ln,
                                                               bias=one_t[:, 0:1], scale=1.0), reads=[k_, 'one'], writes=[k_])
                            S.op('act', lambda e: e.activation(out=t_[:, :cnt], in_=t_[:, :cnt], func=AF.Exp, scale=-1.0),
                                 reads=[k_], writes=[k_])
                        S.op('act', lambda e: e.activation(out=sb_[:, :cnt], in_=rb_[:, :cnt], func=AF.Exp,
                                                           scale=cneg2[:, d, n:n + 1]), reads=[kr, 'cneg2'], writes=[ks])
                        S.op('act', lambda e: e.activation(out=rb_[:, :cnt], in_=rb_[:, :cnt], func=AF.Exp,
                                                           scale=cneg[:, d, n:n + 1]), reads=[kr, 'cneg'], writes=[kr])
                        S.op('act', lambda e: e.activation(out=sb_[:, :cnt], in_=sb_[:, :cnt], func=AF.Ln,
                                                           bias=one_t[:, 0:1], scale=-1.0), reads=[ks, 'one'], writes=[ks])
                        S.op('act', lambda e: e.activation(out=sb_[:, :cnt], in_=sb_[:, :cnt], func=AF.Exp, scale=0.5),
                             reads=[ks], writes=[ks])
                        S.op('dve', lambda e: e.tensor_tensor(out=ib_[:, :cnt], in0=ib_[:, :cnt],
                                                              in1=uc[:, ucoff:ucoff + cnt], op=ALU.mult),
                             reads=[ki, 'uc'], writes=[ki])
                        S.op('dve', lambda e: e.tensor_tensor(out=ib_[:, :cnt], in0=ib_[:, :cnt], in1=sb_[:, :cnt],
                                                              op=ALU.mult), reads=[ki, ks], writes=[ki])
                        if d == 0:
                            if prev is None:
                                init, kin = 0.0, []
                            else:
                                po, pc, puo, _, _ = TILES[prev]
                                init, kin = ub[:, puo + pc - 1:puo + pc], [('ub', prev)]
                            S.op('dve', lambda e: e.tensor_tensor_scan(out=ub[:, uoff:uoff + cnt], data0=rb_[:, :cnt],
                                                                       data1=ib_[:, :cnt], initial=init,
                                                                       op0=ALU.mult, op1=ALU.add),
                                 reads=[kr, ki] + kin, writes=[('ub', ti)])
                        else:
                            if prev is None:
                                init, kin = 0.0, []
                            else:
                                init, kin = hbt[1 - b][:, 0:1], [('hbt', 1 - b)]
                            S.op('dve', lambda e: e.tensor_tensor_scan(out=hb_[:, :cnt][:, ::-1], data0=rb_[:, :cnt][:, ::-1],
                                                                       data1=ib_[:, :cnt][:, ::-1], initial=init,
                                                                       op0=ALU.mult, op1=ALU.add),
                                 reads=[kr, ki] + kin, writes=[kh])
                            S.op('dve', lambda e: e.tensor_tensor(out=sb_[:, :cnt], in0=hb_[:, :cnt],
                                                                  in1=ub[:, uoff:uoff + cnt], op=ALU.add),
                                 reads=[kh, ('ub', ti)], writes=[ks])
                            z_ = zt[b]
                            S.op('dve', lambda e: e.tensor_tensor(out=z_[:, :cnt], in0=sb_[:, :cnt],
                                                                  in1=gy[:, off:off + cnt], op=ALU.mult),
                                 reads=[ks, ('gy', ti)], writes=[('zt', b)])
                            S.dma('sp', zscr[n, :, off:off + cnt], z_[:, :cnt], reads=[('zt', b)], writes=[('zs', ti)])
                        prev = ti
            S.barrier()
        hst.close()

        I32 = mybir.dt.int32
        MAXSUB = 34
        NBMAX = 100
        BIGW = 2 * 32 * 256 + 64
        utri = sb(st, "utri", [128, 128], F32)
        S.op('pool', lambda e: e.memset(utri[:], 1.0), writes=['utri'])
        S.op('pool', lambda e: e.affine_select(out=utri[:], in_=utri[:], pattern=[[1, 128]],
                                               compare_op=ALU.is_gt, fill=0.0, base=0, channel_multiplier=-1),
             reads=['utri'], writes=['utri'])
        pcol = sb(st, "pcol", [128, 1], F32)
        blkoff = sb(st, "blkoff", [128, NBMAX], F32)
        ones_row = sb(st, "ones_row", [128, 32], F32)
        S.dma('sp', pcol[:], pcol_in, writes=['pcol'])
        S.dma('sp', blkoff[:], blkoff_in, writes=['blkoff'])
        S.op('dve', lambda e: e.memset(ones_row[:], 1.0), writes=['ones_row'])
        onesb = sb(st, "onesb", [128, 4, 32], F32)
        c12 = sb(st, "c12", [128, 2, 4, 32], F32)
        S.op('dve', lambda e: e.memset(onesb[:], 1.0), writes=['onesb'])
        for k2_ in range(2):
            for s_ in range(4):
                S.op('dve', lambda e: e.memset(c12[:, k2_, s_, :], float(2 * s_ + 1 + k2_)), writes=['c12'])
        FM = sb(st, "FM", [128, MAXSUB, 2, 32], F32)
        RK = sb(st, "RK", [128, MAXSUB, 2], F32)
        WK = sb(st, "WK", [128, MAXSUB, 2], F32)
        DI = sb(st, "DI", [128, MAXSUB, 2], I32)
        WI = sb(st, "WI", [128, NBMAX, 2], I32)
        cntm = sb(st, "cntm", [128, 32], F32)

        bregs = {}

        def idma(out, out_off, in_, in_off, bounds, reads, writes, slot):
            S._wait('pool', S._deps('pool', reads, writes, skip_same_pe=False))
            if slot not in S.dsem:
                S.dsem[slot] = [S._newsem('d'), 0]
            d = S.dsem[slot]
            if bounds not in bregs:
                bregs[bounds] = nc.gpsimd.to_reg(bounds)
            ins = nc.gpsimd.indirect_dma_start(out=out, out_offset=out_off, in_=in_, in_offset=in_off,
                                               bounds_check=bregs[bounds], oob_is_err=False)
            d[1] += 16
            ins.then_inc(d[0], 16)
            S._commit((d[0], d[1]), reads, writes)

        def phase_C(l, wmat, tiles, xsrc):
            nsub_tot = sum(TILES[ti][1] // 128 for ti in tiles)
            NB = 2 * nsub_tot + 32
            with contextlib.ExitStack() as ph:
                wsb = sb(ph, "wsb", [128, 8, 1024], BF16)
                zts = [sb(ph, f"zts{i}", [128, 8, 512], BF16) for i in range(2)]
                xts = [sb(ph, f"xtc{i}", [128, 8, 512], F32) for i in range(2)]
                h2fs = [sb(ph, f"h2f{i}", [128, 8, 512], F32) for i in range(2)]
                h2t = [sb(ph, f"h2t{i}", [128, 1024], BF16) for i in range(2)]
                tmps = [norm_tmp(ph), norm_tmp(ph)]
                mk3 = lambda nm: [sb(ph, f"{nm}{i}", [128, 4, 32], F32) for i in range(2)]
                ssel, sg, em, mk, cmv, rk, t3 = mk3("ssel"), mk3("sg"), mk3("em"), mk3("mk"), mk3("cmv"), mk3("rk"), mk3("t3")
                mk16 = lambda nm: [sb(ph, f"{nm}{i}", [128, 16], F32) for i in range(2)]
                m1, m2, gs, gm_ = mk16("m1"), mk16("m2"), mk16("gs"), mk16("gmk")
                sm = [sb(ph, f"sm{i}", [128, 4, 4], F32) for i in range(2)]
                t32 = [sb(ph, f"t32{i}", [128, 32], F32) for i in range(2)]
                S.dma('pool', wsb[:], wmat.rearrange("(k p) f -> p k f", p=128), writes=['wsb'])
                S.op('dve', lambda e: e.memset(cntm[:], 0.0), writes=['cntm'])
                nsub_box = [0]

                def stage_W(ti):
                    off, cnt, _, _, r = TILES[ti]
                    b = ti % 2
                    z_, x_ = zts[b], xts[b]
                    h2f, tmp, kh2 = h2fs[b], tmps[b], ('h2f', b)
                    kz, kx = ('zts', b), ('xtc', b)
                    S.dma('sp', z_[:, :, :cnt], zscr[:, :, off:off + cnt].rearrange("j p t -> p j t"),
                          reads=[('zs', ti)], writes=[kz])
                    S.dma('sp', x_[:, :, :cnt], xsrc[:, :, off:off + cnt].rearrange("j p t -> p j t"),
                          reads=[('xr', ti)], writes=[kx])
                    for j in range(8):
                        p = j % 3

                        def mm(e):
                            for kc in range(8):
                                ins = e.matmul(PS[p][:, :cnt], lhsT=wsb[:, kc, j * 128:(j + 1) * 128], rhs=z_[:, kc, :cnt],
                                               start=(kc == 0), stop=(kc == 7))
                            return ins
                        S.op('pe', mm, reads=['wsb', kz], writes=[pk(p)])
                        S.op('dve', lambda e: e.scalar_tensor_tensor(out=x_[:, j, :cnt], in0=PS[p][:, :cnt],
                                                                     scalar=mod_ap(l, 2, j, r), in1=x_[:, j, :cnt],
                                                                     op0=ALU.mult, op1=ALU.add),
                             reads=[pk(p), kx, 'modT'], writes=[kx])
                    S.dma('sp', xr[:, :, off:off + cnt].rearrange("j p t -> p j t"), x_[:, :, :cnt],
                          reads=[kx], writes=[('xr', ti)])
                    if dbg and l == 0:
                        S.dma('sp', dbgo["d_x1"][:, :, off:off + cnt].rearrange("j p t -> p j t"), x_[:, :, :cnt],
                              reads=[kx], writes=['d_x1'])

                def stage_N(ti):
                    off, cnt, _, _, r = TILES[ti]
                    b = ti % 2
                    z_, x_ = zts[b], xts[b]
                    h2f, tmp, kh2 = h2fs[b], tmps[b], ('h2f', b)
                    kz, kx = ('zts', b), ('xtc', b)
                    norm_mod(tmp, x_, cnt, kx, lambda j: h2f[:, j, :cnt], kh2,
                             lambda j: gmT[:, l, 1, j, r:r + 1], lambda j: mod_ap(l, 3, j, r), 3,
                             extra_reads=['gmT', 'modT'], tag=str(b))

                def stage_R(ti):
                    off, cnt, _, _, r = TILES[ti]
                    b = ti % 2
                    z_, x_ = zts[b], xts[b]
                    h2f, tmp, kh2 = h2fs[b], tmps[b], ('h2f', b)
                    kz, kx = ('zts', b), ('xtc', b)
                    nsb = cnt // 128
                    gs0 = nsub_box[0]
                    nsub_box[0] += nsb
                    q = ti % 2
                    pl = 4 + q
                    W_ = nsb * 32
                    for s in range(nsb):
                        gsi = gs0 + s
                        qq = gsi % 2
                        for half in range(2):
                            pt = 6 + half

                            def mmt(e):
                                for jj in range(4):
                                    j = half * 4 + jj
                                    ins = e.transpose(out=PS[pt][:, jj * 128:(jj + 1) * 128],
                                                      in_=h2f[:, j, s * 128:(s + 1) * 128], identity=ident[:])
                                return ins
                            S.op('pe', mmt, reads=[kh2, 'ident'], writes=[pk(pt)])
                            if half == 0:
                                S.op('act', lambda e: e.activation(out=h2t[qq][:, 0:512], in_=PS[pt][:, :], func=AF.Identity),
                                     reads=[pk(pt)], writes=[('h2t', qq)])
                            else:
                                S.op('pool' if False else 'dve', lambda e: e.tensor_copy(out=h2t[qq][:, 512:1024], in_=PS[pt][:, :]),
                                     reads=[pk(pt)], writes=[('h2t', qq)])
                        S.dma('sp', h2tm[gsi * 128:(gsi + 1) * 128, :], h2t[qq][:], reads=[('h2t', qq)], writes=[('h2tm', gsi % 4)])

                        def mmr(e):
                            for kc in range(8):
                                ins = e.matmul(PS[pl][:, s * 32:(s + 1) * 32], lhsT=h2f[:, kc, s * 128:(s + 1) * 128], rhs=rw[:, kc, :],
                                               start=(kc == 0), stop=(kc == 7))
                            return ins
                        S.op('pe', mmr, reads=[kh2, 'rw'], writes=[pk(pl)])
                    kq = ('rt', q)
                    v3 = lambda t: t[:, :nsb, :]
                    v8 = lambda t: t[:, :nsb, :].rearrange("p s (g e) -> p (s g) e", e=8)
                    f2 = lambda t: t[:, :nsb, :].rearrange("p s e -> p (s e)")
                    g4 = lambda t: t[:, :nsb * 4]
                    g43 = lambda t: t[:, :nsb * 4].rearrange("p (s g) -> p s g", g=4)
                    S.op('act', lambda e: e.activation(out=f2(sg[q]), in_=PS[pl][:, :W_], func=AF.Sigmoid),
                         reads=[pk(pl)], writes=[kq])
                    S.op('dve', lambda e: e.tensor_tensor(out=v3(ssel[q]), in0=v3(sg[q]), in1=bc_mid(rb[:], nsb), op=ALU.add),
                         reads=[kq, 'rb'], writes=[kq])
                    S.op('dve', lambda e: e.tensor_reduce(out=g4(m1[q]), in_=v8(ssel[q]), axis=AX.X, op=ALU.max), reads=[kq], writes=[kq])
                    S.op('dve', lambda e: e.tensor_tensor(out=v8(t3[q]), in0=v8(ssel[q]), in1=bc_last(g4(m1[q]), 8), op=ALU.is_equal),
                         reads=[kq], writes=[kq])
                    S.op('dve', lambda e: e.scalar_tensor_tensor(out=f2(t3[q]), in0=f2(t3[q]), scalar=-1.0e9, in1=f2(ssel[q]),
                                                                 op0=ALU.mult, op1=ALU.add), reads=[kq], writes=[kq])
                    S.op('dve', lambda e: e.tensor_reduce(out=g4(m2[q]), in_=v8(t3[q]), axis=AX.X, op=ALU.max), reads=[kq], writes=[kq])
                    S.op('dve', lambda e: e.tensor_tensor(out=g4(gs[q]), in0=g4(m1[q]), in1=g4(m2[q]), op=ALU.add), reads=[kq], writes=[kq])
                    S.op('dve', lambda e: e.tensor_reduce(out=sm[q][:, 0, :nsb], in_=g43(gs[q]), axis=AX.X, op=ALU.max),
                         reads=[kq], writes=[kq])
                    S.op('dve', lambda e: e.tensor_tensor(out=g43(gm_[q]), in0=g43(gs[q]), in1=bc_last(sm[q][:, 0, :nsb], 4),
                                                          op=ALU.is_equal), reads=[kq], writes=[kq])
                    S.op('dve', lambda e: e.tensor_tensor(out=g4(gs[q]), in0=g4(gm_[q]), in1=g4(m2[q]), op=ALU.mult), reads=[kq], writes=[kq])
                    S.op('dve', lambda e: e.tensor_reduce(out=sm[q][:, 1, :nsb], in_=g43(gs[q]), axis=AX.X, op=ALU.add),
                         reads=[kq], writes=[kq])
                    S.op('dve', lambda e: e.tensor_tensor(out=v3(mk[q]), in0=v3(ssel[q]), in1=bc_last(sm[q][:, 1, :nsb], 32),
                                                          op=ALU.is_ge), reads=[kq], writes=[kq])
                    S.op('dve', lambda e: e.tensor_tensor(out=v8(mk[q]), in0=v8(mk[q]), in1=bc_last(g4(gm_[q]), 8), op=ALU.mult),
                         reads=[kq], writes=[kq])
                    S.op('dve', lambda e: e.tensor_tensor(out=f2(em[q]), in0=f2(mk[q]), in1=f2(sg[q]), op=ALU.mult),
                         reads=[kq], writes=[kq])
                    S.op('dve', lambda e: e.tensor_reduce(out=sm[q][:, 2, :nsb], in_=v3(em[q]), axis=AX.X, op=ALU.add),
                         reads=[kq], writes=[kq])
                    S.op('dve', lambda e: e.reciprocal(out=sm[q][:, 3, :nsb], in_=sm[q][:, 2, :nsb]), reads=[kq], writes=[kq])
                    S.op('dve', lambda e: e.tensor_tensor(out=v3(em[q]), in0=v3(em[q]), in1=bc_last(sm[q][:, 3, :nsb], 32), op=ALU.mult),
                         reads=[kq], writes=[kq])

                    def mmk(e):
                        for s in range(nsb):
                            o_ = PS[pl][:, 128 + s * 32:128 + (s + 1) * 32]
                            e.matmul(o_, lhsT=utri[:], rhs=mk[q][:, s, :], start=True, stop=False)
                            for s2 in range(s):
                                e.matmul(o_, lhsT=ones32[:], rhs=mk[q][:, s2, :], start=False, stop=False)
                            ins = e.matmul(o_, lhsT=ones32[:], rhs=cntm[:], start=False, stop=True)
                        return ins
                    S.op('pe', mmk, reads=[kq, 'utri', 'ones32', 'cntm'], writes=[pk(pl)])
                    S.op('dve', lambda e: e.tensor_copy(out=f2(rk[q]), in_=PS[pl][:, 128:128 + W_]), reads=[pk(pl)], writes=[kq])
                    S.op('dve', lambda e: e.tensor_reduce(out=t32[q][:], in_=mk[q][:, :nsb, :].rearrange("p s e -> p e s"), axis=AX.X,
                                                          op=ALU.add), reads=[kq], writes=[kq])
                    S.op('dve', lambda e: e.tensor_tensor(out=cntm[:], in0=cntm[:], in1=t32[q][:], op=ALU.add),
                         reads=[kq, 'cntm'], writes=['cntm'])
                    S.op('dve', lambda e: e.tensor_tensor_scan(out=f2(cmv[q]), data0=f2(onesb), data1=f2(mk[q]), initial=0.0,
                                                               op0=ALU.mult, op1=ALU.add), reads=[kq, 'onesb'], writes=[kq])
                    for k2 in range(2):
                        fm_ = FM[:, gs0:gs0 + nsb, k2, :]
                        S.op('dve', lambda e: e.tensor_tensor(out=v3(t3[q]), in0=v3(cmv[q]), in1=c12[:, k2, :nsb, :], op=ALU.is_equal),
                             reads=[kq, 'c12'], writes=[kq])
                        S.op('dve', lambda e: e.tensor_tensor(out=fm_, in0=v3(t3[q]), in1=v3(mk[q]), op=ALU.mult),
                             reads=[kq], writes=['FM'])
                        S.op('dve', lambda e: e.tensor_tensor(out=v3(t3[q]), in0=fm_, in1=v3(rk[q]), op=ALU.mult),
                             reads=[kq, 'FM'], writes=[kq])
                        S.op('dve', lambda e: e.tensor_reduce(out=RK[:, gs0:gs0 + nsb, k2], in_=v3(t3[q]), axis=AX.X, op=ALU.add),
                             reads=[kq], writes=['RK'])
                        S.op('dve', lambda e: e.tensor_tensor(out=v3(t3[q]), in0=fm_, in1=v3(em[q]), op=ALU.mult),
                             reads=[kq, 'FM'], writes=[kq])
                        S.op('dve', lambda e: e.tensor_reduce(out=WK[:, gs0:gs0 + nsb, k2], in_=v3(t3[q]), axis=AX.X, op=ALU.add),
                             reads=[kq], writes=['WK'])

                stage_W(tiles[0])
                for i_, ti in enumerate(tiles):
                    stage_N(ti)
                    if i_ + 1 < len(tiles):
                        stage_W(tiles[i_ + 1])
                    stage_R(ti)
                S.barrier()
            with contextlib.ExitStack() as ph:
                J = nsub_tot
                cb = sb(ph, "cb", [128, 32], F32)
                nblk = sb(ph, "nblk", [128, 32], F32)
                pend = sb(ph, "pend", [128, 32], F32)
                pst = sb(ph, "pst", [128, 32], F32)
                big = sb(ph, "bigc", [128, 32, NBMAX], F32)
                eb = sb(ph, "eb", [128, NBMAX], F32)
                chg = sb(ph, "chg", [128, NBMAX], F32)
                wif = sb(ph, "wif", [128, NBMAX, 2], F32)
                dtmp2 = sb(ph, "dtmp2", [128, MAXSUB, 32], F32)
                dif = sb(ph, "dif", [128, MAXSUB, 2], F32)
                rows = [sb(ph, f"rows{i}", [128, 1024], BF16) for i in range(2)]
                S.op('pe', lambda e: e.matmul(PS[0][:, 0:32], lhsT=ones32[:], rhs=cntm[:], start=True, stop=True),
                     reads=['ones32', 'cntm'], writes=[pk(0)])
                S.op('dve', lambda e: e.tensor_copy(out=cb[:], in_=PS[0][:, 0:32]), reads=[pk(0)], writes=['cb'])
                S.op('dve', lambda e: e.tensor_tensor(out=big[:, :, :J], in0=bc_last(cb[:], J), in1=bc_mid(blkoff[:, :J], 32),
                                                      op=ALU.is_gt), reads=['cb', 'blkoff'], writes=['big'])
                S.op('dve', lambda e: e.tensor_reduce(out=nblk[:], in_=big[:, :, :J], axis=AX.X, op=ALU.add),
                     reads=['big'], writes=['nblk'])
                S.op('dve', lambda e: e.tensor_tensor_scan(out=pend[:], data0=ones_row[:], data1=nblk[:], initial=0.0,
                                                           op0=ALU.mult, op1=ALU.add), reads=['nblk', 'ones_row'], writes=['pend'])
                S.op('dve', lambda e: e.tensor_tensor(out=pst[:], in0=pend[:], in1=nblk[:], op=ALU.subtract),
                     reads=['pend', 'nblk'], writes=['pst'])
                S.op('dve', lambda e: e.tensor_scalar(out=pst[:], in0=pst[:], scalar1=128.0, scalar2=None, op0=ALU.mult),
                     reads=['pst'], writes=['pst'])
                S.op('dve', lambda e: e.tensor_scalar(out=pend[:], in0=pend[:], scalar1=128.0, scalar2=None, op0=ALU.mult),
                     reads=['pend'], writes=['pend'])
                S.op('dve', lambda e: e.tensor_tensor(out=big[:, :, :NB].rearrange("p e b -> p b e"),
                                                      in0=bc_mid(pend[:], NB), in1=bc_last(blkoff[:, :NB], 32),
                                                      op=ALU.is_le), reads=['pend', 'blkoff', 'big'], writes=['big'])
                S.op('dve', lambda e: e.tensor_reduce(out=eb[:, :NB], in_=big[:, :, :NB].rearrange("p e b -> p b e"),
                                                      axis=AX.X, op=ALU.add), reads=['big'], writes=['eb'])
                S.op('dve', lambda e: e.tensor_scalar(out=eb[:, :NB], in0=eb[:, :NB], scalar1=31.0, scalar2=None, op0=ALU.min),
                     reads=['eb'], writes=['eb'])
                S.op('dve', lambda e: e.memset(chg[:], 1.0), writes=['chg'])
                S.op('dve', lambda e: e.tensor_tensor(out=chg[:, 1:NB], in0=eb[:, 1:NB], in1=eb[:, 0:NB - 1], op=ALU.not_equal),
                     reads=['eb', 'chg'], writes=['chg'])
                for k4 in range(1, 4):
                    S.op('dve', lambda e: e.memset(chg[:, k4 * (NB // 4):k4 * (NB // 4) + 1], 1.0), reads=['chg'], writes=['chg'])
                for h in range(1):
                    S.op('dve', lambda e: e.tensor_scalar(out=wif[:, :NB, h], in0=eb[:, :NB], scalar1=128.0,
                                                          scalar2=float(l * 4096 - BIGW), op0=ALU.mult, op1=ALU.add),
                         reads=['eb', 'wif'], writes=['wif'])
                    S.op('dve', lambda e: e.tensor_scalar(out=wif[:, :NB, h], in0=wif[:, :NB, h], scalar1=pcol[:, 0:1],
                                                          scalar2=None, op0=ALU.add), reads=['wif', 'pcol'], writes=['wif'])
                    S.op('dve', lambda e: e.tensor_tensor(out=wif[:, :NB, h], in0=wif[:, :NB, h], in1=chg[:, :NB], op=ALU.mult),
                         reads=['wif', 'chg'], writes=['wif'])
                    S.op('dve', lambda e: e.tensor_scalar(out=wif[:, :NB, h], in0=wif[:, :NB, h], scalar1=float(BIGW),
                                                          scalar2=None, op0=ALU.add), reads=['wif'], writes=['wif'])
                S.op('dve', lambda e: e.tensor_copy(out=WI[:, :NB, 0:1], in_=wif[:, :NB, 0:1]), reads=['wif'], writes=['WI'])
                for k2 in range(2):
                    S.op('dve', lambda e: e.tensor_tensor(out=dtmp2[:, :J, :], in0=FM[:, :J, k2, :], in1=bc_mid(pst[:], J),
                                                          op=ALU.mult), reads=['FM', 'pst', 'dtmp2'], writes=['dtmp2'])
                    S.op('dve', lambda e: e.tensor_reduce(out=dif[:, :J, k2], in_=dtmp2[:, :J, :], axis=AX.X, op=ALU.add),
                         reads=['dtmp2', 'dif'], writes=['dif'])
                S.op('dve', lambda e: e.tensor_tensor(out=dif[:, :J, :], in0=dif[:, :J, :], in1=RK[:, :J, :], op=ALU.add),
                     reads=['dif', 'RK'], writes=['dif'])
                S.op('dve', lambda e: e.tensor_copy(out=DI[:, :J, :], in_=dif[:, :J, :]), reads=['dif'], writes=['DI'])
                for gsi in range(J):
                    q = gsi % 2
                    S.dma('sp', rows[q][:], h2tm[gsi * 128:(gsi + 1) * 128, :], reads=[('h2tm', gsi % 4)], writes=[('rows', q)])
                    for k2 in range(2):
                        idma(Xs[:, :], bass.IndirectOffsetOnAxis(ap=DI[:, gsi, k2:k2 + 1], axis=0), rows[q][:, :], None,
                             NB * 128 - 1, reads=[('rows', q), 'DI', 'Xs'], writes=[('Xsc', q)], slot=('Xsc', q))
                S.barrier()

        def phase_D(l, tiles, final):
            nsub_tot = sum(TILES[ti][1] // 128 for ti in tiles)
            NB = 2 * nsub_tot + 32
            wgv = wgb.rearrange("(a b) f -> a (b f)", b=2)
            wuv = wub.rearrange("(a b) f -> a (b f)", b=2)
            wdv = wdb.rearrange("(a b) f -> a (b f)", b=2)
            with contextlib.ExitStack() as ph:
                wgs = [sb(ph, f"wgs{i}", [128, 4096], BF16) for i in range(4)]
                wus = [sb(ph, f"wus{i}", [128, 4096], BF16) for i in range(4)]
                wds = [sb(ph, f"wds{i}", [128, 4096], BF16) for i in range(4)]
                blk = lambda p_: (p_ % 4) * (NB // 4) + p_ // 4
                xbs = [sb(ph, f"xbs{i}", [128, 1024], BF16) for i in range(2)]
                XTs = [sb(ph, f"XTs{i}", [128, 8, 128], BF16) for i in range(2)]
                s1s = [sb(ph, f"s1s{i}", [128, 512], F32) for i in range(2)]
                ATs = [sb(ph, f"ATs{i}", [128, 4, 128], BF16) for i in range(2)]
                Yts = [sb(ph, f"Yts{i}", [128, 1024], F32) for i in range(2)]

                def loadw(p_):
                    w_ = p_ % 4
                    for nm, view, tile_ in [('wgs', wgv, wgs[w_]), ('wus', wuv, wus[w_]), ('wds', wdv, wds[w_])]:
                        idma(tile_[:, :], None, view[:, :], bass.IndirectOffsetOnAxis(ap=WI[:, blk(p_), 0:1], axis=0),
                             (l + 1) * 4096 - 1, reads=['WI', ('wcv', l)], writes=[(nm, w_)], slot=(nm, w_))
                for p_ in range(3):
                    loadw(p_)
                def xbload(p_):
                    bn = blk(p_)
                    S.dma('sp', xbs[p_ % 2][:], Xs[bn * 128:(bn + 1) * 128, :], reads=[('Xsc', 0), ('Xsc', 1), 'Xs'],
                          writes=[('xbs', p_ % 2)])

                def stage_T(p_):
                    q = p_ % 2
                    xb, XT = xbs[q], XTs[q]
                    ptb = PS[q].bitcast(BF16)

                    def mmt(e):
                        for c in range(8):
                            ins = e.transpose(out=ptb[:, c * 128:(c + 1) * 128], in_=xb[:, c:1024:8], identity=identb[:])
                        return ins
                    S.op('pe', mmt, reads=[('xbs', q), 'identb'], writes=[pk(q)])
                    S.op('act', lambda e: e.activation(out=XT[:].rearrange("p c s -> p (c s)"), in_=ptb[:, :], func=AF.Identity),
                         reads=[pk(q)], writes=[('XTs', q)])

                def stage_G(p_):
                    q = p_ % 2
                    ws_ = p_ % 4
                    XT, AT = XTs[q], ATs[q]
                    wg_, wu_ = wgs[ws_], wus[ws_]
                    p1, p2 = 2 + 2 * q, 3 + 2 * q

                    def mmg(e, wt, p):
                        for fo in range(4):
                            for c in range(8):
                                c0 = c * 512 + fo
                                ins = e.matmul(PS[p][:, fo * 128:(fo + 1) * 128], lhsT=wt[:, c0:c0 + 509:4], rhs=XT[:, c, :],
                                               start=(c == 0), stop=(c == 7))
                        return ins
                    S.op('pe', lambda e: mmg(e, wg_, p1), reads=[('wgs', ws_), ('XTs', q)], writes=[pk(p1)])
                    S.op('pe', lambda e: mmg(e, wu_, p2), reads=[('wus', ws_), ('XTs', q)], writes=[pk(p2)])
                    S.op('act', lambda e: e.activation(out=s1s[q][:], in_=PS[p1][:, :], func=AF.Silu),
                         reads=[pk(p1)], writes=[('s1s', q)])
                    S.op('dve', lambda e: e.tensor_tensor(out=AT[:].rearrange("p c s -> p (c s)"), in0=s1s[q][:], in1=PS[p2][:, :],
                                                          op=ALU.mult), reads=[('s1s', q), pk(p2)], writes=[('ATs', q)])

                def stage_D(p_):
                    q = p_ % 2
                    ws_ = p_ % 4
                    b = blk(p_)
                    AT, Yt, wd_ = ATs[q], Yts[q], wds[ws_]
                    for dh in range(2):
                        py = 6 + dh

                        def mmd(e):
                            for fo in range(4):
                                ins = e.matmul(PS[py][:, :], lhsT=AT[:, fo, :],
                                               rhs=wd_[:, fo * 1024 + dh * 512:fo * 1024 + (dh + 1) * 512],
                                               start=(fo == 0), stop=(fo == 3))
                            return ins
                        S.op('pe', mmd, reads=[('wds', ws_), ('ATs', q)], writes=[pk(py)])
                        if dh == 0:
                            S.op('act', lambda e: e.activation(out=Yt[:, 0:512], in_=PS[py][:, :], func=AF.Identity),
                                 reads=[pk(py)], writes=[('Yts', q)])
                        else:
                            S.op('dve', lambda e: e.tensor_copy(out=Yt[:, 512:1024], in_=PS[py][:, :]),
                                 reads=[pk(py)], writes=[('Yts', q)])
                    S.dma('sp', Ys[b * 128:(b + 1) * 128, :], Yt[:], reads=[('Yts', q)], writes=[('Ys', q)])

                xbload(0)
                xbload(1)
                stage_T(0)
                for pos in range(NB):
                    if pos + 3 < NB:
                        loadw(pos + 3)
                    stage_G(pos)
                    if pos + 1 < NB:
                        stage_T(pos + 1)
                    if pos + 2 < NB:
                        xbload(pos + 2)
                    stage_D(pos)
                S.barrier()
            with contextlib.ExitStack() as ph3:
                xts = [sb(ph3, f"xtd{i}", [128, 8, 512], F32) for i in range(2)]
                g1 = [sb(ph3, f"g1{i}", [128, 1024], F32) for i in range(4)]
                g2 = [sb(ph3, f"g2{i}", [128, 1024], F32) for i in range(4)]
                tmp = norm_tmp(ph3) if final else None
                ots = [sb(ph3, f"otd{i}", [128, 8, 512], F32) for i in range(2)] if final else None
                subs = []
                for li, ti in enumerate(tiles):
                    for s_ in range(TILES[ti][1] // 128):
                        subs.append((li, ti, s_, len(subs)))

                def stage_a(li, ti, s, gsi):
                    off, cnt, _, _, r = TILES[ti]
                    b = li % 2
                    if s == 0:
                        S.dma('sp', xts[b][:, :, :cnt], xr[:, :, off:off + cnt].rearrange("j p t -> p j t"),
                              reads=[('xr', ti)], writes=[('xtd', b, j) for j in range(8)])
                    q = gsi % 4
                    idma(g1[q][:, :], None, Ys[:, :], bass.IndirectOffsetOnAxis(ap=DI[:, gsi, 0:1], axis=0),
                         NB * 128 - 1, reads=['DI', ('Ys', 0), ('Ys', 1)], writes=[('g1', q)], slot=('g1', q))
                    idma(g2[q][:, :], None, Ys[:, :], bass.IndirectOffsetOnAxis(ap=DI[:, gsi, 1:2], axis=0),
                         NB * 128 - 1, reads=['DI', ('Ys', 0), ('Ys', 1)], writes=[('g2', q)], slot=('g2', q))
                    S.op('dve', lambda e: e.tensor_scalar(out=g1[q][:], in0=g1[q][:], scalar1=WK[:, gsi, 0:1], scalar2=None,
                                                          op0=ALU.mult), reads=[('g1', q), 'WK'], writes=[('g1', q)])
                    S.op('dve', lambda e: e.scalar_tensor_tensor(out=g1[q][:], in0=g2[q][:], scalar=WK[:, gsi, 1:2],
                                                                 in1=g1[q][:], op0=ALU.mult, op1=ALU.add),
                         reads=[('g1', q), ('g2', q), 'WK'], writes=[('g1', q)])
                    for half in range(2):
                        pt = 2 * (q % 2) + half

                        def mmt2(e):
                            for jj in range(4):
                                j = half * 4 + jj
                                ins = e.transpose(out=PS[pt][:, jj * 128:(jj + 1) * 128], in_=g1[q][:, j * 128:(j + 1) * 128],
                                                  identity=ident[:])
                            return ins
                        S.op('pe', mmt2, reads=[('g1', q), 'ident'], writes=[pk(pt)])

                def stage_b(li, ti, s, gsi):
                    off, cnt, _, _, r = TILES[ti]
                    b = li % 2
                    x_ = xts[b]
                    q = gsi % 4
                    kxs = [('xtd', b, j) for j in range(8)]
                    for half in range(2):
                        pt = 2 * (q % 2) + half
                        for jj in range(4):
                            j = half * 4 + jj
                            S.op('dve', lambda e: e.scalar_tensor_tensor(out=x_[:, j, s * 128:(s + 1) * 128],
                                                                         in0=PS[pt][:, jj * 128:(jj + 1) * 128],
                                                                         scalar=mod_ap(l, 5, j, r),
                                                                         in1=x_[:, j, s * 128:(s + 1) * 128],
                                                                         op0=ALU.mult, op1=ALU.add),
                                 reads=[pk(pt), kxs[j], ('modT', l)], writes=[kxs[j]])
                    if s == cnt // 128 - 1:
                        if not final:
                            S.dma('sp', xr[:, :, off:off + cnt].rearrange("j p t -> p j t"), x_[:, :, :cnt],
                                  reads=kxs, writes=[('xr', ti)])
                            if dbg:
                                S.dma('sp', dbgo["d_x2"][:, :, off:off + cnt].rearrange("j p t -> p j t"), x_[:, :, :cnt],
                                      reads=kxs, writes=['d_x2'])
                        else:
                            o_ = ots[b]
                            norm_mod(tmp, x_, cnt, kxs, lambda j: o_[:, j, :cnt], ('otd', b),
                                     lambda j: fing[:, j:j + 1], None, 7, extra_reads=['fing'])
                            S.dma('sp', outT[:, :, off:off + cnt].rearrange("j p t -> p j t"), o_[:, :, :cnt],
                                  reads=[('otd', b)], writes=[('out', b)])

                for idx in range(len(subs) + 1):
                    if idx < len(subs):
                        stage_a(*subs[idx])
                    if idx >= 1:
                        stage_b(*subs[idx - 1])
                S.barrier()

        phase_C(0, w_out, list(range(9)), xc)
        phase_D(0, list(range(9)), final=False)

        hst = contextlib.ExitStack()
        hbuf = sb(hst, "hbuf1", [128, 8, NT], BF16)
        phase_A(1, xr, hbuf)
        with contextlib.ExitStack() as ph:
            cost = sb(ph, "cost", [128, NT], F32)
            sint = sb(ph, "sint", [128, NT], F32)
            dal = sb(ph, "dal", [128, 4, 64], F32)
            dtmp = sb(ph, "dtmp", [128, 64], F32)
            lamv = sb(ph, "lamv", [128, 4], F32)
            subg = sb(ph, "subg", [128, 1], F32)
            wq = sb(ph, "wq", [128, 8, 128], BF16)
            wk = sb(ph, "wk", [128, 8, 128], BF16)
            wv = sb(ph, "wv", [128, 8, 128], BF16)
            qbt = [sb(ph, f"qbt{i}", [128, 512], BF16) for i in range(2)]
            rmb = sb(ph, "rmb", [128, 128], BF16)
            QT = sb(ph, "QT", [128, NQ], BF16)
            vtb = [sb(ph, f"vtb{i}", [128, 512], BF16) for i in range(2)]
            KT = sb(ph, "KT", [128, NT], BF16)
            Vt = sb(ph, "Vt", [128, 34, 128], BF16)
            rt1 = [sb(ph, f"rt1{i}", [128, 512], F32) for i in range(2)]
            rt2 = [sb(ph, f"rt2{i}", [128, 512], F32) for i in range(2)]
            Eb2 = [sb(ph, f"Eb{i}", [128, 2, 512], BF16) for i in range(2)]
            acc2 = sb(ph, "acc2", [128, 2, 512], F32)
            obw = sb(ph, "obw", [128, 2, 512], F32)
            ob = [obw[:, 0, :], obw[:, 1, :]]
            rlw = sb(ph, "rlw", [128, 2, 512], F32)
            rl = sb(ph, "rl", [128, 512], F32)
            accD = sb(ph, "accD", [128, 512], F32)
            accP = sb(ph, "accP", [128, 512], F32)
            osq = sb(ph, "osq", [128, 512], F32)
            aot = [sb(ph, f"aot{i}", [128, 512], BF16) for i in range(2)]
            S.dma('sp', cost[:], cos_in, writes=['cos'])
            S.dma('pool', rmb[:], rmat_in, writes=['rmb'])
            S.dma('sp', sint[:], sin_in, writes=['sin'])
            S.dma('sp', dal[:], dalam_in, writes=['dal'])
            S.dma('sp', subg[:], subg_in, writes=['subg'])
            for i2 in range(2):
                S.op('dve', lambda e: e.tensor_tensor(out=dtmp[:], in0=dal[:, 2 * i2, :], in1=dal[:, 2 * i2 + 1, :], op=ALU.mult),
                     reads=['dal'], writes=['dtmp'])
                S.op('dve', lambda e: e.tensor_reduce(out=lamv[:, i2:i2 + 1], in_=dtmp[:], axis=AX.X, op=ALU.add),
                     reads=['dtmp'], writes=['lamv'])
            S.op('act', lambda e: e.activation(out=lamv[:, 0:2], in_=lamv[:, 0:2], func=AF.Exp), reads=['lamv'], writes=['lamv'])
            S.op('dve', lambda e: e.tensor_tensor(out=lamv[:, 2:3], in0=lamv[:, 1:2], in1=lamv[:, 0:1], op=ALU.subtract),
                 reads=['lamv'], writes=['lamv'])
            S.op('dve', lambda e: e.tensor_scalar(out=lamv[:, 2:3], in0=lamv[:, 2:3], scalar1=-LAMBDA_INIT, scalar2=None,
                                                  op0=ALU.add), reads=['lamv'], writes=['lamv'])
            S.op('dve', lambda e: e.tensor_scalar(out=lamv[:, 3:4], in0=subg[:], scalar1=1.0 - LAMBDA_INIT, scalar2=None,
                                                  op0=ALU.mult), reads=['lamv', 'subg'], writes=['lamv'])
            nkc = 34
            acnt = 0
            for hh in range(8):
                for t_, c0, k_ in [(wq, hh * 128, 'wq'), (wk, 1024 + hh * 128, 'wk'), (wv, 2048 + hh * 128, 'wv')]:
                    S.dma('pool', t_[:], w_qkv[:, c0:c0 + 128].rearrange("(k p) f -> p k f", p=128), writes=[k_])
                convert_experts(1, 4 * hh, 4 * hh + 4)
                jobs = []
                for ti in range(9):
                    jobs.append(('v', ti))
                    if ti < 4:
                        jobs.append(('q', ti))
                    jobs.append(('k', ti))

                def proj_s1(kj, kind, ti):
                    off, cnt = TILES[ti][0], TILES[ti][1]
                    q = kj % 2
                    pa_ = 2 * q
                    wt, kw_ = {'q': (wq, 'wq'), 'k': (wk, 'wk'), 'v': (wv, 'wv')}[kind]

                    def mmp(e):
                        for kc in range(8):
                            ins = e.matmul(PS[pa_][:, :cnt], lhsT=wt[:, kc, :], rhs=hbuf[:, kc, off:off + cnt],
                                           start=(kc == 0), stop=(kc == 7))
                        return ins
                    S.op('pe', mmp, reads=[kw_, ('h', ti)], writes=[pk(pa_)])
                    S.op('act', lambda e: e.activation(out=qbt[q][:, :cnt], in_=PS[pa_][:, :cnt], func=AF.Identity),
                         reads=[pk(pa_)], writes=[('qbt', q)])

                def proj_s2(kj, kind, ti):
                    off, cnt = TILES[ti][0], TILES[ti][1]
                    q = kj % 2
                    pa_, pb_ = 2 * q, 2 * q + 1
                    if kind == 'v':
                        nsb = cnt // 128
                        pbb = PS[pb_].bitcast(BF16)

                        def mmvt(e):
                            for s in range(nsb):
                                ins = e.transpose(out=pbb[:, s * 128:(s + 1) * 128], in_=qbt[q][:, s * 128:(s + 1) * 128],
                                                  identity=identb[:])
                            return ins
                        S.op('pe', mmvt, reads=[('qbt', q), 'identb'], writes=[pk(pb_)])
                        si0 = off // 128
                        S.op('dve', lambda e: e.tensor_copy(out=Vt[:, si0:si0 + nsb, :].rearrange("p s v -> p (s v)"),
                                                            in_=pbb[:, :nsb * 128]), reads=[pk(pb_)], writes=['Vt'])
                        return
                    dst, kdst = (QT, 'QT') if kind == 'q' else (KT, 'KT')
                    S.op('pe', lambda e: e.matmul(PS[pb_][:, :cnt], lhsT=rmb[:], rhs=qbt[q][:, :cnt], start=True, stop=True),
                         reads=['rmb', ('qbt', q)], writes=[pk(pb_)])
                    S.op('dve', lambda e: e.tensor_tensor(out=rt1[q][:, :cnt], in0=PS[pa_][:, :cnt], in1=cost[:, off:off + cnt],
                                                          op=ALU.mult), reads=[pk(pa_), 'cos', ('qbt', q)], writes=[('rt1', q)])
                    S.op('dve', lambda e: e.tensor_tensor(out=rt2[q][:, :cnt], in0=PS[pb_][:, :cnt], in1=sint[:, off:off + cnt],
                                                          op=ALU.mult), reads=[pk(pb_), 'sin'], writes=[('rt2', q)])
                    S.op('dve', lambda e: e.tensor_tensor(out=dst[:, off:off + cnt], in0=rt1[q][:, :cnt], in1=rt2[q][:, :cnt],
                                                          op=ALU.add), reads=[('rt1', q), ('rt2', q)], writes=[kdst])

                proj_s1(0, *jobs[0])
                for kj in range(len(jobs)):
                    if kj + 1 < len(jobs):
                        proj_s1(kj + 1, *jobs[kj + 1])
                    proj_s2(kj, *jobs[kj])
                for qt in range(4):
                    q0 = qt * 512
                    pend_ = None
                    for kc in range(nkc + 1):
                        if kc < nkc:
                            sidx = acnt % 2
                            acnt += 1
                            sb0 = 2 * sidx
                            for mi in range(2):
                                lo_, hi_ = mi * 64, (mi + 1) * 64
                                S.op('pe', lambda e: e.matmul(PS[sb0 + mi][:, :], lhsT=KT[lo_:hi_, kc * 128:(kc + 1) * 128],
                                                              rhs=QT[lo_:hi_, q0:q0 + 512], start=True, stop=True),
                                     reads=['KT', 'QT'], writes=[pk(sb0 + mi)])
                            ke = ('Eb', sidx)
                            S.op('act', lambda e: e.activation(out=Eb2[sidx][:].rearrange("p m q -> p (m q)"),
                                                               in_=psbig[:, sb0 * 512:(sb0 + 2) * 512], func=AF.Exp, scale=0.125),
                                 reads=[pk(sb0), pk(sb0 + 1)], writes=[ke])
                            cur = (Eb2[sidx], ke)
                        if pend_ is not None:
                            kp, (ebp, kep) = pend_
                            for mi in range(2):
                                S.op('pe', lambda e: e.matmul(PS[4 + mi][:, :], lhsT=Vt[:, kp, :], rhs=ebp[:, mi, :], start=(kp == 0),
                                                              stop=(kp == nkc - 1)), reads=['Vt', kep], writes=[pk(4 + mi)])
                            accv = psbig[:, 6 * 512:8 * 512]
                            ebf = ebp[:].rearrange("p m q -> p (m q)")
                            if kp == 0:
                                S.op('dve', lambda e: e.tensor_copy(out=accv, in_=ebf), reads=[kep], writes=[pk(6), pk(7)])
                            else:
                                S.op('dve', lambda e: e.tensor_tensor(out=accv, in0=accv, in1=ebf, op=ALU.add),
                                     reads=[kep, pk(6), pk(7)], writes=[pk(6), pk(7)])
                        pend_ = (kc, cur) if kc < nkc else None
                    S.op('act', lambda e: e.activation(out=acc2[:].rearrange("p m q -> p (m q)"), in_=psbig[:, 6 * 512:8 * 512],
                                                       func=AF.Identity), reads=[pk(6), pk(7)], writes=['acc2'])
                    for mi in range(2):
                        S.op('pe', lambda e: e.matmul(PS[mi][:, :], lhsT=ones32[:], rhs=acc2[:, mi, :], start=True, stop=True),
                             reads=['ones32', 'acc2'], writes=[pk(mi)])
                    rlf = rlw[:].rearrange("p m q -> p (m q)")
                    S.op('act', lambda e: e.activation(out=rlf, in_=psbig[:, 0:1024], func=AF.Ln), reads=[pk(0), pk(1)], writes=['rlw'])
                    S.op('act', lambda e: e.activation(out=rlf, in_=rlf, func=AF.Exp, scale=-1.0), reads=['rlw'], writes=['rlw'])
                    S.op('dve', lambda e: e.tensor_tensor(out=obw[:].rearrange("p m q -> p (m q)"), in0=psbig[:, 4 * 512:6 * 512],
                                                          in1=rlf, op=ALU.mult),
                         reads=[pk(4), pk(5), 'rlw'], writes=[('ob', 0), ('ob', 1)])
                    S.op('dve', lambda e: e.scalar_tensor_tensor(out=ob[0][:], in0=ob[1][:], scalar=lamv[:, 2:3], in1=ob[0][:],
                                                                 op0=ALU.mult, op1=ALU.add),
                         reads=[('ob', 0), ('ob', 1), 'lamv'], writes=[('ob', 0)])
                    S.op('act', lambda e: e.activation(out=osq[:], in_=ob[0][:], func=AF.Square), reads=[('ob', 0)], writes=['osq'])
                    S.op('pe', lambda e: e.matmul(PS[2][:, :], lhsT=ones32[:], rhs=osq[:], start=True, stop=True),
                         reads=['ones32', 'osq'], writes=[pk(2)])
                    S.op('act', lambda e: e.activation(out=osq[:], in_=PS[2][:, :], func=AF.Ln, bias=eps_t[:, 0:1],
                                                       scale=1.0 / 128.0), reads=[pk(2), 'eps'], writes=['osq'])
                    S.op('act', lambda e: e.activation(out=osq[:], in_=osq[:], func=AF.Exp, scale=-0.5), reads=['osq'], writes=['osq'])
                    a_ = aot[qt % 2]
                    S.op('dve', lambda e: e.scalar_tensor_tensor(out=a_[:], in0=ob[0][:], scalar=lamv[:, 3:4], in1=osq[:],
                                                                 op0=ALU.mult, op1=ALU.mult),
                         reads=[('ob', 0), 'osq', 'lamv'], writes=[('aot', qt % 2)])
                    S.dma('sp', zscr[hh, :, q0:q0 + 512], a_[:], reads=[('aot', qt % 2)], writes=[('zs', qt)])
            S.barrier()
        hst.close()

        phase_C(1, w_o, list(range(4)), xr)
        phase_D(1, list(range(4)), final=True)
        S.barrier()
    return nc


def _prep_shared(inp, rev):
    dsl = slice(None, None, -1) if rev else slice(None)
    f = lambda a: np.ascontiguousarray(np.asarray(a, dtype=np.float32))
    pj = lambda v: f(np.asarray(v).reshape(8, 128).T)
    sh = {}
    sh["w_mod"] = f(inp["w_mod"])
    sh["bmod"] = f(np.asarray(inp["b_mod"]).reshape(2, 48, 128).transpose(2, 0, 1))
    sh["n1g"] = f(np.asarray(inp["norm1_g"]).reshape(2, 8, 128).transpose(2, 0, 1))
    sh["n2g"] = f(np.asarray(inp["norm2_g"]).reshape(2, 8, 128).transpose(2, 0, 1))
    sh["fing"] = pj(inp["final_g"])
    sh["w_in"] = f(inp["rg_w_in"][0])
    cw = np.asarray(inp["rg_conv_w"][0])
    z1 = np.zeros((1, 1024), np.float32)
    cw5 = np.concatenate([cw, z1], 0) if not rev else np.concatenate([z1, cw[::-1]], 0)
    sh["convw"] = f(cw5.reshape(5, 8, 128).transpose(2, 1, 0))
    sh["convb"] = pj(inp["rg_conv_b"][0])
    sh["gaw"] = f(np.asarray(inp["rg_gate_a_w"][0])[dsl])
    sh["gxw"] = f(np.asarray(inp["rg_gate_x_w"][0])[dsl])
    sh["gab"] = f(np.asarray(inp["rg_gate_a_b"][0])[dsl].reshape(2, 8, 128).transpose(2, 0, 1))
    sh["gxb"] = f(np.asarray(inp["rg_gate_x_b"][0])[dsl].reshape(2, 8, 128).transpose(2, 0, 1))
    sh["lam"] = f(np.asarray(inp["rg_lambda"][0])[dsl].reshape(2, 8, 128).transpose(2, 0, 1))
    sh["w_out"] = f(inp["rg_w_out"][0])
    sh["w_qkv"] = f(inp["da_w_qkv"][0])
    sh["dalam"] = f(np.broadcast_to(np.asarray(inp["da_lambda"][0])[None], (128, 4, 64)))
    sh["subg"] = f(np.asarray(inp["da_subln_g"][0]).reshape(128, 1))
    sh["w_o"] = f(inp["da_w_o"][0])
    sh["rw"] = f(np.asarray(inp["router_w"]).reshape(8, 128, 32).transpose(1, 0, 2))
    sh["rb"] = f(np.broadcast_to(np.asarray(inp["router_bias"])[None], (128, 32)))
    sh["wg"] = f(inp["moe_w_gate"])
    sh["wu"] = f(inp["moe_w_up"])
    sh["wd"] = f(inp["moe_w_down"])
    t = np.arange(NX)
    row = (t // 64).astype(np.float32)
    col = (t % 64).astype(np.float32)
    inv = (1.0 / (10000.0 ** (np.arange(16, dtype=np.float32) / 16))).astype(np.float32)
    ang = np.stack([row, col], -1)[:, :, None] * inv
    ang = np.broadcast_to(ang[:, :, None, :], (NX, 2, 2, 16)).reshape(NX, 64).astype(np.float32)
    cos = np.ones((128, NT), np.float32)
    sin = np.zeros((128, NT), np.float32)
    if rev:
        ang = ang[::-1]
    cos[:, :NX] = np.tile(np.cos(ang).T, (2, 1))
    sin[:, :NX] = np.tile(np.sin(ang).T, (2, 1))
    sh["pcol"] = np.arange(128, dtype=np.float32).reshape(128, 1)
    sh["blkoff"] = np.ascontiguousarray(np.broadcast_to((128.0 * np.arange(100, dtype=np.float32))[None], (128, 100)))
    rm = np.zeros((128, 128), np.float32)
    for m in range(128):
        if (m % 32) < 16:
            rm[m + 16, m] = -1.0
        else:
            rm[m - 16, m] = 1.0
    sh["rmat"] = rm
    sh["cos"] = cos
    sh["sin"] = sin
    return sh


def _prep_core(inp, b, rev):
    f = lambda a: np.ascontiguousarray(np.asarray(a, dtype=np.float32))
    x = np.asarray(inp["x"][b])
    ctx = np.asarray(inp["ctx"][b])
    if rev:
        x = x[::-1]
        ctx = ctx[::-1]
    tok = np.concatenate([x, ctx], axis=0)
    d = {"xc": f(tok.T.reshape(8, 128, NT))}
    cond = np.stack([np.asarray(inp["c"][b]), np.asarray(inp["c_ctx"])], -1)
    d["cond"] = f(cond.reshape(8, 128, 2).transpose(1, 0, 2))
    return d


def kernel(**inputs):
    nc = build(DEBUG)
    shs = [_prep_shared(inputs, False), _prep_shared(inputs, True)]
    in_maps = []
    for core in range(8):
        b, rev = core // 2, core % 2
        m = dict(shs[rev])
        m.update(_prep_core(inputs, b, bool(rev)))
        in_maps.append(m)
    res = run_bass_kernel_spmd(nc, in_maps, core_ids=list(range(8)))
    out = np.empty((4, NX, 1024), np.float32)
    for core in range(8):
        b, rev = core // 2, core % 2
        o = np.asarray(res.results[core]["outT"]).reshape(1024, NQ).T
        if rev:
            out[b, NX - NQ:] = o[::-1]
        else:
            out[b, :NQ] = o
    return out
```

```python
import contextlib
import math
import numpy as np
import concourse.bass as bass
import concourse.mybir as mybir
from concourse.bass_utils import run_bass_kernel_spmd

F32 = mybir.dt.float32
BF16 = mybir.dt.bfloat16
ALU = mybir.AluOpType
AF = mybir.ActivationFunctionType
AX = mybir.AxisListType

EPS = 1e-6
NT = 4352
NX = 4096
NCTX = 256
TILES = [(i * 512, 512, 2 + i * 512, i * 512, 0) for i in range(8)] + [(4096, 256, 4102, 4100, 1)]
UW = 4360
UCW = 4356
NQ = 2048
LAMBDA_INIT = 0.8 - 0.6 * math.exp(-0.3 * 1)
DEBUG = False


class Sched:
    ROT = 30000

    def __init__(self, nc, stack):
        self.nc = nc
        self.stack = stack
        self.engs = {'pe': nc.tensor, 'act': nc.scalar, 'dve': nc.vector,
                     'pool': nc.gpsimd, 'sp': nc.sync}
        self.cur = {}
        self.nsem = 0
        for e in self.engs:
            self.cur[e] = [self._newsem(e), 0]
        self.lastw = {}
        self.readers = {}
        self.waited = {e: {} for e in self.engs}
        self.dsem = {}
        self.alltok = {}

    def _newsem(self, nm):
        self.nsem += 1
        return self.stack.enter_context(self.nc.semaphore(f"s_{nm}_{self.nsem}"))

    def _wait(self, eng, toks):
        best = {}
        for (s, v) in toks:
            k = id(s)
            if k not in best or best[k][1] < v:
                best[k] = (s, v)
        for k, (s, v) in best.items():
            if self.waited[eng].get(k, 0) >= v:
                continue
            self.engs[eng].wait_ge(s, v)
            self.waited[eng][k] = v

    def _deps(self, eng, reads, writes, skip_same_pe=True):
        toks = []
        for k in reads:
            if k in self.lastw:
                toks.append(self.lastw[k])
        for k in writes:
            if k in self.lastw:
                toks.append(self.lastw[k])
            toks.extend(self.readers.get(k, ()))
        if eng == 'pe' and skip_same_pe:
            toks = [t for t in toks if t[0] is not self.cur['pe'][0]]
        return toks

    def _commit(self, tok, reads, writes):
        for k in writes:
            self.lastw[k] = tok
            self.readers[k] = []
        for k in reads:
            if k in writes:
                continue
            self.readers.setdefault(k, []).append(tok)
        self.alltok[id(tok[0])] = tok

    def op(self, eng, fn, reads=(), writes=()):
        self._wait(eng, self._deps(eng, reads, writes))
        c = self.cur[eng]
        if c[1] >= self.ROT:
            c[0] = self._newsem(eng)
            c[1] = 0
        ins = fn(self.engs[eng])
        c[1] += 1
        ins.then_inc(c[0], 1)
        self._commit((c[0], c[1]), reads, writes)

    def dma(self, eng, out, in_, reads=(), writes=(), slot=None, **kw):
        if slot is None:
            slot = ('auto',) + tuple(writes)
        self._wait(eng, self._deps(eng, reads, writes, skip_same_pe=False))
        if slot not in self.dsem:
            self.dsem[slot] = [self._newsem('d'), 0]
        d = self.dsem[slot]
        ins = self.engs[eng].dma_start(out=out, in_=in_, **kw)
        d[1] += 16
        ins.then_inc(d[0], 16)
        self._commit((d[0], d[1]), reads, writes)

    def barrier(self):
        toks = list(self.alltok.values())
        for e in self.engs:
            self._wait(e, toks)


def bc_last(a, n):
    return bass.AP(a.tensor, a.offset, [list(x) for x in a.ap] + [[0, n]])


def bc_mid(a, n):
    l = [list(x) for x in a.ap]
    return bass.AP(a.tensor, a.offset, [l[0], [0, n]] + l[1:])


def build(dbg=False):
    nc = bass.Bass("TRN2", target_bir_lowering=False)

    def din(name, shape, dt=F32):
        return nc.dram_tensor(name, list(shape), dt, kind="ExternalInput").ap()

    xc = din("xc", [8, 128, NT])
    cond_in = din("cond", [128, 8, 2])
    w_mod = din("w_mod", [2, 1024, 6144])
    bmod_in = din("bmod", [128, 2, 48])
    n1g_in = din("n1g", [128, 2, 8])
    n2g_in = din("n2g", [128, 2, 8])
    fing_in = din("fing", [128, 8])
    w_in = din("w_in", [1024, 2048])
    convw_in = din("convw", [128, 8, 5])
    convb_in = din("convb", [128, 8])
    gaw = din("gaw", [2, 8, 128, 128])
    gxw = din("gxw", [2, 8, 128, 128])
    gab_in = din("gab", [128, 2, 8])
    gxb_in = din("gxb", [128, 2, 8])
    lam_in = din("lam", [128, 2, 8])
    w_out = din("w_out", [1024, 1024])
    w_qkv = din("w_qkv", [1024, 3072])
    dalam_in = din("dalam", [128, 4, 64])
    subg_in = din("subg", [128, 1])
    w_o = din("w_o", [1024, 1024])
    rw_in = din("rw", [128, 8, 32])
    rb_in = din("rb", [128, 32])
    wg = din("wg", [2, 32, 1024, 512])
    wu = din("wu", [2, 32, 1024, 512])
    wd = din("wd", [2, 32, 512, 1024])
    rmat_in = din("rmat", [128, 128])
    cos_in = din("cos", [128, NT])
    sin_in = din("sin", [128, NT])
    outT = nc.dram_tensor("outT", [8, 128, NQ], F32, kind="ExternalOutput").ap()
    xr = nc.dram_tensor("xr", [8, 128, NT], F32, kind="Internal").ap()
    zscr = nc.dram_tensor("zscr", [8, 128, NT], BF16, kind="Internal").ap()
    h2tm = nc.dram_tensor("h2tm", [NT, 1024], BF16, kind="Internal").ap()
    Xs = nc.dram_tensor("Xs", [100 * 128, 1024], BF16, kind="Internal").ap()
    Ys = nc.dram_tensor("Ys", [100 * 128, 1024], F32, kind="Internal").ap()
    wgb = nc.dram_tensor("wgb", [2 * 8192, 2048], BF16, kind="Internal").ap()
    wub = nc.dram_tensor("wub", [2 * 8192, 2048], BF16, kind="Internal").ap()
    wdb = nc.dram_tensor("wdb", [2 * 8192, 2048], BF16, kind="Internal").ap()
    pcol_in = din("pcol", [128, 1])
    blkoff_in = din("blkoff", [128, 100])
    dbgo = {}
    if dbg:
        for nm, shp in [("d_mod", [128, 2 * 48 * 2]), ("d_h", [128, 8, NT]), ("d_z", [8, 128, NT]),
                        ("d_x1", [8, 128, NT]), ("d_x2", [8, 128, NT]),
                        ("d_ao", [8, 128, NT])]:
            dbgo[nm] = nc.dram_tensor(nm, shp, F32, kind="ExternalOutput").ap()

    with contextlib.ExitStack() as st:
        S = Sched(nc, st)

        uid = [0]

        def sb(stack, name, shape, dt):
            uid[0] += 1
            return stack.enter_context(nc.sbuf_tensor(f"sb{uid[0]}_{name}", list(shape), dt))

        psbig = st.enter_context(nc.psum_tensor("psbig", [128, 8 * 512], F32))
        PS = [psbig[:, i * 512:(i + 1) * 512] for i in range(8)]
        pk = lambda i: ('ps', i)
        wgv_all = wg.rearrange("l e (q r) f -> (l e q) (r f)", r=4)
        wuv_all = wu.rearrange("l e (q r) f -> (l e q) (r f)", r=4)
        wdv_all = wd.rearrange("l e (q r) d -> (l e q) (r d)", r=2)

        def convert_experts(l, e0, e1):
            for e_ in range(e0, e1):
                r0 = (l * 32 + e_) * 256
                for dst, src in [(wgb, wgv_all), (wub, wuv_all), (wdb, wdv_all)]:
                    S.dma('pool', dst[r0:r0 + 256, :], src[r0:r0 + 256, :], writes=[('wcv', l)])

        ones_bf = sb(st, "ones_bf", [128, 128], BF16)
        ones32 = sb(st, "ones32", [128, 128], F32)
        ident = sb(st, "ident", [128, 128], F32)
        identb = sb(st, "identb", [128, 128], BF16)
        modT = sb(st, "modT", [128, 2, 48, 2], F32)
        gmT = sb(st, "gmT", [128, 2, 2, 8, 2], F32)
        n1g = sb(st, "n1g", [128, 2, 8], F32)
        n2g = sb(st, "n2g", [128, 2, 8], F32)
        fing = sb(st, "fing", [128, 8], F32)
        rw = sb(st, "rw", [128, 8, 32], F32)
        rb = sb(st, "rb", [128, 32], F32)
        S.op('dve', lambda e: e.memset(ones_bf[:], 1.0), writes=['ones_bf'])
        S.op('dve', lambda e: e.memset(ones32[:], 1.0), writes=['ones32'])
        S.op('pool', lambda e: e.memset(ident[:], 1.0), writes=['ident'])
        S.op('pool', lambda e: e.affine_select(out=ident[:], in_=ident[:], pattern=[[-1, 128]],
                                               compare_op=ALU.is_equal, fill=0.0, base=0, channel_multiplier=1),
             reads=['ident'], writes=['ident'])
        S.op('act', lambda e: e.activation(out=identb[:], in_=ident[:], func=AF.Identity), reads=['ident'], writes=['identb'])
        for t, src, k in [(n1g, n1g_in, 'n1g'), (n2g, n2g_in, 'n2g'), (fing, fing_in, 'fing'),
                          (rw, rw_in, 'rw'), (rb, rb_in, 'rb')]:
            S.dma('sp', t[:], src, writes=[k])

        with contextlib.ExitStack() as ph:
            condt = sb(ph, "condt", [128, 8, 2], F32)
            scond = sb(ph, "scond", [128, 8, 2], F32)
            bm = sb(ph, "bm", [128, 2, 48], F32)
            wm = [sb(ph, f"wm{i}", [128, 8, 1024], F32) for i in range(2)]
            S.dma('sp', condt[:], cond_in, writes=['cond'])
            S.dma('sp', bm[:], bmod_in, writes=['bm'])
            S.op('act', lambda e: e.activation(out=scond[:], in_=condt[:], func=AF.Silu),
                 reads=['cond'], writes=['scond'])
            it = 0
            for l in range(2):
                for gi in range(6):
                    buf = wm[it % 2]
                    key = ('wm', it % 2)
                    S.dma('sp', buf[:], w_mod[l, :, gi * 1024:(gi + 1) * 1024].rearrange("(k p) f -> p k f", p=128),
                          writes=[key])
                    pst = PS[it % 2]

                    def mm(e, buf=buf, pst=pst):
                        for j in range(8):
                            for kc in range(8):
                                ins = e.matmul(pst[:, j * 2:(j + 1) * 2], lhsT=buf[:, kc, j * 128:(j + 1) * 128],
                                               rhs=scond[:, kc, :], start=(kc == 0), stop=(kc == 7))
                        return ins
                    S.op('pe', mm, reads=[key, 'scond'], writes=[pk(it % 2)])
                    S.op('dve', lambda e: e.tensor_tensor(
                        out=modT[:, l, gi * 8:(gi + 1) * 8, :],
                        in0=pst[:, 0:16].rearrange("p (j r) -> p j r", r=2),
                        in1=bc_last(bm[:, l, gi * 8:(gi + 1) * 8], 2), op=ALU.add),
                        reads=[pk(it % 2), 'bm'], writes=['modT'])
                    it += 1
            for l in range(2):
                for w_, (gt, gk, sidx) in enumerate([(n1g, 'n1g', 1), (n2g, 'n2g', 4)]):
                    S.op('dve', lambda e: e.tensor_scalar(out=gmT[:, l, w_], in0=modT[:, l, sidx * 8:(sidx + 1) * 8, :],
                                                          scalar1=1.0, scalar2=None, op0=ALU.add),
                         reads=['modT'], writes=['gmT'])
                    S.op('dve', lambda e: e.tensor_tensor(out=gmT[:, l, w_], in0=gmT[:, l, w_],
                                                          in1=bc_last(gt[:, l, :], 2), op=ALU.mult),
                         reads=['gmT', gk], writes=['gmT'])
            if dbg:
                S.dma('sp', dbgo["d_mod"], modT[:].rearrange("p a b c -> p (a b c)"), reads=['modT'], writes=['d_mod'])
            S.barrier()

        def mod_ap(l, idx, j, r):
            return modT[:, l, idx * 8 + j, r:r + 1]

        def norm_mod(tmp, xt, n, kx, out, kout, gm_of_j, sh_of_j, psi, extra_reads=(), tag=''):
            sq, rt, rstd, xn = tmp
            kxl = list(kx) if isinstance(kx, list) else [kx]
            S.op('act', lambda e: e.activation(out=sq[:, :, :n], in_=xt[:, :, :n], func=AF.Square),
                 reads=kxl, writes=['nm_sq' + tag])

            def mm(e):
                for j in range(8):
                    ins = e.matmul(PS[psi][:, :n], lhsT=ones_bf[:], rhs=sq[:, j, :n], start=(j == 0), stop=(j == 7))
                return ins
            S.op('pe', mm, reads=['nm_sq' + tag, 'ones_bf'], writes=[pk(psi)])
            S.op('act', lambda e: e.activation(out=rt[:, :n], in_=PS[psi][:, :n], func=AF.Ln,
                                               bias=eps_t[:, 0:1], scale=1.0 / 1024.0),
                 reads=[pk(psi), 'eps'], writes=['nm_rt' + tag])
            S.op('act', lambda e: e.activation(out=rstd[:, :n], in_=rt[:, :n], func=AF.Exp, scale=-0.5),
                 reads=['nm_rt' + tag], writes=['nm_rstd' + tag])
            S.op('dve', lambda e: e.tensor_tensor(out=xn[:, :, :n], in0=xt[:, :, :n], in1=bc_mid(rstd[:, :n], 8),
                                                  op=ALU.mult), reads=kxl + ['nm_rstd' + tag], writes=['nm_xn' + tag])
            for j in range(8):
                if sh_of_j is None:
                    S.op('dve', lambda e: e.tensor_scalar(out=out(j), in0=xn[:, j, :n], scalar1=gm_of_j(j),
                                                          scalar2=None, op0=ALU.mult),
                         reads=['nm_xn' + tag] + list(extra_reads), writes=[kout])
                elif j % 2 == 0:
                    S.op('dve', lambda e: e.tensor_scalar(out=out(j), in0=xn[:, j, :n], scalar1=gm_of_j(j),
                                                          scalar2=sh_of_j(j), op0=ALU.mult, op1=ALU.add),
                         reads=['nm_xn' + tag] + list(extra_reads), writes=[kout])
                else:
                    S.op('act', lambda e: e.activation(out=out(j), in_=xn[:, j, :n], func=AF.Identity,
                                                       bias=sh_of_j(j), scale=gm_of_j(j)),
                         reads=['nm_xn' + tag] + list(extra_reads), writes=[kout])

        def norm_tmp(ph):
            return (sb(ph, "nm_sq", [128, 8, 512], BF16), sb(ph, "nm_rt", [128, 512], F32),
                    sb(ph, "nm_rstd", [128, 512], F32), sb(ph, "nm_xn", [128, 8, 512], F32))

        eps_t = sb(st, "eps_t", [128, 1], F32)
        S.op('dve', lambda e: e.memset(eps_t[:], EPS), writes=['eps'])
        one_t = sb(st, "one_t", [128, 1], F32)
        S.op('dve', lambda e: e.memset(one_t[:], 1.0), writes=['one'])


        def phase_A(l, src, hbuf):
            with contextlib.ExitStack() as ph:
                xts = [sb(ph, f"xt{i}", [128, 8, 512], F32) for i in range(2)]
                tmp = norm_tmp(ph)
                for ti, (off, cnt, _, _, r) in enumerate(TILES):
                    xt = xts[ti % 2]
                    kx = ('xt', ti % 2)
                    S.dma('sp', xt[:, :, :cnt], src[:, :, off:off + cnt].rearrange("j p t -> p j t"),
                          reads=[('xr', ti)], writes=[kx])
                    norm_mod(tmp, xt, cnt, kx, lambda j: hbuf[:, j, off:off + cnt], ('h', ti),
                             lambda j: gmT[:, l, 0, j, r:r + 1], lambda j: mod_ap(l, 0, j, r), 7,
                             extra_reads=['gmT', 'modT'])
                S.barrier()

        hst = contextlib.ExitStack()
        hbuf = sb(hst, "hbuf", [128, 8, NT], BF16)
        phase_A(0, xc, hbuf)
        if dbg:
            with contextlib.ExitStack() as ph:
                t32 = sb(ph, "dbg32", [128, 8, 512], F32)
                for ti, (off, cnt, _, _, r) in enumerate(TILES):
                    S.op('dve', lambda e: e.tensor_copy(out=t32[:, :, :cnt], in_=hbuf[:, :, off:off + cnt]),
                         reads=[('h', ti)], writes=['dbg32'])
                    S.dma('sp', dbgo["d_h"][:, :, off:off + cnt], t32[:, :, :cnt], reads=['dbg32'], writes=['d_h'])
                S.barrier()

        with contextlib.ExitStack() as ph:
            ub = sb(ph, "ub", [128, UW], F32)
            uc = sb(ph, "uc", [128, UCW], F32)
            ucb = sb(ph, "ucb", [128, UCW], BF16)
            gy = sb(ph, "gy", [128, NT], BF16)
            wyu = [sb(ph, f"wyu{i}", [128, 8, 256], BF16) for i in range(2)]
            gw = [sb(ph, f"gw{i}", [128, 4, 128], BF16) for i in range(2)]
            convw = sb(ph, "convw", [128, 8, 5], F32)
            convb = sb(ph, "convb", [128, 8], F32)
            gab = sb(ph, "gab", [128, 2, 8], F32)
            gxb = sb(ph, "gxb", [128, 2, 8], F32)
            lamt = sb(ph, "lamt", [128, 2, 8], F32)
            cneg = sb(ph, "cneg", [128, 2, 8], F32)
            cneg2 = sb(ph, "cneg2", [128, 2, 8], F32)
            rbuf = [sb(ph, f"rbuf{i}", [128, 512], F32) for i in range(2)]
            ibuf = [sb(ph, f"ibuf{i}", [128, 512], F32) for i in range(2)]
            sbuf_ = [sb(ph, f"sbuf{i}", [128, 512], F32) for i in range(2)]
            hbt = [sb(ph, f"hbt{i}", [128, 512], F32) for i in range(2)]
            gt1 = [sb(ph, f"gt1{i}", [128, 512], F32) for i in range(2)]
            zt = [sb(ph, f"zt{i}", [128, 512], BF16) for i in range(2)]
            for t, src, k in [(convw, convw_in, 'convw'), (convb, convb_in, 'convb'), (gab, gab_in, 'gab'),
                              (gxb, gxb_in, 'gxb'), (lamt, lam_in, 'lamt')]:
                S.dma('sp', t[:], src, writes=[k])
            S.op('act', lambda e: e.activation(out=cneg[:], in_=lamt[:], func=AF.Exp, scale=-1.0),
                 reads=['lamt'], writes=['cneg'])
            S.op('act', lambda e: e.activation(out=cneg[:], in_=cneg[:], func=AF.Ln, bias=one_t[:, 0:1], scale=1.0),
                 reads=['cneg', 'one'], writes=['cneg'])
            S.op('dve', lambda e: e.tensor_scalar(out=cneg2[:], in0=cneg[:], scalar1=-16.0, scalar2=None, op0=ALU.mult),
                 reads=['cneg'], writes=['cneg2'])
            S.op('dve', lambda e: e.tensor_scalar(out=cneg[:], in0=cneg[:], scalar1=-8.0, scalar2=None, op0=ALU.mult),
                 reads=['cneg', 'cneg2'], writes=['cneg'])
            zer = sb(ph, "zer", [128, 4, 1024], BF16)
            S.op('pool', lambda e: e.memset(zer[:], 0.0), writes=['zer'])
            ngab = sb(ph, "ngab", [128, 2, 8], F32)
            ngxb = sb(ph, "ngxb", [128, 2, 8], F32)
            S.op('dve', lambda e: e.tensor_scalar(out=ngab[:], in0=gab[:], scalar1=-1.0, scalar2=None, op0=ALU.mult),
                 reads=['gab'], writes=['ngab'])
            S.op('dve', lambda e: e.tensor_scalar(out=ngxb[:], in0=gxb[:], scalar1=-1.0, scalar2=None, op0=ALU.mult),
                 reads=['gxb'], writes=['ngxb'])
            S.op('dve', lambda e: e.memset(ub[:], 0.0), writes=[('ub', ti) for ti in range(9)])
            ubkeys = [('ub', ti) for ti in range(9)]
            cnt_sc = 0
            for n in range(8):
                w = wyu[n % 2]
                kw_ = ('wyu', n % 2)
                S.dma('pool', w[:, :, 0:128], w_in[:, n * 128:(n + 1) * 128].rearrange("(k p) f -> p k f", p=128),
                      writes=[kw_])
                S.dma('pool', w[:, :, 128:256],
                      w_in[:, 1024 + n * 128:1024 + (n + 1) * 128].rearrange("(k p) f -> p k f", p=128), writes=[kw_])
                g = gw[n % 2]
                kg = ('gw', n % 2)
                for d in range(2):
                    S.dma('pool', g[:, 2 * d, :], gaw[d, n], writes=[kg])
                    S.dma('pool', g[:, 2 * d + 1, :], gxw[d, n], writes=[kg])
                convert_experts(0, 4 * n, 4 * n + 4)
                for b4 in range(4 * n, min(4 * n + 4, 25)):
                    S.dma('pool', Xs[b4 * 512:(b4 + 1) * 512, :].rearrange("(a p) f -> p a f", p=128), zer[:],
                          reads=['zer'], writes=['Xs'])
                for ti, (off, cnt, uoff, ucoff, r) in enumerate(TILES):
                    pu, py = (ti % 2) * 2, (ti % 2) * 2 + 1

                    def mmu(e, c0=128, p=pu):
                        for kc in range(8):
                            ins = e.matmul(PS[p][:, :cnt], lhsT=w[:, kc, c0:c0 + 128], rhs=hbuf[:, kc, off:off + cnt],
                                           start=(kc == 0), stop=(kc == 7))
                        return ins
                    S.op('pe', mmu, reads=[kw_, ('h', ti)], writes=[pk(pu)])
                    S.op('act', lambda e: e.activation(out=ub[:, uoff:uoff + cnt], in_=PS[pu][:, :cnt], func=AF.Identity),
                         reads=[pk(pu)], writes=[('ub', ti)])
                    S.op('pe', lambda e: mmu(e, 0, py), reads=[kw_, ('h', ti)], writes=[pk(py)])
                    t1 = gt1[ti % 2]
                    k1 = ('gt1', ti % 2)
                    S.op('act', lambda e: e.activation(out=t1[:, :cnt], in_=PS[py][:, :cnt], func=AF.Square),
                         reads=[pk(py)], writes=[k1])
                    S.op('dve', lambda e: e.tensor_scalar(out=t1[:, :cnt], in0=t1[:, :cnt], scalar1=0.044715, scalar2=1.0,
                                                          op0=ALU.mult, op1=ALU.add), reads=[k1], writes=[k1])
                    S.op('dve', lambda e: e.tensor_tensor(out=t1[:, :cnt], in0=t1[:, :cnt], in1=PS[py][:, :cnt], op=ALU.mult),
                         reads=[k1, pk(py)], writes=[k1])
                    S.op('act', lambda e: e.activation(out=t1[:, :cnt], in_=t1[:, :cnt], func=AF.Sigmoid,
                                                       scale=1.5957691216057308), reads=[k1], writes=[k1])
                    S.op('dve', lambda e: e.tensor_tensor(out=gy[:, off:off + cnt], in0=t1[:, :cnt], in1=PS[py][:, :cnt],
                                                          op=ALU.mult), reads=[k1, pk(py)], writes=[('gy', ti)])
                S.op('dve', lambda e: e.tensor_scalar(out=uc[:], in0=ub[:, 0:UCW], scalar1=convw[:, n, 0:1],
                                                      scalar2=convb[:, n:n + 1], op0=ALU.mult, op1=ALU.add),
                     reads=ubkeys + ['convw', 'convb'], writes=['uc'])
                for k in range(1, 5):
                    S.op('dve', lambda e: e.scalar_tensor_tensor(out=uc[:], in0=ub[:, k:k + UCW], scalar=convw[:, n, k:k + 1],
                                                                 in1=uc[:], op0=ALU.mult, op1=ALU.add),
                         reads=ubkeys + ['convw', 'uc'], writes=['uc'])
                S.op('act', lambda e: e.activation(out=ucb[:], in_=uc[:], func=AF.Identity), reads=['uc'], writes=['ucb'])
                for d in range(2):
                    order = [8] + (list(range(8)) if d == 0 else list(range(7, -1, -1)))
                    prev = None
                    for oi, ti in enumerate(order):
                        off, cnt, uoff, ucoff, r = TILES[ti]
                        b = cnt_sc % 2
                        cnt_sc += 1
                        pr, pi = 4 + 2 * b, 5 + 2 * b
                        S.op('pe', lambda e: e.matmul(PS[pr][:, :cnt], lhsT=g[:, 2 * d, :], rhs=ucb[:, ucoff:ucoff + cnt],
                                                      start=True, stop=True), reads=[kg, 'ucb'], writes=[pk(pr)])
                        S.op('pe', lambda e: e.matmul(PS[pi][:, :cnt], lhsT=g[:, 2 * d + 1, :], rhs=ucb[:, ucoff:ucoff + cnt],
                                                      start=True, stop=True), reads=[kg, 'ucb'], writes=[pk(pi)])
                        rb_, ib_, sb_, hb_ = rbuf[b], ibuf[b], sbuf_[b], hbt[b]
                        kr, ki, ks, kh = ('rbuf', b), ('ibuf', b), ('sbuf', b), ('hbt', b)
                        S.op('act', lambda e: e.activation(out=rb_[:, :cnt], in_=PS[pr][:, :cnt], func=AF.Exp,
                                                           bias=ngab[:, d, n:n + 1], scale=-1.0),
                             reads=[pk(pr), 'ngab'], writes=[kr])
                        S.op('act', lambda e: e.activation(out=ib_[:, :cnt], in_=PS[pi][:, :cnt], func=AF.Exp,
                                                           bias=ngxb[:, d, n:n + 1], scale=-1.0),
                             reads=[pk(pi), 'ngxb'], writes=[ki])
                        for t_, k_ in ((rb_, kr), (ib_, ki)):
                            S.op('act', lambda e: e.activation(out=t_[:, :cnt], in_=t_[:, :cnt], func=AF.Ln,
                                                               bias=one_t[:, 0:1], scale=1.0), reads=[k_, 'one'], writes=[k_])
                            S.op('act', lambda e: e.activation(out=t_[:, :cnt], in_=t_[:, :cnt], func=AF.Exp, scale=-1.0),
                                 reads=[k_], writes=[k_])
                        S.op('act', lambda e: e.activation(out=sb_[:, :cnt], in_=rb_[:, :cnt], func=AF.Exp,
                                                           scale=cneg2[:, d, n:n + 1]), reads=[kr, 'cneg2'], writes=[ks])
                        S.op('act', lambda e: e.activation(out=rb_[:, :cnt], in_=rb_[:, :cnt], func=AF.Exp,
                                                           scale=cneg[:, d, n:n + 1]), reads=[kr, 'cneg'], writes=[kr])
                        S.op('act', lambda e: e.activation(out=sb_[:, :cnt], in_=sb_[:, :cnt], func=AF.Ln,
                                                           bias=one_t[:, 0:1], scale=-1.0), reads=[ks, 'one'], writes=[ks])
                        S.op('act', lambda e: e.activation(out=sb_[:, :cnt], in_=sb_[:, :cnt], func=AF.Exp, scale=0.5),
                             reads=[ks], writes=[ks])
                        S.op('dve', lambda e: e.tensor_tensor(out=ib_[:, :cnt], in0=ib_[:, :cnt],
                                                              in1=uc[:, ucoff:ucoff + cnt], op=ALU.mult),
                             reads=[ki, 'uc'], writes=[ki])
                        S.op('dve', lambda e: e.tensor_tensor(out=ib_[:, :cnt], in0=ib_[:, :cnt], in1=sb_[:, :cnt],
                                                              op=ALU.mult), reads=[ki, ks], writes=[ki])
                        if d == 0:
                            if prev is None:
                                init, kin = 0.0, []
                            else:
                                po, pc, puo, _, _ = TILES[prev]
                                init, kin = ub[:, puo + pc - 1:puo + pc], [('ub', prev)]
                            S.op('dve', lambda e: e.tensor_tensor_scan(out=ub[:, uoff:uoff + cnt], data0=rb_[:, :cnt],
                                                                       data1=ib_[:, :cnt], initial=init,
                                                                       op0=ALU.mult, op1=ALU.add),
                                 reads=[kr, ki] + kin, writes=[('ub', ti)])
                        else:
                            if prev is None:
                                init, kin = 0.0, []
                            else:
                                init, kin = hbt[1 - b][:, 0:1], [('hbt', 1 - b)]
                            S.op('dve', lambda e: e.tensor_tensor_scan(out=hb_[:, :cnt][:, ::-1], data0=rb_[:, :cnt][:, ::-1],
                                                                       data1=ib_[:, :cnt][:, ::-1], initial=init,
                                                                       op0=ALU.mult, op1=ALU.add),
                                 reads=[kr, ki] + kin, writes=[kh])
                            S.op('dve', lambda e: e.tensor_tensor(out=sb_[:, :cnt], in0=hb_[:, :cnt],
                                                                  in1=ub[:, uoff:uoff + cnt], op=ALU.add),
                                 reads=[kh, ('ub', ti)], writes=[ks])
                            z_ = zt[b]
                            S.op('dve', lambda e: e.tensor_tensor(out=z_[:, :cnt], in0=sb_[:, :cnt],
                                                                  in1=gy[:, off:off + cnt], op=ALU.mult),
                                 reads=[ks, ('gy', ti)], writes=[('zt', b)])
                            S.dma('sp', zscr[n, :, off:off + cnt], z_[:, :cnt], reads=[('zt', b)], writes=[('zs', ti)])
                        prev = ti
            S.barrier()
        hst.close()

        I32 = mybir.dt.int32
        MAXSUB = 34
        NBMAX = 100
        BIGW = 2 * 32 * 256 + 64
        utri = sb(st, "utri", [128, 128], F32)
        S.op('pool', lambda e: e.memset(utri[:], 1.0), writes=['utri'])
        S.op('pool', lambda e: e.affine_select(out=utri[:], in_=utri[:], pattern=[[1, 128]],
                                               compare_op=ALU.is_gt, fill=0.0, base=0, channel_multiplier=-1),
             reads=['utri'], writes=['utri'])
        pcol = sb(st, "pcol", [128, 1], F32)
        blkoff = sb(st, "blkoff", [128, NBMAX], F32)
        ones_row = sb(st, "ones_row", [128, 32], F32)
        S.dma('sp', pcol[:], pcol_in, writes=['pcol'])
        S.dma('sp', blkoff[:], blkoff_in, writes=['blkoff'])
        S.op('dve', lambda e: e.memset(ones_row[:], 1.0), writes=['ones_row'])
        onesb = sb(st, "onesb", [128, 4, 32], F32)
        c12 = sb(st, "c12", [128, 2, 4, 32], F32)
        S.op('dve', lambda e: e.memset(onesb[:], 1.0), writes=['onesb'])
        for k2_ in range(2):
            for s_ in range(4):
                S.op('dve', lambda e: e.memset(c12[:, k2_, s_, :], float(2 * s_ + 1 + k2_)), writes=['c12'])
        FM = sb(st, "FM", [128, MAXSUB, 2, 32], F32)
        RK = sb(st, "RK", [128, MAXSUB, 2], F32)
        WK = sb(st, "WK", [128, MAXSUB, 2], F32)
        DI = sb(st, "DI", [128, MAXSUB, 2], I32)
        WI = sb(st, "WI", [128, NBMAX, 2], I32)
        cntm = sb(st, "cntm", [128, 32], F32)

        bregs = {}

        def idma(out, out_off, in_, in_off, bounds, reads, writes, slot):
            S._wait('pool', S._deps('pool', reads, writes, skip_same_pe=False))
            if slot not in S.dsem:
                S.dsem[slot] = [S._newsem('d'), 0]
            d = S.dsem[slot]
            if bounds not in bregs:
                bregs[bounds] = nc.gpsimd.to_reg(bounds)
            ins = nc.gpsimd.indirect_dma_start(out=out, out_offset=out_off, in_=in_, in_offset=in_off,
                                               bounds_check=bregs[bounds], oob_is_err=False)
            d[1] += 16
            ins.then_inc(d[0], 16)
            S._commit((d[0], d[1]), reads, writes)

        WST = {}
        wgv = wgb.rearrange("(a b) f -> a (b f)", b=2)
        wuv = wub.rearrange("(a b) f -> a (b f)", b=2)
        wdv = wdb.rearrange("(a b) f -> a (b f)", b=2)

        def moe_blk(NB, p_):
            return (p_ % 4) * (NB // 4) + p_ // 4

        def moe_loadw(l, NB, p_):
            _, wgs, wus, wds = WST[l]
            w_ = p_ % 4
            for nm, view, tile_ in [('wgs', wgv, wgs[w_]), ('wus', wuv, wus[w_]), ('wds', wdv, wds[w_])]:
                idma(tile_[:, :], None, view[:, :], bass.IndirectOffsetOnAxis(ap=WI[:, moe_blk(NB, p_), 0:1], axis=0),
                     (l + 1) * 4096 - 1, reads=['WI', ('wcv', l)], writes=[(nm, w_)], slot=(nm, w_))

        def phase_C(l, wmat, tiles, xsrc):
            nsub_tot = sum(TILES[ti][1] // 128 for ti in tiles)
            NB = 2 * nsub_tot + 32
            with contextlib.ExitStack() as ph:
                wsb = sb(ph, "wsb", [128, 8, 1024], BF16)
                zts = [sb(ph, f"zts{i}", [128, 8, 512], BF16) for i in range(2)]
                xts = [sb(ph, f"xtc{i}", [128, 8, 512], F32) for i in range(2)]
                h2fs = [sb(ph, f"h2f{i}", [128, 8, 512], F32) for i in range(2)]
                h2t = [sb(ph, f"h2t{i}", [128, 1024], BF16) for i in range(2)]
                tmps = [norm_tmp(ph), norm_tmp(ph)]
                mk3 = lambda nm: [sb(ph, f"{nm}{i}", [128, 4, 32], F32) for i in range(2)]
                ssel, sg, em, mk, cmv, rk, t3 = mk3("ssel"), mk3("sg"), mk3("em"), mk3("mk"), mk3("cmv"), mk3("rk"), mk3("t3")
                mk16 = lambda nm: [sb(ph, f"{nm}{i}", [128, 16], F32) for i in range(2)]
                m1, m2, gs, gm_ = mk16("m1"), mk16("m2"), mk16("gs"), mk16("gmk")
                sm = [sb(ph, f"sm{i}", [128, 4, 4], F32) for i in range(2)]
                t32 = [sb(ph, f"t32{i}", [128, 32], F32) for i in range(2)]
                S.dma('pool', wsb[:], wmat.rearrange("(k p) f -> p k f", p=128), writes=['wsb'])
                S.op('dve', lambda e: e.memset(cntm[:], 0.0), writes=['cntm'])
                nsub_box = [0]

                def stage_W(ti):
                    off, cnt, _, _, r = TILES[ti]
                    b = ti % 2
                    z_, x_ = zts[b], xts[b]
                    h2f, tmp, kh2 = h2fs[b], tmps[b], ('h2f', b)
                    kz, kx = ('zts', b), ('xtc', b)
                    S.dma('sp', z_[:, :, :cnt], zscr[:, :, off:off + cnt].rearrange("j p t -> p j t"),
                          reads=[('zs', ti)], writes=[kz])
                    S.dma('sp', x_[:, :, :cnt], xsrc[:, :, off:off + cnt].rearrange("j p t -> p j t"),
                          reads=[('xr', ti)], writes=[kx])
                    for j in range(8):
                        p = j % 3

                        def mm(e):
                            for kc in range(8):
                                ins = e.matmul(PS[p][:, :cnt], lhsT=wsb[:, kc, j * 128:(j + 1) * 128], rhs=z_[:, kc, :cnt],
                                               start=(kc == 0), stop=(kc == 7))
                            return ins
                        S.op('pe', mm, reads=['wsb', kz], writes=[pk(p)])
                        S.op('dve', lambda e: e.scalar_tensor_tensor(out=x_[:, j, :cnt], in0=PS[p][:, :cnt],
                                                                     scalar=mod_ap(l, 2, j, r), in1=x_[:, j, :cnt],
                                                                     op0=ALU.mult, op1=ALU.add),
                             reads=[pk(p), kx, 'modT'], writes=[kx])
                    S.dma('sp', xr[:, :, off:off + cnt].rearrange("j p t -> p j t"), x_[:, :, :cnt],
                          reads=[kx], writes=[('xr', ti)])
                    if dbg and l == 0:
                        S.dma('sp', dbgo["d_x1"][:, :, off:off + cnt].rearrange("j p t -> p j t"), x_[:, :, :cnt],
                              reads=[kx], writes=['d_x1'])

                def stage_N(ti):
                    off, cnt, _, _, r = TILES[ti]
                    b = ti % 2
                    z_, x_ = zts[b], xts[b]
                    h2f, tmp, kh2 = h2fs[b], tmps[b], ('h2f', b)
                    kz, kx = ('zts', b), ('xtc', b)
                    norm_mod(tmp, x_, cnt, kx, lambda j: h2f[:, j, :cnt], kh2,
                             lambda j: gmT[:, l, 1, j, r:r + 1], lambda j: mod_ap(l, 3, j, r), 3,
                             extra_reads=['gmT', 'modT'], tag=str(b))

                def stage_R(ti):
                    off, cnt, _, _, r = TILES[ti]
                    b = ti % 2
                    z_, x_ = zts[b], xts[b]
                    h2f, tmp, kh2 = h2fs[b], tmps[b], ('h2f', b)
                    kz, kx = ('zts', b), ('xtc', b)
                    nsb = cnt // 128
                    gs0 = nsub_box[0]
                    nsub_box[0] += nsb
                    q = ti % 2
                    pl = 4 + q
                    W_ = nsb * 32
                    for s in range(nsb):
                        gsi = gs0 + s
                        qq = gsi % 2
                        for half in range(2):
                            pt = 6 + half

                            def mmt(e):
                                for jj in range(4):
                                    j = half * 4 + jj
                                    ins = e.transpose(out=PS[pt][:, jj * 128:(jj + 1) * 128],
                                                      in_=h2f[:, j, s * 128:(s + 1) * 128], identity=ident[:])
                                return ins
                            S.op('pe', mmt, reads=[kh2, 'ident'], writes=[pk(pt)])
                            if half == 0:
                                S.op('act', lambda e: e.activation(out=h2t[qq][:, 0:512], in_=PS[pt][:, :], func=AF.Identity),
                                     reads=[pk(pt)], writes=[('h2t', qq)])
                            else:
                                S.op('pool' if False else 'dve', lambda e: e.tensor_copy(out=h2t[qq][:, 512:1024], in_=PS[pt][:, :]),
                                     reads=[pk(pt)], writes=[('h2t', qq)])
                        S.dma('sp', h2tm[gsi * 128:(gsi + 1) * 128, :], h2t[qq][:], reads=[('h2t', qq)], writes=[('h2tm', gsi % 4)])

                        def mmr(e):
                            for kc in range(8):
                                ins = e.matmul(PS[pl][:, s * 32:(s + 1) * 32], lhsT=h2f[:, kc, s * 128:(s + 1) * 128], rhs=rw[:, kc, :],
                                               start=(kc == 0), stop=(kc == 7))
                            return ins
                        S.op('pe', mmr, reads=[kh2, 'rw'], writes=[pk(pl)])
                    kq = ('rt', q)
                    v3 = lambda t: t[:, :nsb, :]
                    v8 = lambda t: t[:, :nsb, :].rearrange("p s (g e) -> p (s g) e", e=8)
                    f2 = lambda t: t[:, :nsb, :].rearrange("p s e -> p (s e)")
                    g4 = lambda t: t[:, :nsb * 4]
                    g43 = lambda t: t[:, :nsb * 4].rearrange("p (s g) -> p s g", g=4)
                    S.op('act', lambda e: e.activation(out=f2(sg[q]), in_=PS[pl][:, :W_], func=AF.Sigmoid),
                         reads=[pk(pl)], writes=[kq])
                    S.op('dve', lambda e: e.tensor_tensor(out=v3(ssel[q]), in0=v3(sg[q]), in1=bc_mid(rb[:], nsb), op=ALU.add),
                         reads=[kq, 'rb'], writes=[kq])
                    S.op('dve', lambda e: e.tensor_reduce(out=g4(m1[q]), in_=v8(ssel[q]), axis=AX.X, op=ALU.max), reads=[kq], writes=[kq])
                    S.op('dve', lambda e: e.tensor_tensor(out=v8(t3[q]), in0=v8(ssel[q]), in1=bc_last(g4(m1[q]), 8), op=ALU.is_equal),
                         reads=[kq], writes=[kq])
                    S.op('dve', lambda e: e.scalar_tensor_tensor(out=f2(t3[q]), in0=f2(t3[q]), scalar=-1.0e9, in1=f2(ssel[q]),
                                                                 op0=ALU.mult, op1=ALU.add), reads=[kq], writes=[kq])
                    S.op('dve', lambda e: e.tensor_reduce(out=g4(m2[q]), in_=v8(t3[q]), axis=AX.X, op=ALU.max), reads=[kq], writes=[kq])
                    S.op('dve', lambda e: e.tensor_tensor(out=g4(gs[q]), in0=g4(m1[q]), in1=g4(m2[q]), op=ALU.add), reads=[kq], writes=[kq])
                    S.op('dve', lambda e: e.tensor_reduce(out=sm[q][:, 0, :nsb], in_=g43(gs[q]), axis=AX.X, op=ALU.max),
                         reads=[kq], writes=[kq])
                    S.op('dve', lambda e: e.tensor_tensor(out=g43(gm_[q]), in0=g43(gs[q]), in1=bc_last(sm[q][:, 0, :nsb], 4),
                                                          op=ALU.is_equal), reads=[kq], writes=[kq])
                    S.op('dve', lambda e: e.tensor_tensor(out=g4(gs[q]), in0=g4(gm_[q]), in1=g4(m2[q]), op=ALU.mult), reads=[kq], writes=[kq])
                    S.op('dve', lambda e: e.tensor_reduce(out=sm[q][:, 1, :nsb], in_=g43(gs[q]), axis=AX.X, op=ALU.add),
                         reads=[kq], writes=[kq])
                    S.op('dve', lambda e: e.tensor_tensor(out=v3(mk[q]), in0=v3(ssel[q]), in1=bc_last(sm[q][:, 1, :nsb], 32),
                                                          op=ALU.is_ge), reads=[kq], writes=[kq])
                    S.op('dve', lambda e: e.tensor_tensor(out=v8(mk[q]), in0=v8(mk[q]), in1=bc_last(g4(gm_[q]), 8), op=ALU.mult),
                         reads=[kq], writes=[kq])
                    S.op('dve', lambda e: e.tensor_tensor(out=f2(em[q]), in0=f2(mk[q]), in1=f2(sg[q]), op=ALU.mult),
                         reads=[kq], writes=[kq])
                    S.op('dve', lambda e: e.tensor_reduce(out=sm[q][:, 2, :nsb], in_=v3(em[q]), axis=AX.X, op=ALU.add),
                         reads=[kq], writes=[kq])
                    S.op('dve', lambda e: e.reciprocal(out=sm[q][:, 3, :nsb], in_=sm[q][:, 2, :nsb]), reads=[kq], writes=[kq])
                    S.op('dve', lambda e: e.tensor_tensor(out=v3(em[q]), in0=v3(em[q]), in1=bc_last(sm[q][:, 3, :nsb], 32), op=ALU.mult),
                         reads=[kq], writes=[kq])

                    def mmk(e):
                        for s in range(nsb):
                            o_ = PS[pl][:, 128 + s * 32:128 + (s + 1) * 32]
                            e.matmul(o_, lhsT=utri[:], rhs=mk[q][:, s, :], start=True, stop=False)
                            for s2 in range(s):
                                e.matmul(o_, lhsT=ones32[:], rhs=mk[q][:, s2, :], start=False, stop=False)
                            ins = e.matmul(o_, lhsT=ones32[:], rhs=cntm[:], start=False, stop=True)
                        return ins
                    S.op('pe', mmk, reads=[kq, 'utri', 'ones32', 'cntm'], writes=[pk(pl)])
                    S.op('dve', lambda e: e.tensor_copy(out=f2(rk[q]), in_=PS[pl][:, 128:128 + W_]), reads=[pk(pl)], writes=[kq])
                    S.op('dve', lambda e: e.tensor_reduce(out=t32[q][:], in_=mk[q][:, :nsb, :].rearrange("p s e -> p e s"), axis=AX.X,
                                                          op=ALU.add), reads=[kq], writes=[kq])
                    S.op('dve', lambda e: e.tensor_tensor(out=cntm[:], in0=cntm[:], in1=t32[q][:], op=ALU.add),
                         reads=[kq, 'cntm'], writes=['cntm'])
                    S.op('dve', lambda e: e.tensor_tensor_scan(out=f2(cmv[q]), data0=f2(onesb), data1=f2(mk[q]), initial=0.0,
                                                               op0=ALU.mult, op1=ALU.add), reads=[kq, 'onesb'], writes=[kq])
                    for k2 in range(2):
                        fm_ = FM[:, gs0:gs0 + nsb, k2, :]
                        S.op('dve', lambda e: e.tensor_tensor(out=v3(t3[q]), in0=v3(cmv[q]), in1=c12[:, k2, :nsb, :], op=ALU.is_equal),
                             reads=[kq, 'c12'], writes=[kq])
                        S.op('dve', lambda e: e.tensor_tensor(out=fm_, in0=v3(t3[q]), in1=v3(mk[q]), op=ALU.mult),
                             reads=[kq], writes=['FM'])
                        S.op('dve', lambda e: e.tensor_tensor(out=v3(t3[q]), in0=fm_, in1=v3(rk[q]), op=ALU.mult),
                             reads=[kq, 'FM'], writes=[kq])
                        S.op('dve', lambda e: e.tensor_reduce(out=RK[:, gs0:gs0 + nsb, k2], in_=v3(t3[q]), axis=AX.X, op=ALU.add),
                             reads=[kq], writes=['RK'])
                        S.op('dve', lambda e: e.tensor_tensor(out=v3(t3[q]), in0=fm_, in1=v3(em[q]), op=ALU.mult),
                             reads=[kq, 'FM'], writes=[kq])
                        S.op('dve', lambda e: e.tensor_reduce(out=WK[:, gs0:gs0 + nsb, k2], in_=v3(t3[q]), axis=AX.X, op=ALU.add),
                             reads=[kq], writes=['WK'])

                stage_W(tiles[0])
                for i_, ti in enumerate(tiles):
                    stage_N(ti)
                    if i_ + 1 < len(tiles):
                        stage_W(tiles[i_ + 1])
                    stage_R(ti)
                S.barrier()
            wst = contextlib.ExitStack()
            WST[l] = (wst,
                      [sb(wst, f"wgs{i}", [128, 4096], BF16) for i in range(4)],
                      [sb(wst, f"wus{i}", [128, 4096], BF16) for i in range(4)],
                      [sb(wst, f"wds{i}", [128, 4096], BF16) for i in range(4)])
            with contextlib.ExitStack() as ph:
                J = nsub_tot
                cb = sb(ph, "cb", [128, 32], F32)
                nblk = sb(ph, "nblk", [128, 32], F32)
                pend = sb(ph, "pend", [128, 32], F32)
                pst = sb(ph, "pst", [128, 32], F32)
                big = sb(ph, "bigc", [128, 32, NBMAX], F32)
                eb = sb(ph, "eb", [128, NBMAX], F32)
                chg = sb(ph, "chg", [128, NBMAX], F32)
                wif = sb(ph, "wif", [128, NBMAX, 2], F32)
                dtmp2 = sb(ph, "dtmp2", [128, MAXSUB, 32], F32)
                dif = sb(ph, "dif", [128, MAXSUB, 2], F32)
                rows = [sb(ph, f"rows{i}", [128, 1024], BF16) for i in range(4)]
                S.op('pe', lambda e: e.matmul(PS[0][:, 0:32], lhsT=ones32[:], rhs=cntm[:], start=True, stop=True),
                     reads=['ones32', 'cntm'], writes=[pk(0)])
                S.op('dve', lambda e: e.tensor_copy(out=cb[:], in_=PS[0][:, 0:32]), reads=[pk(0)], writes=['cb'])
                S.op('dve', lambda e: e.tensor_tensor(out=big[:, :, :J], in0=bc_last(cb[:], J), in1=bc_mid(blkoff[:, :J], 32),
                                                      op=ALU.is_gt), reads=['cb', 'blkoff'], writes=['big'])
                S.op('dve', lambda e: e.tensor_reduce(out=nblk[:], in_=big[:, :, :J], axis=AX.X, op=ALU.add),
                     reads=['big'], writes=['nblk'])
                S.op('dve', lambda e: e.tensor_tensor_scan(out=pend[:], data0=ones_row[:], data1=nblk[:], initial=0.0,
                                                           op0=ALU.mult, op1=ALU.add), reads=['nblk', 'ones_row'], writes=['pend'])
                S.op('dve', lambda e: e.tensor_tensor(out=pst[:], in0=pend[:], in1=nblk[:], op=ALU.subtract),
                     reads=['pend', 'nblk'], writes=['pst'])
                S.op('dve', lambda e: e.tensor_scalar(out=pst[:], in0=pst[:], scalar1=128.0, scalar2=None, op0=ALU.mult),
                     reads=['pst'], writes=['pst'])
                S.op('dve', lambda e: e.tensor_scalar(out=pend[:], in0=pend[:], scalar1=128.0, scalar2=None, op0=ALU.mult),
                     reads=['pend'], writes=['pend'])
                S.op('dve', lambda e: e.tensor_tensor(out=big[:, :, :NB].rearrange("p e b -> p b e"),
                                                      in0=bc_mid(pend[:], NB), in1=bc_last(blkoff[:, :NB], 32),
                                                      op=ALU.is_le), reads=['pend', 'blkoff', 'big'], writes=['big'])
                S.op('dve', lambda e: e.tensor_reduce(out=eb[:, :NB], in_=big[:, :, :NB].rearrange("p e b -> p b e"),
                                                      axis=AX.X, op=ALU.add), reads=['big'], writes=['eb'])
                S.op('dve', lambda e: e.tensor_scalar(out=eb[:, :NB], in0=eb[:, :NB], scalar1=31.0, scalar2=None, op0=ALU.min),
                     reads=['eb'], writes=['eb'])
                S.op('dve', lambda e: e.memset(chg[:], 1.0), writes=['chg'])
                S.op('dve', lambda e: e.tensor_tensor(out=chg[:, 1:NB], in0=eb[:, 1:NB], in1=eb[:, 0:NB - 1], op=ALU.not_equal),
                     reads=['eb', 'chg'], writes=['chg'])
                for k4 in range(1, 4):
                    S.op('dve', lambda e: e.memset(chg[:, k4 * (NB // 4):k4 * (NB // 4) + 1], 1.0), reads=['chg'], writes=['chg'])
                for h in range(1):
                    S.op('dve', lambda e: e.tensor_scalar(out=wif[:, :NB, h], in0=eb[:, :NB], scalar1=128.0,
                                                          scalar2=float(l * 4096 - BIGW), op0=ALU.mult, op1=ALU.add),
                         reads=['eb', 'wif'], writes=['wif'])
                    S.op('dve', lambda e: e.tensor_scalar(out=wif[:, :NB, h], in0=wif[:, :NB, h], scalar1=pcol[:, 0:1],
                                                          scalar2=None, op0=ALU.add), reads=['wif', 'pcol'], writes=['wif'])
                    S.op('dve', lambda e: e.tensor_tensor(out=wif[:, :NB, h], in0=wif[:, :NB, h], in1=chg[:, :NB], op=ALU.mult),
                         reads=['wif', 'chg'], writes=['wif'])
                    S.op('dve', lambda e: e.tensor_scalar(out=wif[:, :NB, h], in0=wif[:, :NB, h], scalar1=float(BIGW),
                                                          scalar2=None, op0=ALU.add), reads=['wif'], writes=['wif'])
                S.op('dve', lambda e: e.tensor_copy(out=WI[:, :NB, 0:1], in_=wif[:, :NB, 0:1]), reads=['wif'], writes=['WI'])
                for k2 in range(2):
                    S.op('dve', lambda e: e.tensor_tensor(out=dtmp2[:, :J, :], in0=FM[:, :J, k2, :], in1=bc_mid(pst[:], J),
                                                          op=ALU.mult), reads=['FM', 'pst', 'dtmp2'], writes=['dtmp2'])
                    S.op('dve', lambda e: e.tensor_reduce(out=dif[:, :J, k2], in_=dtmp2[:, :J, :], axis=AX.X, op=ALU.add),
                         reads=['dtmp2', 'dif'], writes=['dif'])
                S.op('dve', lambda e: e.tensor_tensor(out=dif[:, :J, :], in0=dif[:, :J, :], in1=RK[:, :J, :], op=ALU.add),
                     reads=['dif', 'RK'], writes=['dif'])
                S.op('dve', lambda e: e.tensor_copy(out=DI[:, :J, :], in_=dif[:, :J, :]), reads=['dif'], writes=['DI'])
                for p_ in range(3):
                    moe_loadw(l, NB, p_)
                for gsi in range(J):
                    q = gsi % 4
                    S.dma('sp', rows[q][:], h2tm[gsi * 128:(gsi + 1) * 128, :], reads=[('h2tm', gsi % 4)], writes=[('rows', q)])
                    for k2 in range(2):
                        idma(Xs[:, :], bass.IndirectOffsetOnAxis(ap=DI[:, gsi, k2:k2 + 1], axis=0), rows[q][:, :], None,
                             NB * 128 - 1, reads=[('rows', q), 'DI', 'Xs'], writes=[('Xsc', q)], slot=('Xsc', q))
                S.barrier()

        def phase_D(l, tiles, final):
            nsub_tot = sum(TILES[ti][1] // 128 for ti in tiles)
            NB = 2 * nsub_tot + 32
            with contextlib.ExitStack() as ph:
                _, wgs, wus, wds = WST[l]
                blk = lambda p_: moe_blk(NB, p_)
                xbs = [sb(ph, f"xbs{i}", [128, 1024], BF16) for i in range(2)]
                XTs = [sb(ph, f"XTs{i}", [128, 8, 128], BF16) for i in range(2)]
                s1s = [sb(ph, f"s1s{i}", [128, 512], F32) for i in range(2)]
                ATs = [sb(ph, f"ATs{i}", [128, 4, 128], BF16) for i in range(2)]
                Yts = [sb(ph, f"Yts{i}", [128, 1024], F32) for i in range(2)]

                loadw = lambda p_: moe_loadw(l, NB, p_)
                def xbload(p_):
                    bn = blk(p_)
                    S.dma('sp', xbs[p_ % 2][:], Xs[bn * 128:(bn + 1) * 128, :], reads=[('Xsc', 0), ('Xsc', 1), ('Xsc', 2), ('Xsc', 3), 'Xs'],
                          writes=[('xbs', p_ % 2)])

                def stage_T(p_):
                    q = p_ % 2
                    xb, XT = xbs[q], XTs[q]
                    ptb = PS[q].bitcast(BF16)

                    def mmt(e):
                        for c in range(8):
                            ins = e.transpose(out=ptb[:, c * 128:(c + 1) * 128], in_=xb[:, c:1024:8], identity=identb[:])
                        return ins
                    S.op('pe', mmt, reads=[('xbs', q), 'identb'], writes=[pk(q)])
                    S.op('act', lambda e: e.activation(out=XT[:].rearrange("p c s -> p (c s)"), in_=ptb[:, :], func=AF.Identity),
                         reads=[pk(q)], writes=[('XTs', q)])

                def stage_G(p_):
                    q = p_ % 2
                    ws_ = p_ % 4
                    XT, AT = XTs[q], ATs[q]
                    wg_, wu_ = wgs[ws_], wus[ws_]
                    p1, p2 = 2 + 2 * q, 3 + 2 * q

                    def mmg(e, wt, p):
                        for fo in range(4):
                            for c in range(8):
                                c0 = c * 512 + fo
                                ins = e.matmul(PS[p][:, fo * 128:(fo + 1) * 128], lhsT=wt[:, c0:c0 + 509:4], rhs=XT[:, c, :],
                                               start=(c == 0), stop=(c == 7))
                        return ins
                    S.op('pe', lambda e: mmg(e, wg_, p1), reads=[('wgs', ws_), ('XTs', q)], writes=[pk(p1)])
                    S.op('pe', lambda e: mmg(e, wu_, p2), reads=[('wus', ws_), ('XTs', q)], writes=[pk(p2)])
                    S.op('act', lambda e: e.activation(out=s1s[q][:], in_=PS[p1][:, :], func=AF.Silu),
                         reads=[pk(p1)], writes=[('s1s', q)])
                    S.op('dve', lambda e: e.tensor_tensor(out=AT[:].rearrange("p c s -> p (c s)"), in0=s1s[q][:], in1=PS[p2][:, :],
                                                          op=ALU.mult), reads=[('s1s', q), pk(p2)], writes=[('ATs', q)])

                def stage_D(p_):
                    q = p_ % 2
                    ws_ = p_ % 4
                    b = blk(p_)
                    AT, Yt, wd_ = ATs[q], Yts[q], wds[ws_]
                    for dh in range(2):
                        py = 6 + dh

                        def mmd(e):
                            for fo in range(4):
                                ins = e.matmul(PS[py][:, :], lhsT=AT[:, fo, :],
                                               rhs=wd_[:, fo * 1024 + dh * 512:fo * 1024 + (dh + 1) * 512],
                                               start=(fo == 0), stop=(fo == 3))
                            return ins
                        S.op('pe', mmd, reads=[('wds', ws_), ('ATs', q)], writes=[pk(py)])
                        if dh == 0:
                            S.op('act', lambda e: e.activation(out=Yt[:, 0:512], in_=PS[py][:, :], func=AF.Identity),
                                 reads=[pk(py)], writes=[('Yts', q)])
                        else:
                            S.op('dve', lambda e: e.tensor_copy(out=Yt[:, 512:1024], in_=PS[py][:, :]),
                                 reads=[pk(py)], writes=[('Yts', q)])
                    S.dma('sp', Ys[b * 128:(b + 1) * 128, :], Yt[:], reads=[('Yts', q)], writes=[('Ys', q)])

                xbload(0)
                xbload(1)
                stage_T(0)
                for pos in range(NB):
                    if pos + 3 < NB:
                        loadw(pos + 3)
                    stage_G(pos)
                    if pos + 1 < NB:
                        stage_T(pos + 1)
                    if pos + 2 < NB:
                        xbload(pos + 2)
                    stage_D(pos)
                S.barrier()
            WST[l][0].close()
            with contextlib.ExitStack() as ph3:
                xts = [sb(ph3, f"xtd{i}", [128, 8, 512], F32) for i in range(2)]
                g1 = [sb(ph3, f"g1{i}", [128, 1024], F32) for i in range(4)]
                g2 = [sb(ph3, f"g2{i}", [128, 1024], F32) for i in range(4)]
                tmp = norm_tmp(ph3) if final else None
                ots = [sb(ph3, f"otd{i}", [128, 8, 512], F32) for i in range(2)] if final else None
                subs = []
                for li, ti in enumerate(tiles):
                    for s_ in range(TILES[ti][1] // 128):
                        subs.append((li, ti, s_, len(subs)))

                def stage_a(li, ti, s, gsi):
                    off, cnt, _, _, r = TILES[ti]
                    b = li % 2
                    if s == 0:
                        S.dma('sp', xts[b][:, :, :cnt], xr[:, :, off:off + cnt].rearrange("j p t -> p j t"),
                              reads=[('xr', ti)], writes=[('xtd', b, j) for j in range(8)])
                    q = gsi % 4
                    idma(g1[q][:, :], None, Ys[:, :], bass.IndirectOffsetOnAxis(ap=DI[:, gsi, 0:1], axis=0),
                         NB * 128 - 1, reads=['DI', ('Ys', 0), ('Ys', 1)], writes=[('g1', q)], slot=('g1', q))
                    idma(g2[q][:, :], None, Ys[:, :], bass.IndirectOffsetOnAxis(ap=DI[:, gsi, 1:2], axis=0),
                         NB * 128 - 1, reads=['DI', ('Ys', 0), ('Ys', 1)], writes=[('g2', q)], slot=('g2', q))
                    S.op('dve', lambda e: e.tensor_scalar(out=g1[q][:], in0=g1[q][:], scalar1=WK[:, gsi, 0:1], scalar2=None,
                                                          op0=ALU.mult), reads=[('g1', q), 'WK'], writes=[('g1', q)])
                    S.op('dve', lambda e: e.scalar_tensor_tensor(out=g1[q][:], in0=g2[q][:], scalar=WK[:, gsi, 1:2],
                                                                 in1=g1[q][:], op0=ALU.mult, op1=ALU.add),
                         reads=[('g1', q), ('g2', q), 'WK'], writes=[('g1', q)])
                    for half in range(2):
                        pt = 2 * (q % 2) + half

                        def mmt2(e):
                            for jj in range(4):
                                j = half * 4 + jj
                                ins = e.transpose(out=PS[pt][:, jj * 128:(jj + 1) * 128], in_=g1[q][:, j * 128:(j + 1) * 128],
                                                  identity=ident[:])
                            return ins
                        S.op('pe', mmt2, reads=[('g1', q), 'ident'], writes=[pk(pt)])

                def stage_b(li, ti, s, gsi):
                    off, cnt, _, _, r = TILES[ti]
                    b = li % 2
                    x_ = xts[b]
                    q = gsi % 4
                    kxs = [('xtd', b, j) for j in range(8)]
                    for half in range(2):
                        pt = 2 * (q % 2) + half
                        for jj in range(4):
                            j = half * 4 + jj
                            S.op('dve', lambda e: e.scalar_tensor_tensor(out=x_[:, j, s * 128:(s + 1) * 128],
                                                                         in0=PS[pt][:, jj * 128:(jj + 1) * 128],
                                                                         scalar=mod_ap(l, 5, j, r),
                                                                         in1=x_[:, j, s * 128:(s + 1) * 128],
                                                                         op0=ALU.mult, op1=ALU.add),
                                 reads=[pk(pt), kxs[j], ('modT', l)], writes=[kxs[j]])
                    if s == cnt // 128 - 1:
                        if not final:
                            S.dma('sp', xr[:, :, off:off + cnt].rearrange("j p t -> p j t"), x_[:, :, :cnt],
                                  reads=kxs, writes=[('xr', ti)])
                            if dbg:
                                S.dma('sp', dbgo["d_x2"][:, :, off:off + cnt].rearrange("j p t -> p j t"), x_[:, :, :cnt],
                                      reads=kxs, writes=['d_x2'])
                        else:
                            o_ = ots[b]
                            norm_mod(tmp, x_, cnt, kxs, lambda j: o_[:, j, :cnt], ('otd', b),
                                     lambda j: fing[:, j:j + 1], None, 7, extra_reads=['fing'])
                            S.dma('sp', outT[:, :, off:off + cnt].rearrange("j p t -> p j t"), o_[:, :, :cnt],
                                  reads=[('otd', b)], writes=[('out', b)])

                for idx in range(len(subs) + 1):
                    if idx < len(subs):
                        stage_a(*subs[idx])
                    if idx >= 1:
                        stage_b(*subs[idx - 1])
                S.barrier()

        phase_C(0, w_out, list(range(9)), xc)
        phase_D(0, list(range(9)), final=False)

        hst = contextlib.ExitStack()
        hbuf = sb(hst, "hbuf1", [128, 8, NT], BF16)
        phase_A(1, xr, hbuf)
        with contextlib.ExitStack() as ph:
            cost = sb(ph, "cost", [128, NT], F32)
            sint = sb(ph, "sint", [128, NT], F32)
            dal = sb(ph, "dal", [128, 4, 64], F32)
            dtmp = sb(ph, "dtmp", [128, 64], F32)
            lamv = sb(ph, "lamv", [128, 4], F32)
            subg = sb(ph, "subg", [128, 1], F32)
            wq = sb(ph, "wq", [128, 8, 128], BF16)
            wk = sb(ph, "wk", [128, 8, 128], BF16)
            wv = sb(ph, "wv", [128, 8, 128], BF16)
            qbt = [sb(ph, f"qbt{i}", [128, 512], BF16) for i in range(2)]
            rmb = sb(ph, "rmb", [128, 128], BF16)
            QT = sb(ph, "QT", [128, NQ], BF16)
            vtb = [sb(ph, f"vtb{i}", [128, 512], BF16) for i in range(2)]
            KT = sb(ph, "KT", [128, NT], BF16)
            Vt = sb(ph, "Vt", [128, 34, 128], BF16)
            rt1 = [sb(ph, f"rt1{i}", [128, 512], F32) for i in range(2)]
            rt2 = [sb(ph, f"rt2{i}", [128, 512], F32) for i in range(2)]
            Eb2 = [sb(ph, f"Eb{i}", [128, 2, 512], BF16) for i in range(2)]
            acc2 = sb(ph, "acc2", [128, 2, 512], F32)
            obw = sb(ph, "obw", [128, 2, 512], F32)
            ob = [obw[:, 0, :], obw[:, 1, :]]
            rlw = sb(ph, "rlw", [128, 2, 512], F32)
            rl = sb(ph, "rl", [128, 512], F32)
            accD = sb(ph, "accD", [128, 512], F32)
            accP = sb(ph, "accP", [128, 512], F32)
            osq = sb(ph, "osq", [128, 512], F32)
            aot = [sb(ph, f"aot{i}", [128, 512], BF16) for i in range(2)]
            S.dma('sp', cost[:], cos_in, writes=['cos'])
            S.dma('pool', rmb[:], rmat_in, writes=['rmb'])
            S.dma('sp', sint[:], sin_in, writes=['sin'])
            S.dma('sp', dal[:], dalam_in, writes=['dal'])
            S.dma('sp', subg[:], subg_in, writes=['subg'])
            for i2 in range(2):
                S.op('dve', lambda e: e.tensor_tensor(out=dtmp[:], in0=dal[:, 2 * i2, :], in1=dal[:, 2 * i2 + 1, :], op=ALU.mult),
                     reads=['dal'], writes=['dtmp'])
                S.op('dve', lambda e: e.tensor_reduce(out=lamv[:, i2:i2 + 1], in_=dtmp[:], axis=AX.X, op=ALU.add),
                     reads=['dtmp'], writes=['lamv'])
            S.op('act', lambda e: e.activation(out=lamv[:, 0:2], in_=lamv[:, 0:2], func=AF.Exp), reads=['lamv'], writes=['lamv'])
            S.op('dve', lambda e: e.tensor_tensor(out=lamv[:, 2:3], in0=lamv[:, 1:2], in1=lamv[:, 0:1], op=ALU.subtract),
                 reads=['lamv'], writes=['lamv'])
            S.op('dve', lambda e: e.tensor_scalar(out=lamv[:, 2:3], in0=lamv[:, 2:3], scalar1=-LAMBDA_INIT, scalar2=None,
                                                  op0=ALU.add), reads=['lamv'], writes=['lamv'])
            S.op('dve', lambda e: e.tensor_scalar(out=lamv[:, 3:4], in0=subg[:], scalar1=1.0 - LAMBDA_INIT, scalar2=None,
                                                  op0=ALU.mult), reads=['lamv', 'subg'], writes=['lamv'])
            nkc = 34
            acnt = 0
            for hh in range(8):
                for t_, c0, k_ in [(wq, hh * 128, 'wq'), (wk, 1024 + hh * 128, 'wk'), (wv, 2048 + hh * 128, 'wv')]:
                    S.dma('pool', t_[:], w_qkv[:, c0:c0 + 128].rearrange("(k p) f -> p k f", p=128), writes=[k_])
                convert_experts(1, 4 * hh, 4 * hh + 4)
                jobs = []
                for ti in range(9):
                    jobs.append(('v', ti))
                    if ti < 4:
                        jobs.append(('q', ti))
                    jobs.append(('k', ti))

                def proj_s1(kj, kind, ti):
                    off, cnt = TILES[ti][0], TILES[ti][1]
                    q = kj % 2
                    pa_ = 2 * q
                    wt, kw_ = {'q': (wq, 'wq'), 'k': (wk, 'wk'), 'v': (wv, 'wv')}[kind]

                    def mmp(e):
                        for kc in range(8):
                            ins = e.matmul(PS[pa_][:, :cnt], lhsT=wt[:, kc, :], rhs=hbuf[:, kc, off:off + cnt],
                                           start=(kc == 0), stop=(kc == 7))
                        return ins
                    S.op('pe', mmp, reads=[kw_, ('h', ti)], writes=[pk(pa_)])
                    S.op('act', lambda e: e.activation(out=qbt[q][:, :cnt], in_=PS[pa_][:, :cnt], func=AF.Identity),
                         reads=[pk(pa_)], writes=[('qbt', q)])

                def proj_s2(kj, kind, ti):
                    off, cnt = TILES[ti][0], TILES[ti][1]
                    q = kj % 2
                    pa_, pb_ = 2 * q, 2 * q + 1
                    if kind == 'v':
                        nsb = cnt // 128
                        pbb = PS[pb_].bitcast(BF16)

                        def mmvt(e):
                            for s in range(nsb):
                                ins = e.transpose(out=pbb[:, s * 128:(s + 1) * 128], in_=qbt[q][:, s * 128:(s + 1) * 128],
                                                  identity=identb[:])
                            return ins
                        S.op('pe', mmvt, reads=[('qbt', q), 'identb'], writes=[pk(pb_)])
                        si0 = off // 128
                        S.op('dve', lambda e: e.tensor_copy(out=Vt[:, si0:si0 + nsb, :].rearrange("p s v -> p (s v)"),
                                                            in_=pbb[:, :nsb * 128]), reads=[pk(pb_)], writes=['Vt'])
                        return
                    dst, kdst = (QT, 'QT') if kind == 'q' else (KT, 'KT')
                    S.op('pe', lambda e: e.matmul(PS[pb_][:, :cnt], lhsT=rmb[:], rhs=qbt[q][:, :cnt], start=True, stop=True),
                         reads=['rmb', ('qbt', q)], writes=[pk(pb_)])
                    S.op('dve', lambda e: e.tensor_tensor(out=rt1[q][:, :cnt], in0=PS[pa_][:, :cnt], in1=cost[:, off:off + cnt],
                                                          op=ALU.mult), reads=[pk(pa_), 'cos', ('qbt', q)], writes=[('rt1', q)])
                    S.op('dve', lambda e: e.tensor_tensor(out=rt2[q][:, :cnt], in0=PS[pb_][:, :cnt], in1=sint[:, off:off + cnt],
                                                          op=ALU.mult), reads=[pk(pb_), 'sin'], writes=[('rt2', q)])
                    S.op('dve', lambda e: e.tensor_tensor(out=dst[:, off:off + cnt], in0=rt1[q][:, :cnt], in1=rt2[q][:, :cnt],
                                                          op=ALU.add), reads=[('rt1', q), ('rt2', q)], writes=[kdst])

                proj_s1(0, *jobs[0])
                for kj in range(len(jobs)):
                    if kj + 1 < len(jobs):
                        proj_s1(kj + 1, *jobs[kj + 1])
                    proj_s2(kj, *jobs[kj])
                for qt in range(4):
                    q0 = qt * 512
                    pend_ = None
                    for kc in range(nkc + 1):
                        if kc < nkc:
                            sidx = acnt % 2
                            acnt += 1
                            sb0 = 2 * sidx
                            for mi in range(2):
                                lo_, hi_ = mi * 64, (mi + 1) * 64
                                S.op('pe', lambda e: e.matmul(PS[sb0 + mi][:, :], lhsT=KT[lo_:hi_, kc * 128:(kc + 1) * 128],
                                                              rhs=QT[lo_:hi_, q0:q0 + 512], start=True, stop=True),
                                     reads=['KT', 'QT'], writes=[pk(sb0 + mi)])
                            ke = ('Eb', sidx)
                            S.op('act', lambda e: e.activation(out=Eb2[sidx][:].rearrange("p m q -> p (m q)"),
                                                               in_=psbig[:, sb0 * 512:(sb0 + 2) * 512], func=AF.Exp, scale=0.125),
                                 reads=[pk(sb0), pk(sb0 + 1)], writes=[ke])
                            cur = (Eb2[sidx], ke)
                        if pend_ is not None:
                            kp, (ebp, kep) = pend_
                            for mi in range(2):
                                S.op('pe', lambda e: e.matmul(PS[4 + mi][:, :], lhsT=Vt[:, kp, :], rhs=ebp[:, mi, :], start=(kp == 0),
                                                              stop=(kp == nkc - 1)), reads=['Vt', kep], writes=[pk(4 + mi)])
                            accv = psbig[:, 6 * 512:8 * 512]
                            ebf = ebp[:].rearrange("p m q -> p (m q)")
                            if kp == 0:
                                S.op('dve', lambda e: e.tensor_copy(out=accv, in_=ebf), reads=[kep], writes=[pk(6), pk(7)])
                            else:
                                S.op('dve', lambda e: e.tensor_tensor(out=accv, in0=accv, in1=ebf, op=ALU.add),
                                     reads=[kep, pk(6), pk(7)], writes=[pk(6), pk(7)])
                        pend_ = (kc, cur) if kc < nkc else None
                    S.op('act', lambda e: e.activation(out=acc2[:].rearrange("p m q -> p (m q)"), in_=psbig[:, 6 * 512:8 * 512],
                                                       func=AF.Identity), reads=[pk(6), pk(7)], writes=['acc2'])
                    for mi in range(2):
                        S.op('pe', lambda e: e.matmul(PS[mi][:, :], lhsT=ones32[:], rhs=acc2[:, mi, :], start=True, stop=True),
                             reads=['ones32', 'acc2'], writes=[pk(mi)])
                    rlf = rlw[:].rearrange("p m q -> p (m q)")
                    S.op('act', lambda e: e.activation(out=rlf, in_=psbig[:, 0:1024], func=AF.Ln), reads=[pk(0), pk(1)], writes=['rlw'])
                    S.op('act', lambda e: e.activation(out=rlf, in_=rlf, func=AF.Exp, scale=-1.0), reads=['rlw'], writes=['rlw'])
                    S.op('dve', lambda e: e.tensor_tensor(out=obw[:].rearrange("p m q -> p (m q)"), in0=psbig[:, 4 * 512:6 * 512],
                                                          in1=rlf, op=ALU.mult),
                         reads=[pk(4), pk(5), 'rlw'], writes=[('ob', 0), ('ob', 1)])
                    S.op('dve', lambda e: e.scalar_tensor_tensor(out=ob[0][:], in0=ob[1][:], scalar=lamv[:, 2:3], in1=ob[0][:],
                                                                 op0=ALU.mult, op1=ALU.add),
                         reads=[('ob', 0), ('ob', 1), 'lamv'], writes=[('ob', 0)])
                    S.op('act', lambda e: e.activation(out=osq[:], in_=ob[0][:], func=AF.Square), reads=[('ob', 0)], writes=['osq'])
                    S.op('pe', lambda e: e.matmul(PS[2][:, :], lhsT=ones32[:], rhs=osq[:], start=True, stop=True),
                         reads=['ones32', 'osq'], writes=[pk(2)])
                    S.op('act', lambda e: e.activation(out=osq[:], in_=PS[2][:, :], func=AF.Ln, bias=eps_t[:, 0:1],
                                                       scale=1.0 / 128.0), reads=[pk(2), 'eps'], writes=['osq'])
                    S.op('act', lambda e: e.activation(out=osq[:], in_=osq[:], func=AF.Exp, scale=-0.5), reads=['osq'], writes=['osq'])
                    a_ = aot[qt % 2]
                    S.op('dve', lambda e: e.scalar_tensor_tensor(out=a_[:], in0=ob[0][:], scalar=lamv[:, 3:4], in1=osq[:],
                                                                 op0=ALU.mult, op1=ALU.mult),
                         reads=[('ob', 0), 'osq', 'lamv'], writes=[('aot', qt % 2)])
                    S.dma('sp', zscr[hh, :, q0:q0 + 512], a_[:], reads=[('aot', qt % 2)], writes=[('zs', qt)])
            S.barrier()
        hst.close()

        phase_C(1, w_o, list(range(4)), xr)
        phase_D(1, list(range(4)), final=True)
        S.barrier()
    return nc


def _prep_shared(inp, rev):
    dsl = slice(None, None, -1) if rev else slice(None)
    f = lambda a: np.ascontiguousarray(np.asarray(a, dtype=np.float32))
    pj = lambda v: f(np.asarray(v).reshape(8, 128).T)
    sh = {}
    sh["w_mod"] = f(inp["w_mod"])
    sh["bmod"] = f(np.asarray(inp["b_mod"]).reshape(2, 48, 128).transpose(2, 0, 1))
    sh["n1g"] = f(np.asarray(inp["norm1_g"]).reshape(2, 8, 128).transpose(2, 0, 1))
    sh["n2g"] = f(np.asarray(inp["norm2_g"]).reshape(2, 8, 128).transpose(2, 0, 1))
    sh["fing"] = pj(inp["final_g"])
    sh["w_in"] = f(inp["rg_w_in"][0])
    cw = np.asarray(inp["rg_conv_w"][0])
    z1 = np.zeros((1, 1024), np.float32)
    cw5 = np.concatenate([cw, z1], 0) if not rev else np.concatenate([z1, cw[::-1]], 0)
    sh["convw"] = f(cw5.reshape(5, 8, 128).transpose(2, 1, 0))
    sh["convb"] = pj(inp["rg_conv_b"][0])
    sh["gaw"] = f(np.asarray(inp["rg_gate_a_w"][0])[dsl])
    sh["gxw"] = f(np.asarray(inp["rg_gate_x_w"][0])[dsl])
    sh["gab"] = f(np.asarray(inp["rg_gate_a_b"][0])[dsl].reshape(2, 8, 128).transpose(2, 0, 1))
    sh["gxb"] = f(np.asarray(inp["rg_gate_x_b"][0])[dsl].reshape(2, 8, 128).transpose(2, 0, 1))
    sh["lam"] = f(np.asarray(inp["rg_lambda"][0])[dsl].reshape(2, 8, 128).transpose(2, 0, 1))
    sh["w_out"] = f(inp["rg_w_out"][0])
    sh["w_qkv"] = f(inp["da_w_qkv"][0])
    sh["dalam"] = f(np.broadcast_to(np.asarray(inp["da_lambda"][0])[None], (128, 4, 64)))
    sh["subg"] = f(np.asarray(inp["da_subln_g"][0]).reshape(128, 1))
    sh["w_o"] = f(inp["da_w_o"][0])
    sh["rw"] = f(np.asarray(inp["router_w"]).reshape(8, 128, 32).transpose(1, 0, 2))
    sh["rb"] = f(np.broadcast_to(np.asarray(inp["router_bias"])[None], (128, 32)))
    sh["wg"] = f(inp["moe_w_gate"])
    sh["wu"] = f(inp["moe_w_up"])
    sh["wd"] = f(inp["moe_w_down"])
    t = np.arange(NX)
    row = (t // 64).astype(np.float32)
    col = (t % 64).astype(np.float32)
    inv = (1.0 / (10000.0 ** (np.arange(16, dtype=np.float32) / 16))).astype(np.float32)
    ang = np.stack([row, col], -1)[:, :, None] * inv
    ang = np.broadcast_to(ang[:, :, None, :], (NX, 2, 2, 16)).reshape(NX, 64).astype(np.float32)
    cos = np.ones((128, NT), np.float32)
    sin = np.zeros((128, NT), np.float32)
    if rev:
        ang = ang[::-1]
    cos[:, :NX] = np.tile(np.cos(ang).T, (2, 1))
    sin[:, :NX] = np.tile(np.sin(ang).T, (2, 1))
    sh["pcol"] = np.arange(128, dtype=np.float32).reshape(128, 1)
    sh["blkoff"] = np.ascontiguousarray(np.broadcast_to((128.0 * np.arange(100, dtype=np.float32))[None], (128, 100)))
    rm = np.zeros((128, 128), np.float32)
    for m in range(128):
        if (m % 32) < 16:
            rm[m + 16, m] = -1.0
        else:
            rm[m - 16, m] = 1.0
    sh["rmat"] = rm
    sh["cos"] = cos
    sh["sin"] = sin
    return sh


def _prep_core(inp, b, rev):
    f = lambda a: np.ascontiguousarray(np.asarray(a, dtype=np.float32))
    x = np.asarray(inp["x"][b])
    ctx = np.asarray(inp["ctx"][b])
    if rev:
        x = x[::-1]
        ctx = ctx[::-1]
    tok = np.concatenate([x, ctx], axis=0)
    d = {"xc": f(tok.T.reshape(8, 128, NT))}
    cond = np.stack([np.asarray(inp["c"][b]), np.asarray(inp["c_ctx"])], -1)
    d["cond"] = f(cond.reshape(8, 128, 2).transpose(1, 0, 2))
    return d


def kernel(**inputs):
    nc = build(DEBUG)
    shs = [_prep_shared(inputs, False), _prep_shared(inputs, True)]
    in_maps = []
    for core in range(8):
        b, rev = core // 2, core % 2
        m = dict(shs[rev])
        m.update(_prep_core(inputs, b, bool(rev)))
        in_maps.append(m)
    res = run_bass_kernel_spmd(nc, in_maps, core_ids=list(range(8)))
    out = np.empty((4, NX, 1024), np.float32)
    for core in range(8):
        b, rev = core // 2, core % 2
        o = np.asarray(res.results[core]["outT"]).reshape(1024, NQ).T
        if rev:
            out[b, NX - NQ:] = o[::-1]
        else:
            out[b, :NQ] = o
    return out
```

```python
import contextlib
import math
import numpy as np
import concourse.bass as bass
import concourse.mybir as mybir
from concourse.bass_utils import run_bass_kernel_spmd

F32 = mybir.dt.float32
BF16 = mybir.dt.bfloat16
ALU = mybir.AluOpType
AF = mybir.ActivationFunctionType
AX = mybir.AxisListType

EPS = 1e-6
NT = 4352
NX = 4096
NCTX = 256
TILES = [(i * 512, 512, 2 + i * 512, i * 512, 0) for i in range(8)] + [(4096, 256, 4102, 4100, 1)]
UW = 4360
UCW = 4356
NQ = 2048
LAMBDA_INIT = 0.8 - 0.6 * math.exp(-0.3 * 1)
DEBUG = False


class Sched:
    ROT = 30000

    def __init__(self, nc, stack):
        self.nc = nc
        self.stack = stack
        self.engs = {'pe': nc.tensor, 'act': nc.scalar, 'dve': nc.vector,
                     'pool': nc.gpsimd, 'sp': nc.sync}
        self.cur = {}
        self.nsem = 0
        for e in self.engs:
            self.cur[e] = [self._newsem(e), 0]
        self.lastw = {}
        self.readers = {}
        self.waited = {e: {} for e in self.engs}
        self.dsem = {}
        self.alltok = {}

    def _newsem(self, nm):
        self.nsem += 1
        return self.stack.enter_context(self.nc.semaphore(f"s_{nm}_{self.nsem}"))

    def _wait(self, eng, toks):
        best = {}
        for (s, v) in toks:
            k = id(s)
            if k not in best or best[k][1] < v:
                best[k] = (s, v)
        for k, (s, v) in best.items():
            if self.waited[eng].get(k, 0) >= v:
                continue
            self.engs[eng].wait_ge(s, v)
            self.waited[eng][k] = v

    def _deps(self, eng, reads, writes, skip_same_pe=True):
        toks = []
        for k in reads:
            if k in self.lastw:
                toks.append(self.lastw[k])
        for k in writes:
            if k in self.lastw:
                toks.append(self.lastw[k])
            toks.extend(self.readers.get(k, ()))
        if eng == 'pe' and skip_same_pe:
            toks = [t for t in toks if t[0] is not self.cur['pe'][0]]
        return toks

    def _commit(self, tok, reads, writes):
        for k in writes:
            self.lastw[k] = tok
            self.readers[k] = []
        for k in reads:
            if k in writes:
                continue
            self.readers.setdefault(k, []).append(tok)
        self.alltok[id(tok[0])] = tok

    def op(self, eng, fn, reads=(), writes=()):
        self._wait(eng, self._deps(eng, reads, writes))
        c = self.cur[eng]
        if c[1] >= self.ROT:
            c[0] = self._newsem(eng)
            c[1] = 0
        ins = fn(self.engs[eng])
        c[1] += 1
        ins.then_inc(c[0], 1)
        self._commit((c[0], c[1]), reads, writes)

    def dma(self, eng, out, in_, reads=(), writes=(), slot=None, **kw):
        if slot is None:
            slot = ('auto',) + tuple(writes)
        self._wait(eng, self._deps(eng, reads, writes, skip_same_pe=False))
        if slot not in self.dsem:
            self.dsem[slot] = [self._newsem('d'), 0]
        d = self.dsem[slot]
        ins = self.engs[eng].dma_start(out=out, in_=in_, **kw)
        d[1] += 16
        ins.then_inc(d[0], 16)
        self._commit((d[0], d[1]), reads, writes)

    def barrier(self):
        toks = list(self.alltok.values())
        for e in self.engs:
            self._wait(e, toks)


def bc_last(a, n):
    return bass.AP(a.tensor, a.offset, [list(x) for x in a.ap] + [[0, n]])


def bc_mid(a, n):
    l = [list(x) for x in a.ap]
    return bass.AP(a.tensor, a.offset, [l[0], [0, n]] + l[1:])


def build(dbg=False):
    nc = bass.Bass("TRN2", target_bir_lowering=False)

    def din(name, shape, dt=F32):
        return nc.dram_tensor(name, list(shape), dt, kind="ExternalInput").ap()

    xc = din("xc", [8, 128, NT])
    cond_in = din("cond", [128, 8, 2])
    w_mod = din("w_mod", [2, 1024, 6144])
    bmod_in = din("bmod", [128, 2, 48])
    n1g_in = din("n1g", [128, 2, 8])
    n2g_in = din("n2g", [128, 2, 8])
    fing_in = din("fing", [128, 8])
    w_in = din("w_in", [1024, 2048])
    convw_in = din("convw", [128, 8, 5])
    convb_in = din("convb", [128, 8])
    gaw = din("gaw", [2, 8, 128, 128])
    gxw = din("gxw", [2, 8, 128, 128])
    gab_in = din("gab", [128, 2, 8])
    gxb_in = din("gxb", [128, 2, 8])
    lam_in = din("lam", [128, 2, 8])
    w_out = din("w_out", [1024, 1024])
    w_qkv = din("w_qkv", [1024, 3072])
    dalam_in = din("dalam", [128, 4, 64])
    subg_in = din("subg", [128, 1])
    w_o = din("w_o", [1024, 1024])
    rw_in = din("rw", [128, 8, 32])
    rb_in = din("rb", [128, 32])
    wg = din("wg", [2, 32, 1024, 512])
    wu = din("wu", [2, 32, 1024, 512])
    wd = din("wd", [2, 32, 512, 1024])
    rmat_in = din("rmat", [128, 128])
    cos_in = din("cos", [128, NT])
    sin_in = din("sin", [128, NT])
    outT = nc.dram_tensor("outT", [8, 128, NQ], F32, kind="ExternalOutput").ap()
    xr = nc.dram_tensor("xr", [8, 128, NT], F32, kind="Internal").ap()
    zscr = nc.dram_tensor("zscr", [8, 128, NT], BF16, kind="Internal").ap()
    h2tm = nc.dram_tensor("h2tm", [NT, 1024], BF16, kind="Internal").ap()
    Xs = nc.dram_tensor("Xs", [100 * 128, 1024], BF16, kind="Internal").ap()
    Ys = nc.dram_tensor("Ys", [100 * 128, 1024], F32, kind="Internal").ap()
    wgb = nc.dram_tensor("wgb", [2 * 8192, 2048], BF16, kind="Internal").ap()
    wub = nc.dram_tensor("wub", [2 * 8192, 2048], BF16, kind="Internal").ap()
    wdb = nc.dram_tensor("wdb", [2 * 8192, 2048], BF16, kind="Internal").ap()
    pcol_in = din("pcol", [128, 1])
    blkoff_in = din("blkoff", [128, 100])
    dbgo = {}
    if dbg:
        for nm, shp in [("d_mod", [128, 2 * 48 * 2]), ("d_h", [128, 8, NT]), ("d_z", [8, 128, NT]),
                        ("d_x1", [8, 128, NT]), ("d_x2", [8, 128, NT]),
                        ("d_ao", [8, 128, NT])]:
            dbgo[nm] = nc.dram_tensor(nm, shp, F32, kind="ExternalOutput").ap()

    with contextlib.ExitStack() as st:
        S = Sched(nc, st)

        uid = [0]

        def sb(stack, name, shape, dt):
            uid[0] += 1
            return stack.enter_context(nc.sbuf_tensor(f"sb{uid[0]}_{name}", list(shape), dt))

        psbig = st.enter_context(nc.psum_tensor("psbig", [128, 8 * 512], F32))
        PS = [psbig[:, i * 512:(i + 1) * 512] for i in range(8)]
        pk = lambda i: ('ps', i)
        wgv_all = wg.rearrange("l e (q r) f -> (l e q) (r f)", r=4)
        wuv_all = wu.rearrange("l e (q r) f -> (l e q) (r f)", r=4)
        wdv_all = wd.rearrange("l e (q r) d -> (l e q) (r d)", r=2)

        def convert_experts(l, e0, e1):
            for e_ in range(e0, e1):
                r0 = (l * 32 + e_) * 256
                for dst, src in [(wgb, wgv_all), (wub, wuv_all), (wdb, wdv_all)]:
                    S.dma('pool', dst[r0:r0 + 256, :], src[r0:r0 + 256, :], writes=[('wcv', l)])

        ones_bf = sb(st, "ones_bf", [128, 128], BF16)
        ones32 = sb(st, "ones32", [128, 128], F32)
        ident = sb(st, "ident", [128, 128], F32)
        identb = sb(st, "identb", [128, 128], BF16)
        modT = sb(st, "modT", [128, 2, 48, 2], F32)
        gmT = sb(st, "gmT", [128, 2, 2, 8, 2], F32)
        n1g = sb(st, "n1g", [128, 2, 8], F32)
        n2g = sb(st, "n2g", [128, 2, 8], F32)
        fing = sb(st, "fing", [128, 8], F32)
        rw = sb(st, "rw", [128, 8, 32], F32)
        rb = sb(st, "rb", [128, 32], F32)
        S.op('dve', lambda e: e.memset(ones_bf[:], 1.0), writes=['ones_bf'])
        S.op('dve', lambda e: e.memset(ones32[:], 1.0), writes=['ones32'])
        S.op('pool', lambda e: e.memset(ident[:], 1.0), writes=['ident'])
        S.op('pool', lambda e: e.affine_select(out=ident[:], in_=ident[:], pattern=[[-1, 128]],
                                               compare_op=ALU.is_equal, fill=0.0, base=0, channel_multiplier=1),
             reads=['ident'], writes=['ident'])
        S.op('act', lambda e: e.activation(out=identb[:], in_=ident[:], func=AF.Identity), reads=['ident'], writes=['identb'])
        for t, src, k in [(n1g, n1g_in, 'n1g'), (n2g, n2g_in, 'n2g'), (fing, fing_in, 'fing'),
                          (rw, rw_in, 'rw'), (rb, rb_in, 'rb')]:
            S.dma('sp', t[:], src, writes=[k])

        with contextlib.ExitStack() as ph:
            condt = sb(ph, "condt", [128, 8, 2], F32)
            scond = sb(ph, "scond", [128, 8, 2], F32)
            bm = sb(ph, "bm", [128, 2, 48], F32)
            wm = [sb(ph, f"wm{i}", [128, 8, 1024], F32) for i in range(2)]
            S.dma('sp', condt[:], cond_in, writes=['cond'])
            S.dma('sp', bm[:], bmod_in, writes=['bm'])
            S.op('act', lambda e: e.activation(out=scond[:], in_=condt[:], func=AF.Silu),
                 reads=['cond'], writes=['scond'])
            it = 0
            for l in range(2):
                for gi in range(6):
                    buf = wm[it % 2]
                    key = ('wm', it % 2)
                    S.dma('sp', buf[:], w_mod[l, :, gi * 1024:(gi + 1) * 1024].rearrange("(k p) f -> p k f", p=128),
                          writes=[key])
                    pst = PS[it % 2]

                    def mm(e, buf=buf, pst=pst):
                        for j in range(8):
                            for kc in range(8):
                                ins = e.matmul(pst[:, j * 2:(j + 1) * 2], lhsT=buf[:, kc, j * 128:(j + 1) * 128],
                                               rhs=scond[:, kc, :], start=(kc == 0), stop=(kc == 7))
                        return ins
                    S.op('pe', mm, reads=[key, 'scond'], writes=[pk(it % 2)])
                    S.op('dve', lambda e: e.tensor_tensor(
                        out=modT[:, l, gi * 8:(gi + 1) * 8, :],
                        in0=pst[:, 0:16].rearrange("p (j r) -> p j r", r=2),
                        in1=bc_last(bm[:, l, gi * 8:(gi + 1) * 8], 2), op=ALU.add),
                        reads=[pk(it % 2), 'bm'], writes=['modT'])
                    it += 1
            for l in range(2):
                for w_, (gt, gk, sidx) in enumerate([(n1g, 'n1g', 1), (n2g, 'n2g', 4)]):
                    S.op('dve', lambda e: e.tensor_scalar(out=gmT[:, l, w_], in0=modT[:, l, sidx * 8:(sidx + 1) * 8, :],
                                                          scalar1=1.0, scalar2=None, op0=ALU.add),
                         reads=['modT'], writes=['gmT'])
                    S.op('dve', lambda e: e.tensor_tensor(out=gmT[:, l, w_], in0=gmT[:, l, w_],
                                                          in1=bc_last(gt[:, l, :], 2), op=ALU.mult),
                         reads=['gmT', gk], writes=['gmT'])
            if dbg:
                S.dma('sp', dbgo["d_mod"], modT[:].rearrange("p a b c -> p (a b c)"), reads=['modT'], writes=['d_mod'])
            S.barrier()

        def mod_ap(l, idx, j, r):
            return modT[:, l, idx * 8 + j, r:r + 1]

        def norm_mod(tmp, xt, n, kx, out, kout, gm_of_j, sh_of_j, psi, extra_reads=(), tag=''):
            sq, rt, rstd, xn = tmp
            kxl = list(kx) if isinstance(kx, list) else [kx]
            S.op('act', lambda e: e.activation(out=sq[:, :, :n], in_=xt[:, :, :n], func=AF.Square),
                 reads=kxl, writes=['nm_sq' + tag])

            def mm(e):
                for j in range(8):
                    ins = e.matmul(PS[psi][:, :n], lhsT=ones_bf[:], rhs=sq[:, j, :n], start=(j == 0), stop=(j == 7))
                return ins
            S.op('pe', mm, reads=['nm_sq' + tag, 'ones_bf'], writes=[pk(psi)])
            S.op('act', lambda e: e.activation(out=rt[:, :n], in_=PS[psi][:, :n], func=AF.Ln,
                                               bias=eps_t[:, 0:1], scale=1.0 / 1024.0),
                 reads=[pk(psi), 'eps'], writes=['nm_rt' + tag])
            S.op('act', lambda e: e.activation(out=rstd[:, :n], in_=rt[:, :n], func=AF.Exp, scale=-0.5),
                 reads=['nm_rt' + tag], writes=['nm_rstd' + tag])
            S.op('dve', lambda e: e.tensor_tensor(out=xn[:, :, :n], in0=xt[:, :, :n], in1=bc_mid(rstd[:, :n], 8),
                                                  op=ALU.mult), reads=kxl + ['nm_rstd' + tag], writes=['nm_xn' + tag])
            for j in range(8):
                if sh_of_j is None:
                    S.op('dve', lambda e: e.tensor_scalar(out=out(j), in0=xn[:, j, :n], scalar1=gm_of_j(j),
                                                          scalar2=None, op0=ALU.mult),
                         reads=['nm_xn' + tag] + list(extra_reads), writes=[kout])
                elif j % 2 == 0:
                    S.op('dve', lambda e: e.tensor_scalar(out=out(j), in0=xn[:, j, :n], scalar1=gm_of_j(j),
                                                          scalar2=sh_of_j(j), op0=ALU.mult, op1=ALU.add),
                         reads=['nm_xn' + tag] + list(extra_reads), writes=[kout])
                else:
                    S.op('act', lambda e: e.activation(out=out(j), in_=xn[:, j, :n], func=AF.Identity,
                                                       bias=sh_of_j(j), scale=gm_of_j(j)),
                         reads=['nm_xn' + tag] + list(extra_reads), writes=[kout])

        def norm_tmp(ph):
            return (sb(ph, "nm_sq", [128, 8, 512], BF16), sb(ph, "nm_rt", [128, 512], F32),
                    sb(ph, "nm_rstd", [128, 512], F32), sb(ph, "nm_xn", [128, 8, 512], F32))

        eps_t = sb(st, "eps_t", [128, 1], F32)
        S.op('dve', lambda e: e.memset(eps_t[:], EPS), writes=['eps'])
        one_t = sb(st, "one_t", [128, 1], F32)
        S.op('dve', lambda e: e.memset(one_t[:], 1.0), writes=['one'])


        def phase_A(l, src, hbuf):
            with contextlib.ExitStack() as ph:
                xts = [sb(ph, f"xt{i}", [128, 8, 512], F32) for i in range(2)]
                tmp = norm_tmp(ph)
                for ti, (off, cnt, _, _, r) in enumerate(TILES):
                    xt = xts[ti % 2]
                    kx = ('xt', ti % 2)
                    S.dma('sp', xt[:, :, :cnt], src[:, :, off:off + cnt].rearrange("j p t -> p j t"),
                          reads=[('xr', ti)], writes=[kx])
                    norm_mod(tmp, xt, cnt, kx, lambda j: hbuf[:, j, off:off + cnt], ('h', ti),
                             lambda j: gmT[:, l, 0, j, r:r + 1], lambda j: mod_ap(l, 0, j, r), 7,
                             extra_reads=['gmT', 'modT'])
                S.barrier()

        hst = contextlib.ExitStack()
        hbuf = sb(hst, "hbuf", [128, 8, NT], BF16)
        phase_A(0, xc, hbuf)
        if dbg:
            with contextlib.ExitStack() as ph:
                t32 = sb(ph, "dbg32", [128, 8, 512], F32)
                for ti, (off, cnt, _, _, r) in enumerate(TILES):
                    S.op('dve', lambda e: e.tensor_copy(out=t32[:, :, :cnt], in_=hbuf[:, :, off:off + cnt]),
                         reads=[('h', ti)], writes=['dbg32'])
                    S.dma('sp', dbgo["d_h"][:, :, off:off + cnt], t32[:, :, :cnt], reads=['dbg32'], writes=['d_h'])
                S.barrier()

        with contextlib.ExitStack() as ph:
            ub = sb(ph, "ub", [128, UW], F32)
            uc = sb(ph, "uc", [128, UCW], F32)
            ucb = sb(ph, "ucb", [128, UCW], BF16)
            gy = sb(ph, "gy", [128, NT], BF16)
            wyu = [sb(ph, f"wyu{i}", [128, 8, 256], BF16) for i in range(2)]
            gw = [sb(ph, f"gw{i}", [128, 4, 128], BF16) for i in range(2)]
            convw = sb(ph, "convw", [128, 8, 5], F32)
            convb = sb(ph, "convb", [128, 8], F32)
            gab = sb(ph, "gab", [128, 2, 8], F32)
            gxb = sb(ph, "gxb", [128, 2, 8], F32)
            lamt = sb(ph, "lamt", [128, 2, 8], F32)
            cneg = sb(ph, "cneg", [128, 2, 8], F32)
            cneg2 = sb(ph, "cneg2", [128, 2, 8], F32)
            rbuf = [sb(ph, f"rbuf{i}", [128, 512], F32) for i in range(2)]
            ibuf = [sb(ph, f"ibuf{i}", [128, 512], F32) for i in range(2)]
            sbuf_ = [sb(ph, f"sbuf{i}", [128, 512], F32) for i in range(2)]
            hbt = [sb(ph, f"hbt{i}", [128, 512], F32) for i in range(2)]
            gt1 = [sb(ph, f"gt1{i}", [128, 512], F32) for i in range(2)]
            zt = [sb(ph, f"zt{i}", [128, 512], BF16) for i in range(2)]
            for t, src, k in [(convw, convw_in, 'convw'), (convb, convb_in, 'convb'), (gab, gab_in, 'gab'),
                              (gxb, gxb_in, 'gxb'), (lamt, lam_in, 'lamt')]:
                S.dma('sp', t[:], src, writes=[k])
            S.op('act', lambda e: e.activation(out=cneg[:], in_=lamt[:], func=AF.Exp, scale=-1.0),
                 reads=['lamt'], writes=['cneg'])
            S.op('act', lambda e: e.activation(out=cneg[:], in_=cneg[:], func=AF.Ln, bias=one_t[:, 0:1], scale=1.0),
                 reads=['cneg', 'one'], writes=['cneg'])
            S.op('dve', lambda e: e.tensor_scalar(out=cneg2[:], in0=cneg[:], scalar1=-16.0, scalar2=None, op0=ALU.mult),
                 reads=['cneg'], writes=['cneg2'])
            S.op('dve', lambda e: e.tensor_scalar(out=cneg[:], in0=cneg[:], scalar1=-8.0, scalar2=None, op0=ALU.mult),
                 reads=['cneg', 'cneg2'], writes=['cneg'])
            zer = sb(ph, "zer", [128, 4, 1024], BF16)
            S.op('pool', lambda e: e.memset(zer[:], 0.0), writes=['zer'])
            ngab = sb(ph, "ngab", [128, 2, 8], F32)
            ngxb = sb(ph, "ngxb", [128, 2, 8], F32)
            S.op('dve', lambda e: e.tensor_scalar(out=ngab[:], in0=gab[:], scalar1=-1.0, scalar2=None, op0=ALU.mult),
                 reads=['gab'], writes=['ngab'])
            S.op('dve', lambda e: e.tensor_scalar(out=ngxb[:], in0=gxb[:], scalar1=-1.0, scalar2=None, op0=ALU.mult),
                 reads=['gxb'], writes=['ngxb'])
            S.op('dve', lambda e: e.memset(ub[:], 0.0), writes=[('ub', ti) for ti in range(9)])
            ubkeys = [('ub', ti) for ti in range(9)]
            cnt_sc = 0
            for n in range(8):
                w = wyu[n % 2]
                kw_ = ('wyu', n % 2)
                S.dma('pool', w[:, :, 0:128], w_in[:, n * 128:(n + 1) * 128].rearrange("(k p) f -> p k f", p=128),
                      writes=[kw_])
                S.dma('pool', w[:, :, 128:256],
                      w_in[:, 1024 + n * 128:1024 + (n + 1) * 128].rearrange("(k p) f -> p k f", p=128), writes=[kw_])
                g = gw[n % 2]
                kg = ('gw', n % 2)
                for d in range(2):
                    S.dma('pool', g[:, 2 * d, :], gaw[d, n], writes=[kg])
                    S.dma('pool', g[:, 2 * d + 1, :], gxw[d, n], writes=[kg])
                convert_experts(0, 4 * n, 4 * n + 4)
                for b4 in range(4 * n, min(4 * n + 4, 25)):
                    S.dma('pool', Xs[b4 * 512:(b4 + 1) * 512, :].rearrange("(a p) f -> p a f", p=128), zer[:],
                          reads=['zer'], writes=['Xs'])
                for ti, (off, cnt, uoff, ucoff, r) in enumerate(TILES):
                    pu, py = (ti % 2) * 2, (ti % 2) * 2 + 1

                    def mmu(e, c0=128, p=pu):
                        for kc in range(8):
                            ins = e.matmul(PS[p][:, :cnt], lhsT=w[:, kc, c0:c0 + 128], rhs=hbuf[:, kc, off:off + cnt],
                                           start=(kc == 0), stop=(kc == 7))
                        return ins
                    S.op('pe', mmu, reads=[kw_, ('h', ti)], writes=[pk(pu)])
                    S.op('act', lambda e: e.activation(out=ub[:, uoff:uoff + cnt], in_=PS[pu][:, :cnt], func=AF.Identity),
                         reads=[pk(pu)], writes=[('ub', ti)])
                    S.op('pe', lambda e: mmu(e, 0, py), reads=[kw_, ('h', ti)], writes=[pk(py)])
                    t1 = gt1[ti % 2]
                    k1 = ('gt1', ti % 2)
                    S.op('act', lambda e: e.activation(out=t1[:, :cnt], in_=PS[py][:, :cnt], func=AF.Square),
                         reads=[pk(py)], writes=[k1])
                    S.op('dve', lambda e: e.tensor_scalar(out=t1[:, :cnt], in0=t1[:, :cnt], scalar1=0.044715, scalar2=1.0,
                                                          op0=ALU.mult, op1=ALU.add), reads=[k1], writes=[k1])
                    S.op('dve', lambda e: e.tensor_tensor(out=t1[:, :cnt], in0=t1[:, :cnt], in1=PS[py][:, :cnt], op=ALU.mult),
                         reads=[k1, pk(py)], writes=[k1])
                    S.op('act', lambda e: e.activation(out=t1[:, :cnt], in_=t1[:, :cnt], func=AF.Sigmoid,
                                                       scale=1.5957691216057308), reads=[k1], writes=[k1])
                    S.op('dve', lambda e: e.tensor_tensor(out=gy[:, off:off + cnt], in0=t1[:, :cnt], in1=PS[py][:, :cnt],
                                                          op=ALU.mult), reads=[k1, pk(py)], writes=[('gy', ti)])
                S.op('dve', lambda e: e.tensor_scalar(out=uc[:], in0=ub[:, 0:UCW], scalar1=convw[:, n, 0:1],
                                                      scalar2=convb[:, n:n + 1], op0=ALU.mult, op1=ALU.add),
                     reads=ubkeys + ['convw', 'convb'], writes=['uc'])
                for k in range(1, 5):
                    S.op('dve', lambda e: e.scalar_tensor_tensor(out=uc[:], in0=ub[:, k:k + UCW], scalar=convw[:, n, k:k + 1],
                                                                 in1=uc[:], op0=ALU.mult, op1=ALU.add),
                         reads=ubkeys + ['convw', 'uc'], writes=['uc'])
                S.op('act', lambda e: e.activation(out=ucb[:], in_=uc[:], func=AF.Identity), reads=['uc'], writes=['ucb'])
                for d in range(2):
                    order = [8] + (list(range(8)) if d == 0 else list(range(7, -1, -1)))
                    prev = None
                    for oi, ti in enumerate(order):
                        off, cnt, uoff, ucoff, r = TILES[ti]
                        b = cnt_sc % 2
                        cnt_sc += 1
                        pr, pi = 4 + 2 * b, 5 + 2 * b
                        S.op('pe', lambda e: e.matmul(PS[pr][:, :cnt], lhsT=g[:, 2 * d, :], rhs=ucb[:, ucoff:ucoff + cnt],
                                                      start=True, stop=True), reads=[kg, 'ucb'], writes=[pk(pr)])
                        S.op('pe', lambda e: e.matmul(PS[pi][:, :cnt], lhsT=g[:, 2 * d + 1, :], rhs=ucb[:, ucoff:ucoff + cnt],
                                                      start=True, stop=True), reads=[kg, 'ucb'], writes=[pk(pi)])
                        rb_, ib_, sb_, hb_ = rbuf[b], ibuf[b], sbuf_[b], hbt[b]
                        kr, ki, ks, kh = ('rbuf', b), ('ibuf', b), ('sbuf', b), ('hbt', b)
                        S.op('act', lambda e: e.activation(out=rb_[:, :cnt], in_=PS[pr][:, :cnt], func=AF.Exp,
                                                           bias=ngab[:, d, n:n + 1], scale=-1.0),
                             reads=[pk(pr), 'ngab'], writes=[kr])
                        S.op('act', lambda e: e.activation(out=ib_[:, :cnt], in_=PS[pi][:, :cnt], func=AF.Exp,
                                                           bias=ngxb[:, d, n:n + 1], scale=-1.0),
                             reads=[pk(pi), 'ngxb'], writes=[ki])
                        S.op('act', lambda e: e.activation(out=rb_[:, :cnt], in_=rb_[:, :cnt], func=AF.Ln,
                                                           bias=one_t[:, 0:1], scale=1.0), reads=[kr, 'one'], writes=[kr])
                        S.op('act', lambda e: e.activation(out=ib_[:, :cnt], in_=ib_[:, :cnt], func=AF.Ln,
                                                           bias=one_t[:, 0:1], scale=1.0), reads=[ki, 'one'], writes=[ki])
                        S.op('act', lambda e: e.activation(out=rb_[:, :cnt], in_=rb_[:, :cnt], func=AF.Exp, scale=-1.0),
                             reads=[kr], writes=[kr])
                        S.op('act', lambda e: e.activation(out=sb_[:, :cnt], in_=rb_[:, :cnt], func=AF.Exp,
                                                           scale=cneg2[:, d, n:n + 1]), reads=[kr, 'cneg2'], writes=[ks])
                        S.op('act', lambda e: e.activation(out=rb_[:, :cnt], in_=rb_[:, :cnt], func=AF.Exp,
                                                           scale=cneg[:, d, n:n + 1]), reads=[kr, 'cneg'], writes=[kr])
                        S.op('act', lambda e: e.activation(out=sb_[:, :cnt], in_=sb_[:, :cnt], func=AF.Ln,
                                                           bias=one_t[:, 0:1], scale=-1.0), reads=[ks, 'one'], writes=[ks])
                        S.op('dve', lambda e: e.scalar_tensor_tensor(out=sb_[:, :cnt], in0=sb_[:, :cnt], scalar=0.5, in1=ib_[:, :cnt],
                                                                     op0=ALU.mult, op1=ALU.subtract),
                             reads=[ks, ki], writes=[ks])
                        S.op('act', lambda e: e.activation(out=sb_[:, :cnt], in_=sb_[:, :cnt], func=AF.Exp),
                             reads=[ks], writes=[ks])
                        S.op('dve', lambda e: e.tensor_tensor(out=ib_[:, :cnt], in0=sb_[:, :cnt],
                                                              in1=uc[:, ucoff:ucoff + cnt], op=ALU.mult),
                             reads=[ks, 'uc', ki], writes=[ki])
                        if d == 0:
                            if prev is None:
                                init, kin = 0.0, []
                            else:
                                po, pc, puo, _, _ = TILES[prev]
                                init, kin = ub[:, puo + pc - 1:puo + pc], [('ub', prev)]
                            S.op('dve', lambda e: e.tensor_tensor_scan(out=ub[:, uoff:uoff + cnt], data0=rb_[:, :cnt],
                                                                       data1=ib_[:, :cnt], initial=init,
                                                                       op0=ALU.mult, op1=ALU.add),
                                 reads=[kr, ki] + kin, writes=[('ub', ti)])
                        else:
                            if prev is None:
                                init, kin = 0.0, []
                            else:
                                init, kin = hbt[1 - b][:, 0:1], [('hbt', 1 - b)]
                            S.op('dve', lambda e: e.tensor_tensor_scan(out=hb_[:, :cnt][:, ::-1], data0=rb_[:, :cnt][:, ::-1],
                                                                       data1=ib_[:, :cnt][:, ::-1], initial=init,
                                                                       op0=ALU.mult, op1=ALU.add),
                                 reads=[kr, ki] + kin, writes=[kh])
                            S.op('dve', lambda e: e.tensor_tensor(out=sb_[:, :cnt], in0=hb_[:, :cnt],
                                                                  in1=ub[:, uoff:uoff + cnt], op=ALU.add),
                                 reads=[kh, ('ub', ti)], writes=[ks])
                            z_ = zt[b]
                            S.op('dve', lambda e: e.tensor_tensor(out=z_[:, :cnt], in0=sb_[:, :cnt],
                                                                  in1=gy[:, off:off + cnt], op=ALU.mult),
                                 reads=[ks, ('gy', ti)], writes=[('zt', b)])
                            S.dma('sp', zscr[n, :, off:off + cnt], z_[:, :cnt], reads=[('zt', b)], writes=[('zs', ti)])
                        prev = ti
            S.barrier()
        hst.close()

        I32 = mybir.dt.int32
        MAXSUB = 34
        NBMAX = 100
        BIGW = 2 * 32 * 256 + 64
        utri = sb(st, "utri", [128, 128], F32)
        S.op('pool', lambda e: e.memset(utri[:], 1.0), writes=['utri'])
        S.op('pool', lambda e: e.affine_select(out=utri[:], in_=utri[:], pattern=[[1, 128]],
                                               compare_op=ALU.is_gt, fill=0.0, base=0, channel_multiplier=-1),
             reads=['utri'], writes=['utri'])
        pcol = sb(st, "pcol", [128, 1], F32)
        blkoff = sb(st, "blkoff", [128, NBMAX], F32)
        ones_row = sb(st, "ones_row", [128, 32], F32)
        S.dma('sp', pcol[:], pcol_in, writes=['pcol'])
        S.dma('sp', blkoff[:], blkoff_in, writes=['blkoff'])
        S.op('dve', lambda e: e.memset(ones_row[:], 1.0), writes=['ones_row'])
        onesb = sb(st, "onesb", [128, 4, 32], F32)
        c12 = sb(st, "c12", [128, 2, 4, 32], F32)
        S.op('dve', lambda e: e.memset(onesb[:], 1.0), writes=['onesb'])
        for k2_ in range(2):
            for s_ in range(4):
                S.op('dve', lambda e: e.memset(c12[:, k2_, s_, :], float(2 * s_ + 1 + k2_)), writes=['c12'])
        FM = sb(st, "FM", [128, MAXSUB, 2, 32], F32)
        RK = sb(st, "RK", [128, MAXSUB, 2], F32)
        WK = sb(st, "WK", [128, MAXSUB, 2], F32)
        DI = sb(st, "DI", [128, MAXSUB, 2], I32)
        WI = sb(st, "WI", [128, NBMAX, 2], I32)
        cntm = sb(st, "cntm", [128, 32], F32)

        bregs = {}

        def idma(out, out_off, in_, in_off, bounds, reads, writes, slot):
            S._wait('pool', S._deps('pool', reads, writes, skip_same_pe=False))
            if slot not in S.dsem:
                S.dsem[slot] = [S._newsem('d'), 0]
            d = S.dsem[slot]
            if bounds not in bregs:
                bregs[bounds] = nc.gpsimd.to_reg(bounds)
            ins = nc.gpsimd.indirect_dma_start(out=out, out_offset=out_off, in_=in_, in_offset=in_off,
                                               bounds_check=bregs[bounds], oob_is_err=False)
            d[1] += 16
            ins.then_inc(d[0], 16)
            S._commit((d[0], d[1]), reads, writes)

        WST = {}
        wgv = wgb.rearrange("(a b) f -> a (b f)", b=2)
        wuv = wub.rearrange("(a b) f -> a (b f)", b=2)
        wdv = wdb.rearrange("(a b) f -> a (b f)", b=2)

        def moe_blk(NB, p_):
            return (p_ % 4) * (NB // 4) + p_ // 4

        def moe_loadw(l, NB, p_):
            _, wgs, wus, wds = WST[l]
            w_ = p_ % 4
            for nm, view, tile_ in [('wgs', wgv, wgs[w_]), ('wus', wuv, wus[w_]), ('wds', wdv, wds[w_])]:
                idma(tile_[:, :], None, view[:, :], bass.IndirectOffsetOnAxis(ap=WI[:, moe_blk(NB, p_), 0:1], axis=0),
                     (l + 1) * 4096 - 1, reads=['WI', ('wcv', l)], writes=[(nm, w_)], slot=(nm, w_))

        def phase_C(l, wmat, tiles, xsrc):
            nsub_tot = sum(TILES[ti][1] // 128 for ti in tiles)
            NB = 2 * nsub_tot + 32
            with contextlib.ExitStack() as ph:
                wsb = sb(ph, "wsb", [128, 8, 1024], BF16)
                zts = [sb(ph, f"zts{i}", [128, 8, 512], BF16) for i in range(2)]
                xts = [sb(ph, f"xtc{i}", [128, 8, 512], F32) for i in range(2)]
                h2fs = [sb(ph, f"h2f{i}", [128, 8, 512], F32) for i in range(2)]
                h2t = [sb(ph, f"h2t{i}", [128, 1024], BF16) for i in range(2)]
                tmps = [norm_tmp(ph), norm_tmp(ph)]
                mk3 = lambda nm: [sb(ph, f"{nm}{i}", [128, 4, 32], F32) for i in range(2)]
                ssel, sg, em, mk, cmv, rk, t3 = mk3("ssel"), mk3("sg"), mk3("em"), mk3("mk"), mk3("cmv"), mk3("rk"), mk3("t3")
                mk16 = lambda nm: [sb(ph, f"{nm}{i}", [128, 16], F32) for i in range(2)]
                m1, m2, gs, gm_ = mk16("m1"), mk16("m2"), mk16("gs"), mk16("gmk")
                sm = [sb(ph, f"sm{i}", [128, 4, 4], F32) for i in range(2)]
                t32 = [sb(ph, f"t32{i}", [128, 32], F32) for i in range(2)]
                S.dma('pool', wsb[:], wmat.rearrange("(k p) f -> p k f", p=128), writes=['wsb'])
                S.op('dve', lambda e: e.memset(cntm[:], 0.0), writes=['cntm'])
                nsub_box = [0]

                def stage_W(ti):
                    off, cnt, _, _, r = TILES[ti]
                    b = ti % 2
                    z_, x_ = zts[b], xts[b]
                    h2f, tmp, kh2 = h2fs[b], tmps[b], ('h2f', b)
                    kz, kx = ('zts', b), ('xtc', b)
                    S.dma('sp', z_[:, :, :cnt], zscr[:, :, off:off + cnt].rearrange("j p t -> p j t"),
                          reads=[('zs', ti)], writes=[kz])
                    S.dma('sp', x_[:, :, :cnt], xsrc[:, :, off:off + cnt].rearrange("j p t -> p j t"),
                          reads=[('xr', ti)], writes=[kx])
                    for j in range(8):
                        p = j % 3

                        def mm(e):
                            for kc in range(8):
                                ins = e.matmul(PS[p][:, :cnt], lhsT=wsb[:, kc, j * 128:(j + 1) * 128], rhs=z_[:, kc, :cnt],
                                               start=(kc == 0), stop=(kc == 7))
                            return ins
                        S.op('pe', mm, reads=['wsb', kz], writes=[pk(p)])
                        S.op('dve', lambda e: e.scalar_tensor_tensor(out=x_[:, j, :cnt], in0=PS[p][:, :cnt],
                                                                     scalar=mod_ap(l, 2, j, r), in1=x_[:, j, :cnt],
                                                                     op0=ALU.mult, op1=ALU.add),
                             reads=[pk(p), kx, 'modT'], writes=[kx])
                    S.dma('sp', xr[:, :, off:off + cnt].rearrange("j p t -> p j t"), x_[:, :, :cnt],
                          reads=[kx], writes=[('xr', ti)])
                    if dbg and l == 0:
                        S.dma('sp', dbgo["d_x1"][:, :, off:off + cnt].rearrange("j p t -> p j t"), x_[:, :, :cnt],
                              reads=[kx], writes=['d_x1'])

                def stage_N(ti):
                    off, cnt, _, _, r = TILES[ti]
                    b = ti % 2
                    z_, x_ = zts[b], xts[b]
                    h2f, tmp, kh2 = h2fs[b], tmps[b], ('h2f', b)
                    kz, kx = ('zts', b), ('xtc', b)
                    norm_mod(tmp, x_, cnt, kx, lambda j: h2f[:, j, :cnt], kh2,
                             lambda j: gmT[:, l, 1, j, r:r + 1], lambda j: mod_ap(l, 3, j, r), 3,
                             extra_reads=['gmT', 'modT'], tag=str(b))

                def stage_R(ti):
                    off, cnt, _, _, r = TILES[ti]
                    b = ti % 2
                    z_, x_ = zts[b], xts[b]
                    h2f, tmp, kh2 = h2fs[b], tmps[b], ('h2f', b)
                    kz, kx = ('zts', b), ('xtc', b)
                    nsb = cnt // 128
                    gs0 = nsub_box[0]
                    nsub_box[0] += nsb
                    q = ti % 2
                    pl = 4 + q
                    W_ = nsb * 32
                    for s in range(nsb):
                        gsi = gs0 + s
                        qq = gsi % 2
                        for half in range(2):
                            pt = 6 + half

                            def mmt(e):
                                for jj in range(4):
                                    j = half * 4 + jj
                                    ins = e.transpose(out=PS[pt][:, jj * 128:(jj + 1) * 128],
                                                      in_=h2f[:, j, s * 128:(s + 1) * 128], identity=ident[:])
                                return ins
                            S.op('pe', mmt, reads=[kh2, 'ident'], writes=[pk(pt)])
                            if half == 0:
                                S.op('act', lambda e: e.activation(out=h2t[qq][:, 0:512], in_=PS[pt][:, :], func=AF.Identity),
                                     reads=[pk(pt)], writes=[('h2t', qq)])
                            else:
                                S.op('pool' if False else 'dve', lambda e: e.tensor_copy(out=h2t[qq][:, 512:1024], in_=PS[pt][:, :]),
                                     reads=[pk(pt)], writes=[('h2t', qq)])
                        S.dma('sp', h2tm[gsi * 128:(gsi + 1) * 128, :], h2t[qq][:], reads=[('h2t', qq)], writes=[('h2tm', gsi % 4)])

                        def mmr(e):
                            for kc in range(8):
                                ins = e.matmul(PS[pl][:, s * 32:(s + 1) * 32], lhsT=h2f[:, kc, s * 128:(s + 1) * 128], rhs=rw[:, kc, :],
                                               start=(kc == 0), stop=(kc == 7))
                            return ins
                        S.op('pe', mmr, reads=[kh2, 'rw'], writes=[pk(pl)])
                    kq = ('rt', q)
                    v3 = lambda t: t[:, :nsb, :]
                    v8 = lambda t: t[:, :nsb, :].rearrange("p s (g e) -> p (s g) e", e=8)
                    f2 = lambda t: t[:, :nsb, :].rearrange("p s e -> p (s e)")
                    g4 = lambda t: t[:, :nsb * 4]
                    g43 = lambda t: t[:, :nsb * 4].rearrange("p (s g) -> p s g", g=4)
                    S.op('act', lambda e: e.activation(out=f2(sg[q]), in_=PS[pl][:, :W_], func=AF.Sigmoid),
                         reads=[pk(pl)], writes=[kq])
                    S.op('dve', lambda e: e.tensor_tensor(out=v3(ssel[q]), in0=v3(sg[q]), in1=bc_mid(rb[:], nsb), op=ALU.add),
                         reads=[kq, 'rb'], writes=[kq])
                    S.op('dve', lambda e: e.tensor_reduce(out=g4(m1[q]), in_=v8(ssel[q]), axis=AX.X, op=ALU.max), reads=[kq], writes=[kq])
                    S.op('dve', lambda e: e.tensor_tensor(out=v8(t3[q]), in0=v8(ssel[q]), in1=bc_last(g4(m1[q]), 8), op=ALU.is_equal),
                         reads=[kq], writes=[kq])
                    S.op('dve', lambda e: e.scalar_tensor_tensor(out=f2(t3[q]), in0=f2(t3[q]), scalar=-1.0e9, in1=f2(ssel[q]),
                                                                 op0=ALU.mult, op1=ALU.add), reads=[kq], writes=[kq])
                    S.op('dve', lambda e: e.tensor_reduce(out=g4(m2[q]), in_=v8(t3[q]), axis=AX.X, op=ALU.max), reads=[kq], writes=[kq])
                    S.op('dve', lambda e: e.tensor_tensor(out=g4(gs[q]), in0=g4(m1[q]), in1=g4(m2[q]), op=ALU.add), reads=[kq], writes=[kq])
                    S.op('dve', lambda e: e.tensor_reduce(out=sm[q][:, 0, :nsb], in_=g43(gs[q]), axis=AX.X, op=ALU.max),
                         reads=[kq], writes=[kq])
                    S.op('dve', lambda e: e.tensor_tensor(out=g43(gm_[q]), in0=g43(gs[q]), in1=bc_last(sm[q][:, 0, :nsb], 4),
                                                          op=ALU.is_equal), reads=[kq], writes=[kq])
                    S.op('dve', lambda e: e.tensor_tensor(out=g4(gs[q]), in0=g4(gm_[q]), in1=g4(m2[q]), op=ALU.mult), reads=[kq], writes=[kq])
                    S.op('dve', lambda e: e.tensor_reduce(out=sm[q][:, 1, :nsb], in_=g43(gs[q]), axis=AX.X, op=ALU.add),
                         reads=[kq], writes=[kq])
                    S.op('dve', lambda e: e.tensor_tensor(out=v3(mk[q]), in0=v3(ssel[q]), in1=bc_last(sm[q][:, 1, :nsb], 32),
                                                          op=ALU.is_ge), reads=[kq], writes=[kq])
                    S.op('dve', lambda e: e.tensor_tensor(out=v8(mk[q]), in0=v8(mk[q]), in1=bc_last(g4(gm_[q]), 8), op=ALU.mult),
                         reads=[kq], writes=[kq])
                    S.op('dve', lambda e: e.tensor_tensor(out=f2(em[q]), in0=f2(mk[q]), in1=f2(sg[q]), op=ALU.mult),
                         reads=[kq], writes=[kq])
                    S.op('dve', lambda e: e.tensor_reduce(out=sm[q][:, 2, :nsb], in_=v3(em[q]), axis=AX.X, op=ALU.add),
                         reads=[kq], writes=[kq])
                    S.op('dve', lambda e: e.reciprocal(out=sm[q][:, 3, :nsb], in_=sm[q][:, 2, :nsb]), reads=[kq], writes=[kq])
                    S.op('dve', lambda e: e.tensor_tensor(out=v3(em[q]), in0=v3(em[q]), in1=bc_last(sm[q][:, 3, :nsb], 32), op=ALU.mult),
                         reads=[kq], writes=[kq])

                    def mmk(e):
                        for s in range(nsb):
                            o_ = PS[pl][:, 128 + s * 32:128 + (s + 1) * 32]
                            e.matmul(o_, lhsT=utri[:], rhs=mk[q][:, s, :], start=True, stop=False)
                            for s2 in range(s):
                                e.matmul(o_, lhsT=ones32[:], rhs=mk[q][:, s2, :], start=False, stop=False)
                            ins = e.matmul(o_, lhsT=ones32[:], rhs=cntm[:], start=False, stop=True)
                        return ins
                    S.op('pe', mmk, reads=[kq, 'utri', 'ones32', 'cntm'], writes=[pk(pl)])
                    S.op('dve', lambda e: e.tensor_copy(out=f2(rk[q]), in_=PS[pl][:, 128:128 + W_]), reads=[pk(pl)], writes=[kq])
                    S.op('dve', lambda e: e.tensor_reduce(out=t32[q][:], in_=mk[q][:, :nsb, :].rearrange("p s e -> p e s"), axis=AX.X,
                                                          op=ALU.add), reads=[kq], writes=[kq])
                    S.op('dve', lambda e: e.tensor_tensor(out=cntm[:], in0=cntm[:], in1=t32[q][:], op=ALU.add),
                         reads=[kq, 'cntm'], writes=['cntm'])
                    S.op('dve', lambda e: e.tensor_tensor_scan(out=f2(cmv[q]), data0=f2(onesb), data1=f2(mk[q]), initial=0.0,
                                                               op0=ALU.mult, op1=ALU.add), reads=[kq, 'onesb'], writes=[kq])
                    for k2 in range(2):
                        fm_ = FM[:, gs0:gs0 + nsb, k2, :]
                        S.op('dve', lambda e: e.tensor_tensor(out=v3(t3[q]), in0=v3(cmv[q]), in1=c12[:, k2, :nsb, :], op=ALU.is_equal),
                             reads=[kq, 'c12'], writes=[kq])
                        S.op('dve', lambda e: e.tensor_tensor(out=fm_, in0=v3(t3[q]), in1=v3(mk[q]), op=ALU.mult),
                             reads=[kq], writes=['FM'])
                        S.op('dve', lambda e: e.tensor_tensor(out=v3(t3[q]), in0=fm_, in1=v3(rk[q]), op=ALU.mult),
                             reads=[kq, 'FM'], writes=[kq])
                        S.op('dve', lambda e: e.tensor_reduce(out=RK[:, gs0:gs0 + nsb, k2], in_=v3(t3[q]), axis=AX.X, op=ALU.add),
                             reads=[kq], writes=['RK'])
                        S.op('dve', lambda e: e.tensor_tensor(out=v3(t3[q]), in0=fm_, in1=v3(em[q]), op=ALU.mult),
                             reads=[kq, 'FM'], writes=[kq])
                        S.op('dve', lambda e: e.tensor_reduce(out=WK[:, gs0:gs0 + nsb, k2], in_=v3(t3[q]), axis=AX.X, op=ALU.add),
                             reads=[kq], writes=['WK'])

                stage_W(tiles[0])
                for i_, ti in enumerate(tiles):
                    stage_N(ti)
                    if i_ + 1 < len(tiles):
                        stage_W(tiles[i_ + 1])
                    stage_R(ti)
                S.barrier()
            wst = contextlib.ExitStack()
            WST[l] = (wst,
                      [sb(wst, f"wgs{i}", [128, 4096], BF16) for i in range(4)],
                      [sb(wst, f"wus{i}", [128, 4096], BF16) for i in range(4)],
                      [sb(wst, f"wds{i}", [128, 4096], BF16) for i in range(4)])
            with contextlib.ExitStack() as ph:
                J = nsub_tot
                cb = sb(ph, "cb", [128, 32], F32)
                nblk = sb(ph, "nblk", [128, 32], F32)
                pend = sb(ph, "pend", [128, 32], F32)
                pst = sb(ph, "pst", [128, 32], F32)
                big = sb(ph, "bigc", [128, 32, NBMAX], F32)
                eb = sb(ph, "eb", [128, NBMAX], F32)
                chg = sb(ph, "chg", [128, NBMAX], F32)
                wif = sb(ph, "wif", [128, NBMAX, 2], F32)
                dtmp2 = sb(ph, "dtmp2", [128, MAXSUB, 32], F32)
                dif = sb(ph, "dif", [128, MAXSUB, 2], F32)
                rows = [sb(ph, f"rows{i}", [128, 1024], BF16) for i in range(4)]
                S.op('pe', lambda e: e.matmul(PS[0][:, 0:32], lhsT=ones32[:], rhs=cntm[:], start=True, stop=True),
                     reads=['ones32', 'cntm'], writes=[pk(0)])
                S.op('dve', lambda e: e.tensor_copy(out=cb[:], in_=PS[0][:, 0:32]), reads=[pk(0)], writes=['cb'])
                S.op('dve', lambda e: e.tensor_tensor(out=big[:, :, :J], in0=bc_last(cb[:], J), in1=bc_mid(blkoff[:, :J], 32),
                                                      op=ALU.is_gt), reads=['cb', 'blkoff'], writes=['big'])
                S.op('dve', lambda e: e.tensor_reduce(out=nblk[:], in_=big[:, :, :J], axis=AX.X, op=ALU.add),
                     reads=['big'], writes=['nblk'])
                S.op('dve', lambda e: e.tensor_tensor_scan(out=pend[:], data0=ones_row[:], data1=nblk[:], initial=0.0,
                                                           op0=ALU.mult, op1=ALU.add), reads=['nblk', 'ones_row'], writes=['pend'])
                S.op('dve', lambda e: e.tensor_tensor(out=pst[:], in0=pend[:], in1=nblk[:], op=ALU.subtract),
                     reads=['pend', 'nblk'], writes=['pst'])
                S.op('dve', lambda e: e.tensor_scalar(out=pst[:], in0=pst[:], scalar1=128.0, scalar2=None, op0=ALU.mult),
                     reads=['pst'], writes=['pst'])
                S.op('dve', lambda e: e.tensor_scalar(out=pend[:], in0=pend[:], scalar1=128.0, scalar2=None, op0=ALU.mult),
                     reads=['pend'], writes=['pend'])
                S.op('dve', lambda e: e.tensor_tensor(out=big[:, :, :NB].rearrange("p e b -> p b e"),
                                                      in0=bc_mid(pend[:], NB), in1=bc_last(blkoff[:, :NB], 32),
                                                      op=ALU.is_le), reads=['pend', 'blkoff', 'big'], writes=['big'])
                S.op('dve', lambda e: e.tensor_reduce(out=eb[:, :NB], in_=big[:, :, :NB].rearrange("p e b -> p b e"),
                                                      axis=AX.X, op=ALU.add), reads=['big'], writes=['eb'])
                S.op('dve', lambda e: e.tensor_scalar(out=eb[:, :NB], in0=eb[:, :NB], scalar1=31.0, scalar2=None, op0=ALU.min),
                     reads=['eb'], writes=['eb'])
                S.op('dve', lambda e: e.memset(chg[:], 1.0), writes=['chg'])
                S.op('dve', lambda e: e.tensor_tensor(out=chg[:, 1:NB], in0=eb[:, 1:NB], in1=eb[:, 0:NB - 1], op=ALU.not_equal),
                     reads=['eb', 'chg'], writes=['chg'])
                for k4 in range(1, 4):
                    S.op('dve', lambda e: e.memset(chg[:, k4 * (NB // 4):k4 * (NB // 4) + 1], 1.0), reads=['chg'], writes=['chg'])
                for h in range(1):
                    S.op('dve', lambda e: e.tensor_scalar(out=wif[:, :NB, h], in0=eb[:, :NB], scalar1=128.0,
                                                          scalar2=float(l * 4096 - BIGW), op0=ALU.mult, op1=ALU.add),
                         reads=['eb', 'wif'], writes=['wif'])
                    S.op('dve', lambda e: e.tensor_scalar(out=wif[:, :NB, h], in0=wif[:, :NB, h], scalar1=pcol[:, 0:1],
                                                          scalar2=None, op0=ALU.add), reads=['wif', 'pcol'], writes=['wif'])
                    S.op('dve', lambda e: e.tensor_tensor(out=wif[:, :NB, h], in0=wif[:, :NB, h], in1=chg[:, :NB], op=ALU.mult),
                         reads=['wif', 'chg'], writes=['wif'])
                    S.op('dve', lambda e: e.tensor_scalar(out=wif[:, :NB, h], in0=wif[:, :NB, h], scalar1=float(BIGW),
                                                          scalar2=None, op0=ALU.add), reads=['wif'], writes=['wif'])
                S.op('dve', lambda e: e.tensor_copy(out=WI[:, :NB, 0:1], in_=wif[:, :NB, 0:1]), reads=['wif'], writes=['WI'])
                for k2 in range(2):
                    S.op('dve', lambda e: e.tensor_tensor(out=dtmp2[:, :J, :], in0=FM[:, :J, k2, :], in1=bc_mid(pst[:], J),
                                                          op=ALU.mult), reads=['FM', 'pst', 'dtmp2'], writes=['dtmp2'])
                    S.op('dve', lambda e: e.tensor_reduce(out=dif[:, :J, k2], in_=dtmp2[:, :J, :], axis=AX.X, op=ALU.add),
                         reads=['dtmp2', 'dif'], writes=['dif'])
                S.op('dve', lambda e: e.tensor_tensor(out=dif[:, :J, :], in0=dif[:, :J, :], in1=RK[:, :J, :], op=ALU.add),
                     reads=['dif', 'RK'], writes=['dif'])
                S.op('dve', lambda e: e.tensor_copy(out=DI[:, :J, :], in_=dif[:, :J, :]), reads=['dif'], writes=['DI'])
                for p_ in range(3):
                    moe_loadw(l, NB, p_)
                for gsi in range(J):
                    q = gsi % 4
                    S.dma('sp', rows[q][:], h2tm[gsi * 128:(gsi + 1) * 128, :], reads=[('h2tm', gsi % 4)], writes=[('rows', q)])
                    for k2 in range(2):
                        idma(Xs[:, :], bass.IndirectOffsetOnAxis(ap=DI[:, gsi, k2:k2 + 1], axis=0), rows[q][:, :], None,
                             NB * 128 - 1, reads=[('rows', q), 'DI', 'Xs'], writes=[('Xsc', q)], slot=('Xsc', q))
                S.barrier()

        def phase_D(l, tiles, final):
            nsub_tot = sum(TILES[ti][1] // 128 for ti in tiles)
            NB = 2 * nsub_tot + 32
            with contextlib.ExitStack() as ph:
                _, wgs, wus, wds = WST[l]
                blk = lambda p_: moe_blk(NB, p_)
                xbs = [sb(ph, f"xbs{i}", [128, 1024], BF16) for i in range(2)]
                XTs = [sb(ph, f"XTs{i}", [128, 8, 128], BF16) for i in range(2)]
                s1s = [sb(ph, f"s1s{i}", [128, 512], F32) for i in range(2)]
                ATs = [sb(ph, f"ATs{i}", [128, 4, 128], BF16) for i in range(2)]
                Yts = [sb(ph, f"Yts{i}", [128, 1024], F32) for i in range(2)]

                loadw = lambda p_: moe_loadw(l, NB, p_)
                def xbload(p_):
                    bn = blk(p_)
                    S.dma('sp', xbs[p_ % 2][:], Xs[bn * 128:(bn + 1) * 128, :], reads=[('Xsc', 0), ('Xsc', 1), ('Xsc', 2), ('Xsc', 3), 'Xs'],
                          writes=[('xbs', p_ % 2)])

                def stage_T(p_):
                    q = p_ % 2
                    xb, XT = xbs[q], XTs[q]
                    ptb = PS[q].bitcast(BF16)

                    def mmt(e):
                        for c in range(8):
                            ins = e.transpose(out=ptb[:, c * 128:(c + 1) * 128], in_=xb[:, c:1024:8], identity=identb[:])
                        return ins
                    S.op('pe', mmt, reads=[('xbs', q), 'identb'], writes=[pk(q)])
                    S.op('act', lambda e: e.activation(out=XT[:].rearrange("p c s -> p (c s)"), in_=ptb[:, :], func=AF.Identity),
                         reads=[pk(q)], writes=[('XTs', q)])

                def stage_G(p_):
                    q = p_ % 2
                    ws_ = p_ % 4
                    XT, AT = XTs[q], ATs[q]
                    wg_, wu_ = wgs[ws_], wus[ws_]
                    p1, p2 = 2 + 2 * q, 3 + 2 * q

                    def mmg(e, wt, p):
                        for fo in range(4):
                            for c in range(8):
                                c0 = c * 512 + fo
                                ins = e.matmul(PS[p][:, fo * 128:(fo + 1) * 128], lhsT=wt[:, c0:c0 + 509:4], rhs=XT[:, c, :],
                                               start=(c == 0), stop=(c == 7))
                        return ins
                    S.op('pe', lambda e: mmg(e, wg_, p1), reads=[('wgs', ws_), ('XTs', q)], writes=[pk(p1)])
                    S.op('pe', lambda e: mmg(e, wu_, p2), reads=[('wus', ws_), ('XTs', q)], writes=[pk(p2)])
                    S.op('act', lambda e: e.activation(out=s1s[q][:], in_=PS[p1][:, :], func=AF.Silu),
                         reads=[pk(p1)], writes=[('s1s', q)])
                    S.op('dve', lambda e: e.tensor_tensor(out=AT[:].rearrange("p c s -> p (c s)"), in0=s1s[q][:], in1=PS[p2][:, :],
                                                          op=ALU.mult), reads=[('s1s', q), pk(p2)], writes=[('ATs', q)])

                def stage_D(p_):
                    q = p_ % 2
                    ws_ = p_ % 4
                    b = blk(p_)
                    AT, Yt, wd_ = ATs[q], Yts[q], wds[ws_]
                    for dh in range(2):
                        py = 6 + dh

                        def mmd(e):
                            for fo in range(4):
                                ins = e.matmul(PS[py][:, :], lhsT=AT[:, fo, :],
                                               rhs=wd_[:, fo * 1024 + dh * 512:fo * 1024 + (dh + 1) * 512],
                                               start=(fo == 0), stop=(fo == 3))
                            return ins
                        S.op('pe', mmd, reads=[('wds', ws_), ('ATs', q)], writes=[pk(py)])
                        if dh == 0:
                            S.op('act', lambda e: e.activation(out=Yt[:, 0:512], in_=PS[py][:, :], func=AF.Identity),
                                 reads=[pk(py)], writes=[('Yts', q)])
                        else:
                            S.op('dve', lambda e: e.tensor_copy(out=Yt[:, 512:1024], in_=PS[py][:, :]),
                                 reads=[pk(py)], writes=[('Yts', q)])
                    S.dma('sp', Ys[b * 128:(b + 1) * 128, :], Yt[:], reads=[('Yts', q)], writes=[('Ys', q)])

                xbload(0)
                xbload(1)
                stage_T(0)
                for pos in range(NB):
                    if pos + 3 < NB:
                        loadw(pos + 3)
                    stage_G(pos)
                    if pos + 1 < NB:
                        stage_T(pos + 1)
                    if pos + 2 < NB:
                        xbload(pos + 2)
                    stage_D(pos)
                S.barrier()
            WST[l][0].close()
            with contextlib.ExitStack() as ph3:
                xts = [sb(ph3, f"xtd{i}", [128, 8, 512], F32) for i in range(2)]
                g1 = [sb(ph3, f"g1{i}", [128, 1024], F32) for i in range(4)]
                g2 = [sb(ph3, f"g2{i}", [128, 1024], F32) for i in range(4)]
                tmp = norm_tmp(ph3) if final else None
                ots = [sb(ph3, f"otd{i}", [128, 8, 512], F32) for i in range(2)] if final else None
                subs = []
                for li, ti in enumerate(tiles):
                    for s_ in range(TILES[ti][1] // 128):
                        subs.append((li, ti, s_, len(subs)))

                def stage_a(li, ti, s, gsi):
                    off, cnt, _, _, r = TILES[ti]
                    b = li % 2
                    if s == 0:
                        S.dma('sp', xts[b][:, :, :cnt], xr[:, :, off:off + cnt].rearrange("j p t -> p j t"),
                              reads=[('xr', ti)], writes=[('xtd', b, j) for j in range(8)])
                    q = gsi % 4
                    idma(g1[q][:, :], None, Ys[:, :], bass.IndirectOffsetOnAxis(ap=DI[:, gsi, 0:1], axis=0),
                         NB * 128 - 1, reads=['DI', ('Ys', 0), ('Ys', 1)], writes=[('g1', q)], slot=('g1', q))
                    idma(g2[q][:, :], None, Ys[:, :], bass.IndirectOffsetOnAxis(ap=DI[:, gsi, 1:2], axis=0),
                         NB * 128 - 1, reads=['DI', ('Ys', 0), ('Ys', 1)], writes=[('g2', q)], slot=('g2', q))
                    S.op('dve', lambda e: e.tensor_scalar(out=g1[q][:], in0=g1[q][:], scalar1=WK[:, gsi, 0:1], scalar2=None,
                                                          op0=ALU.mult), reads=[('g1', q), 'WK'], writes=[('g1', q)])
                    S.op('dve', lambda e: e.scalar_tensor_tensor(out=g1[q][:], in0=g2[q][:], scalar=WK[:, gsi, 1:2],
                                                                 in1=g1[q][:], op0=ALU.mult, op1=ALU.add),
                         reads=[('g1', q), ('g2', q), 'WK'], writes=[('g1', q)])
                    for half in range(2):
                        pt = 2 * (q % 2) + half

                        def mmt2(e):
                            for jj in range(4):
                                j = half * 4 + jj
                                ins = e.transpose(out=PS[pt][:, jj * 128:(jj + 1) * 128], in_=g1[q][:, j * 128:(j + 1) * 128],
                                                  identity=ident[:])
                            return ins
                        S.op('pe', mmt2, reads=[('g1', q), 'ident'], writes=[pk(pt)])

                def stage_b(li, ti, s, gsi):
                    off, cnt, _, _, r = TILES[ti]
                    b = li % 2
                    x_ = xts[b]
                    q = gsi % 4
                    kxs = [('xtd', b, j) for j in range(8)]
                    for half in range(2):
                        pt = 2 * (q % 2) + half
                        for jj in range(4):
                            j = half * 4 + jj
                            S.op('dve', lambda e: e.scalar_tensor_tensor(out=x_[:, j, s * 128:(s + 1) * 128],
                                                                         in0=PS[pt][:, jj * 128:(jj + 1) * 128],
                                                                         scalar=mod_ap(l, 5, j, r),
                                                                         in1=x_[:, j, s * 128:(s + 1) * 128],
                                                                         op0=ALU.mult, op1=ALU.add),
                                 reads=[pk(pt), kxs[j], ('modT', l)], writes=[kxs[j]])
                    if s == cnt // 128 - 1:
                        if not final:
                            S.dma('sp', xr[:, :, off:off + cnt].rearrange("j p t -> p j t"), x_[:, :, :cnt],
                                  reads=kxs, writes=[('xr', ti)])
                            if dbg:
                                S.dma('sp', dbgo["d_x2"][:, :, off:off + cnt].rearrange("j p t -> p j t"), x_[:, :, :cnt],
                                      reads=kxs, writes=['d_x2'])
                        else:
                            o_ = ots[b]
                            norm_mod(tmp, x_, cnt, kxs, lambda j: o_[:, j, :cnt], ('otd', b),
                                     lambda j: fing[:, j:j + 1], None, 7, extra_reads=['fing'])
                            S.dma('sp', outT[:, :, off:off + cnt].rearrange("j p t -> p j t"), o_[:, :, :cnt],
                                  reads=[('otd', b)], writes=[('out', b)])

                for idx in range(len(subs) + 1):
                    if idx < len(subs):
                        stage_a(*subs[idx])
                    if idx >= 1:
                        stage_b(*subs[idx - 1])
                S.barrier()

        phase_C(0, w_out, list(range(9)), xc)
        phase_D(0, list(range(9)), final=False)

        hst = contextlib.ExitStack()
        hbuf = sb(hst, "hbuf1", [128, 8, NT], BF16)
        phase_A(1, xr, hbuf)
        with contextlib.ExitStack() as ph:
            cost = sb(ph, "cost", [128, NT], F32)
            sint = sb(ph, "sint", [128, NT], F32)
            dal = sb(ph, "dal", [128, 4, 64], F32)
            dtmp = sb(ph, "dtmp", [128, 64], F32)
            lamv = sb(ph, "lamv", [128, 4], F32)
            subg = sb(ph, "subg", [128, 1], F32)
            wq = sb(ph, "wq", [128, 8, 128], BF16)
            wk = sb(ph, "wk", [128, 8, 128], BF16)
            wv = sb(ph, "wv", [128, 8, 128], BF16)
            qbt = [sb(ph, f"qbt{i}", [128, 512], BF16) for i in range(2)]
            rmb = sb(ph, "rmb", [128, 128], BF16)
            QT = sb(ph, "QT", [128, NQ], BF16)
            vtb = [sb(ph, f"vtb{i}", [128, 512], BF16) for i in range(2)]
            KT = sb(ph, "KT", [128, NT], BF16)
            Vt = sb(ph, "Vt", [128, 34, 128], BF16)
            rt1 = [sb(ph, f"rt1{i}", [128, 512], F32) for i in range(2)]
            rt2 = [sb(ph, f"rt2{i}", [128, 512], F32) for i in range(2)]
            Eb2 = [sb(ph, f"Eb{i}", [128, 2, 512], BF16) for i in range(2)]
            acc2 = sb(ph, "acc2", [128, 2, 512], F32)
            obw = sb(ph, "obw", [128, 2, 512], F32)
            ob = [obw[:, 0, :], obw[:, 1, :]]
            rlw = sb(ph, "rlw", [128, 2, 512], F32)
            rl = sb(ph, "rl", [128, 512], F32)
            accD = sb(ph, "accD", [128, 512], F32)
            accP = sb(ph, "accP", [128, 512], F32)
            osq = sb(ph, "osq", [128, 512], F32)
            aot = [sb(ph, f"aot{i}", [128, 512], BF16) for i in range(2)]
            S.dma('sp', cost[:], cos_in, writes=['cos'])
            S.dma('pool', rmb[:], rmat_in, writes=['rmb'])
            S.dma('sp', sint[:], sin_in, writes=['sin'])
            S.dma('sp', dal[:], dalam_in, writes=['dal'])
            S.dma('sp', subg[:], subg_in, writes=['subg'])
            for i2 in range(2):
                S.op('dve', lambda e: e.tensor_tensor(out=dtmp[:], in0=dal[:, 2 * i2, :], in1=dal[:, 2 * i2 + 1, :], op=ALU.mult),
                     reads=['dal'], writes=['dtmp'])
                S.op('dve', lambda e: e.tensor_reduce(out=lamv[:, i2:i2 + 1], in_=dtmp[:], axis=AX.X, op=ALU.add),
                     reads=['dtmp'], writes=['lamv'])
            S.op('act', lambda e: e.activation(out=lamv[:, 0:2], in_=lamv[:, 0:2], func=AF.Exp), reads=['lamv'], writes=['lamv'])
            S.op('dve', lambda e: e.tensor_tensor(out=lamv[:, 2:3], in0=lamv[:, 1:2], in1=lamv[:, 0:1], op=ALU.subtract),
                 reads=['lamv'], writes=['lamv'])
            S.op('dve', lambda e: e.tensor_scalar(out=lamv[:, 2:3], in0=lamv[:, 2:3], scalar1=-LAMBDA_INIT, scalar2=None,
                                                  op0=ALU.add), reads=['lamv'], writes=['lamv'])
            S.op('dve', lambda e: e.tensor_scalar(out=lamv[:, 3:4], in0=subg[:], scalar1=1.0 - LAMBDA_INIT, scalar2=None,
                                                  op0=ALU.mult), reads=['lamv', 'subg'], writes=['lamv'])
            nkc = 34
            acnt = 0
            for hh in range(8):
                for t_, c0, k_ in [(wq, hh * 128, 'wq'), (wk, 1024 + hh * 128, 'wk'), (wv, 2048 + hh * 128, 'wv')]:
                    S.dma('pool', t_[:], w_qkv[:, c0:c0 + 128].rearrange("(k p) f -> p k f", p=128), writes=[k_])
                convert_experts(1, 4 * hh, 4 * hh + 4)
                jobs = []
                for ti in range(9):
                    jobs.append(('v', ti))
                    if ti < 4:
                        jobs.append(('q', ti))
                    jobs.append(('k', ti))

                def proj_s1(kj, kind, ti):
                    off, cnt = TILES[ti][0], TILES[ti][1]
                    q = kj % 2
                    pa_ = 2 * q
                    wt, kw_ = {'q': (wq, 'wq'), 'k': (wk, 'wk'), 'v': (wv, 'wv')}[kind]

                    def mmp(e):
                        for kc in range(8):
                            ins = e.matmul(PS[pa_][:, :cnt], lhsT=wt[:, kc, :], rhs=hbuf[:, kc, off:off + cnt],
                                           start=(kc == 0), stop=(kc == 7))
                        return ins
                    S.op('pe', mmp, reads=[kw_, ('h', ti)], writes=[pk(pa_)])
                    S.op('act', lambda e: e.activation(out=qbt[q][:, :cnt], in_=PS[pa_][:, :cnt], func=AF.Identity),
                         reads=[pk(pa_)], writes=[('qbt', q)])

                def proj_s2(kj, kind, ti):
                    off, cnt = TILES[ti][0], TILES[ti][1]
                    q = kj % 2
                    pa_, pb_ = 2 * q, 2 * q + 1
                    if kind == 'v':
                        nsb = cnt // 128
                        pbb = PS[pb_].bitcast(BF16)

                        def mmvt(e):
                            for s in range(nsb):
                                ins = e.transpose(out=pbb[:, s * 128:(s + 1) * 128], in_=qbt[q][:, s * 128:(s + 1) * 128],
                                                  identity=identb[:])
                            return ins
                        S.op('pe', mmvt, reads=[('qbt', q), 'identb'], writes=[pk(pb_)])
                        si0 = off // 128
                        S.op('dve', lambda e: e.tensor_copy(out=Vt[:, si0:si0 + nsb, :].rearrange("p s v -> p (s v)"),
                                                            in_=pbb[:, :nsb * 128]), reads=[pk(pb_)], writes=['Vt'])
                        return
                    dst, kdst = (QT, 'QT') if kind == 'q' else (KT, 'KT')
                    S.op('pe', lambda e: e.matmul(PS[pb_][:, :cnt], lhsT=rmb[:], rhs=qbt[q][:, :cnt], start=True, stop=True),
                         reads=['rmb', ('qbt', q)], writes=[pk(pb_)])
                    S.op('dve', lambda e: e.tensor_tensor(out=rt1[q][:, :cnt], in0=PS[pa_][:, :cnt], in1=cost[:, off:off + cnt],
                                                          op=ALU.mult), reads=[pk(pa_), 'cos', ('qbt', q)], writes=[('rt1', q)])
                    S.op('dve', lambda e: e.tensor_tensor(out=rt2[q][:, :cnt], in0=PS[pb_][:, :cnt], in1=sint[:, off:off + cnt],
                                                          op=ALU.mult), reads=[pk(pb_), 'sin'], writes=[('rt2', q)])
                    S.op('dve', lambda e: e.tensor_tensor(out=dst[:, off:off + cnt], in0=rt1[q][:, :cnt], in1=rt2[q][:, :cnt],
                                                          op=ALU.add), reads=[('rt1', q), ('rt2', q)], writes=[kdst])

                proj_s1(0, *jobs[0])
                for kj in range(len(jobs)):
                    if kj + 1 < len(jobs):
                        proj_s1(kj + 1, *jobs[kj + 1])
                    proj_s2(kj, *jobs[kj])
                for qt in range(4):
                    q0 = qt * 512
                    pend_ = None
                    for kc in range(nkc + 1):
                        if kc < nkc:
                            sidx = acnt % 2
                            acnt += 1
                            sb0 = 2 * sidx
                            for mi in range(2):
                                lo_, hi_ = mi * 64, (mi + 1) * 64
                                S.op('pe', lambda e: e.matmul(PS[sb0 + mi][:, :], lhsT=KT[lo_:hi_, kc * 128:(kc + 1) * 128],
                                                              rhs=QT[lo_:hi_, q0:q0 + 512], start=True, stop=True),
                                     reads=['KT', 'QT'], writes=[pk(sb0 + mi)])
                            ke = ('Eb', sidx)
                            S.op('act', lambda e: e.activation(out=Eb2[sidx][:].rearrange("p m q -> p (m q)"),
                                                               in_=psbig[:, sb0 * 512:(sb0 + 2) * 512], func=AF.Exp, scale=0.125),
                                 reads=[pk(sb0), pk(sb0 + 1)], writes=[ke])
                            cur = (Eb2[sidx], ke)
                        if pend_ is not None:
                            kp, (ebp, kep) = pend_
                            for mi in range(2):
                                S.op('pe', lambda e: e.matmul(PS[4 + mi][:, :], lhsT=Vt[:, kp, :], rhs=ebp[:, mi, :], start=(kp == 0),
                                                              stop=(kp == nkc - 1)), reads=['Vt', kep], writes=[pk(4 + mi)])
                            accv = psbig[:, 6 * 512:8 * 512]
                            ebf = ebp[:].rearrange("p m q -> p (m q)")
                            if kp == 0:
                                S.op('dve', lambda e: e.tensor_copy(out=accv, in_=ebf), reads=[kep], writes=[pk(6), pk(7)])
                            else:
                                S.op('dve', lambda e: e.tensor_tensor(out=accv, in0=accv, in1=ebf, op=ALU.add),
                                     reads=[kep, pk(6), pk(7)], writes=[pk(6), pk(7)])
                        pend_ = (kc, cur) if kc < nkc else None
                    S.op('act', lambda e: e.activation(out=acc2[:].rearrange("p m q -> p (m q)"), in_=psbig[:, 6 * 512:8 * 512],
                                                       func=AF.Identity), reads=[pk(6), pk(7)], writes=['acc2'])
                    for mi in range(2):
                        S.op('pe', lambda e: e.matmul(PS[mi][:, :], lhsT=ones32[:], rhs=acc2[:, mi, :], start=True, stop=True),
                             reads=['ones32', 'acc2'], writes=[pk(mi)])
                    rlf = rlw[:].rearrange("p m q -> p (m q)")
                    S.op('act', lambda e: e.activation(out=rlf, in_=psbig[:, 0:1024], func=AF.Ln), reads=[pk(0), pk(1)], writes=['rlw'])
                    S.op('act', lambda e: e.activation(out=rlf, in_=rlf, func=AF.Exp, scale=-1.0), reads=['rlw'], writes=['rlw'])
                    S.op('dve', lambda e: e.tensor_tensor(out=obw[:].rearrange("p m q -> p (m q)"), in0=psbig[:, 4 * 512:6 * 512],
                                                          in1=rlf, op=ALU.mult),
                         reads=[pk(4), pk(5), 'rlw'], writes=[('ob', 0), ('ob', 1)])
                    S.op('dve', lambda e: e.scalar_tensor_tensor(out=ob[0][:], in0=ob[1][:], scalar=lamv[:, 2:3], in1=ob[0][:],
                                                                 op0=ALU.mult, op1=ALU.add),
                         reads=[('ob', 0), ('ob', 1), 'lamv'], writes=[('ob', 0)])
                    S.op('act', lambda e: e.activation(out=osq[:], in_=ob[0][:], func=AF.Square), reads=[('ob', 0)], writes=['osq'])
                    S.op('pe', lambda e: e.matmul(PS[2][:, :], lhsT=ones32[:], rhs=osq[:], start=True, stop=True),
                         reads=['ones32', 'osq'], writes=[pk(2)])
                    S.op('act', lambda e: e.activation(out=osq[:], in_=PS[2][:, :], func=AF.Ln, bias=eps_t[:, 0:1],
                                                       scale=1.0 / 128.0), reads=[pk(2), 'eps'], writes=['osq'])
                    S.op('act', lambda e: e.activation(out=osq[:], in_=osq[:], func=AF.Exp, scale=-0.5), reads=['osq'], writes=['osq'])
                    a_ = aot[qt % 2]
                    S.op('dve', lambda e: e.scalar_tensor_tensor(out=a_[:], in0=ob[0][:], scalar=lamv[:, 3:4], in1=osq[:],
                                                                 op0=ALU.mult, op1=ALU.mult),
                         reads=[('ob', 0), 'osq', 'lamv'], writes=[('aot', qt % 2)])
                    S.dma('sp', zscr[hh, :, q0:q0 + 512], a_[:], reads=[('aot', qt % 2)], writes=[('zs', qt)])
            S.barrier()
        hst.close()

        phase_C(1, w_o, list(range(4)), xr)
        phase_D(1, list(range(4)), final=True)
        S.barrier()
    return nc


def _prep_shared(inp, rev):
    dsl = slice(None, None, -1) if rev else slice(None)
    f = lambda a: np.ascontiguousarray(np.asarray(a, dtype=np.float32))
    pj = lambda v: f(np.asarray(v).reshape(8, 128).T)
    sh = {}
    sh["w_mod"] = f(inp["w_mod"])
    sh["bmod"] = f(np.asarray(inp["b_mod"]).reshape(2, 48, 128).transpose(2, 0, 1))
    sh["n1g"] = f(np.asarray(inp["norm1_g"]).reshape(2, 8, 128).transpose(2, 0, 1))
    sh["n2g"] = f(np.asarray(inp["norm2_g"]).reshape(2, 8, 128).transpose(2, 0, 1))
    sh["fing"] = pj(inp["final_g"])
    sh["w_in"] = f(inp["rg_w_in"][0])
    cw = np.asarray(inp["rg_conv_w"][0])
    z1 = np.zeros((1, 1024), np.float32)
    cw5 = np.concatenate([cw, z1], 0) if not rev else np.concatenate([z1, cw[::-1]], 0)
    sh["convw"] = f(cw5.reshape(5, 8, 128).transpose(2, 1, 0))
    sh["convb"] = pj(inp["rg_conv_b"][0])
    sh["gaw"] = f(np.asarray(inp["rg_gate_a_w"][0])[dsl])
    sh["gxw"] = f(np.asarray(inp["rg_gate_x_w"][0])[dsl])
    sh["gab"] = f(np.asarray(inp["rg_gate_a_b"][0])[dsl].reshape(2, 8, 128).transpose(2, 0, 1))
    sh["gxb"] = f(np.asarray(inp["rg_gate_x_b"][0])[dsl].reshape(2, 8, 128).transpose(2, 0, 1))
    sh["lam"] = f(np.asarray(inp["rg_lambda"][0])[dsl].reshape(2, 8, 128).transpose(2, 0, 1))
    sh["w_out"] = f(inp["rg_w_out"][0])
    sh["w_qkv"] = f(inp["da_w_qkv"][0])
    sh["dalam"] = f(np.broadcast_to(np.asarray(inp["da_lambda"][0])[None], (128, 4, 64)))
    sh["subg"] = f(np.asarray(inp["da_subln_g"][0]).reshape(128, 1))
    sh["w_o"] = f(inp["da_w_o"][0])
    sh["rw"] = f(np.asarray(inp["router_w"]).reshape(8, 128, 32).transpose(1, 0, 2))
    sh["rb"] = f(np.broadcast_to(np.asarray(inp["router_bias"])[None], (128, 32)))
    sh["wg"] = f(inp["moe_w_gate"])
    sh["wu"] = f(inp["moe_w_up"])
    sh["wd"] = f(inp["moe_w_down"])
    t = np.arange(NX)
    row = (t // 64).astype(np.float32)
    col = (t % 64).astype(np.float32)
    inv = (1.0 / (10000.0 ** (np.arange(16, dtype=np.float32) / 16))).astype(np.float32)
    ang = np.stack([row, col], -1)[:, :, None] * inv
    ang = np.broadcast_to(ang[:, :, None, :], (NX, 2, 2, 16)).reshape(NX, 64).astype(np.float32)
    cos = np.ones((128, NT), np.float32)
    sin = np.zeros((128, NT), np.float32)
    if rev:
        ang = ang[::-1]
    cos[:, :NX] = np.tile(np.cos(ang).T, (2, 1))
    sin[:, :NX] = np.tile(np.sin(ang).T, (2, 1))
    sh["pcol"] = np.arange(128, dtype=np.float32).reshape(128, 1)
    sh["blkoff"] = np.ascontiguousarray(np.broadcast_to((128.0 * np.arange(100, dtype=np.float32))[None], (128, 100)))
    rm = np.zeros((128, 128), np.float32)
    for m in range(128):
        if (m % 32) < 16:
            rm[m + 16, m] = -1.0
        else:
            rm[m - 16, m] = 1.0
    sh["rmat"] = rm
    sh["cos"] = cos
    sh["sin"] = sin
    return sh


def _prep_core(inp, b, rev):
    f = lambda a: np.ascontiguousarray(np.asarray(a, dtype=np.float32))
    x = np.asarray(inp["x"][b])
    ctx = np.asarray(inp["ctx"][b])
    if rev:
        x = x[::-1]
        ctx = ctx[::-1]
    tok = np.concatenate([x, ctx], axis=0)
    d = {"xc": f(tok.T.reshape(8, 128, NT))}
    cond = np.stack([np.asarray(inp["c"][b]), np.asarray(inp["c_ctx"])], -1)
    d["cond"] = f(cond.reshape(8, 128, 2).transpose(1, 0, 2))
    return d


def kernel(**inputs):
    nc = build(DEBUG)
    shs = [_prep_shared(inputs, False), _prep_shared(inputs, True)]
    in_maps = []
    for core in range(8):
        b, rev = core // 2, core % 2
        m = dict(shs[rev])
        m.update(_prep_core(inputs, b, bool(rev)))
        in_maps.append(m)
    res = run_bass_kernel_spmd(nc, in_maps, core_ids=list(range(8)))
    out = np.empty((4, NX, 1024), np.float32)
    for core in range(8):
        b, rev = core // 2, core % 2
        o = np.asarray(res.results[core]["outT"]).reshape(1024, NQ).T
        if rev:
            out[b, NX - NQ:] = o[::-1]
        else:
            out[b, :NQ] = o
    return out
```

```python
import contextlib
import math
import numpy as np
import concourse.bass as bass
import concourse.mybir as mybir
from concourse.bass_utils import run_bass_kernel_spmd

F32 = mybir.dt.float32
BF16 = mybir.dt.bfloat16
ALU = mybir.AluOpType
AF = mybir.ActivationFunctionType
AX = mybir.AxisListType

EPS = 1e-6
NT = 4352
NX = 4096
NCTX = 256
TILES = [(i * 512, 512, 2 + i * 512, i * 512, 0) for i in range(8)] + [(4096, 256, 4102, 4100, 1)]
UW = 4360
UCW = 4356
NQ = 2048
LAMBDA_INIT = 0.8 - 0.6 * math.exp(-0.3 * 1)
DEBUG = False


class Sched:
    ROT = 30000

    def __init__(self, nc, stack):
        self.nc = nc
        self.stack = stack
        self.engs = {'pe': nc.tensor, 'act': nc.scalar, 'dve': nc.vector,
                     'pool': nc.gpsimd, 'sp': nc.sync}
        self.cur = {}
        self.nsem = 0
        for e in self.engs:
            self.cur[e] = [self._newsem(e), 0]
        self.lastw = {}
        self.readers = {}
        self.waited = {e: {} for e in self.engs}
        self.dsem = {}
        self.alltok = {}

    def _newsem(self, nm):
        self.nsem += 1
        return self.stack.enter_context(self.nc.semaphore(f"s_{nm}_{self.nsem}"))

    def _wait(self, eng, toks):
        best = {}
        for (s, v) in toks:
            k = id(s)
            if k not in best or best[k][1] < v:
                best[k] = (s, v)
        for k, (s, v) in best.items():
            if self.waited[eng].get(k, 0) >= v:
                continue
            self.engs[eng].wait_ge(s, v)
            self.waited[eng][k] = v

    def _deps(self, eng, reads, writes, skip_same_pe=True):
        toks = []
        for k in reads:
            if k in self.lastw:
                toks.append(self.lastw[k])
        for k in writes:
            if k in self.lastw:
                toks.append(self.lastw[k])
            toks.extend(self.readers.get(k, ()))
        if eng == 'pe' and skip_same_pe:
            toks = [t for t in toks if t[0] is not self.cur['pe'][0]]
        return toks

    def _commit(self, tok, reads, writes):
        for k in writes:
            self.lastw[k] = tok
            self.readers[k] = []
        for k in reads:
            if k in writes:
                continue
            self.readers.setdefault(k, []).append(tok)
        self.alltok[id(tok[0])] = tok

    def op(self, eng, fn, reads=(), writes=()):
        self._wait(eng, self._deps(eng, reads, writes))
        c = self.cur[eng]
        if c[1] >= self.ROT:
            c[0] = self._newsem(eng)
            c[1] = 0
        ins = fn(self.engs[eng])
        c[1] += 1
        ins.then_inc(c[0], 1)
        self._commit((c[0], c[1]), reads, writes)

    def dma(self, eng, out, in_, reads=(), writes=(), slot=None, **kw):
        if slot is None:
            slot = ('auto',) + tuple(writes)
        self._wait(eng, self._deps(eng, reads, writes, skip_same_pe=False))
        if slot not in self.dsem:
            self.dsem[slot] = [self._newsem('d'), 0]
        d = self.dsem[slot]
        ins = self.engs[eng].dma_start(out=out, in_=in_, **kw)
        d[1] += 16
        ins.then_inc(d[0], 16)
        self._commit((d[0], d[1]), reads, writes)

    def barrier(self):
        toks = list(self.alltok.values())
        for e in self.engs:
            self._wait(e, toks)


def bc_last(a, n):
    return bass.AP(a.tensor, a.offset, [list(x) for x in a.ap] + [[0, n]])


def bc_mid(a, n):
    l = [list(x) for x in a.ap]
    return bass.AP(a.tensor, a.offset, [l[0], [0, n]] + l[1:])


def build(dbg=False):
    nc = bass.Bass("TRN2", target_bir_lowering=False)

    def din(name, shape, dt=F32):
        return nc.dram_tensor(name, list(shape), dt, kind="ExternalInput").ap()

    xc = din("xc", [8, 128, NT])
    cond_in = din("cond", [128, 8, 2])
    w_mod = din("w_mod", [2, 1024, 6144])
    bmod_in = din("bmod", [128, 2, 48])
    n1g_in = din("n1g", [128, 2, 8])
    n2g_in = din("n2g", [128, 2, 8])
    fing_in = din("fing", [128, 8])
    w_in = din("w_in", [1024, 2048])
    convw_in = din("convw", [128, 8, 5])
    convb_in = din("convb", [128, 8])
    gaw = din("gaw", [2, 8, 128, 128])
    gxw = din("gxw", [2, 8, 128, 128])
    gab_in = din("gab", [128, 2, 8])
    gxb_in = din("gxb", [128, 2, 8])
    lam_in = din("lam", [128, 2, 8])
    w_out = din("w_out", [1024, 1024])
    w_qkv = din("w_qkv", [1024, 3072])
    dalam_in = din("dalam", [128, 4, 64])
    subg_in = din("subg", [128, 1])
    w_o = din("w_o", [1024, 1024])
    rw_in = din("rw", [128, 8, 32])
    rb_in = din("rb", [128, 32])
    wg = din("wg", [2, 32, 1024, 512])
    wu = din("wu", [2, 32, 1024, 512])
    wd = din("wd", [2, 32, 512, 1024])
    rmat_in = din("rmat", [128, 128])
    cos_in = din("cos", [128, NT])
    sin_in = din("sin", [128, NT])
    outT = nc.dram_tensor("outT", [8, 128, NQ], F32, kind="ExternalOutput").ap()
    xr = nc.dram_tensor("xr", [8, 128, NT], F32, kind="Internal").ap()
    zscr = nc.dram_tensor("zscr", [8, 128, NT], BF16, kind="Internal").ap()
    h2tm = nc.dram_tensor("h2tm", [NT, 1024], BF16, kind="Internal").ap()
    Xs = nc.dram_tensor("Xs", [100 * 128, 1024], BF16, kind="Internal").ap()
    Ys = nc.dram_tensor("Ys", [100 * 128, 1024], F32, kind="Internal").ap()
    wgb = nc.dram_tensor("wgb", [2 * 8192, 2048], BF16, kind="Internal").ap()
    wub = nc.dram_tensor("wub", [2 * 8192, 2048], BF16, kind="Internal").ap()
    wdb = nc.dram_tensor("wdb", [2 * 8192, 2048], BF16, kind="Internal").ap()
    pcol_in = din("pcol", [128, 1])
    blkoff_in = din("blkoff", [128, 100])
    dbgo = {}
    if dbg:
        for nm, shp in [("d_mod", [128, 2 * 48 * 2]), ("d_h", [128, 8, NT]), ("d_z", [8, 128, NT]),
                        ("d_x1", [8, 128, NT]), ("d_x2", [8, 128, NT]),
                        ("d_ao", [8, 128, NT])]:
            dbgo[nm] = nc.dram_tensor(nm, shp, F32, kind="ExternalOutput").ap()

    with contextlib.ExitStack() as st:
        S = Sched(nc, st)

        uid = [0]

        def sb(stack, name, shape, dt):
            uid[0] += 1
            return stack.enter_context(nc.sbuf_tensor(f"sb{uid[0]}_{name}", list(shape), dt))

        psbig = st.enter_context(nc.psum_tensor("psbig", [128, 8 * 512], F32))
        PS = [psbig[:, i * 512:(i + 1) * 512] for i in range(8)]
        pk = lambda i: ('ps', i)
        wgv_all = wg.rearrange("l e (q r) f -> (l e q) (r f)", r=4)
        wuv_all = wu.rearrange("l e (q r) f -> (l e q) (r f)", r=4)
        wdv_all = wd.rearrange("l e (q r) d -> (l e q) (r d)", r=2)

        def convert_experts(l, e0, e1):
            for e_ in range(e0, e1):
                r0 = (l * 32 + e_) * 256
                for dst, src in [(wgb, wgv_all), (wub, wuv_all), (wdb, wdv_all)]:
                    S.dma('pool', dst[r0:r0 + 256, :], src[r0:r0 + 256, :], writes=[('wcv', l)])

        ones_bf = sb(st, "ones_bf", [128, 128], BF16)
        ones32 = sb(st, "ones32", [128, 128], F32)
        ident = sb(st, "ident", [128, 128], F32)
        identb = sb(st, "identb", [128, 128], BF16)
        modT = sb(st, "modT", [128, 2, 48, 2], F32)
        gmT = sb(st, "gmT", [128, 2, 2, 8, 2], F32)
        n1g = sb(st, "n1g", [128, 2, 8], F32)
        n2g = sb(st, "n2g", [128, 2, 8], F32)
        fing = sb(st, "fing", [128, 8], F32)
        rw = sb(st, "rw", [128, 8, 32], F32)
        rb = sb(st, "rb", [128, 32], F32)
        S.op('dve', lambda e: e.memset(ones_bf[:], 1.0), writes=['ones_bf'])
        S.op('dve', lambda e: e.memset(ones32[:], 1.0), writes=['ones32'])
        S.op('pool', lambda e: e.memset(ident[:], 1.0), writes=['ident'])
        S.op('pool', lambda e: e.affine_select(out=ident[:], in_=ident[:], pattern=[[-1, 128]],
                                               compare_op=ALU.is_equal, fill=0.0, base=0, channel_multiplier=1),
             reads=['ident'], writes=['ident'])
        S.op('act', lambda e: e.activation(out=identb[:], in_=ident[:], func=AF.Identity), reads=['ident'], writes=['identb'])
        for t, src, k in [(n1g, n1g_in, 'n1g'), (n2g, n2g_in, 'n2g'), (fing, fing_in, 'fing'),
                          (rw, rw_in, 'rw'), (rb, rb_in, 'rb')]:
            S.dma('sp', t[:], src, writes=[k])

        with contextlib.ExitStack() as ph:
            condt = sb(ph, "condt", [128, 8, 2], F32)
            scond = sb(ph, "scond", [128, 8, 2], F32)
            bm = sb(ph, "bm", [128, 2, 48], F32)
            wm = [sb(ph, f"wm{i}", [128, 8, 1024], F32) for i in range(2)]
            S.dma('sp', condt[:], cond_in, writes=['cond'])
            S.dma('sp', bm[:], bmod_in, writes=['bm'])
            S.op('act', lambda e: e.activation(out=scond[:], in_=condt[:], func=AF.Silu),
                 reads=['cond'], writes=['scond'])
            it = 0
            for l in range(2):
                for gi in range(6):
                    buf = wm[it % 2]
                    key = ('wm', it % 2)
                    S.dma('sp', buf[:], w_mod[l, :, gi * 1024:(gi + 1) * 1024].rearrange("(k p) f -> p k f", p=128),
                          writes=[key])
                    pst = PS[it % 2]

                    def mm(e, buf=buf, pst=pst):
                        for j in range(8):
                            for kc in range(8):
                                ins = e.matmul(pst[:, j * 2:(j + 1) * 2], lhsT=buf[:, kc, j * 128:(j + 1) * 128],
                                               rhs=scond[:, kc, :], start=(kc == 0), stop=(kc == 7))
                        return ins
                    S.op('pe', mm, reads=[key, 'scond'], writes=[pk(it % 2)])
                    S.op('dve', lambda e: e.tensor_tensor(
                        out=modT[:, l, gi * 8:(gi + 1) * 8, :],
                        in0=pst[:, 0:16].rearrange("p (j r) -> p j r", r=2),
                        in1=bc_last(bm[:, l, gi * 8:(gi + 1) * 8], 2), op=ALU.add),
                        reads=[pk(it % 2), 'bm'], writes=['modT'])
                    it += 1
            for l in range(2):
                for w_, (gt, gk, sidx) in enumerate([(n1g, 'n1g', 1), (n2g, 'n2g', 4)]):
                    S.op('dve', lambda e: e.tensor_scalar(out=gmT[:, l, w_], in0=modT[:, l, sidx * 8:(sidx + 1) * 8, :],
                                                          scalar1=1.0, scalar2=None, op0=ALU.add),
                         reads=['modT'], writes=['gmT'])
                    S.op('dve', lambda e: e.tensor_tensor(out=gmT[:, l, w_], in0=gmT[:, l, w_],
                                                          in1=bc_last(gt[:, l, :], 2), op=ALU.mult),
                         reads=['gmT', gk], writes=['gmT'])
            if dbg:
                S.dma('sp', dbgo["d_mod"], modT[:].rearrange("p a b c -> p (a b c)"), reads=['modT'], writes=['d_mod'])
            S.barrier()

        def mod_ap(l, idx, j, r):
            return modT[:, l, idx * 8 + j, r:r + 1]

        def norm_mod(tmp, xt, n, kx, out, kout, gm_of_j, sh_of_j, psi, extra_reads=(), tag='', part=0):
            sq, rt, rstd, xn = tmp
            kxl = list(kx) if isinstance(kx, list) else [kx]
            if part in (0, 1):
                norm_stats(sq, rt, rstd, xt, n, kxl, psi, tag)
            if part in (0, 2):
                norm_apply(rstd, xn, xt, n, kxl, out, kout, gm_of_j, sh_of_j, extra_reads, tag)

        def norm_stats(sq, rt, rstd, xt, n, kxl, psi, tag):
            S.op('act', lambda e: e.activation(out=sq[:, :, :n], in_=xt[:, :, :n], func=AF.Square),
                 reads=kxl, writes=['nm_sq' + tag])

            def mm(e):
                for j in range(8):
                    ins = e.matmul(PS[psi][:, :n], lhsT=ones_bf[:], rhs=sq[:, j, :n], start=(j == 0), stop=(j == 7))
                return ins
            S.op('pe', mm, reads=['nm_sq' + tag, 'ones_bf'], writes=[pk(psi)])
            S.op('act', lambda e: e.activation(out=rt[:, :n], in_=PS[psi][:, :n], func=AF.Ln,
                                               bias=eps_t[:, 0:1], scale=1.0 / 1024.0),
                 reads=[pk(psi), 'eps'], writes=['nm_rt' + tag])
            S.op('act', lambda e: e.activation(out=rstd[:, :n], in_=rt[:, :n], func=AF.Exp, scale=-0.5),
                 reads=['nm_rt' + tag], writes=['nm_rstd' + tag])

        def norm_apply(rstd, xn, xt, n, kxl, out, kout, gm_of_j, sh_of_j, extra_reads, tag):
            S.op('dve', lambda e: e.tensor_tensor(out=xn[:, :, :n], in0=xt[:, :, :n], in1=bc_mid(rstd[:, :n], 8),
                                                  op=ALU.mult), reads=kxl + ['nm_rstd' + tag], writes=['nm_xn' + tag])
            for j in range(8):
                if sh_of_j is None:
                    S.op('dve', lambda e: e.tensor_scalar(out=out(j), in0=xn[:, j, :n], scalar1=gm_of_j(j),
                                                          scalar2=None, op0=ALU.mult),
                         reads=['nm_xn' + tag] + list(extra_reads), writes=[kout])
                elif j % 2 == 0:
                    S.op('dve', lambda e: e.tensor_scalar(out=out(j), in0=xn[:, j, :n], scalar1=gm_of_j(j),
                                                          scalar2=sh_of_j(j), op0=ALU.mult, op1=ALU.add),
                         reads=['nm_xn' + tag] + list(extra_reads), writes=[kout])
                else:
                    S.op('act', lambda e: e.activation(out=out(j), in_=xn[:, j, :n], func=AF.Identity,
                                                       bias=sh_of_j(j), scale=gm_of_j(j)),
                         reads=['nm_xn' + tag] + list(extra_reads), writes=[kout])

        def norm_tmp(ph):
            return (sb(ph, "nm_sq", [128, 8, 512], BF16), sb(ph, "nm_rt", [128, 512], F32),
                    sb(ph, "nm_rstd", [128, 512], F32), sb(ph, "nm_xn", [128, 8, 512], F32))

        eps_t = sb(st, "eps_t", [128, 1], F32)
        S.op('dve', lambda e: e.memset(eps_t[:], EPS), writes=['eps'])
        one_t = sb(st, "one_t", [128, 1], F32)
        S.op('dve', lambda e: e.memset(one_t[:], 1.0), writes=['one'])


        def phase_A(l, src, hbuf):
            with contextlib.ExitStack() as ph:
                xts = [sb(ph, f"xt{i}", [128, 8, 512], F32) for i in range(2)]
                tmps = [norm_tmp(ph), norm_tmp(ph)]

                def a_part(ti, part):
                    off, cnt, _, _, r = TILES[ti]
                    xt = xts[ti % 2]
                    kx = ('xt', ti % 2)
                    if part == 1:
                        S.dma('sp', xt[:, :, :cnt], src[:, :, off:off + cnt].rearrange("j p t -> p j t"),
                              reads=[('xr', ti)], writes=[kx])
                    norm_mod(tmps[ti % 2], xt, cnt, kx, lambda j: hbuf[:, j, off:off + cnt], ('h', ti),
                             lambda j: gmT[:, l, 0, j, r:r + 1], lambda j: mod_ap(l, 0, j, r), 7 - (ti % 2),
                             extra_reads=['gmT', 'modT'], tag='A' + str(ti % 2), part=part)
                a_part(0, 1)
                for ti in range(len(TILES)):
                    if ti + 1 < len(TILES):
                        a_part(ti + 1, 1)
                    a_part(ti, 2)
                S.barrier()

        hst = contextlib.ExitStack()
        hbuf = sb(hst, "hbuf", [128, 8, NT], BF16)
        phase_A(0, xc, hbuf)
        if dbg:
            with contextlib.ExitStack() as ph:
                t32 = sb(ph, "dbg32", [128, 8, 512], F32)
                for ti, (off, cnt, _, _, r) in enumerate(TILES):
                    S.op('dve', lambda e: e.tensor_copy(out=t32[:, :, :cnt], in_=hbuf[:, :, off:off + cnt]),
                         reads=[('h', ti)], writes=['dbg32'])
                    S.dma('sp', dbgo["d_h"][:, :, off:off + cnt], t32[:, :, :cnt], reads=['dbg32'], writes=['d_h'])
                S.barrier()

        with contextlib.ExitStack() as ph:
            ub = sb(ph, "ub", [128, UW], F32)
            uc = sb(ph, "uc", [128, UCW], F32)
            ucb = sb(ph, "ucb", [128, UCW], BF16)
            gy = sb(ph, "gy", [128, NT], BF16)
            wyu = [sb(ph, f"wyu{i}", [128, 8, 256], BF16) for i in range(2)]
            gw = [sb(ph, f"gw{i}", [128, 4, 128], BF16) for i in range(2)]
            convw = sb(ph, "convw", [128, 8, 5], F32)
            convb = sb(ph, "convb", [128, 8], F32)
            gab = sb(ph, "gab", [128, 2, 8], F32)
            gxb = sb(ph, "gxb", [128, 2, 8], F32)
            lamt = sb(ph, "lamt", [128, 2, 8], F32)
            cneg = sb(ph, "cneg", [128, 2, 8], F32)
            cneg2 = sb(ph, "cneg2", [128, 2, 8], F32)
            rbuf = [sb(ph, f"rbuf{i}", [128, 512], F32) for i in range(2)]
            ibuf = [sb(ph, f"ibuf{i}", [128, 512], F32) for i in range(2)]
            sbuf_ = [sb(ph, f"sbuf{i}", [128, 512], F32) for i in range(2)]
            hbt = [sb(ph, f"hbt{i}", [128, 512], F32) for i in range(2)]
            gt1 = [sb(ph, f"gt1{i}", [128, 512], F32) for i in range(2)]
            zt = [sb(ph, f"zt{i}", [128, 512], BF16) for i in range(2)]
            for t, src, k in [(convw, convw_in, 'convw'), (convb, convb_in, 'convb'), (gab, gab_in, 'gab'),
                              (gxb, gxb_in, 'gxb'), (lamt, lam_in, 'lamt')]:
                S.dma('sp', t[:], src, writes=[k])
            S.op('act', lambda e: e.activation(out=cneg[:], in_=lamt[:], func=AF.Exp, scale=-1.0),
                 reads=['lamt'], writes=['cneg'])
            S.op('act', lambda e: e.activation(out=cneg[:], in_=cneg[:], func=AF.Ln, bias=one_t[:, 0:1], scale=1.0),
                 reads=['cneg', 'one'], writes=['cneg'])
            S.op('dve', lambda e: e.tensor_scalar(out=cneg2[:], in0=cneg[:], scalar1=-16.0, scalar2=None, op0=ALU.mult),
                 reads=['cneg'], writes=['cneg2'])
            S.op('dve', lambda e: e.tensor_scalar(out=cneg[:], in0=cneg[:], scalar1=-8.0, scalar2=None, op0=ALU.mult),
                 reads=['cneg', 'cneg2'], writes=['cneg'])
            zer = sb(ph, "zer", [128, 4, 1024], BF16)
            S.op('pool', lambda e: e.memset(zer[:], 0.0), writes=['zer'])
            ngab = sb(ph, "ngab", [128, 2, 8], F32)
            ngxb = sb(ph, "ngxb", [128, 2, 8], F32)
            S.op('dve', lambda e: e.tensor_scalar(out=ngab[:], in0=gab[:], scalar1=-1.0, scalar2=None, op0=ALU.mult),
                 reads=['gab'], writes=['ngab'])
            S.op('dve', lambda e: e.tensor_scalar(out=ngxb[:], in0=gxb[:], scalar1=-1.0, scalar2=None, op0=ALU.mult),
                 reads=['gxb'], writes=['ngxb'])
            S.op('dve', lambda e: e.memset(ub[:], 0.0), writes=[('ub', ti) for ti in range(9)])
            ubkeys = [('ub', ti) for ti in range(9)]
            cnt_sc = 0
            for n in range(8):
                w = wyu[n % 2]
                kw_ = ('wyu', n % 2)
                S.dma('pool', w[:, :, 0:128], w_in[:, n * 128:(n + 1) * 128].rearrange("(k p) f -> p k f", p=128),
                      writes=[kw_])
                S.dma('pool', w[:, :, 128:256],
                      w_in[:, 1024 + n * 128:1024 + (n + 1) * 128].rearrange("(k p) f -> p k f", p=128), writes=[kw_])
                g = gw[n % 2]
                kg = ('gw', n % 2)
                for d in range(2):
                    S.dma('pool', g[:, 2 * d, :], gaw[d, n], writes=[kg])
                    S.dma('pool', g[:, 2 * d + 1, :], gxw[d, n], writes=[kg])
                convert_experts(0, 4 * n, 4 * n + 4)
                for b4 in range(4 * n, min(4 * n + 4, 25)):
                    S.dma('pool', Xs[b4 * 512:(b4 + 1) * 512, :].rearrange("(a p) f -> p a f", p=128), zer[:],
                          reads=['zer'], writes=['Xs'])
                for ti, (off, cnt, uoff, ucoff, r) in enumerate(TILES):
                    pu, py = (ti % 2) * 2, (ti % 2) * 2 + 1

                    def mmu(e, c0=128, p=pu):
                        for kc in range(8):
                            ins = e.matmul(PS[p][:, :cnt], lhsT=w[:, kc, c0:c0 + 128], rhs=hbuf[:, kc, off:off + cnt],
                                           start=(kc == 0), stop=(kc == 7))
                        return ins
                    S.op('pe', mmu, reads=[kw_, ('h', ti)], writes=[pk(pu)])
                    S.op('act', lambda e: e.activation(out=ub[:, uoff:uoff + cnt], in_=PS[pu][:, :cnt], func=AF.Identity),
                         reads=[pk(pu)], writes=[('ub', ti)])
                    S.op('pe', lambda e: mmu(e, 0, py), reads=[kw_, ('h', ti)], writes=[pk(py)])
                    t1 = gt1[ti % 2]
                    k1 = ('gt1', ti % 2)
                    S.op('act', lambda e: e.activation(out=t1[:, :cnt], in_=PS[py][:, :cnt], func=AF.Square),
                         reads=[pk(py)], writes=[k1])
                    S.op('dve', lambda e: e.tensor_scalar(out=t1[:, :cnt], in0=t1[:, :cnt], scalar1=0.044715, scalar2=1.0,
                                                          op0=ALU.mult, op1=ALU.add), reads=[k1], writes=[k1])
                    S.op('dve', lambda e: e.tensor_tensor(out=t1[:, :cnt], in0=t1[:, :cnt], in1=PS[py][:, :cnt], op=ALU.mult),
                         reads=[k1, pk(py)], writes=[k1])
                    S.op('act', lambda e: e.activation(out=t1[:, :cnt], in_=t1[:, :cnt], func=AF.Sigmoid,
                                                       scale=1.5957691216057308), reads=[k1], writes=[k1])
                    S.op('dve', lambda e: e.tensor_tensor(out=gy[:, off:off + cnt], in0=t1[:, :cnt], in1=PS[py][:, :cnt],
                                                          op=ALU.mult), reads=[k1, pk(py)], writes=[('gy', ti)])
                S.op('dve', lambda e: e.tensor_scalar(out=uc[:], in0=ub[:, 0:UCW], scalar1=convw[:, n, 0:1],
                                                      scalar2=convb[:, n:n + 1], op0=ALU.mult, op1=ALU.add),
                     reads=ubkeys + ['convw', 'convb'], writes=['uc'])
                for k in range(1, 5):
                    S.op('dve', lambda e: e.scalar_tensor_tensor(out=uc[:], in0=ub[:, k:k + UCW], scalar=convw[:, n, k:k + 1],
                                                                 in1=uc[:], op0=ALU.mult, op1=ALU.add),
                         reads=ubkeys + ['convw', 'uc'], writes=['uc'])
                S.op('act', lambda e: e.activation(out=ucb[:], in_=uc[:], func=AF.Identity), reads=['uc'], writes=['ucb'])
                for d in range(2):
                    order = [8] + (list(range(8)) if d == 0 else list(range(7, -1, -1)))
                    prev = None
                    for oi, ti in enumerate(order):
                        off, cnt, uoff, ucoff, r = TILES[ti]
                        b = cnt_sc % 2
                        cnt_sc += 1
                        pr, pi = 4 + 2 * b, 5 + 2 * b
                        S.op('pe', lambda e: e.matmul(PS[pr][:, :cnt], lhsT=g[:, 2 * d, :], rhs=ucb[:, ucoff:ucoff + cnt],
                                                      start=True, stop=True), reads=[kg, 'ucb'], writes=[pk(pr)])
                        S.op('pe', lambda e: e.matmul(PS[pi][:, :cnt], lhsT=g[:, 2 * d + 1, :], rhs=ucb[:, ucoff:ucoff + cnt],
                                                      start=True, stop=True), reads=[kg, 'ucb'], writes=[pk(pi)])
                        rb_, ib_, sb_, hb_ = rbuf[b], ibuf[b], sbuf_[b], hbt[b]
                        kr, ki, ks, kh = ('rbuf', b), ('ibuf', b), ('sbuf', b), ('hbt', b)
                        S.op('act', lambda e: e.activation(out=rb_[:, :cnt], in_=PS[pr][:, :cnt], func=AF.Exp,
                                                           bias=ngab[:, d, n:n + 1], scale=-1.0),
                             reads=[pk(pr), 'ngab'], writes=[kr])
                        S.op('act', lambda e: e.activation(out=ib_[:, :cnt], in_=PS[pi][:, :cnt], func=AF.Exp,
                                                           bias=ngxb[:, d, n:n + 1], scale=-1.0),
                             reads=[pk(pi), 'ngxb'], writes=[ki])
                        for t_, k_ in ((rb_, kr), (ib_, ki)):
                            S.op('act', lambda e: e.activation(out=t_[:, :cnt], in_=t_[:, :cnt], func=AF.Ln,
                                                               bias=one_t[:, 0:1], scale=1.0), reads=[k_, 'one'], writes=[k_])
                            S.op('act', lambda e: e.activation(out=t_[:, :cnt], in_=t_[:, :cnt], func=AF.Exp, scale=-1.0),
                                 reads=[k_], writes=[k_])
                        S.op('act', lambda e: e.activation(out=sb_[:, :cnt], in_=rb_[:, :cnt], func=AF.Exp,
                                                           scale=cneg2[:, d, n:n + 1]), reads=[kr, 'cneg2'], writes=[ks])
                        S.op('act', lambda e: e.activation(out=rb_[:, :cnt], in_=rb_[:, :cnt], func=AF.Exp,
                                                           scale=cneg[:, d, n:n + 1]), reads=[kr, 'cneg'], writes=[kr])
                        S.op('act', lambda e: e.activation(out=sb_[:, :cnt], in_=sb_[:, :cnt], func=AF.Ln,
                                                           bias=one_t[:, 0:1], scale=-1.0), reads=[ks, 'one'], writes=[ks])
                        S.op('act', lambda e: e.activation(out=sb_[:, :cnt], in_=sb_[:, :cnt], func=AF.Exp, scale=0.5),
                             reads=[ks], writes=[ks])
                        S.op('dve', lambda e: e.tensor_tensor(out=ib_[:, :cnt], in0=ib_[:, :cnt],
                                                              in1=uc[:, ucoff:ucoff + cnt], op=ALU.mult),
                             reads=[ki, 'uc'], writes=[ki])
                        S.op('dve', lambda e: e.tensor_tensor(out=ib_[:, :cnt], in0=ib_[:, :cnt], in1=sb_[:, :cnt],
                                                              op=ALU.mult), reads=[ki, ks], writes=[ki])
                        if d == 0:
                            if prev is None:
                                init, kin = 0.0, []
                            else:
                                po, pc, puo, _, _ = TILES[prev]
                                init, kin = ub[:, puo + pc - 1:puo + pc], [('ub', prev)]
                            S.op('dve', lambda e: e.tensor_tensor_scan(out=ub[:, uoff:uoff + cnt], data0=rb_[:, :cnt],
                                                                       data1=ib_[:, :cnt], initial=init,
                                                                       op0=ALU.mult, op1=ALU.add),
                                 reads=[kr, ki] + kin, writes=[('ub', ti)])
                        else:
                            if prev is None:
                                init, kin = 0.0, []
                            else:
                                init, kin = hbt[1 - b][:, 0:1], [('hbt', 1 - b)]
                            S.op('dve', lambda e: e.tensor_tensor_scan(out=hb_[:, :cnt][:, ::-1], data0=rb_[:, :cnt][:, ::-1],
                                                                       data1=ib_[:, :cnt][:, ::-1], initial=init,
                                                                       op0=ALU.mult, op1=ALU.add),
                                 reads=[kr, ki] + kin, writes=[kh])
                            S.op('dve', lambda e: e.tensor_tensor(out=sb_[:, :cnt], in0=hb_[:, :cnt],
                                                                  in1=ub[:, uoff:uoff + cnt], op=ALU.add),
                                 reads=[kh, ('ub', ti)], writes=[ks])
                            z_ = zt[b]
                            S.op('dve', lambda e: e.tensor_tensor(out=z_[:, :cnt], in0=sb_[:, :cnt],
                                                                  in1=gy[:, off:off + cnt], op=ALU.mult),
                                 reads=[ks, ('gy', ti)], writes=[('zt', b)])
                            S.dma('sp', zscr[n, :, off:off + cnt], z_[:, :cnt], reads=[('zt', b)], writes=[('zs', ti)])
                        prev = ti
            S.barrier()
        hst.close()

        I32 = mybir.dt.int32
        MAXSUB = 34
        NBMAX = 100
        BIGW = 2 * 32 * 256 + 64
        utri = sb(st, "utri", [128, 128], F32)
        S.op('pool', lambda e: e.memset(utri[:], 1.0), writes=['utri'])
        S.op('pool', lambda e: e.affine_select(out=utri[:], in_=utri[:], pattern=[[1, 128]],
                                               compare_op=ALU.is_gt, fill=0.0, base=0, channel_multiplier=-1),
             reads=['utri'], writes=['utri'])
        pcol = sb(st, "pcol", [128, 1], F32)
        blkoff = sb(st, "blkoff", [128, NBMAX], F32)
        ones_row = sb(st, "ones_row", [128, 32], F32)
        S.dma('sp', pcol[:], pcol_in, writes=['pcol'])
        S.dma('sp', blkoff[:], blkoff_in, writes=['blkoff'])
        S.op('dve', lambda e: e.memset(ones_row[:], 1.0), writes=['ones_row'])
        onesb = sb(st, "onesb", [128, 4, 32], F32)
        c12 = sb(st, "c12", [128, 2, 4, 32], F32)
        S.op('dve', lambda e: e.memset(onesb[:], 1.0), writes=['onesb'])
        for k2_ in range(2):
            for s_ in range(4):
                S.op('dve', lambda e: e.memset(c12[:, k2_, s_, :], float(2 * s_ + 1 + k2_)), writes=['c12'])
        FM = sb(st, "FM", [128, MAXSUB, 2, 32], F32)
        RK = sb(st, "RK", [128, MAXSUB, 2], F32)
        WK = sb(st, "WK", [128, MAXSUB, 2], F32)
        DI = sb(st, "DI", [128, MAXSUB, 2], I32)
        WI = sb(st, "WI", [128, NBMAX, 2], I32)
        cntm = sb(st, "cntm", [128, 32], F32)

        bregs = {}

        def idma(out, out_off, in_, in_off, bounds, reads, writes, slot):
            S._wait('pool', S._deps('pool', reads, writes, skip_same_pe=False))
            if slot not in S.dsem:
                S.dsem[slot] = [S._newsem('d'), 0]
            d = S.dsem[slot]
            if bounds not in bregs:
                bregs[bounds] = nc.gpsimd.to_reg(bounds)
            ins = nc.gpsimd.indirect_dma_start(out=out, out_offset=out_off, in_=in_, in_offset=in_off,
                                               bounds_check=bregs[bounds], oob_is_err=False)
            d[1] += 16
            ins.then_inc(d[0], 16)
            S._commit((d[0], d[1]), reads, writes)

        WST = {}
        wgv = wgb.rearrange("(a b) f -> a (b f)", b=2)
        wuv = wub.rearrange("(a b) f -> a (b f)", b=2)
        wdv = wdb.rearrange("(a b) f -> a (b f)", b=2)

        def moe_blk(NB, p_):
            return (p_ % 4) * (NB // 4) + p_ // 4

        def moe_loadw(l, NB, p_):
            _, wgs, wus, wds = WST[l]
            w_ = p_ % 4
            for nm, view, tile_ in [('wgs', wgv, wgs[w_]), ('wus', wuv, wus[w_]), ('wds', wdv, wds[w_])]:
                idma(tile_[:, :], None, view[:, :], bass.IndirectOffsetOnAxis(ap=WI[:, moe_blk(NB, p_), 0:1], axis=0),
                     (l + 1) * 4096 - 1, reads=['WI', ('wcv', l)], writes=[(nm, w_)], slot=(nm, w_))

        def phase_C(l, wmat, tiles, xsrc):
            nsub_tot = sum(TILES[ti][1] // 128 for ti in tiles)
            NB = 2 * nsub_tot + 32
            with contextlib.ExitStack() as ph:
                wsb = sb(ph, "wsb", [128, 8, 1024], BF16)
                zts = [sb(ph, f"zts{i}", [128, 8, 512], BF16) for i in range(2)]
                xts = [sb(ph, f"xtc{i}", [128, 8, 512], F32) for i in range(2)]
                h2fs = [sb(ph, f"h2f{i}", [128, 8, 512], F32) for i in range(2)]
                h2t = [sb(ph, f"h2t{i}", [128, 1024], BF16) for i in range(2)]
                tmps = [norm_tmp(ph), norm_tmp(ph)]
                mk3 = lambda nm: [sb(ph, f"{nm}{i}", [128, 4, 32], F32) for i in range(2)]
                ssel, sg, em, mk, cmv, rk, t3 = mk3("ssel"), mk3("sg"), mk3("em"), mk3("mk"), mk3("cmv"), mk3("rk"), mk3("t3")
                mk16 = lambda nm: [sb(ph, f"{nm}{i}", [128, 16], F32) for i in range(2)]
                m1, m2, gs, gm_ = mk16("m1"), mk16("m2"), mk16("gs"), mk16("gmk")
                sm = [sb(ph, f"sm{i}", [128, 4, 4], F32) for i in range(2)]
                t32 = [sb(ph, f"t32{i}", [128, 32], F32) for i in range(2)]
                S.dma('pool', wsb[:], wmat.rearrange("(k p) f -> p k f", p=128), writes=['wsb'])
                S.op('dve', lambda e: e.memset(cntm[:], 0.0), writes=['cntm'])
                nsub_box = [0]

                def stage_W(ti):
                    off, cnt, _, _, r = TILES[ti]
                    b = ti % 2
                    z_, x_ = zts[b], xts[b]
                    h2f, tmp, kh2 = h2fs[b], tmps[b], ('h2f', b)
                    kz, kx = ('zts', b), ('xtc', b)
                    S.dma('sp', z_[:, :, :cnt], zscr[:, :, off:off + cnt].rearrange("j p t -> p j t"),
                          reads=[('zs', ti)], writes=[kz])
                    S.dma('sp', x_[:, :, :cnt], xsrc[:, :, off:off + cnt].rearrange("j p t -> p j t"),
                          reads=[('xr', ti)], writes=[kx])
                    for j in range(8):
                        p = j % 3

                        def mm(e):
                            for kc in range(8):
                                ins = e.matmul(PS[p][:, :cnt], lhsT=wsb[:, kc, j * 128:(j + 1) * 128], rhs=z_[:, kc, :cnt],
                                               start=(kc == 0), stop=(kc == 7))
                            return ins
                        S.op('pe', mm, reads=['wsb', kz], writes=[pk(p)])
                        S.op('dve', lambda e: e.scalar_tensor_tensor(out=x_[:, j, :cnt], in0=PS[p][:, :cnt],
                                                                     scalar=mod_ap(l, 2, j, r), in1=x_[:, j, :cnt],
                                                                     op0=ALU.mult, op1=ALU.add),
                             reads=[pk(p), kx, 'modT'], writes=[kx])
                    S.dma('sp', xr[:, :, off:off + cnt].rearrange("j p t -> p j t"), x_[:, :, :cnt],
                          reads=[kx], writes=[('xr', ti)])
                    if dbg and l == 0:
                        S.dma('sp', dbgo["d_x1"][:, :, off:off + cnt].rearrange("j p t -> p j t"), x_[:, :, :cnt],
                              reads=[kx], writes=['d_x1'])

                def stage_N(ti):
                    off, cnt, _, _, r = TILES[ti]
                    b = ti % 2
                    z_, x_ = zts[b], xts[b]
                    h2f, tmp, kh2 = h2fs[b], tmps[b], ('h2f', b)
                    kz, kx = ('zts', b), ('xtc', b)
                    norm_mod(tmp, x_, cnt, kx, lambda j: h2f[:, j, :cnt], kh2,
                             lambda j: gmT[:, l, 1, j, r:r + 1], lambda j: mod_ap(l, 3, j, r), 3,
                             extra_reads=['gmT', 'modT'], tag=str(b))

                def stage_R(ti):
                    off, cnt, _, _, r = TILES[ti]
                    b = ti % 2
                    z_, x_ = zts[b], xts[b]
                    h2f, tmp, kh2 = h2fs[b], tmps[b], ('h2f', b)
                    kz, kx = ('zts', b), ('xtc', b)
                    nsb = cnt // 128
                    gs0 = nsub_box[0]
                    nsub_box[0] += nsb
                    q = ti % 2
                    pl = 4 + q
                    W_ = nsb * 32
                    for s in range(nsb):
                        gsi = gs0 + s
                        qq = gsi % 2
                        for half in range(2):
                            pt = 6 + half

                            def mmt(e):
                                for jj in range(4):
                                    j = half * 4 + jj
                                    ins = e.transpose(out=PS[pt][:, jj * 128:(jj + 1) * 128],
                                                      in_=h2f[:, j, s * 128:(s + 1) * 128], identity=ident[:])
                                return ins
                            S.op('pe', mmt, reads=[kh2, 'ident'], writes=[pk(pt)])
                            if half == 0:
                                S.op('act', lambda e: e.activation(out=h2t[qq][:, 0:512], in_=PS[pt][:, :], func=AF.Identity),
                                     reads=[pk(pt)], writes=[('h2t', qq)])
                            else:
                                S.op('pool' if False else 'dve', lambda e: e.tensor_copy(out=h2t[qq][:, 512:1024], in_=PS[pt][:, :]),
                                     reads=[pk(pt)], writes=[('h2t', qq)])
                        S.dma('sp', h2tm[gsi * 128:(gsi + 1) * 128, :], h2t[qq][:], reads=[('h2t', qq)], writes=[('h2tm', gsi % 4)])

                        def mmr(e):
                            for kc in range(8):
                                ins = e.matmul(PS[pl][:, s * 32:(s + 1) * 32], lhsT=h2f[:, kc, s * 128:(s + 1) * 128], rhs=rw[:, kc, :],
                                               start=(kc == 0), stop=(kc == 7))
                            return ins
                        S.op('pe', mmr, reads=[kh2, 'rw'], writes=[pk(pl)])
                    kq = ('rt', q)
                    v3 = lambda t: t[:, :nsb, :]
                    v8 = lambda t: t[:, :nsb, :].rearrange("p s (g e) -> p (s g) e", e=8)
                    f2 = lambda t: t[:, :nsb, :].rearrange("p s e -> p (s e)")
                    g4 = lambda t: t[:, :nsb * 4]
                    g43 = lambda t: t[:, :nsb * 4].rearrange("p (s g) -> p s g", g=4)
                    S.op('act', lambda e: e.activation(out=f2(sg[q]), in_=PS[pl][:, :W_], func=AF.Sigmoid),
                         reads=[pk(pl)], writes=[kq])
                    S.op('dve', lambda e: e.tensor_tensor(out=v3(ssel[q]), in0=v3(sg[q]), in1=bc_mid(rb[:], nsb), op=ALU.add),
                         reads=[kq, 'rb'], writes=[kq])
                    S.op('dve', lambda e: e.tensor_reduce(out=g4(m1[q]), in_=v8(ssel[q]), axis=AX.X, op=ALU.max), reads=[kq], writes=[kq])
                    S.op('dve', lambda e: e.tensor_tensor(out=v8(t3[q]), in0=v8(ssel[q]), in1=bc_last(g4(m1[q]), 8), op=ALU.is_equal),
                         reads=[kq], writes=[kq])
                    S.op('dve', lambda e: e.scalar_tensor_tensor(out=f2(t3[q]), in0=f2(t3[q]), scalar=-1.0e9, in1=f2(ssel[q]),
                                                                 op0=ALU.mult, op1=ALU.add), reads=[kq], writes=[kq])
                    S.op('dve', lambda e: e.tensor_reduce(out=g4(m2[q]), in_=v8(t3[q]), axis=AX.X, op=ALU.max), reads=[kq], writes=[kq])
                    S.op('dve', lambda e: e.tensor_tensor(out=g4(gs[q]), in0=g4(m1[q]), in1=g4(m2[q]), op=ALU.add), reads=[kq], writes=[kq])
                    S.op('dve', lambda e: e.tensor_reduce(out=sm[q][:, 0, :nsb], in_=g43(gs[q]), axis=AX.X, op=ALU.max),
                         reads=[kq], writes=[kq])
                    S.op('dve', lambda e: e.tensor_tensor(out=g43(gm_[q]), in0=g43(gs[q]), in1=bc_last(sm[q][:, 0, :nsb], 4),
                                                          op=ALU.is_equal), reads=[kq], writes=[kq])
                    S.op('dve', lambda e: e.tensor_tensor(out=g4(gs[q]), in0=g4(gm_[q]), in1=g4(m2[q]), op=ALU.mult), reads=[kq], writes=[kq])
                    S.op('dve', lambda e: e.tensor_reduce(out=sm[q][:, 1, :nsb], in_=g43(gs[q]), axis=AX.X, op=ALU.add),
                         reads=[kq], writes=[kq])
                    S.op('dve', lambda e: e.tensor_tensor(out=v3(mk[q]), in0=v3(ssel[q]), in1=bc_last(sm[q][:, 1, :nsb], 32),
                                                          op=ALU.is_ge), reads=[kq], writes=[kq])
                    S.op('dve', lambda e: e.tensor_tensor(out=v8(mk[q]), in0=v8(mk[q]), in1=bc_last(g4(gm_[q]), 8), op=ALU.mult),
                         reads=[kq], writes=[kq])
                    S.op('dve', lambda e: e.tensor_tensor(out=f2(em[q]), in0=f2(mk[q]), in1=f2(sg[q]), op=ALU.mult),
                         reads=[kq], writes=[kq])
                    S.op('dve', lambda e: e.tensor_reduce(out=sm[q][:, 2, :nsb], in_=v3(em[q]), axis=AX.X, op=ALU.add),
                         reads=[kq], writes=[kq])
                    S.op('dve', lambda e: e.reciprocal(out=sm[q][:, 3, :nsb], in_=sm[q][:, 2, :nsb]), reads=[kq], writes=[kq])
                    S.op('dve', lambda e: e.tensor_tensor(out=v3(em[q]), in0=v3(em[q]), in1=bc_last(sm[q][:, 3, :nsb], 32), op=ALU.mult),
                         reads=[kq], writes=[kq])

                    def mmk(e):
                        for s in range(nsb):
                            o_ = PS[pl][:, 128 + s * 32:128 + (s + 1) * 32]
                            e.matmul(o_, lhsT=utri[:], rhs=mk[q][:, s, :], start=True, stop=False)
                            for s2 in range(s):
                                e.matmul(o_, lhsT=ones32[:], rhs=mk[q][:, s2, :], start=False, stop=False)
                            ins = e.matmul(o_, lhsT=ones32[:], rhs=cntm[:], start=False, stop=True)
                        return ins
                    S.op('pe', mmk, reads=[kq, 'utri', 'ones32', 'cntm'], writes=[pk(pl)])
                    S.op('dve', lambda e: e.tensor_copy(out=f2(rk[q]), in_=PS[pl][:, 128:128 + W_]), reads=[pk(pl)], writes=[kq])
                    S.op('dve', lambda e: e.tensor_reduce(out=t32[q][:], in_=mk[q][:, :nsb, :].rearrange("p s e -> p e s"), axis=AX.X,
                                                          op=ALU.add), reads=[kq], writes=[kq])
                    S.op('dve', lambda e: e.tensor_tensor(out=cntm[:], in0=cntm[:], in1=t32[q][:], op=ALU.add),
                         reads=[kq, 'cntm'], writes=['cntm'])
                    S.op('dve', lambda e: e.tensor_tensor_scan(out=f2(cmv[q]), data0=f2(onesb), data1=f2(mk[q]), initial=0.0,
                                                               op0=ALU.mult, op1=ALU.add), reads=[kq, 'onesb'], writes=[kq])
                    for k2 in range(2):
                        fm_ = FM[:, gs0:gs0 + nsb, k2, :]
                        S.op('dve', lambda e: e.tensor_tensor(out=v3(t3[q]), in0=v3(cmv[q]), in1=c12[:, k2, :nsb, :], op=ALU.is_equal),
                             reads=[kq, 'c12'], writes=[kq])
                        S.op('dve', lambda e: e.tensor_tensor(out=fm_, in0=v3(t3[q]), in1=v3(mk[q]), op=ALU.mult),
                             reads=[kq], writes=['FM'])
                        S.op('dve', lambda e: e.tensor_tensor(out=v3(t3[q]), in0=fm_, in1=v3(rk[q]), op=ALU.mult),
                             reads=[kq, 'FM'], writes=[kq])
                        S.op('dve', lambda e: e.tensor_reduce(out=RK[:, gs0:gs0 + nsb, k2], in_=v3(t3[q]), axis=AX.X, op=ALU.add),
                             reads=[kq], writes=['RK'])
                        S.op('dve', lambda e: e.tensor_tensor(out=v3(t3[q]), in0=fm_, in1=v3(em[q]), op=ALU.mult),
                             reads=[kq, 'FM'], writes=[kq])
                        S.op('dve', lambda e: e.tensor_reduce(out=WK[:, gs0:gs0 + nsb, k2], in_=v3(t3[q]), axis=AX.X, op=ALU.add),
                             reads=[kq], writes=['WK'])

                stage_W(tiles[0])
                for i_, ti in enumerate(tiles):
                    stage_N(ti)
                    if i_ + 1 < len(tiles):
                        stage_W(tiles[i_ + 1])
                    stage_R(ti)
                S.barrier()
            wst = contextlib.ExitStack()
            WST[l] = (wst,
                      [sb(wst, f"wgs{i}", [128, 4096], BF16) for i in range(4)],
                      [sb(wst, f"wus{i}", [128, 4096], BF16) for i in range(4)],
                      [sb(wst, f"wds{i}", [128, 4096], BF16) for i in range(4)])
            with contextlib.ExitStack() as ph:
                J = nsub_tot
                cb = sb(ph, "cb", [128, 32], F32)
                nblk = sb(ph, "nblk", [128, 32], F32)
                pend = sb(ph, "pend", [128, 32], F32)
                pst = sb(ph, "pst", [128, 32], F32)
                big = sb(ph, "bigc", [128, 32, NBMAX], F32)
                eb = sb(ph, "eb", [128, NBMAX], F32)
                chg = sb(ph, "chg", [128, NBMAX], F32)
                wif = sb(ph, "wif", [128, NBMAX, 2], F32)
                dtmp2 = sb(ph, "dtmp2", [128, MAXSUB, 32], F32)
                dif = sb(ph, "dif", [128, MAXSUB, 2], F32)
                rows = [sb(ph, f"rows{i}", [128, 1024], BF16) for i in range(4)]
                S.op('pe', lambda e: e.matmul(PS[0][:, 0:32], lhsT=ones32[:], rhs=cntm[:], start=True, stop=True),
                     reads=['ones32', 'cntm'], writes=[pk(0)])
                S.op('dve', lambda e: e.tensor_copy(out=cb[:], in_=PS[0][:, 0:32]), reads=[pk(0)], writes=['cb'])
                S.op('dve', lambda e: e.tensor_tensor(out=big[:, :, :J], in0=bc_last(cb[:], J), in1=bc_mid(blkoff[:, :J], 32),
                                                      op=ALU.is_gt), reads=['cb', 'blkoff'], writes=['big'])
                S.op('dve', lambda e: e.tensor_reduce(out=nblk[:], in_=big[:, :, :J], axis=AX.X, op=ALU.add),
                     reads=['big'], writes=['nblk'])
                S.op('dve', lambda e: e.tensor_tensor_scan(out=pend[:], data0=ones_row[:], data1=nblk[:], initial=0.0,
                                                           op0=ALU.mult, op1=ALU.add), reads=['nblk', 'ones_row'], writes=['pend'])
                S.op('dve', lambda e: e.tensor_tensor(out=pst[:], in0=pend[:], in1=nblk[:], op=ALU.subtract),
                     reads=['pend', 'nblk'], writes=['pst'])
                S.op('dve', lambda e: e.tensor_scalar(out=pst[:], in0=pst[:], scalar1=128.0, scalar2=None, op0=ALU.mult),
                     reads=['pst'], writes=['pst'])
                S.op('dve', lambda e: e.tensor_scalar(out=pend[:], in0=pend[:], scalar1=128.0, scalar2=None, op0=ALU.mult),
                     reads=['pend'], writes=['pend'])
                S.op('dve', lambda e: e.tensor_tensor(out=big[:, :, :NB].rearrange("p e b -> p b e"),
                                                      in0=bc_mid(pend[:], NB), in1=bc_last(blkoff[:, :NB], 32),
                                                      op=ALU.is_le), reads=['pend', 'blkoff', 'big'], writes=['big'])
                S.op('dve', lambda e: e.tensor_reduce(out=eb[:, :NB], in_=big[:, :, :NB].rearrange("p e b -> p b e"),
                                                      axis=AX.X, op=ALU.add), reads=['big'], writes=['eb'])
                S.op('dve', lambda e: e.tensor_scalar(out=eb[:, :NB], in0=eb[:, :NB], scalar1=31.0, scalar2=None, op0=ALU.min),
                     reads=['eb'], writes=['eb'])
                S.op('dve', lambda e: e.memset(chg[:], 1.0), writes=['chg'])
                S.op('dve', lambda e: e.tensor_tensor(out=chg[:, 1:NB], in0=eb[:, 1:NB], in1=eb[:, 0:NB - 1], op=ALU.not_equal),
                     reads=['eb', 'chg'], writes=['chg'])
                for k4 in range(1, 4):
                    S.op('dve', lambda e: e.memset(chg[:, k4 * (NB // 4):k4 * (NB // 4) + 1], 1.0), reads=['chg'], writes=['chg'])
                for h in range(1):
                    S.op('dve', lambda e: e.tensor_scalar(out=wif[:, :NB, h], in0=eb[:, :NB], scalar1=128.0,
                                                          scalar2=float(l * 4096 - BIGW), op0=ALU.mult, op1=ALU.add),
                         reads=['eb', 'wif'], writes=['wif'])
                    S.op('dve', lambda e: e.tensor_scalar(out=wif[:, :NB, h], in0=wif[:, :NB, h], scalar1=pcol[:, 0:1],
                                                          scalar2=None, op0=ALU.add), reads=['wif', 'pcol'], writes=['wif'])
                    S.op('dve', lambda e: e.tensor_tensor(out=wif[:, :NB, h], in0=wif[:, :NB, h], in1=chg[:, :NB], op=ALU.mult),
                         reads=['wif', 'chg'], writes=['wif'])
                    S.op('dve', lambda e: e.tensor_scalar(out=wif[:, :NB, h], in0=wif[:, :NB, h], scalar1=float(BIGW),
                                                          scalar2=None, op0=ALU.add), reads=['wif'], writes=['wif'])
                S.op('dve', lambda e: e.tensor_copy(out=WI[:, :NB, 0:1], in_=wif[:, :NB, 0:1]), reads=['wif'], writes=['WI'])
                for k2 in range(2):
                    S.op('dve', lambda e: e.tensor_tensor(out=dtmp2[:, :J, :], in0=FM[:, :J, k2, :], in1=bc_mid(pst[:], J),
                                                          op=ALU.mult), reads=['FM', 'pst', 'dtmp2'], writes=['dtmp2'])
                    S.op('dve', lambda e: e.tensor_reduce(out=dif[:, :J, k2], in_=dtmp2[:, :J, :], axis=AX.X, op=ALU.add),
                         reads=['dtmp2', 'dif'], writes=['dif'])
                S.op('dve', lambda e: e.tensor_tensor(out=dif[:, :J, :], in0=dif[:, :J, :], in1=RK[:, :J, :], op=ALU.add),
                     reads=['dif', 'RK'], writes=['dif'])
                S.op('dve', lambda e: e.tensor_copy(out=DI[:, :J, :], in_=dif[:, :J, :]), reads=['dif'], writes=['DI'])
                for p_ in range(3):
                    moe_loadw(l, NB, p_)
                for gsi in range(J):
                    q = gsi % 4
                    S.dma('sp', rows[q][:], h2tm[gsi * 128:(gsi + 1) * 128, :], reads=[('h2tm', gsi % 4)], writes=[('rows', q)])
                    for k2 in range(2):
                        idma(Xs[:, :], bass.IndirectOffsetOnAxis(ap=DI[:, gsi, k2:k2 + 1], axis=0), rows[q][:, :], None,
                             NB * 128 - 1, reads=[('rows', q), 'DI', 'Xs'], writes=[('Xsc', q)], slot=('Xsc', q))
                S.barrier()

        def phase_D(l, tiles, final):
            nsub_tot = sum(TILES[ti][1] // 128 for ti in tiles)
            NB = 2 * nsub_tot + 32
            with contextlib.ExitStack() as ph:
                _, wgs, wus, wds = WST[l]
                blk = lambda p_: moe_blk(NB, p_)
                xbs = [sb(ph, f"xbs{i}", [128, 1024], BF16) for i in range(2)]
                XTs = [sb(ph, f"XTs{i}", [128, 8, 128], BF16) for i in range(2)]
                s1s = [sb(ph, f"s1s{i}", [128, 512], F32) for i in range(2)]
                ATs = [sb(ph, f"ATs{i}", [128, 4, 128], BF16) for i in range(2)]
                Yts = [sb(ph, f"Yts{i}", [128, 1024], F32) for i in range(2)]

                loadw = lambda p_: moe_loadw(l, NB, p_)
                def xbload(p_):
                    bn = blk(p_)
                    S.dma('sp', xbs[p_ % 2][:], Xs[bn * 128:(bn + 1) * 128, :], reads=[('Xsc', 0), ('Xsc', 1), ('Xsc', 2), ('Xsc', 3), 'Xs'],
                          writes=[('xbs', p_ % 2)])

                def stage_T(p_):
                    q = p_ % 2
                    xb, XT = xbs[q], XTs[q]
                    ptb = PS[q].bitcast(BF16)

                    def mmt(e):
                        for c in range(8):
                            ins = e.transpose(out=ptb[:, c * 128:(c + 1) * 128], in_=xb[:, c:1024:8], identity=identb[:])
                        return ins
                    S.op('pe', mmt, reads=[('xbs', q), 'identb'], writes=[pk(q)])
                    S.op('act', lambda e: e.activation(out=XT[:].rearrange("p c s -> p (c s)"), in_=ptb[:, :], func=AF.Identity),
                         reads=[pk(q)], writes=[('XTs', q)])

                def stage_G(p_):
                    q = p_ % 2
                    ws_ = p_ % 4
                    XT, AT = XTs[q], ATs[q]
                    wg_, wu_ = wgs[ws_], wus[ws_]
                    p1, p2 = 2 + 2 * q, 3 + 2 * q

                    def mmg(e, wt, p):
                        for fo in range(4):
                            for c in range(8):
                                c0 = c * 512 + fo
                                ins = e.matmul(PS[p][:, fo * 128:(fo + 1) * 128], lhsT=wt[:, c0:c0 + 509:4], rhs=XT[:, c, :],
                                               start=(c == 0), stop=(c == 7))
                        return ins
                    S.op('pe', lambda e: mmg(e, wg_, p1), reads=[('wgs', ws_), ('XTs', q)], writes=[pk(p1)])
                    S.op('pe', lambda e: mmg(e, wu_, p2), reads=[('wus', ws_), ('XTs', q)], writes=[pk(p2)])
                    S.op('act', lambda e: e.activation(out=s1s[q][:], in_=PS[p1][:, :], func=AF.Silu),
                         reads=[pk(p1)], writes=[('s1s', q)])
                    S.op('dve', lambda e: e.tensor_tensor(out=AT[:].rearrange("p c s -> p (c s)"), in0=s1s[q][:], in1=PS[p2][:, :],
                                                          op=ALU.mult), reads=[('s1s', q), pk(p2)], writes=[('ATs', q)])

                def stage_D(p_):
                    q = p_ % 2
                    ws_ = p_ % 4
                    b = blk(p_)
                    AT, Yt, wd_ = ATs[q], Yts[q], wds[ws_]
                    for dh in range(2):
                        py = 6 + dh

                        def mmd(e):
                            for fo in range(4):
                                ins = e.matmul(PS[py][:, :], lhsT=AT[:, fo, :],
                                               rhs=wd_[:, fo * 1024 + dh * 512:fo * 1024 + (dh + 1) * 512],
                                               start=(fo == 0), stop=(fo == 3))
                            return ins
                        S.op('pe', mmd, reads=[('wds', ws_), ('ATs', q)], writes=[pk(py)])
                        if dh == 0:
                            S.op('act', lambda e: e.activation(out=Yt[:, 0:512], in_=PS[py][:, :], func=AF.Identity),
                                 reads=[pk(py)], writes=[('Yts', q)])
                        else:
                            S.op('dve', lambda e: e.tensor_copy(out=Yt[:, 512:1024], in_=PS[py][:, :]),
                                 reads=[pk(py)], writes=[('Yts', q)])
                    S.dma('sp', Ys[b * 128:(b + 1) * 128, :], Yt[:], reads=[('Yts', q)], writes=[('Ys', q)])

                xbload(0)
                xbload(1)
                stage_T(0)
                for pos in range(NB):
                    if pos + 3 < NB:
                        loadw(pos + 3)
                    stage_G(pos)
                    if pos + 1 < NB:
                        stage_T(pos + 1)
                    if pos + 2 < NB:
                        xbload(pos + 2)
                    stage_D(pos)
                S.barrier()
            WST[l][0].close()
            with contextlib.ExitStack() as ph3:
                xts = [sb(ph3, f"xtd{i}", [128, 8, 512], F32) for i in range(2)]
                g1 = [sb(ph3, f"g1{i}", [128, 1024], F32) for i in range(4)]
                g2 = [sb(ph3, f"g2{i}", [128, 1024], F32) for i in range(4)]
                tmp = norm_tmp(ph3) if final else None
                ots = [sb(ph3, f"otd{i}", [128, 8, 512], F32) for i in range(2)] if final else None
                subs = []
                for li, ti in enumerate(tiles):
                    for s_ in range(TILES[ti][1] // 128):
                        subs.append((li, ti, s_, len(subs)))

                def stage_a(li, ti, s, gsi):
                    off, cnt, _, _, r = TILES[ti]
                    b = li % 2
                    if s == 0:
                        S.dma('sp', xts[b][:, :, :cnt], xr[:, :, off:off + cnt].rearrange("j p t -> p j t"),
                              reads=[('xr', ti)], writes=[('xtd', b, j) for j in range(8)])
                    q = gsi % 4
                    idma(g1[q][:, :], None, Ys[:, :], bass.IndirectOffsetOnAxis(ap=DI[:, gsi, 0:1], axis=0),
                         NB * 128 - 1, reads=['DI', ('Ys', 0), ('Ys', 1)], writes=[('g1', q)], slot=('g1', q))
                    idma(g2[q][:, :], None, Ys[:, :], bass.IndirectOffsetOnAxis(ap=DI[:, gsi, 1:2], axis=0),
                         NB * 128 - 1, reads=['DI', ('Ys', 0), ('Ys', 1)], writes=[('g2', q)], slot=('g2', q))
                    S.op('dve', lambda e: e.tensor_scalar(out=g1[q][:], in0=g1[q][:], scalar1=WK[:, gsi, 0:1], scalar2=None,
                                                          op0=ALU.mult), reads=[('g1', q), 'WK'], writes=[('g1', q)])
                    S.op('dve', lambda e: e.scalar_tensor_tensor(out=g1[q][:], in0=g2[q][:], scalar=WK[:, gsi, 1:2],
                                                                 in1=g1[q][:], op0=ALU.mult, op1=ALU.add),
                         reads=[('g1', q), ('g2', q), 'WK'], writes=[('g1', q)])
                    for half in range(2):
                        pt = 2 * (q % 2) + half

                        def mmt2(e):
                            for jj in range(4):
                                j = half * 4 + jj
                                ins = e.transpose(out=PS[pt][:, jj * 128:(jj + 1) * 128], in_=g1[q][:, j * 128:(j + 1) * 128],
                                                  identity=ident[:])
                            return ins
                        S.op('pe', mmt2, reads=[('g1', q), 'ident'], writes=[pk(pt)])

                def stage_b(li, ti, s, gsi):
                    off, cnt, _, _, r = TILES[ti]
                    b = li % 2
                    x_ = xts[b]
                    q = gsi % 4
                    kxs = [('xtd', b, j) for j in range(8)]
                    for half in range(2):
                        pt = 2 * (q % 2) + half
                        for jj in range(4):
                            j = half * 4 + jj
                            S.op('dve', lambda e: e.scalar_tensor_tensor(out=x_[:, j, s * 128:(s + 1) * 128],
                                                                         in0=PS[pt][:, jj * 128:(jj + 1) * 128],
                                                                         scalar=mod_ap(l, 5, j, r),
                                                                         in1=x_[:, j, s * 128:(s + 1) * 128],
                                                                         op0=ALU.mult, op1=ALU.add),
                                 reads=[pk(pt), kxs[j], ('modT', l)], writes=[kxs[j]])
                    if s == cnt // 128 - 1:
                        if not final:
                            S.dma('sp', xr[:, :, off:off + cnt].rearrange("j p t -> p j t"), x_[:, :, :cnt],
                                  reads=kxs, writes=[('xr', ti)])
                            if dbg:
                                S.dma('sp', dbgo["d_x2"][:, :, off:off + cnt].rearrange("j p t -> p j t"), x_[:, :, :cnt],
                                      reads=kxs, writes=['d_x2'])
                        else:
                            o_ = ots[b]
                            norm_mod(tmp, x_, cnt, kxs, lambda j: o_[:, j, :cnt], ('otd', b),
                                     lambda j: fing[:, j:j + 1], None, 7, extra_reads=['fing'])
                            S.dma('sp', outT[:, :, off:off + cnt].rearrange("j p t -> p j t"), o_[:, :, :cnt],
                                  reads=[('otd', b)], writes=[('out', b)])

                for idx in range(len(subs) + 1):
                    if idx < len(subs):
                        stage_a(*subs[idx])
                    if idx >= 1:
                        stage_b(*subs[idx - 1])
                S.barrier()

        phase_C(0, w_out, list(range(9)), xc)
        phase_D(0, list(range(9)), final=False)

        hst = contextlib.ExitStack()
        hbuf = sb(hst, "hbuf1", [128, 8, NT], BF16)
        phase_A(1, xr, hbuf)
        with contextlib.ExitStack() as ph:
            cost = sb(ph, "cost", [128, NT], F32)
            sint = sb(ph, "sint", [128, NT], F32)
            dal = sb(ph, "dal", [128, 4, 64], F32)
            dtmp = sb(ph, "dtmp", [128, 64], F32)
            lamv = sb(ph, "lamv", [128, 4], F32)
            subg = sb(ph, "subg", [128, 1], F32)
            wq = sb(ph, "wq", [128, 8, 128], BF16)
            wk = sb(ph, "wk", [128, 8, 128], BF16)
            wv = sb(ph, "wv", [128, 8, 128], BF16)
            qbt = [sb(ph, f"qbt{i}", [128, 512], BF16) for i in range(2)]
            rmb = sb(ph, "rmb", [128, 128], BF16)
            QT = sb(ph, "QT", [128, NQ], BF16)
            vtb = [sb(ph, f"vtb{i}", [128, 512], BF16) for i in range(2)]
            KT = sb(ph, "KT", [128, NT], BF16)
            Vt = sb(ph, "Vt", [128, 34, 128], BF16)
            rt1 = [sb(ph, f"rt1{i}", [128, 512], F32) for i in range(2)]
            rt2 = [sb(ph, f"rt2{i}", [128, 512], F32) for i in range(2)]
            Eb2 = [sb(ph, f"Eb{i}", [128, 2, 512], BF16) for i in range(2)]
            acc2 = sb(ph, "acc2", [128, 2, 512], F32)
            obw = sb(ph, "obw", [128, 2, 512], F32)
            ob = [obw[:, 0, :], obw[:, 1, :]]
            rlw = sb(ph, "rlw", [128, 2, 512], F32)
            rl = sb(ph, "rl", [128, 512], F32)
            accD = sb(ph, "accD", [128, 512], F32)
            accP = sb(ph, "accP", [128, 512], F32)
            osq = sb(ph, "osq", [128, 512], F32)
            aot = [sb(ph, f"aot{i}", [128, 512], BF16) for i in range(2)]
            S.dma('sp', cost[:], cos_in, writes=['cos'])
            S.dma('pool', rmb[:], rmat_in, writes=['rmb'])
            S.dma('sp', sint[:], sin_in, writes=['sin'])
            S.dma('sp', dal[:], dalam_in, writes=['dal'])
            S.dma('sp', subg[:], subg_in, writes=['subg'])
            for i2 in range(2):
                S.op('dve', lambda e: e.tensor_tensor(out=dtmp[:], in0=dal[:, 2 * i2, :], in1=dal[:, 2 * i2 + 1, :], op=ALU.mult),
                     reads=['dal'], writes=['dtmp'])
                S.op('dve', lambda e: e.tensor_reduce(out=lamv[:, i2:i2 + 1], in_=dtmp[:], axis=AX.X, op=ALU.add),
                     reads=['dtmp'], writes=['lamv'])
            S.op('act', lambda e: e.activation(out=lamv[:, 0:2], in_=lamv[:, 0:2], func=AF.Exp), reads=['lamv'], writes=['lamv'])
            S.op('dve', lambda e: e.tensor_tensor(out=lamv[:, 2:3], in0=lamv[:, 1:2], in1=lamv[:, 0:1], op=ALU.subtract),
                 reads=['lamv'], writes=['lamv'])
            S.op('dve', lambda e: e.tensor_scalar(out=lamv[:, 2:3], in0=lamv[:, 2:3], scalar1=-LAMBDA_INIT, scalar2=None,
                                                  op0=ALU.add), reads=['lamv'], writes=['lamv'])
            S.op('dve', lambda e: e.tensor_scalar(out=lamv[:, 3:4], in0=subg[:], scalar1=1.0 - LAMBDA_INIT, scalar2=None,
                                                  op0=ALU.mult), reads=['lamv', 'subg'], writes=['lamv'])
            nkc = 34
            acnt = 0
            for hh in range(8):
                for t_, c0, k_ in [(wq, hh * 128, 'wq'), (wk, 1024 + hh * 128, 'wk'), (wv, 2048 + hh * 128, 'wv')]:
                    S.dma('pool', t_[:], w_qkv[:, c0:c0 + 128].rearrange("(k p) f -> p k f", p=128), writes=[k_])
                convert_experts(1, 4 * hh, 4 * hh + 4)
                jobs = []
                for ti in range(9):
                    jobs.append(('v', ti))
                    if ti < 4:
                        jobs.append(('q', ti))
                    jobs.append(('k', ti))

                def proj_s1(kj, kind, ti):
                    off, cnt = TILES[ti][0], TILES[ti][1]
                    q = kj % 2
                    pa_ = 2 * q
                    wt, kw_ = {'q': (wq, 'wq'), 'k': (wk, 'wk'), 'v': (wv, 'wv')}[kind]

                    def mmp(e):
                        for kc in range(8):
                            ins = e.matmul(PS[pa_][:, :cnt], lhsT=wt[:, kc, :], rhs=hbuf[:, kc, off:off + cnt],
                                           start=(kc == 0), stop=(kc == 7))
                        return ins
                    S.op('pe', mmp, reads=[kw_, ('h', ti)], writes=[pk(pa_)])
                    S.op('act', lambda e: e.activation(out=qbt[q][:, :cnt], in_=PS[pa_][:, :cnt], func=AF.Identity),
                         reads=[pk(pa_)], writes=[('qbt', q)])

                def proj_s2(kj, kind, ti):
                    off, cnt = TILES[ti][0], TILES[ti][1]
                    q = kj % 2
                    pa_, pb_ = 2 * q, 2 * q + 1
                    if kind == 'v':
                        nsb = cnt // 128
                        pbb = PS[pb_].bitcast(BF16)

                        def mmvt(e):
                            for s in range(nsb):
                                ins = e.transpose(out=pbb[:, s * 128:(s + 1) * 128], in_=qbt[q][:, s * 128:(s + 1) * 128],
                                                  identity=identb[:])
                            return ins
                        S.op('pe', mmvt, reads=[('qbt', q), 'identb'], writes=[pk(pb_)])
                        si0 = off // 128
                        S.op('dve', lambda e: e.tensor_copy(out=Vt[:, si0:si0 + nsb, :].rearrange("p s v -> p (s v)"),
                                                            in_=pbb[:, :nsb * 128]), reads=[pk(pb_)], writes=['Vt'])
                        return
                    dst, kdst = (QT, 'QT') if kind == 'q' else (KT, 'KT')
                    S.op('pe', lambda e: e.matmul(PS[pb_][:, :cnt], lhsT=rmb[:], rhs=qbt[q][:, :cnt], start=True, stop=True),
                         reads=['rmb', ('qbt', q)], writes=[pk(pb_)])
                    S.op('dve', lambda e: e.tensor_tensor(out=rt1[q][:, :cnt], in0=PS[pa_][:, :cnt], in1=cost[:, off:off + cnt],
                                                          op=ALU.mult), reads=[pk(pa_), 'cos', ('qbt', q)], writes=[('rt1', q)])
                    S.op('dve', lambda e: e.tensor_tensor(out=rt2[q][:, :cnt], in0=PS[pb_][:, :cnt], in1=sint[:, off:off + cnt],
                                                          op=ALU.mult), reads=[pk(pb_), 'sin'], writes=[('rt2', q)])
                    S.op('dve', lambda e: e.tensor_tensor(out=dst[:, off:off + cnt], in0=rt1[q][:, :cnt], in1=rt2[q][:, :cnt],
                                                          op=ALU.add), reads=[('rt1', q), ('rt2', q)], writes=[kdst])

                proj_s1(0, *jobs[0])
                for kj in range(len(jobs)):
                    if kj + 1 < len(jobs):
                        proj_s1(kj + 1, *jobs[kj + 1])
                    proj_s2(kj, *jobs[kj])
                for qt in range(4):
                    q0 = qt * 512
                    pend_ = None
                    for kc in range(nkc + 1):
                        if kc < nkc:
                            sidx = acnt % 2
                            acnt += 1
                            sb0 = 2 * sidx
                            for mi in range(2):
                                lo_, hi_ = mi * 64, (mi + 1) * 64
                                S.op('pe', lambda e: e.matmul(PS[sb0 + mi][:, :], lhsT=KT[lo_:hi_, kc * 128:(kc + 1) * 128],
                                                              rhs=QT[lo_:hi_, q0:q0 + 512], start=True, stop=True),
                                     reads=['KT', 'QT'], writes=[pk(sb0 + mi)])
                            ke = ('Eb', sidx)
                            S.op('act', lambda e: e.activation(out=Eb2[sidx][:].rearrange("p m q -> p (m q)"),
                                                               in_=psbig[:, sb0 * 512:(sb0 + 2) * 512], func=AF.Exp, scale=0.125),
                                 reads=[pk(sb0), pk(sb0 + 1)], writes=[ke])
                            cur = (Eb2[sidx], ke)
                        if pend_ is not None:
                            kp, (ebp, kep) = pend_
                            for mi in range(2):
                                S.op('pe', lambda e: e.matmul(PS[4 + mi][:, :], lhsT=Vt[:, kp, :], rhs=ebp[:, mi, :], start=(kp == 0),
                                                              stop=(kp == nkc - 1)), reads=['Vt', kep], writes=[pk(4 + mi)])
                            accv = psbig[:, 6 * 512:8 * 512]
                            ebf = ebp[:].rearrange("p m q -> p (m q)")
                            if kp == 0:
                                S.op('dve', lambda e: e.tensor_copy(out=accv, in_=ebf), reads=[kep], writes=[pk(6), pk(7)])
                            else:
                                S.op('dve', lambda e: e.tensor_tensor(out=accv, in0=accv, in1=ebf, op=ALU.add),
                                     reads=[kep, pk(6), pk(7)], writes=[pk(6), pk(7)])
                        pend_ = (kc, cur) if kc < nkc else None
                    S.op('act', lambda e: e.activation(out=acc2[:].rearrange("p m q -> p (m q)"), in_=psbig[:, 6 * 512:8 * 512],
                                                       func=AF.Identity), reads=[pk(6), pk(7)], writes=['acc2'])
                    for mi in range(2):
                        S.op('pe', lambda e: e.matmul(PS[mi][:, :], lhsT=ones32[:], rhs=acc2[:, mi, :], start=True, stop=True),
                             reads=['ones32', 'acc2'], writes=[pk(mi)])
                    rlf = rlw[:].rearrange("p m q -> p (m q)")
                    S.op('act', lambda e: e.activation(out=rlf, in_=psbig[:, 0:1024], func=AF.Ln), reads=[pk(0), pk(1)], writes=['rlw'])
                    S.op('act', lambda e: e.activation(out=rlf, in_=rlf, func=AF.Exp, scale=-1.0), reads=['rlw'], writes=['rlw'])
                    S.op('dve', lambda e: e.tensor_tensor(out=obw[:].rearrange("p m q -> p (m q)"), in0=psbig[:, 4 * 512:6 * 512],
                                                          in1=rlf, op=ALU.mult),
                         reads=[pk(4), pk(5), 'rlw'], writes=[('ob', 0), ('ob', 1)])
                    S.op('dve', lambda e: e.scalar_tensor_tensor(out=ob[0][:], in0=ob[1][:], scalar=lamv[:, 2:3], in1=ob[0][:],
                                                                 op0=ALU.mult, op1=ALU.add),
                         reads=[('ob', 0), ('ob', 1), 'lamv'], writes=[('ob', 0)])
                    S.op('act', lambda e: e.activation(out=osq[:], in_=ob[0][:], func=AF.Square), reads=[('ob', 0)], writes=['osq'])
                    S.op('pe', lambda e: e.matmul(PS[2][:, :], lhsT=ones32[:], rhs=osq[:], start=True, stop=True),
                         reads=['ones32', 'osq'], writes=[pk(2)])
                    S.op('act', lambda e: e.activation(out=osq[:], in_=PS[2][:, :], func=AF.Ln, bias=eps_t[:, 0:1],
                                                       scale=1.0 / 128.0), reads=[pk(2), 'eps'], writes=['osq'])
                    S.op('act', lambda e: e.activation(out=osq[:], in_=osq[:], func=AF.Exp, scale=-0.5), reads=['osq'], writes=['osq'])
                    a_ = aot[qt % 2]
                    S.op('dve', lambda e: e.scalar_tensor_tensor(out=a_[:], in0=ob[0][:], scalar=lamv[:, 3:4], in1=osq[:],
                                                                 op0=ALU.mult, op1=ALU.mult),
                         reads=[('ob', 0), 'osq', 'lamv'], writes=[('aot', qt % 2)])
                    S.dma('sp', zscr[hh, :, q0:q0 + 512], a_[:], reads=[('aot', qt % 2)], writes=[('zs', qt)])
            S.barrier()
        hst.close()

        phase_C(1, w_o, list(range(4)), xr)
        phase_D(1, list(range(4)), final=True)
        S.barrier()
    return nc


def _prep_shared(inp, rev):
    dsl = slice(None, None, -1) if rev else slice(None)
    f = lambda a: np.ascontiguousarray(np.asarray(a, dtype=np.float32))
    pj = lambda v: f(np.asarray(v).reshape(8, 128).T)
    sh = {}
    sh["w_mod"] = f(inp["w_mod"])
    sh["bmod"] = f(np.asarray(inp["b_mod"]).reshape(2, 48, 128).transpose(2, 0, 1))
    sh["n1g"] = f(np.asarray(inp["norm1_g"]).reshape(2, 8, 128).transpose(2, 0, 1))
    sh["n2g"] = f(np.asarray(inp["norm2_g"]).reshape(2, 8, 128).transpose(2, 0, 1))
    sh["fing"] = pj(inp["final_g"])
    sh["w_in"] = f(inp["rg_w_in"][0])
    cw = np.asarray(inp["rg_conv_w"][0])
    z1 = np.zeros((1, 1024), np.float32)
    cw5 = np.concatenate([cw, z1], 0) if not rev else np.concatenate([z1, cw[::-1]], 0)
    sh["convw"] = f(cw5.reshape(5, 8, 128).transpose(2, 1, 0))
    sh["convb"] = pj(inp["rg_conv_b"][0])
    sh["gaw"] = f(np.asarray(inp["rg_gate_a_w"][0])[dsl])
    sh["gxw"] = f(np.asarray(inp["rg_gate_x_w"][0])[dsl])
    sh["gab"] = f(np.asarray(inp["rg_gate_a_b"][0])[dsl].reshape(2, 8, 128).transpose(2, 0, 1))
    sh["gxb"] = f(np.asarray(inp["rg_gate_x_b"][0])[dsl].reshape(2, 8, 128).transpose(2, 0, 1))
    sh["lam"] = f(np.asarray(inp["rg_lambda"][0])[dsl].reshape(2, 8, 128).transpose(2, 0, 1))
    sh["w_out"] = f(inp["rg_w_out"][0])
    sh["w_qkv"] = f(inp["da_w_qkv"][0])
    sh["dalam"] = f(np.broadcast_to(np.asarray(inp["da_lambda"][0])[None], (128, 4, 64)))
    sh["subg"] = f(np.asarray(inp["da_subln_g"][0]).reshape(128, 1))
    sh["w_o"] = f(inp["da_w_o"][0])
    sh["rw"] = f(np.asarray(inp["router_w"]).reshape(8, 128, 32).transpose(1, 0, 2))
    sh["rb"] = f(np.broadcast_to(np.asarray(inp["router_bias"])[None], (128, 32)))
    sh["wg"] = f(inp["moe_w_gate"])
    sh["wu"] = f(inp["moe_w_up"])
    sh["wd"] = f(inp["moe_w_down"])
    t = np.arange(NX)
    row = (t // 64).astype(np.float32)
    col = (t % 64).astype(np.float32)
    inv = (1.0 / (10000.0 ** (np.arange(16, dtype=np.float32) / 16))).astype(np.float32)
    ang = np.stack([row, col], -1)[:, :, None] * inv
    ang = np.broadcast_to(ang[:, :, None, :], (NX, 2, 2, 16)).reshape(NX, 64).astype(np.float32)
    cos = np.ones((128, NT), np.float32)
    sin = np.zeros((128, NT), np.float32)
    if rev:
        ang = ang[::-1]
    cos[:, :NX] = np.tile(np.cos(ang).T, (2, 1))
    sin[:, :NX] = np.tile(np.sin(ang).T, (2, 1))
    sh["pcol"] = np.arange(128, dtype=np.float32).reshape(128, 1)
    sh["blkoff"] = np.ascontiguousarray(np.broadcast_to((128.0 * np.arange(100, dtype=np.float32))[None], (128, 100)))
    rm = np.zeros((128, 128), np.float32)
    for m in range(128):
        if (m % 32) < 16:
            rm[m + 16, m] = -1.0
        else:
            rm[m - 16, m] = 1.0
    sh["rmat"] = rm
    sh["cos"] = cos
    sh["sin"] = sin
    return sh


def _prep_core(inp, b, rev):
    f = lambda a: np.ascontiguousarray(np.asarray(a, dtype=np.float32))
    x = np.asarray(inp["x"][b])
    ctx = np.asarray(inp["ctx"][b])
    if rev:
        x = x[::-1]
        ctx = ctx[::-1]
    tok = np.concatenate([x, ctx], axis=0)
    d = {"xc": f(tok.T.reshape(8, 128, NT))}
    cond = np.stack([np.asarray(inp["c"][b]), np.asarray(inp["c_ctx"])], -1)
    d["cond"] = f(cond.reshape(8, 128, 2).transpose(1, 0, 2))
    return d


def kernel(**inputs):
    nc = build(DEBUG)
    shs = [_prep_shared(inputs, False), _prep_shared(inputs, True)]
    in_maps = []
    for core in range(8):
        b, rev = core // 2, core % 2
        m = dict(shs[rev])
        m.update(_prep_core(inputs, b, bool(rev)))
        in_maps.append(m)
    res = run_bass_kernel_spmd(nc, in_maps, core_ids=list(range(8)))
    out = np.empty((4, NX, 1024), np.float32)
    for core in range(8):
        b, rev = core // 2, core % 2
        o = np.asarray(res.results[core]["outT"]).reshape(1024, NQ).T
        if rev:
            out[b, NX - NQ:] = o[::-1]
        else:
            out[b, :NQ] = o
    return out
```

```python
import contextlib
import math
import numpy as np
import concourse.bass as bass
import concourse.mybir as mybir
from concourse.bass_utils import run_bass_kernel_spmd

F32 = mybir.dt.float32
BF16 = mybir.dt.bfloat16
ALU = mybir.AluOpType
AF = mybir.ActivationFunctionType
AX = mybir.AxisListType

EPS = 1e-6
NT = 4352
NX = 4096
NCTX = 256
TILES = [(i * 512, 512, 2 + i * 512, i * 512, 0) for i in range(8)] + [(4096, 256, 4102, 4100, 1)]
UW = 4360
UCW = 4356
NQ = 2048
LAMBDA_INIT = 0.8 - 0.6 * math.exp(-0.3 * 1)
DEBUG = False


class Sched:
    ROT = 30000

    def __init__(self, nc, stack):
        self.nc = nc
        self.stack = stack
        self.engs = {'pe': nc.tensor, 'act': nc.scalar, 'dve': nc.vector,
                     'pool': nc.gpsimd, 'sp': nc.sync}
        self.cur = {}
        self.nsem = 0
        for e in self.engs:
            self.cur[e] = [self._newsem(e), 0]
        self.lastw = {}
        self.readers = {}
        self.waited = {e: {} for e in self.engs}
        self.dsem = {}
        self.alltok = {}

    def _newsem(self, nm):
        self.nsem += 1
        return self.stack.enter_context(self.nc.semaphore(f"s_{nm}_{self.nsem}"))

    def _wait(self, eng, toks):
        best = {}
        for (s, v) in toks:
            k = id(s)
            if k not in best or best[k][1] < v:
                best[k] = (s, v)
        for k, (s, v) in best.items():
            if self.waited[eng].get(k, 0) >= v:
                continue
            self.engs[eng].wait_ge(s, v)
            self.waited[eng][k] = v

    def _deps(self, eng, reads, writes, skip_same_pe=True):
        toks = []
        for k in reads:
            if k in self.lastw:
                toks.append(self.lastw[k])
        for k in writes:
            if k in self.lastw:
                toks.append(self.lastw[k])
            toks.extend(self.readers.get(k, ()))
        if eng == 'pe' and skip_same_pe:
            toks = [t for t in toks if t[0] is not self.cur['pe'][0]]
        return toks

    def _commit(self, tok, reads, writes):
        for k in writes:
            self.lastw[k] = tok
            self.readers[k] = []
        for k in reads:
            if k in writes:
                continue
            self.readers.setdefault(k, []).append(tok)
        self.alltok[id(tok[0])] = tok

    def op(self, eng, fn, reads=(), writes=()):
        self._wait(eng, self._deps(eng, reads, writes))
        c = self.cur[eng]
        if c[1] >= self.ROT:
            c[0] = self._newsem(eng)
            c[1] = 0
        ins = fn(self.engs[eng])
        c[1] += 1
        ins.then_inc(c[0], 1)
        self._commit((c[0], c[1]), reads, writes)

    def dma(self, eng, out, in_, reads=(), writes=(), slot=None, **kw):
        if slot is None:
            slot = ('auto',) + tuple(writes)
        self._wait(eng, self._deps(eng, reads, writes, skip_same_pe=False))
        if slot not in self.dsem:
            self.dsem[slot] = [self._newsem('d'), 0]
        d = self.dsem[slot]
        ins = self.engs[eng].dma_start(out=out, in_=in_, **kw)
        d[1] += 16
        ins.then_inc(d[0], 16)
        self._commit((d[0], d[1]), reads, writes)

    def barrier(self):
        toks = list(self.alltok.values())
        for e in self.engs:
            self._wait(e, toks)


def bc_last(a, n):
    return bass.AP(a.tensor, a.offset, [list(x) for x in a.ap] + [[0, n]])


def bc_mid(a, n):
    l = [list(x) for x in a.ap]
    return bass.AP(a.tensor, a.offset, [l[0], [0, n]] + l[1:])


def build(dbg=False):
    nc = bass.Bass("TRN2", target_bir_lowering=False)

    def din(name, shape, dt=F32):
        return nc.dram_tensor(name, list(shape), dt, kind="ExternalInput").ap()

    xc = din("xc", [8, 128, NT])
    cond_in = din("cond", [128, 8, 2])
    w_mod = din("w_mod", [2, 1024, 6144])
    bmod_in = din("bmod", [128, 2, 48])
    n1g_in = din("n1g", [128, 2, 8])
    n2g_in = din("n2g", [128, 2, 8])
    fing_in = din("fing", [128, 8])
    w_in = din("w_in", [1024, 2048])
    convw_in = din("convw", [128, 8, 5])
    convb_in = din("convb", [128, 8])
    gaw = din("gaw", [2, 8, 128, 128])
    gxw = din("gxw", [2, 8, 128, 128])
    gab_in = din("gab", [128, 2, 8])
    gxb_in = din("gxb", [128, 2, 8])
    lam_in = din("lam", [128, 2, 8])
    w_out = din("w_out", [1024, 1024])
    w_qkv = din("w_qkv", [1024, 3072])
    dalam_in = din("dalam", [128, 4, 64])
    subg_in = din("subg", [128, 1])
    w_o = din("w_o", [1024, 1024])
    rw_in = din("rw", [128, 8, 32])
    rb_in = din("rb", [128, 32])
    wg = din("wg", [2, 32, 1024, 512])
    wu = din("wu", [2, 32, 1024, 512])
    wd = din("wd", [2, 32, 512, 1024])
    rmat_in = din("rmat", [128, 128])
    cos_in = din("cos", [128, NT])
    sin_in = din("sin", [128, NT])
    outT = nc.dram_tensor("outT", [8, 128, NQ], F32, kind="ExternalOutput").ap()
    xr = nc.dram_tensor("xr", [8, 128, NT], F32, kind="Internal").ap()
    zscr = nc.dram_tensor("zscr", [8, 128, NT], BF16, kind="Internal").ap()
    h2tm = nc.dram_tensor("h2tm", [NT, 1024], BF16, kind="Internal").ap()
    Xs = nc.dram_tensor("Xs", [100 * 128, 1024], BF16, kind="Internal").ap()
    Ys = nc.dram_tensor("Ys", [100 * 128, 1024], F32, kind="Internal").ap()
    wgb = nc.dram_tensor("wgb", [2 * 8192, 2048], BF16, kind="Internal").ap()
    wub = nc.dram_tensor("wub", [2 * 8192, 2048], BF16, kind="Internal").ap()
    wdb = nc.dram_tensor("wdb", [2 * 8192, 2048], BF16, kind="Internal").ap()
    pcol_in = din("pcol", [128, 1])
    blkoff_in = din("blkoff", [128, 100])
    dbgo = {}
    if dbg:
        for nm, shp in [("d_mod", [128, 2 * 48 * 2]), ("d_h", [128, 8, NT]), ("d_z", [8, 128, NT]),
                        ("d_x1", [8, 128, NT]), ("d_x2", [8, 128, NT]),
                        ("d_ao", [8, 128, NT])]:
            dbgo[nm] = nc.dram_tensor(nm, shp, F32, kind="ExternalOutput").ap()

    with contextlib.ExitStack() as st:
        S = Sched(nc, st)

        uid = [0]

        def sb(stack, name, shape, dt):
            uid[0] += 1
            return stack.enter_context(nc.sbuf_tensor(f"sb{uid[0]}_{name}", list(shape), dt))

        psbig = st.enter_context(nc.psum_tensor("psbig", [128, 8 * 512], F32))
        PS = [psbig[:, i * 512:(i + 1) * 512] for i in range(8)]
        pk = lambda i: ('ps', i)
        wgv_all = wg.rearrange("l e (q r) f -> (l e q) (r f)", r=4)
        wuv_all = wu.rearrange("l e (q r) f -> (l e q) (r f)", r=4)
        wdv_all = wd.rearrange("l e (q r) d -> (l e q) (r d)", r=2)

        def convert_experts(l, e0, e1):
            for e_ in range(e0, e1):
                r0 = (l * 32 + e_) * 256
                for dst, src in [(wgb, wgv_all), (wub, wuv_all), (wdb, wdv_all)]:
                    S.dma('pool', dst[r0:r0 + 256, :], src[r0:r0 + 256, :], writes=[('wcv', l)])

        ones_bf = sb(st, "ones_bf", [128, 128], BF16)
        ones32 = sb(st, "ones32", [128, 128], F32)
        ident = sb(st, "ident", [128, 128], F32)
        identb = sb(st, "identb", [128, 128], BF16)
        modT = sb(st, "modT", [128, 2, 48, 2], F32)
        gmT = sb(st, "gmT", [128, 2, 2, 8, 2], F32)
        n1g = sb(st, "n1g", [128, 2, 8], F32)
        n2g = sb(st, "n2g", [128, 2, 8], F32)
        fing = sb(st, "fing", [128, 8], F32)
        rw = sb(st, "rw", [128, 8, 32], F32)
        rb = sb(st, "rb", [128, 32], F32)
        S.op('dve', lambda e: e.memset(ones_bf[:], 1.0), writes=['ones_bf'])
        S.op('dve', lambda e: e.memset(ones32[:], 1.0), writes=['ones32'])
        S.op('pool', lambda e: e.memset(ident[:], 1.0), writes=['ident'])
        S.op('pool', lambda e: e.affine_select(out=ident[:], in_=ident[:], pattern=[[-1, 128]],
                                               compare_op=ALU.is_equal, fill=0.0, base=0, channel_multiplier=1),
             reads=['ident'], writes=['ident'])
        S.op('act', lambda e: e.activation(out=identb[:], in_=ident[:], func=AF.Identity), reads=['ident'], writes=['identb'])
        for t, src, k in [(n1g, n1g_in, 'n1g'), (n2g, n2g_in, 'n2g'), (fing, fing_in, 'fing'),
                          (rw, rw_in, 'rw'), (rb, rb_in, 'rb')]:
            S.dma('sp', t[:], src, writes=[k])

        with contextlib.ExitStack() as ph:
            condt = sb(ph, "condt", [128, 8, 2], F32)
            scond = sb(ph, "scond", [128, 8, 2], F32)
            bm = sb(ph, "bm", [128, 2, 48], F32)
            wm = [sb(ph, f"wm{i}", [128, 8, 1024], F32) for i in range(2)]
            S.dma('sp', condt[:], cond_in, writes=['cond'])
            S.dma('sp', bm[:], bmod_in, writes=['bm'])
            S.op('act', lambda e: e.activation(out=scond[:], in_=condt[:], func=AF.Silu),
                 reads=['cond'], writes=['scond'])
            it = 0
            for l in range(2):
                for gi in range(6):
                    buf = wm[it % 2]
                    key = ('wm', it % 2)
                    S.dma('sp', buf[:], w_mod[l, :, gi * 1024:(gi + 1) * 1024].rearrange("(k p) f -> p k f", p=128),
                          writes=[key])
                    pst = PS[it % 2]

                    def mm(e, buf=buf, pst=pst):
                        for j in range(8):
                            for kc in range(8):
                                ins = e.matmul(pst[:, j * 2:(j + 1) * 2], lhsT=buf[:, kc, j * 128:(j + 1) * 128],
                                               rhs=scond[:, kc, :], start=(kc == 0), stop=(kc == 7))
                        return ins
                    S.op('pe', mm, reads=[key, 'scond'], writes=[pk(it % 2)])
                    S.op('dve', lambda e: e.tensor_tensor(
                        out=modT[:, l, gi * 8:(gi + 1) * 8, :],
                        in0=pst[:, 0:16].rearrange("p (j r) -> p j r", r=2),
                        in1=bc_last(bm[:, l, gi * 8:(gi + 1) * 8], 2), op=ALU.add),
                        reads=[pk(it % 2), 'bm'], writes=['modT'])
                    it += 1
            for l in range(2):
                for w_, (gt, gk, sidx) in enumerate([(n1g, 'n1g', 1), (n2g, 'n2g', 4)]):
                    S.op('dve', lambda e: e.tensor_scalar(out=gmT[:, l, w_], in0=modT[:, l, sidx * 8:(sidx + 1) * 8, :],
                                                          scalar1=1.0, scalar2=None, op0=ALU.add),
                         reads=['modT'], writes=['gmT'])
                    S.op('dve', lambda e: e.tensor_tensor(out=gmT[:, l, w_], in0=gmT[:, l, w_],
                                                          in1=bc_last(gt[:, l, :], 2), op=ALU.mult),
                         reads=['gmT', gk], writes=['gmT'])
            if dbg:
                S.dma('sp', dbgo["d_mod"], modT[:].rearrange("p a b c -> p (a b c)"), reads=['modT'], writes=['d_mod'])
            S.barrier()

        def mod_ap(l, idx, j, r):
            return modT[:, l, idx * 8 + j, r:r + 1]

        def norm_mod(tmp, xt, n, kx, out, kout, gm_of_j, sh_of_j, psi, extra_reads=(), tag='', part=0):
            sq, rt, rstd, xn = tmp
            kxl = list(kx) if isinstance(kx, list) else [kx]
            if part in (0, 1):
                norm_stats(sq, rt, rstd, xt, n, kxl, psi, tag)
            if part in (0, 2):
                norm_apply(rstd, xn, xt, n, kxl, out, kout, gm_of_j, sh_of_j, extra_reads, tag)

        def norm_stats(sq, rt, rstd, xt, n, kxl, psi, tag):
            S.op('act', lambda e: e.activation(out=sq[:, :, :n], in_=xt[:, :, :n], func=AF.Square),
                 reads=kxl, writes=['nm_sq' + tag])

            def mm(e):
                for j in range(8):
                    ins = e.matmul(PS[psi][:, :n], lhsT=ones_bf[:], rhs=sq[:, j, :n], start=(j == 0), stop=(j == 7))
                return ins
            S.op('pe', mm, reads=['nm_sq' + tag, 'ones_bf'], writes=[pk(psi)])
            S.op('act', lambda e: e.activation(out=rt[:, :n], in_=PS[psi][:, :n], func=AF.Ln,
                                               bias=eps_t[:, 0:1], scale=1.0 / 1024.0),
                 reads=[pk(psi), 'eps'], writes=['nm_rt' + tag])
            S.op('act', lambda e: e.activation(out=rstd[:, :n], in_=rt[:, :n], func=AF.Exp, scale=-0.5),
                 reads=['nm_rt' + tag], writes=['nm_rstd' + tag])

        def norm_apply(rstd, xn, xt, n, kxl, out, kout, gm_of_j, sh_of_j, extra_reads, tag):
            S.op('dve', lambda e: e.tensor_tensor(out=xn[:, :, :n], in0=xt[:, :, :n], in1=bc_mid(rstd[:, :n], 8),
                                                  op=ALU.mult), reads=kxl + ['nm_rstd' + tag], writes=['nm_xn' + tag])
            for j in range(8):
                if sh_of_j is None:
                    S.op('dve', lambda e: e.tensor_scalar(out=out(j), in0=xn[:, j, :n], scalar1=gm_of_j(j),
                                                          scalar2=None, op0=ALU.mult),
                         reads=['nm_xn' + tag] + list(extra_reads), writes=[kout])
                elif j % 2 == 0:
                    S.op('dve', lambda e: e.tensor_scalar(out=out(j), in0=xn[:, j, :n], scalar1=gm_of_j(j),
                                                          scalar2=sh_of_j(j), op0=ALU.mult, op1=ALU.add),
                         reads=['nm_xn' + tag] + list(extra_reads), writes=[kout])
                else:
                    S.op('act', lambda e: e.activation(out=out(j), in_=xn[:, j, :n], func=AF.Identity,
                                                       bias=sh_of_j(j), scale=gm_of_j(j)),
                         reads=['nm_xn' + tag] + list(extra_reads), writes=[kout])

        def norm_tmp(ph):
            return (sb(ph, "nm_sq", [128, 8, 512], BF16), sb(ph, "nm_rt", [128, 512], F32),
                    sb(ph, "nm_rstd", [128, 512], F32), sb(ph, "nm_xn", [128, 8, 512], F32))

        eps_t = sb(st, "eps_t", [128, 1], F32)
        S.op('dve', lambda e: e.memset(eps_t[:], EPS), writes=['eps'])
        one_t = sb(st, "one_t", [128, 1], F32)
        S.op('dve', lambda e: e.memset(one_t[:], 1.0), writes=['one'])


        def phase_A(l, src, hbuf):
            with contextlib.ExitStack() as ph:
                xts = [sb(ph, f"xt{i}", [128, 8, 512], F32) for i in range(2)]
                tmps = [norm_tmp(ph), norm_tmp(ph)]

                def a_part(ti, part):
                    off, cnt, _, _, r = TILES[ti]
                    xt = xts[ti % 2]
                    kx = ('xt', ti % 2)
                    if part == 1:
                        S.dma('sp', xt[:, :, :cnt], src[:, :, off:off + cnt].rearrange("j p t -> p j t"),
                              reads=[('xr', ti)], writes=[kx])
                    norm_mod(tmps[ti % 2], xt, cnt, kx, lambda j: hbuf[:, j, off:off + cnt], ('h', ti),
                             lambda j: gmT[:, l, 0, j, r:r + 1], lambda j: mod_ap(l, 0, j, r), 7 - (ti % 2),
                             extra_reads=['gmT', 'modT'], tag='A' + str(ti % 2), part=part)
                a_part(0, 1)
                for ti in range(len(TILES)):
                    if ti + 1 < len(TILES):
                        a_part(ti + 1, 1)
                    a_part(ti, 2)
                S.barrier()

        hst = contextlib.ExitStack()
        hbuf = sb(hst, "hbuf", [128, 8, NT], BF16)
        phase_A(0, xc, hbuf)
        if dbg:
            with contextlib.ExitStack() as ph:
                t32 = sb(ph, "dbg32", [128, 8, 512], F32)
                for ti, (off, cnt, _, _, r) in enumerate(TILES):
                    S.op('dve', lambda e: e.tensor_copy(out=t32[:, :, :cnt], in_=hbuf[:, :, off:off + cnt]),
                         reads=[('h', ti)], writes=['dbg32'])
                    S.dma('sp', dbgo["d_h"][:, :, off:off + cnt], t32[:, :, :cnt], reads=['dbg32'], writes=['d_h'])
                S.barrier()

        with contextlib.ExitStack() as ph:
            ub = sb(ph, "ub", [128, UW], F32)
            uc = sb(ph, "uc", [128, UCW], F32)
            ucb = sb(ph, "ucb", [128, UCW], BF16)
            gy = sb(ph, "gy", [128, NT], BF16)
            wyu = [sb(ph, f"wyu{i}", [128, 8, 256], BF16) for i in range(2)]
            gw = [sb(ph, f"gw{i}", [128, 4, 128], BF16) for i in range(2)]
            convw = sb(ph, "convw", [128, 8, 5], F32)
            convb = sb(ph, "convb", [128, 8], F32)
            gab = sb(ph, "gab", [128, 2, 8], F32)
            gxb = sb(ph, "gxb", [128, 2, 8], F32)
            lamt = sb(ph, "lamt", [128, 2, 8], F32)
            cneg = sb(ph, "cneg", [128, 2, 8], F32)
            cneg2 = sb(ph, "cneg2", [128, 2, 8], F32)
            rbuf = [sb(ph, f"rbuf{i}", [128, 512], F32) for i in range(2)]
            ibuf = [sb(ph, f"ibuf{i}", [128, 512], F32) for i in range(2)]
            sbuf_ = [sb(ph, f"sbuf{i}", [128, 512], F32) for i in range(2)]
            hbt = [sb(ph, f"hbt{i}", [128, 512], F32) for i in range(2)]
            gt1 = [sb(ph, f"gt1{i}", [128, 512], F32) for i in range(2)]
            zt = [sb(ph, f"zt{i}", [128, 512], BF16) for i in range(2)]
            for t, src, k in [(convw, convw_in, 'convw'), (convb, convb_in, 'convb'), (gab, gab_in, 'gab'),
                              (gxb, gxb_in, 'gxb'), (lamt, lam_in, 'lamt')]:
                S.dma('sp', t[:], src, writes=[k])
            S.op('act', lambda e: e.activation(out=cneg[:], in_=lamt[:], func=AF.Exp, scale=-1.0),
                 reads=['lamt'], writes=['cneg'])
            S.op('act', lambda e: e.activation(out=cneg[:], in_=cneg[:], func=AF.Ln, bias=one_t[:, 0:1], scale=1.0),
                 reads=['cneg', 'one'], writes=['cneg'])
            S.op('dve', lambda e: e.tensor_scalar(out=cneg2[:], in0=cneg[:], scalar1=-16.0, scalar2=None, op0=ALU.mult),
                 reads=['cneg'], writes=['cneg2'])
            S.op('dve', lambda e: e.tensor_scalar(out=cneg[:], in0=cneg[:], scalar1=-8.0, scalar2=None, op0=ALU.mult),
                 reads=['cneg', 'cneg2'], writes=['cneg'])
            zer = sb(ph, "zer", [128, 4, 1024], BF16)
            S.op('pool', lambda e: e.memset(zer[:], 0.0), writes=['zer'])
            ngab = sb(ph, "ngab", [128, 2, 8], F32)
            ngxb = sb(ph, "ngxb", [128, 2, 8], F32)
            S.op('dve', lambda e: e.tensor_scalar(out=ngab[:], in0=gab[:], scalar1=-1.0, scalar2=None, op0=ALU.mult),
                 reads=['gab'], writes=['ngab'])
            S.op('dve', lambda e: e.tensor_scalar(out=ngxb[:], in0=gxb[:], scalar1=-1.0, scalar2=None, op0=ALU.mult),
                 reads=['gxb'], writes=['ngxb'])
            S.op('dve', lambda e: e.memset(ub[:], 0.0), writes=[('ub', ti) for ti in range(9)])
            ubkeys = [('ub', ti) for ti in range(9)]
            cnt_sc = 0
            for n in range(8):
                w = wyu[n % 2]
                kw_ = ('wyu', n % 2)
                S.dma('pool', w[:, :, 0:128], w_in[:, n * 128:(n + 1) * 128].rearrange("(k p) f -> p k f", p=128),
                      writes=[kw_])
                S.dma('pool', w[:, :, 128:256],
                      w_in[:, 1024 + n * 128:1024 + (n + 1) * 128].rearrange("(k p) f -> p k f", p=128), writes=[kw_])
                g = gw[n % 2]
                kg = ('gw', n % 2)
                for d in range(2):
                    S.dma('pool', g[:, 2 * d, :], gaw[d, n], writes=[kg])
                    S.dma('pool', g[:, 2 * d + 1, :], gxw[d, n], writes=[kg])
                convert_experts(0, 4 * n, 4 * n + 4)
                for b4 in range(4 * n, min(4 * n + 4, 25)):
                    S.dma('pool', Xs[b4 * 512:(b4 + 1) * 512, :].rearrange("(a p) f -> p a f", p=128), zer[:],
                          reads=['zer'], writes=['Xs'])
                for ti, (off, cnt, uoff, ucoff, r) in enumerate(TILES):
                    pu, py = (ti % 2) * 2, (ti % 2) * 2 + 1

                    def mmu(e, c0=128, p=pu):
                        for kc in range(8):
                            ins = e.matmul(PS[p][:, :cnt], lhsT=w[:, kc, c0:c0 + 128], rhs=hbuf[:, kc, off:off + cnt],
                                           start=(kc == 0), stop=(kc == 7))
                        return ins
                    S.op('pe', mmu, reads=[kw_, ('h', ti)], writes=[pk(pu)])
                    S.op('act', lambda e: e.activation(out=ub[:, uoff:uoff + cnt], in_=PS[pu][:, :cnt], func=AF.Identity),
                         reads=[pk(pu)], writes=[('ub', ti)])
                    S.op('pe', lambda e: mmu(e, 0, py), reads=[kw_, ('h', ti)], writes=[pk(py)])
                    t1 = gt1[ti % 2]
                    k1 = ('gt1', ti % 2)
                    S.op('act', lambda e: e.activation(out=t1[:, :cnt], in_=PS[py][:, :cnt], func=AF.Square),
                         reads=[pk(py)], writes=[k1])
                    S.op('dve', lambda e: e.tensor_scalar(out=t1[:, :cnt], in0=t1[:, :cnt], scalar1=0.044715, scalar2=1.0,
                                                          op0=ALU.mult, op1=ALU.add), reads=[k1], writes=[k1])
                    S.op('dve', lambda e: e.tensor_tensor(out=t1[:, :cnt], in0=t1[:, :cnt], in1=PS[py][:, :cnt], op=ALU.mult),
                         reads=[k1, pk(py)], writes=[k1])
                    S.op('act', lambda e: e.activation(out=t1[:, :cnt], in_=t1[:, :cnt], func=AF.Sigmoid,
                                                       scale=1.5957691216057308), reads=[k1], writes=[k1])
                    S.op('dve', lambda e: e.tensor_tensor(out=gy[:, off:off + cnt], in0=t1[:, :cnt], in1=PS[py][:, :cnt],
                                                          op=ALU.mult), reads=[k1, pk(py)], writes=[('gy', ti)])
                S.op('dve', lambda e: e.tensor_scalar(out=uc[:], in0=ub[:, 0:UCW], scalar1=convw[:, n, 0:1],
                                                      scalar2=convb[:, n:n + 1], op0=ALU.mult, op1=ALU.add),
                     reads=ubkeys + ['convw', 'convb'], writes=['uc'])
                for k in range(1, 5):
                    S.op('dve', lambda e: e.scalar_tensor_tensor(out=uc[:], in0=ub[:, k:k + UCW], scalar=convw[:, n, k:k + 1],
                                                                 in1=uc[:], op0=ALU.mult, op1=ALU.add),
                         reads=ubkeys + ['convw', 'uc'], writes=['uc'])
                S.op('act', lambda e: e.activation(out=ucb[:], in_=uc[:], func=AF.Identity), reads=['uc'], writes=['ucb'])
                for d in range(2):
                    order = [8] + (list(range(8)) if d == 0 else list(range(7, -1, -1)))
                    prev = None
                    for oi, ti in enumerate(order):
                        off, cnt, uoff, ucoff, r = TILES[ti]
                        b = cnt_sc % 2
                        cnt_sc += 1
                        pr, pi = 4 + 2 * b, 5 + 2 * b
                        S.op('pe', lambda e: e.matmul(PS[pr][:, :cnt], lhsT=g[:, 2 * d, :], rhs=ucb[:, ucoff:ucoff + cnt],
                                                      start=True, stop=True), reads=[kg, 'ucb'], writes=[pk(pr)])
                        S.op('pe', lambda e: e.matmul(PS[pi][:, :cnt], lhsT=g[:, 2 * d + 1, :], rhs=ucb[:, ucoff:ucoff + cnt],
                                                      start=True, stop=True), reads=[kg, 'ucb'], writes=[pk(pi)])
                        rb_, ib_, sb_, hb_ = rbuf[b], ibuf[b], sbuf_[b], hbt[b]
                        kr, ki, ks, kh = ('rbuf', b), ('ibuf', b), ('sbuf', b), ('hbt', b)
                        S.op('act', lambda e: e.activation(out=rb_[:, :cnt], in_=PS[pr][:, :cnt], func=AF.Exp,
                                                           bias=ngab[:, d, n:n + 1], scale=-1.0),
                             reads=[pk(pr), 'ngab'], writes=[kr])
                        S.op('act', lambda e: e.activation(out=ib_[:, :cnt], in_=PS[pi][:, :cnt], func=AF.Exp,
                                                           bias=ngxb[:, d, n:n + 1], scale=-1.0),
                             reads=[pk(pi), 'ngxb'], writes=[ki])
                        for t_, k_ in ((rb_, kr), (ib_, ki)):
                            S.op('act', lambda e: e.activation(out=t_[:, :cnt], in_=t_[:, :cnt], func=AF.Ln,
                                                               bias=one_t[:, 0:1], scale=1.0), reads=[k_, 'one'], writes=[k_])
                        for t_, k_ in ((rb_, kr), (ib_, ki)):
                            S.op('act', lambda e: e.activation(out=t_[:, :cnt], in_=t_[:, :cnt], func=AF.Exp, scale=-1.0),
                                 reads=[k_], writes=[k_])
                        S.op('act', lambda e: e.activation(out=sb_[:, :cnt], in_=rb_[:, :cnt], func=AF.Exp,
                                                           scale=cneg2[:, d, n:n + 1]), reads=[kr, 'cneg2'], writes=[ks])
                        S.op('act', lambda e: e.activation(out=rb_[:, :cnt], in_=rb_[:, :cnt], func=AF.Exp,
                                                           scale=cneg[:, d, n:n + 1]), reads=[kr, 'cneg'], writes=[kr])
                        S.op('act', lambda e: e.activation(out=sb_[:, :cnt], in_=sb_[:, :cnt], func=AF.Ln,
                                                           bias=one_t[:, 0:1], scale=-1.0), reads=[ks, 'one'], writes=[ks])
                        S.op('act', lambda e: e.activation(out=sb_[:, :cnt], in_=sb_[:, :cnt], func=AF.Exp, scale=0.5),
                             reads=[ks], writes=[ks])
                        S.op('dve', lambda e: e.tensor_tensor(out=ib_[:, :cnt], in0=ib_[:, :cnt],
                                                              in1=uc[:, ucoff:ucoff + cnt], op=ALU.mult),
                             reads=[ki, 'uc'], writes=[ki])
                        S.op('dve', lambda e: e.tensor_tensor(out=ib_[:, :cnt], in0=ib_[:, :cnt], in1=sb_[:, :cnt],
                                                              op=ALU.mult), reads=[ki, ks], writes=[ki])
                        if d == 0:
                            if prev is None:
                                init, kin = 0.0, []
                            else:
                                po, pc, puo, _, _ = TILES[prev]
                                init, kin = ub[:, puo + pc - 1:puo + pc], [('ub', prev)]
                            S.op('dve', lambda e: e.tensor_tensor_scan(out=ub[:, uoff:uoff + cnt], data0=rb_[:, :cnt],
                                                                       data1=ib_[:, :cnt], initial=init,
                                                                       op0=ALU.mult, op1=ALU.add),
                                 reads=[kr, ki] + kin, writes=[('ub', ti)])
                        else:
                            if prev is None:
                                init, kin = 0.0, []
                            else:
                                init, kin = hbt[1 - b][:, 0:1], [('hbt', 1 - b)]
                            S.op('dve', lambda e: e.tensor_tensor_scan(out=hb_[:, :cnt][:, ::-1], data0=rb_[:, :cnt][:, ::-1],
                                                                       data1=ib_[:, :cnt][:, ::-1], initial=init,
                                                                       op0=ALU.mult, op1=ALU.add),
                                 reads=[kr, ki] + kin, writes=[kh])
                            S.op('dve', lambda e: e.tensor_tensor(out=sb_[:, :cnt], in0=hb_[:, :cnt],
                                                                  in1=ub[:, uoff:uoff + cnt], op=ALU.add),
                                 reads=[kh, ('ub', ti)], writes=[ks])
                            z_ = zt[b]
                            S.op('dve', lambda e: e.tensor_tensor(out=z_[:, :cnt], in0=sb_[:, :cnt],
                                                                  in1=gy[:, off:off + cnt], op=ALU.mult),
                                 reads=[ks, ('gy', ti)], writes=[('zt', b)])
                            S.dma('sp', zscr[n, :, off:off + cnt], z_[:, :cnt], reads=[('zt', b)], writes=[('zs', ti)])
                        prev = ti
            S.barrier()
        hst.close()

        I32 = mybir.dt.int32
        MAXSUB = 34
        NBMAX = 100
        BIGW = 2 * 32 * 256 + 64
        utri = sb(st, "utri", [128, 128], F32)
        S.op('pool', lambda e: e.memset(utri[:], 1.0), writes=['utri'])
        S.op('pool', lambda e: e.affine_select(out=utri[:], in_=utri[:], pattern=[[1, 128]],
                                               compare_op=ALU.is_gt, fill=0.0, base=0, channel_multiplier=-1),
             reads=['utri'], writes=['utri'])
        pcol = sb(st, "pcol", [128, 1], F32)
        blkoff = sb(st, "blkoff", [128, NBMAX], F32)
        ones_row = sb(st, "ones_row", [128, 32], F32)
        S.dma('sp', pcol[:], pcol_in, writes=['pcol'])
        S.dma('sp', blkoff[:], blkoff_in, writes=['blkoff'])
        S.op('dve', lambda e: e.memset(ones_row[:], 1.0), writes=['ones_row'])
        onesb = sb(st, "onesb", [128, 4, 32], F32)
        c12 = sb(st, "c12", [128, 2, 4, 32], F32)
        S.op('dve', lambda e: e.memset(onesb[:], 1.0), writes=['onesb'])
        for k2_ in range(2):
            for s_ in range(4):
                S.op('dve', lambda e: e.memset(c12[:, k2_, s_, :], float(2 * s_ + 1 + k2_)), writes=['c12'])
        FM = sb(st, "FM", [128, MAXSUB, 2, 32], F32)
        RK = sb(st, "RK", [128, MAXSUB, 2], F32)
        WK = sb(st, "WK", [128, MAXSUB, 2], F32)
        DI = sb(st, "DI", [128, MAXSUB, 2], I32)
        WI = sb(st, "WI", [128, NBMAX, 2], I32)
        cntm = sb(st, "cntm", [128, 32], F32)

        bregs = {}

        def idma(out, out_off, in_, in_off, bounds, reads, writes, slot):
            S._wait('pool', S._deps('pool', reads, writes, skip_same_pe=False))
            if slot not in S.dsem:
                S.dsem[slot] = [S._newsem('d'), 0]
            d = S.dsem[slot]
            if bounds not in bregs:
                bregs[bounds] = nc.gpsimd.to_reg(bounds)
            ins = nc.gpsimd.indirect_dma_start(out=out, out_offset=out_off, in_=in_, in_offset=in_off,
                                               bounds_check=bregs[bounds], oob_is_err=False)
            d[1] += 16
            ins.then_inc(d[0], 16)
            S._commit((d[0], d[1]), reads, writes)

        WST = {}
        wgv = wgb.rearrange("(a b) f -> a (b f)", b=2)
        wuv = wub.rearrange("(a b) f -> a (b f)", b=2)
        wdv = wdb.rearrange("(a b) f -> a (b f)", b=2)

        def moe_blk(NB, p_):
            return (p_ % 4) * (NB // 4) + p_ // 4

        def moe_loadw(l, NB, p_):
            _, wgs, wus, wds = WST[l]
            w_ = p_ % 4
            for nm, view, tile_ in [('wgs', wgv, wgs[w_]), ('wus', wuv, wus[w_]), ('wds', wdv, wds[w_])]:
                idma(tile_[:, :], None, view[:, :], bass.IndirectOffsetOnAxis(ap=WI[:, moe_blk(NB, p_), 0:1], axis=0),
                     (l + 1) * 4096 - 1, reads=['WI', ('wcv', l)], writes=[(nm, w_)], slot=(nm, w_))

        def phase_C(l, wmat, tiles, xsrc):
            nsub_tot = sum(TILES[ti][1] // 128 for ti in tiles)
            NB = 2 * nsub_tot + 32
            with contextlib.ExitStack() as ph:
                wsb = sb(ph, "wsb", [128, 8, 1024], BF16)
                zts = [sb(ph, f"zts{i}", [128, 8, 512], BF16) for i in range(2)]
                xts = [sb(ph, f"xtc{i}", [128, 8, 512], F32) for i in range(2)]
                h2fs = [sb(ph, f"h2f{i}", [128, 8, 512], F32) for i in range(2)]
                h2t = [sb(ph, f"h2t{i}", [128, 1024], BF16) for i in range(2)]
                tmps = [norm_tmp(ph), norm_tmp(ph)]
                mk3 = lambda nm: [sb(ph, f"{nm}{i}", [128, 4, 32], F32) for i in range(2)]
                ssel, sg, em, mk, cmv, rk, t3 = mk3("ssel"), mk3("sg"), mk3("em"), mk3("mk"), mk3("cmv"), mk3("rk"), mk3("t3")
                mk16 = lambda nm: [sb(ph, f"{nm}{i}", [128, 16], F32) for i in range(2)]
                m1, m2, gs, gm_ = mk16("m1"), mk16("m2"), mk16("gs"), mk16("gmk")
                sm = [sb(ph, f"sm{i}", [128, 4, 4], F32) for i in range(2)]
                t32 = [sb(ph, f"t32{i}", [128, 32], F32) for i in range(2)]
                S.dma('pool', wsb[:], wmat.rearrange("(k p) f -> p k f", p=128), writes=['wsb'])
                S.op('dve', lambda e: e.memset(cntm[:], 0.0), writes=['cntm'])
                nsub_box = [0]

                def stage_W(ti):
                    off, cnt, _, _, r = TILES[ti]
                    b = ti % 2
                    z_, x_ = zts[b], xts[b]
                    h2f, tmp, kh2 = h2fs[b], tmps[b], ('h2f', b)
                    kz, kx = ('zts', b), ('xtc', b)
                    S.dma('sp', z_[:, :, :cnt], zscr[:, :, off:off + cnt].rearrange("j p t -> p j t"),
                          reads=[('zs', ti)], writes=[kz])
                    S.dma('sp', x_[:, :, :cnt], xsrc[:, :, off:off + cnt].rearrange("j p t -> p j t"),
                          reads=[('xr', ti)], writes=[kx])
                    for j in range(8):
                        p = j % 3

                        def mm(e):
                            for kc in range(8):
                                ins = e.matmul(PS[p][:, :cnt], lhsT=wsb[:, kc, j * 128:(j + 1) * 128], rhs=z_[:, kc, :cnt],
                                               start=(kc == 0), stop=(kc == 7))
                            return ins
                        S.op('pe', mm, reads=['wsb', kz], writes=[pk(p)])
                        S.op('dve', lambda e: e.scalar_tensor_tensor(out=x_[:, j, :cnt], in0=PS[p][:, :cnt],
                                                                     scalar=mod_ap(l, 2, j, r), in1=x_[:, j, :cnt],
                                                                     op0=ALU.mult, op1=ALU.add),
                             reads=[pk(p), kx, 'modT'], writes=[kx])
                    S.dma('sp', xr[:, :, off:off + cnt].rearrange("j p t -> p j t"), x_[:, :, :cnt],
                          reads=[kx], writes=[('xr', ti)])
                    if dbg and l == 0:
                        S.dma('sp', dbgo["d_x1"][:, :, off:off + cnt].rearrange("j p t -> p j t"), x_[:, :, :cnt],
                              reads=[kx], writes=['d_x1'])

                def stage_N(ti):
                    off, cnt, _, _, r = TILES[ti]
                    b = ti % 2
                    z_, x_ = zts[b], xts[b]
                    h2f, tmp, kh2 = h2fs[b], tmps[b], ('h2f', b)
                    kz, kx = ('zts', b), ('xtc', b)
                    norm_mod(tmp, x_, cnt, kx, lambda j: h2f[:, j, :cnt], kh2,
                             lambda j: gmT[:, l, 1, j, r:r + 1], lambda j: mod_ap(l, 3, j, r), 3,
                             extra_reads=['gmT', 'modT'], tag=str(b))

                def stage_R(ti):
                    off, cnt, _, _, r = TILES[ti]
                    b = ti % 2
                    z_, x_ = zts[b], xts[b]
                    h2f, tmp, kh2 = h2fs[b], tmps[b], ('h2f', b)
                    kz, kx = ('zts', b), ('xtc', b)
                    nsb = cnt // 128
                    gs0 = nsub_box[0]
                    nsub_box[0] += nsb
                    q = ti % 2
                    pl = 4 + q
                    W_ = nsb * 32
                    for s in range(nsb):
                        gsi = gs0 + s
                        qq = gsi % 2
                        for half in range(2):
                            pt = 6 + half

                            def mmt(e):
                                for jj in range(4):
                                    j = half * 4 + jj
                                    ins = e.transpose(out=PS[pt][:, jj * 128:(jj + 1) * 128],
                                                      in_=h2f[:, j, s * 128:(s + 1) * 128], identity=ident[:])
                                return ins
                            S.op('pe', mmt, reads=[kh2, 'ident'], writes=[pk(pt)])
                            if half == 0:
                                S.op('act', lambda e: e.activation(out=h2t[qq][:, 0:512], in_=PS[pt][:, :], func=AF.Identity),
                                     reads=[pk(pt)], writes=[('h2t', qq)])
                            else:
                                S.op('pool' if False else 'dve', lambda e: e.tensor_copy(out=h2t[qq][:, 512:1024], in_=PS[pt][:, :]),
                                     reads=[pk(pt)], writes=[('h2t', qq)])
                        S.dma('sp', h2tm[gsi * 128:(gsi + 1) * 128, :], h2t[qq][:], reads=[('h2t', qq)], writes=[('h2tm', gsi % 4)])

                        def mmr(e):
                            for kc in range(8):
                                ins = e.matmul(PS[pl][:, s * 32:(s + 1) * 32], lhsT=h2f[:, kc, s * 128:(s + 1) * 128], rhs=rw[:, kc, :],
                                               start=(kc == 0), stop=(kc == 7))
                            return ins
                        S.op('pe', mmr, reads=[kh2, 'rw'], writes=[pk(pl)])
                    kq = ('rt', q)
                    v3 = lambda t: t[:, :nsb, :]
                    v8 = lambda t: t[:, :nsb, :].rearrange("p s (g e) -> p (s g) e", e=8)
                    f2 = lambda t: t[:, :nsb, :].rearrange("p s e -> p (s e)")
                    g4 = lambda t: t[:, :nsb * 4]
                    g43 = lambda t: t[:, :nsb * 4].rearrange("p (s g) -> p s g", g=4)
                    S.op('act', lambda e: e.activation(out=f2(sg[q]), in_=PS[pl][:, :W_], func=AF.Sigmoid),
                         reads=[pk(pl)], writes=[kq])
                    S.op('dve', lambda e: e.tensor_tensor(out=v3(ssel[q]), in0=v3(sg[q]), in1=bc_mid(rb[:], nsb), op=ALU.add),
                         reads=[kq, 'rb'], writes=[kq])
                    S.op('dve', lambda e: e.tensor_reduce(out=g4(m1[q]), in_=v8(ssel[q]), axis=AX.X, op=ALU.max), reads=[kq], writes=[kq])
                    S.op('dve', lambda e: e.tensor_tensor(out=v8(t3[q]), in0=v8(ssel[q]), in1=bc_last(g4(m1[q]), 8), op=ALU.is_equal),
                         reads=[kq], writes=[kq])
                    S.op('dve', lambda e: e.scalar_tensor_tensor(out=f2(t3[q]), in0=f2(t3[q]), scalar=-1.0e9, in1=f2(ssel[q]),
                                                                 op0=ALU.mult, op1=ALU.add), reads=[kq], writes=[kq])
                    S.op('dve', lambda e: e.tensor_reduce(out=g4(m2[q]), in_=v8(t3[q]), axis=AX.X, op=ALU.max), reads=[kq], writes=[kq])
                    S.op('dve', lambda e: e.tensor_tensor(out=g4(gs[q]), in0=g4(m1[q]), in1=g4(m2[q]), op=ALU.add), reads=[kq], writes=[kq])
                    S.op('dve', lambda e: e.tensor_reduce(out=sm[q][:, 0, :nsb], in_=g43(gs[q]), axis=AX.X, op=ALU.max),
                         reads=[kq], writes=[kq])
                    S.op('dve', lambda e: e.tensor_tensor(out=g43(gm_[q]), in0=g43(gs[q]), in1=bc_last(sm[q][:, 0, :nsb], 4),
                                                          op=ALU.is_equal), reads=[kq], writes=[kq])
                    S.op('dve', lambda e: e.tensor_tensor(out=g4(gs[q]), in0=g4(gm_[q]), in1=g4(m2[q]), op=ALU.mult), reads=[kq], writes=[kq])
                    S.op('dve', lambda e: e.tensor_reduce(out=sm[q][:, 1, :nsb], in_=g43(gs[q]), axis=AX.X, op=ALU.add),
                         reads=[kq], writes=[kq])
                    S.op('dve', lambda e: e.tensor_tensor(out=v3(mk[q]), in0=v3(ssel[q]), in1=bc_last(sm[q][:, 1, :nsb], 32),
                                                          op=ALU.is_ge), reads=[kq], writes=[kq])
                    S.op('dve', lambda e: e.tensor_tensor(out=v8(mk[q]), in0=v8(mk[q]), in1=bc_last(g4(gm_[q]), 8), op=ALU.mult),
                         reads=[kq], writes=[kq])
                    S.op('dve', lambda e: e.tensor_tensor(out=f2(em[q]), in0=f2(mk[q]), in1=f2(sg[q]), op=ALU.mult),
                         reads=[kq], writes=[kq])
                    S.op('dve', lambda e: e.tensor_reduce(out=sm[q][:, 2, :nsb], in_=v3(em[q]), axis=AX.X, op=ALU.add),
                         reads=[kq], writes=[kq])
                    S.op('dve', lambda e: e.reciprocal(out=sm[q][:, 3, :nsb], in_=sm[q][:, 2, :nsb]), reads=[kq], writes=[kq])
                    S.op('dve', lambda e: e.tensor_tensor(out=v3(em[q]), in0=v3(em[q]), in1=bc_last(sm[q][:, 3, :nsb], 32), op=ALU.mult),
                         reads=[kq], writes=[kq])

                    def mmk(e):
                        for s in range(nsb):
                            o_ = PS[pl][:, 128 + s * 32:128 + (s + 1) * 32]
                            e.matmul(o_, lhsT=utri[:], rhs=mk[q][:, s, :], start=True, stop=False)
                            for s2 in range(s):
                                e.matmul(o_, lhsT=ones32[:], rhs=mk[q][:, s2, :], start=False, stop=False)
                            ins = e.matmul(o_, lhsT=ones32[:], rhs=cntm[:], start=False, stop=True)
                        return ins
                    S.op('pe', mmk, reads=[kq, 'utri', 'ones32', 'cntm'], writes=[pk(pl)])
                    S.op('dve', lambda e: e.tensor_copy(out=f2(rk[q]), in_=PS[pl][:, 128:128 + W_]), reads=[pk(pl)], writes=[kq])
                    S.op('dve', lambda e: e.tensor_reduce(out=t32[q][:], in_=mk[q][:, :nsb, :].rearrange("p s e -> p e s"), axis=AX.X,
                                                          op=ALU.add), reads=[kq], writes=[kq])
                    S.op('dve', lambda e: e.tensor_tensor(out=cntm[:], in0=cntm[:], in1=t32[q][:], op=ALU.add),
                         reads=[kq, 'cntm'], writes=['cntm'])
                    S.op('dve', lambda e: e.tensor_tensor_scan(out=f2(cmv[q]), data0=f2(onesb), data1=f2(mk[q]), initial=0.0,
                                                               op0=ALU.mult, op1=ALU.add), reads=[kq, 'onesb'], writes=[kq])
                    for k2 in range(2):
                        fm_ = FM[:, gs0:gs0 + nsb, k2, :]
                        S.op('dve', lambda e: e.tensor_tensor(out=v3(t3[q]), in0=v3(cmv[q]), in1=c12[:, k2, :nsb, :], op=ALU.is_equal),
                             reads=[kq, 'c12'], writes=[kq])
                        S.op('dve', lambda e: e.tensor_tensor(out=fm_, in0=v3(t3[q]), in1=v3(mk[q]), op=ALU.mult),
                             reads=[kq], writes=['FM'])
                        S.op('dve', lambda e: e.tensor_tensor(out=v3(t3[q]), in0=fm_, in1=v3(rk[q]), op=ALU.mult),
                             reads=[kq, 'FM'], writes=[kq])
                        S.op('dve', lambda e: e.tensor_reduce(out=RK[:, gs0:gs0 + nsb, k2], in_=v3(t3[q]), axis=AX.X, op=ALU.add),
                             reads=[kq], writes=['RK'])
                        S.op('dve', lambda e: e.tensor_tensor(out=v3(t3[q]), in0=fm_, in1=v3(em[q]), op=ALU.mult),
                             reads=[kq, 'FM'], writes=[kq])
                        S.op('dve', lambda e: e.tensor_reduce(out=WK[:, gs0:gs0 + nsb, k2], in_=v3(t3[q]), axis=AX.X, op=ALU.add),
                             reads=[kq], writes=['WK'])

                stage_W(tiles[0])
                for i_, ti in enumerate(tiles):
                    stage_N(ti)
                    if i_ + 1 < len(tiles):
                        stage_W(tiles[i_ + 1])
                    stage_R(ti)
                S.barrier()
            wst = contextlib.ExitStack()
            WST[l] = (wst,
                      [sb(wst, f"wgs{i}", [128, 4096], BF16) for i in range(4)],
                      [sb(wst, f"wus{i}", [128, 4096], BF16) for i in range(4)],
                      [sb(wst, f"wds{i}", [128, 4096], BF16) for i in range(4)])
            with contextlib.ExitStack() as ph:
                J = nsub_tot
                cb = sb(ph, "cb", [128, 32], F32)
                nblk = sb(ph, "nblk", [128, 32], F32)
                pend = sb(ph, "pend", [128, 32], F32)
                pst = sb(ph, "pst", [128, 32], F32)
                big = sb(ph, "bigc", [128, 32, NBMAX], F32)
                eb = sb(ph, "eb", [128, NBMAX], F32)
                chg = sb(ph, "chg", [128, NBMAX], F32)
                wif = sb(ph, "wif", [128, NBMAX, 2], F32)
                dtmp2 = sb(ph, "dtmp2", [128, MAXSUB, 32], F32)
                dif = sb(ph, "dif", [128, MAXSUB, 2], F32)
                rows = [sb(ph, f"rows{i}", [128, 1024], BF16) for i in range(4)]
                S.op('pe', lambda e: e.matmul(PS[0][:, 0:32], lhsT=ones32[:], rhs=cntm[:], start=True, stop=True),
                     reads=['ones32', 'cntm'], writes=[pk(0)])
                S.op('dve', lambda e: e.tensor_copy(out=cb[:], in_=PS[0][:, 0:32]), reads=[pk(0)], writes=['cb'])
                S.op('dve', lambda e: e.tensor_tensor(out=big[:, :, :J], in0=bc_last(cb[:], J), in1=bc_mid(blkoff[:, :J], 32),
                                                      op=ALU.is_gt), reads=['cb', 'blkoff'], writes=['big'])
                S.op('dve', lambda e: e.tensor_reduce(out=nblk[:], in_=big[:, :, :J], axis=AX.X, op=ALU.add),
                     reads=['big'], writes=['nblk'])
                S.op('dve', lambda e: e.tensor_tensor_scan(out=pend[:], data0=ones_row[:], data1=nblk[:], initial=0.0,
                                                           op0=ALU.mult, op1=ALU.add), reads=['nblk', 'ones_row'], writes=['pend'])
                S.op('dve', lambda e: e.tensor_tensor(out=pst[:], in0=pend[:], in1=nblk[:], op=ALU.subtract),
                     reads=['pend', 'nblk'], writes=['pst'])
                S.op('dve', lambda e: e.tensor_scalar(out=pst[:], in0=pst[:], scalar1=128.0, scalar2=None, op0=ALU.mult),
                     reads=['pst'], writes=['pst'])
                S.op('dve', lambda e: e.tensor_scalar(out=pend[:], in0=pend[:], scalar1=128.0, scalar2=None, op0=ALU.mult),
                     reads=['pend'], writes=['pend'])
                S.op('dve', lambda e: e.tensor_tensor(out=big[:, :, :NB].rearrange("p e b -> p b e"),
                                                      in0=bc_mid(pend[:], NB), in1=bc_last(blkoff[:, :NB], 32),
                                                      op=ALU.is_le), reads=['pend', 'blkoff', 'big'], writes=['big'])
                S.op('dve', lambda e: e.tensor_reduce(out=eb[:, :NB], in_=big[:, :, :NB].rearrange("p e b -> p b e"),
                                                      axis=AX.X, op=ALU.add), reads=['big'], writes=['eb'])
                S.op('dve', lambda e: e.tensor_scalar(out=eb[:, :NB], in0=eb[:, :NB], scalar1=31.0, scalar2=None, op0=ALU.min),
                     reads=['eb'], writes=['eb'])
                S.op('dve', lambda e: e.memset(chg[:], 1.0), writes=['chg'])
                S.op('dve', lambda e: e.tensor_tensor(out=chg[:, 1:NB], in0=eb[:, 1:NB], in1=eb[:, 0:NB - 1], op=ALU.not_equal),
                     reads=['eb', 'chg'], writes=['chg'])
                for k4 in range(1, 4):
                    S.op('dve', lambda e: e.memset(chg[:, k4 * (NB // 4):k4 * (NB // 4) + 1], 1.0), reads=['chg'], writes=['chg'])
                for h in range(1):
                    S.op('dve', lambda e: e.tensor_scalar(out=wif[:, :NB, h], in0=eb[:, :NB], scalar1=128.0,
                                                          scalar2=float(l * 4096 - BIGW), op0=ALU.mult, op1=ALU.add),
                         reads=['eb', 'wif'], writes=['wif'])
                    S.op('dve', lambda e: e.tensor_scalar(out=wif[:, :NB, h], in0=wif[:, :NB, h], scalar1=pcol[:, 0:1],
                                                          scalar2=None, op0=ALU.add), reads=['wif', 'pcol'], writes=['wif'])
                    S.op('dve', lambda e: e.tensor_tensor(out=wif[:, :NB, h], in0=wif[:, :NB, h], in1=chg[:, :NB], op=ALU.mult),
                         reads=['wif', 'chg'], writes=['wif'])
                    S.op('dve', lambda e: e.tensor_scalar(out=wif[:, :NB, h], in0=wif[:, :NB, h], scalar1=float(BIGW),
                                                          scalar2=None, op0=ALU.add), reads=['wif'], writes=['wif'])
                S.op('dve', lambda e: e.tensor_copy(out=WI[:, :NB, 0:1], in_=wif[:, :NB, 0:1]), reads=['wif'], writes=['WI'])
                for k2 in range(2):
                    S.op('dve', lambda e: e.tensor_tensor(out=dtmp2[:, :J, :], in0=FM[:, :J, k2, :], in1=bc_mid(pst[:], J),
                                                          op=ALU.mult), reads=['FM', 'pst', 'dtmp2'], writes=['dtmp2'])
                    S.op('dve', lambda e: e.tensor_reduce(out=dif[:, :J, k2], in_=dtmp2[:, :J, :], axis=AX.X, op=ALU.add),
                         reads=['dtmp2', 'dif'], writes=['dif'])
                S.op('dve', lambda e: e.tensor_tensor(out=dif[:, :J, :], in0=dif[:, :J, :], in1=RK[:, :J, :], op=ALU.add),
                     reads=['dif', 'RK'], writes=['dif'])
                S.op('dve', lambda e: e.tensor_copy(out=DI[:, :J, :], in_=dif[:, :J, :]), reads=['dif'], writes=['DI'])
                for p_ in range(3):
                    moe_loadw(l, NB, p_)
                for gsi in range(J):
                    q = gsi % 4
                    S.dma('sp', rows[q][:], h2tm[gsi * 128:(gsi + 1) * 128, :], reads=[('h2tm', gsi % 4)], writes=[('rows', q)])
                    for k2 in range(2):
                        idma(Xs[:, :], bass.IndirectOffsetOnAxis(ap=DI[:, gsi, k2:k2 + 1], axis=0), rows[q][:, :], None,
                             NB * 128 - 1, reads=[('rows', q), 'DI', 'Xs'], writes=[('Xsc', q)], slot=('Xsc', q))
                S.barrier()

        def phase_D(l, tiles, final):
            nsub_tot = sum(TILES[ti][1] // 128 for ti in tiles)
            NB = 2 * nsub_tot + 32
            with contextlib.ExitStack() as ph:
                _, wgs, wus, wds = WST[l]
                blk = lambda p_: moe_blk(NB, p_)
                xbs = [sb(ph, f"xbs{i}", [128, 1024], BF16) for i in range(2)]
                XTs = [sb(ph, f"XTs{i}", [128, 8, 128], BF16) for i in range(2)]
                s1s = [sb(ph, f"s1s{i}", [128, 512], F32) for i in range(2)]
                ATs = [sb(ph, f"ATs{i}", [128, 4, 128], BF16) for i in range(2)]
                Yts = [sb(ph, f"Yts{i}", [128, 1024], F32) for i in range(2)]

                loadw = lambda p_: moe_loadw(l, NB, p_)
                def xbload(p_):
                    bn = blk(p_)
                    S.dma('sp', xbs[p_ % 2][:], Xs[bn * 128:(bn + 1) * 128, :], reads=[('Xsc', 0), ('Xsc', 1), ('Xsc', 2), ('Xsc', 3), 'Xs'],
                          writes=[('xbs', p_ % 2)])

                def stage_T(p_):
                    q = p_ % 2
                    xb, XT = xbs[q], XTs[q]
                    ptb = PS[q].bitcast(BF16)

                    def mmt(e):
                        for c in range(8):
                            ins = e.transpose(out=ptb[:, c * 128:(c + 1) * 128], in_=xb[:, c:1024:8], identity=identb[:])
                        return ins
                    S.op('pe', mmt, reads=[('xbs', q), 'identb'], writes=[pk(q)])
                    S.op('act', lambda e: e.activation(out=XT[:].rearrange("p c s -> p (c s)"), in_=ptb[:, :], func=AF.Identity),
                         reads=[pk(q)], writes=[('XTs', q)])

                def stage_G(p_):
                    q = p_ % 2
                    ws_ = p_ % 4
                    XT, AT = XTs[q], ATs[q]
                    wg_, wu_ = wgs[ws_], wus[ws_]
                    p1, p2 = 2 + 2 * q, 3 + 2 * q

                    def mmg(e, wt, p):
                        for fo in range(4):
                            for c in range(8):
                                c0 = c * 512 + fo
                                ins = e.matmul(PS[p][:, fo * 128:(fo + 1) * 128], lhsT=wt[:, c0:c0 + 509:4], rhs=XT[:, c, :],
                                               start=(c == 0), stop=(c == 7))
                        return ins
                    S.op('pe', lambda e: mmg(e, wg_, p1), reads=[('wgs', ws_), ('XTs', q)], writes=[pk(p1)])
                    S.op('pe', lambda e: mmg(e, wu_, p2), reads=[('wus', ws_), ('XTs', q)], writes=[pk(p2)])
                    S.op('act', lambda e: e.activation(out=s1s[q][:], in_=PS[p1][:, :], func=AF.Silu),
                         reads=[pk(p1)], writes=[('s1s', q)])
                    S.op('dve', lambda e: e.tensor_tensor(out=AT[:].rearrange("p c s -> p (c s)"), in0=s1s[q][:], in1=PS[p2][:, :],
                                                          op=ALU.mult), reads=[('s1s', q), pk(p2)], writes=[('ATs', q)])

                def stage_D(p_):
                    q = p_ % 2
                    ws_ = p_ % 4
                    b = blk(p_)
                    AT, Yt, wd_ = ATs[q], Yts[q], wds[ws_]
                    for dh in range(2):
                        py = 6 + dh

                        def mmd(e):
                            for fo in range(4):
                                ins = e.matmul(PS[py][:, :], lhsT=AT[:, fo, :],
                                               rhs=wd_[:, fo * 1024 + dh * 512:fo * 1024 + (dh + 1) * 512],
                                               start=(fo == 0), stop=(fo == 3))
                            return ins
                        S.op('pe', mmd, reads=[('wds', ws_), ('ATs', q)], writes=[pk(py)])
                        if dh == 0:
                            S.op('act', lambda e: e.activation(out=Yt[:, 0:512], in_=PS[py][:, :], func=AF.Identity),
                                 reads=[pk(py)], writes=[('Yts', q)])
                        else:
                            S.op('dve', lambda e: e.tensor_copy(out=Yt[:, 512:1024], in_=PS[py][:, :]),
                                 reads=[pk(py)], writes=[('Yts', q)])
                    S.dma('sp', Ys[b * 128:(b + 1) * 128, :], Yt[:], reads=[('Yts', q)], writes=[('Ys', q)])

                xbload(0)
                xbload(1)
                stage_T(0)
                for pos in range(NB):
                    if pos + 3 < NB:
                        loadw(pos + 3)
                    stage_G(pos)
                    if pos + 1 < NB:
                        stage_T(pos + 1)
                    if pos + 2 < NB:
                        xbload(pos + 2)
                    stage_D(pos)
                S.barrier()
            WST[l][0].close()
            with contextlib.ExitStack() as ph3:
                xts = [sb(ph3, f"xtd{i}", [128, 8, 512], F32) for i in range(2)]
                g1 = [sb(ph3, f"g1{i}", [128, 1024], F32) for i in range(4)]
                g2 = [sb(ph3, f"g2{i}", [128, 1024], F32) for i in range(4)]
                tmp = norm_tmp(ph3) if final else None
                ots = [sb(ph3, f"otd{i}", [128, 8, 512], F32) for i in range(2)] if final else None
                subs = []
                for li, ti in enumerate(tiles):
                    for s_ in range(TILES[ti][1] // 128):
                        subs.append((li, ti, s_, len(subs)))

                def stage_a(li, ti, s, gsi):
                    off, cnt, _, _, r = TILES[ti]
                    b = li % 2
                    if s == 0:
                        S.dma('sp', xts[b][:, :, :cnt], xr[:, :, off:off + cnt].rearrange("j p t -> p j t"),
                              reads=[('xr', ti)], writes=[('xtd', b, j) for j in range(8)])
                    q = gsi % 4
                    idma(g1[q][:, :], None, Ys[:, :], bass.IndirectOffsetOnAxis(ap=DI[:, gsi, 0:1], axis=0),
                         NB * 128 - 1, reads=['DI', ('Ys', 0), ('Ys', 1)], writes=[('g1', q)], slot=('g1', q))
                    idma(g2[q][:, :], None, Ys[:, :], bass.IndirectOffsetOnAxis(ap=DI[:, gsi, 1:2], axis=0),
                         NB * 128 - 1, reads=['DI', ('Ys', 0), ('Ys', 1)], writes=[('g2', q)], slot=('g2', q))
                    S.op('dve', lambda e: e.tensor_scalar(out=g1[q][:], in0=g1[q][:], scalar1=WK[:, gsi, 0:1], scalar2=None,
                                                          op0=ALU.mult), reads=[('g1', q), 'WK'], writes=[('g1', q)])
                    S.op('dve', lambda e: e.scalar_tensor_tensor(out=g1[q][:], in0=g2[q][:], scalar=WK[:, gsi, 1:2],
                                                                 in1=g1[q][:], op0=ALU.mult, op1=ALU.add),
                         reads=[('g1', q), ('g2', q), 'WK'], writes=[('g1', q)])
                    for half in range(2):
                        pt = 2 * (q % 2) + half

                        def mmt2(e):
                            for jj in range(4):
                                j = half * 4 + jj
                                ins = e.transpose(out=PS[pt][:, jj * 128:(jj + 1) * 128], in_=g1[q][:, j * 128:(j + 1) * 128],
                                                  identity=ident[:])
                            return ins
                        S.op('pe', mmt2, reads=[('g1', q), 'ident'], writes=[pk(pt)])

                def stage_b(li, ti, s, gsi):
                    off, cnt, _, _, r = TILES[ti]
                    b = li % 2
                    x_ = xts[b]
                    q = gsi % 4
                    kxs = [('xtd', b, j) for j in range(8)]
                    for half in range(2):
                        pt = 2 * (q % 2) + half
                        for jj in range(4):
                            j = half * 4 + jj
                            S.op('dve', lambda e: e.scalar_tensor_tensor(out=x_[:, j, s * 128:(s + 1) * 128],
                                                                         in0=PS[pt][:, jj * 128:(jj + 1) * 128],
                                                                         scalar=mod_ap(l, 5, j, r),
                                                                         in1=x_[:, j, s * 128:(s + 1) * 128],
                                                                         op0=ALU.mult, op1=ALU.add),
                                 reads=[pk(pt), kxs[j], ('modT', l)], writes=[kxs[j]])
                    if s == cnt // 128 - 1:
                        if not final:
                            S.dma('sp', xr[:, :, off:off + cnt].rearrange("j p t -> p j t"), x_[:, :, :cnt],
                                  reads=kxs, writes=[('xr', ti)])
                            if dbg:
                                S.dma('sp', dbgo["d_x2"][:, :, off:off + cnt].rearrange("j p t -> p j t"), x_[:, :, :cnt],
                                      reads=kxs, writes=['d_x2'])
                        else:
                            o_ = ots[b]
                            norm_mod(tmp, x_, cnt, kxs, lambda j: o_[:, j, :cnt], ('otd', b),
                                     lambda j: fing[:, j:j + 1], None, 7, extra_reads=['fing'])
                            S.dma('sp', outT[:, :, off:off + cnt].rearrange("j p t -> p j t"), o_[:, :, :cnt],
                                  reads=[('otd', b)], writes=[('out', b)])

                for idx in range(len(subs) + 1):
                    if idx < len(subs):
                        stage_a(*subs[idx])
                    if idx >= 1:
                        stage_b(*subs[idx - 1])
                S.barrier()

        phase_C(0, w_out, list(range(9)), xc)
        phase_D(0, list(range(9)), final=False)

        hst = contextlib.ExitStack()
        hbuf = sb(hst, "hbuf1", [128, 8, NT], BF16)
        phase_A(1, xr, hbuf)
        with contextlib.ExitStack() as ph:
            cost = sb(ph, "cost", [128, NT], F32)
            sint = sb(ph, "sint", [128, NT], F32)
            dal = sb(ph, "dal", [128, 4, 64], F32)
            dtmp = sb(ph, "dtmp", [128, 64], F32)
            lamv = sb(ph, "lamv", [128, 4], F32)
            subg = sb(ph, "subg", [128, 1], F32)
            wq = sb(ph, "wq", [128, 8, 128], BF16)
            wk = sb(ph, "wk", [128, 8, 128], BF16)
            wv = sb(ph, "wv", [128, 8, 128], BF16)
            qbt = [sb(ph, f"qbt{i}", [128, 512], BF16) for i in range(2)]
            rmb = sb(ph, "rmb", [128, 128], BF16)
            QT = sb(ph, "QT", [128, NQ], BF16)
            vtb = [sb(ph, f"vtb{i}", [128, 512], BF16) for i in range(2)]
            KT = sb(ph, "KT", [128, NT], BF16)
            Vt = sb(ph, "Vt", [128, 34, 128], BF16)
            rt1 = [sb(ph, f"rt1{i}", [128, 512], F32) for i in range(2)]
            rt2 = [sb(ph, f"rt2{i}", [128, 512], F32) for i in range(2)]
            Eb2 = [sb(ph, f"Eb{i}", [128, 2, 512], BF16) for i in range(2)]
            acc2 = sb(ph, "acc2", [128, 2, 512], F32)
            obw = sb(ph, "obw", [128, 2, 512], F32)
            ob = [obw[:, 0, :], obw[:, 1, :]]
            rlw = sb(ph, "rlw", [128, 2, 512], F32)
            rl = sb(ph, "rl", [128, 512], F32)
            accD = sb(ph, "accD", [128, 512], F32)
            accP = sb(ph, "accP", [128, 512], F32)
            osq = sb(ph, "osq", [128, 512], F32)
            aot = [sb(ph, f"aot{i}", [128, 512], BF16) for i in range(2)]
            S.dma('sp', cost[:], cos_in, writes=['cos'])
            S.dma('pool', rmb[:], rmat_in, writes=['rmb'])
            S.dma('sp', sint[:], sin_in, writes=['sin'])
            S.dma('sp', dal[:], dalam_in, writes=['dal'])
            S.dma('sp', subg[:], subg_in, writes=['subg'])
            for i2 in range(2):
                S.op('dve', lambda e: e.tensor_tensor(out=dtmp[:], in0=dal[:, 2 * i2, :], in1=dal[:, 2 * i2 + 1, :], op=ALU.mult),
                     reads=['dal'], writes=['dtmp'])
                S.op('dve', lambda e: e.tensor_reduce(out=lamv[:, i2:i2 + 1], in_=dtmp[:], axis=AX.X, op=ALU.add),
                     reads=['dtmp'], writes=['lamv'])
            S.op('act', lambda e: e.activation(out=lamv[:, 0:2], in_=lamv[:, 0:2], func=AF.Exp), reads=['lamv'], writes=['lamv'])
            S.op('dve', lambda e: e.tensor_tensor(out=lamv[:, 2:3], in0=lamv[:, 1:2], in1=lamv[:, 0:1], op=ALU.subtract),
                 reads=['lamv'], writes=['lamv'])
            S.op('dve', lambda e: e.tensor_scalar(out=lamv[:, 2:3], in0=lamv[:, 2:3], scalar1=-LAMBDA_INIT, scalar2=None,
                                                  op0=ALU.add), reads=['lamv'], writes=['lamv'])
            S.op('dve', lambda e: e.tensor_scalar(out=lamv[:, 3:4], in0=subg[:], scalar1=1.0 - LAMBDA_INIT, scalar2=None,
                                                  op0=ALU.mult), reads=['lamv', 'subg'], writes=['lamv'])
            nkc = 34
            acnt = 0
            for hh in range(8):
                for t_, c0, k_ in [(wq, hh * 128, 'wq'), (wk, 1024 + hh * 128, 'wk'), (wv, 2048 + hh * 128, 'wv')]:
                    S.dma('pool', t_[:], w_qkv[:, c0:c0 + 128].rearrange("(k p) f -> p k f", p=128), writes=[k_])
                convert_experts(1, 4 * hh, 4 * hh + 4)
                jobs = []
                for ti in range(9):
                    jobs.append(('v', ti))
                    if ti < 4:
                        jobs.append(('q', ti))
                    jobs.append(('k', ti))

                def proj_s1(kj, kind, ti):
                    off, cnt = TILES[ti][0], TILES[ti][1]
                    q = kj % 2
                    pa_ = 2 * q
                    wt, kw_ = {'q': (wq, 'wq'), 'k': (wk, 'wk'), 'v': (wv, 'wv')}[kind]

                    def mmp(e):
                        for kc in range(8):
                            ins = e.matmul(PS[pa_][:, :cnt], lhsT=wt[:, kc, :], rhs=hbuf[:, kc, off:off + cnt],
                                           start=(kc == 0), stop=(kc == 7))
                        return ins
                    S.op('pe', mmp, reads=[kw_, ('h', ti)], writes=[pk(pa_)])
                    S.op('act', lambda e: e.activation(out=qbt[q][:, :cnt], in_=PS[pa_][:, :cnt], func=AF.Identity),
                         reads=[pk(pa_)], writes=[('qbt', q)])

                def proj_s2(kj, kind, ti):
                    off, cnt = TILES[ti][0], TILES[ti][1]
                    q = kj % 2
                    pa_, pb_ = 2 * q, 2 * q + 1
                    if kind == 'v':
                        nsb = cnt // 128
                        pbb = PS[pb_].bitcast(BF16)

                        def mmvt(e):
                            for s in range(nsb):
                                ins = e.transpose(out=pbb[:, s * 128:(s + 1) * 128], in_=qbt[q][:, s * 128:(s + 1) * 128],
                                                  identity=identb[:])
                            return ins
                        S.op('pe', mmvt, reads=[('qbt', q), 'identb'], writes=[pk(pb_)])
                        si0 = off // 128
                        S.op('dve', lambda e: e.tensor_copy(out=Vt[:, si0:si0 + nsb, :].rearrange("p s v -> p (s v)"),
                                                            in_=pbb[:, :nsb * 128]), reads=[pk(pb_)], writes=['Vt'])
                        return
                    dst, kdst = (QT, 'QT') if kind == 'q' else (KT, 'KT')
                    S.op('pe', lambda e: e.matmul(PS[pb_][:, :cnt], lhsT=rmb[:], rhs=qbt[q][:, :cnt], start=True, stop=True),
                         reads=['rmb', ('qbt', q)], writes=[pk(pb_)])
                    S.op('dve', lambda e: e.tensor_tensor(out=rt1[q][:, :cnt], in0=PS[pa_][:, :cnt], in1=cost[:, off:off + cnt],
                                                          op=ALU.mult), reads=[pk(pa_), 'cos', ('qbt', q)], writes=[('rt1', q)])
                    S.op('dve', lambda e: e.tensor_tensor(out=rt2[q][:, :cnt], in0=PS[pb_][:, :cnt], in1=sint[:, off:off + cnt],
                                                          op=ALU.mult), reads=[pk(pb_), 'sin'], writes=[('rt2', q)])
                    S.op('dve', lambda e: e.tensor_tensor(out=dst[:, off:off + cnt], in0=rt1[q][:, :cnt], in1=rt2[q][:, :cnt],
                                                          op=ALU.add), reads=[('rt1', q), ('rt2', q)], writes=[kdst])

                proj_s1(0, *jobs[0])
                for kj in range(len(jobs)):
                    if kj + 1 < len(jobs):
                        proj_s1(kj + 1, *jobs[kj + 1])
                    proj_s2(kj, *jobs[kj])
                for qt in range(4):
                    q0 = qt * 512
                    pend_ = None
                    for kc in range(nkc + 1):
                        if kc < nkc:
                            sidx = acnt % 2
                            acnt += 1
                            sb0 = 2 * sidx
                            for mi in range(2):
                                lo_, hi_ = mi * 64, (mi + 1) * 64
                                S.op('pe', lambda e: e.matmul(PS[sb0 + mi][:, :], lhsT=KT[lo_:hi_, kc * 128:(kc + 1) * 128],
                                                              rhs=QT[lo_:hi_, q0:q0 + 512], start=True, stop=True),
                                     reads=['KT', 'QT'], writes=[pk(sb0 + mi)])
                            ke = ('Eb', sidx)
                            S.op('act', lambda e: e.activation(out=Eb2[sidx][:].rearrange("p m q -> p (m q)"),
                                                               in_=psbig[:, sb0 * 512:(sb0 + 2) * 512], func=AF.Exp, scale=0.125),
                                 reads=[pk(sb0), pk(sb0 + 1)], writes=[ke])
                            cur = (Eb2[sidx], ke)
                        if pend_ is not None:
                            kp, (ebp, kep) = pend_
                            for mi in range(2):
                                S.op('pe', lambda e: e.matmul(PS[4 + mi][:, :], lhsT=Vt[:, kp, :], rhs=ebp[:, mi, :], start=(kp == 0),
                                                              stop=(kp == nkc - 1)), reads=['Vt', kep], writes=[pk(4 + mi)])
                            accv = psbig[:, 6 * 512:8 * 512]
                            ebf = ebp[:].rearrange("p m q -> p (m q)")
                            if kp == 0:
                                S.op('dve', lambda e: e.tensor_copy(out=accv, in_=ebf), reads=[kep], writes=[pk(6), pk(7)])
                            else:
                                S.op('dve', lambda e: e.tensor_tensor(out=accv, in0=accv, in1=ebf, op=ALU.add),
                                     reads=[kep, pk(6), pk(7)], writes=[pk(6), pk(7)])
                        pend_ = (kc, cur) if kc < nkc else None
                    S.op('act', lambda e: e.activation(out=acc2[:].rearrange("p m q -> p (m q)"), in_=psbig[:, 6 * 512:8 * 512],
                                                       func=AF.Identity), reads=[pk(6), pk(7)], writes=['acc2'])
                    for mi in range(2):
                        S.op('pe', lambda e: e.matmul(PS[mi][:, :], lhsT=ones32[:], rhs=acc2[:, mi, :], start=True, stop=True),
                             reads=['ones32', 'acc2'], writes=[pk(mi)])
                    rlf = rlw[:].rearrange("p m q -> p (m q)")
                    S.op('act', lambda e: e.activation(out=rlf, in_=psbig[:, 0:1024], func=AF.Ln), reads=[pk(0), pk(1)], writes=['rlw'])
                    S.op('act', lambda e: e.activation(out=rlf, in_=rlf, func=AF.Exp, scale=-1.0), reads=['rlw'], writes=['rlw'])
                    S.op('dve', lambda e: e.tensor_tensor(out=obw[:].rearrange("p m q -> p (m q)"), in0=psbig[:, 4 * 512:6 * 512],
                                                          in1=rlf, op=ALU.mult),
                         reads=[pk(4), pk(5), 'rlw'], writes=[('ob', 0), ('ob', 1)])
                    S.op('dve', lambda e: e.scalar_tensor_tensor(out=ob[0][:], in0=ob[1][:], scalar=lamv[:, 2:3], in1=ob[0][:],
                                                                 op0=ALU.mult, op1=ALU.add),
                         reads=[('ob', 0), ('ob', 1), 'lamv'], writes=[('ob', 0)])
                    S.op('act', lambda e: e.activation(out=osq[:], in_=ob[0][:], func=AF.Square), reads=[('ob', 0)], writes=['osq'])
                    S.op('pe', lambda e: e.matmul(PS[2][:, :], lhsT=ones32[:], rhs=osq[:], start=True, stop=True),
                         reads=['ones32', 'osq'], writes=[pk(2)])
                    S.op('act', lambda e: e.activation(out=osq[:], in_=PS[2][:, :], func=AF.Ln, bias=eps_t[:, 0:1],
                                                       scale=1.0 / 128.0), reads=[pk(2), 'eps'], writes=['osq'])
                    S.op('act', lambda e: e.activation(out=osq[:], in_=osq[:], func=AF.Exp, scale=-0.5), reads=['osq'], writes=['osq'])
                    a_ = aot[qt % 2]
                    S.op('dve', lambda e: e.scalar_tensor_tensor(out=a_[:], in0=ob[0][:], scalar=lamv[:, 3:4], in1=osq[:],
                                                                 op0=ALU.mult, op1=ALU.mult),
                         reads=[('ob', 0), 'osq', 'lamv'], writes=[('aot', qt % 2)])
                    S.dma('sp', zscr[hh, :, q0:q0 + 512], a_[:], reads=[('aot', qt % 2)], writes=[('zs', qt)])
            S.barrier()
        hst.close()

        phase_C(1, w_o, list(range(4)), xr)
        phase_D(1, list(range(4)), final=True)
        S.barrier()
    return nc


def _prep_shared(inp, rev):
    dsl = slice(None, None, -1) if rev else slice(None)
    f = lambda a: np.ascontiguousarray(np.asarray(a, dtype=np.float32))
    pj = lambda v: f(np.asarray(v).reshape(8, 128).T)
    sh = {}
    sh["w_mod"] = f(inp["w_mod"])
    sh["bmod"] = f(np.asarray(inp["b_mod"]).reshape(2, 48, 128).transpose(2, 0, 1))
    sh["n1g"] = f(np.asarray(inp["norm1_g"]).reshape(2, 8, 128).transpose(2, 0, 1))
    sh["n2g"] = f(np.asarray(inp["norm2_g"]).reshape(2, 8, 128).transpose(2, 0, 1))
    sh["fing"] = pj(inp["final_g"])
    sh["w_in"] = f(inp["rg_w_in"][0])
    cw = np.asarray(inp["rg_conv_w"][0])
    z1 = np.zeros((1, 1024), np.float32)
    cw5 = np.concatenate([cw, z1], 0) if not rev else np.concatenate([z1, cw[::-1]], 0)
    sh["convw"] = f(cw5.reshape(5, 8, 128).transpose(2, 1, 0))
    sh["convb"] = pj(inp["rg_conv_b"][0])
    sh["gaw"] = f(np.asarray(inp["rg_gate_a_w"][0])[dsl])
    sh["gxw"] = f(np.asarray(inp["rg_gate_x_w"][0])[dsl])
    sh["gab"] = f(np.asarray(inp["rg_gate_a_b"][0])[dsl].reshape(2, 8, 128).transpose(2, 0, 1))
    sh["gxb"] = f(np.asarray(inp["rg_gate_x_b"][0])[dsl].reshape(2, 8, 128).transpose(2, 0, 1))
    sh["lam"] = f(np.asarray(inp["rg_lambda"][0])[dsl].reshape(2, 8, 128).transpose(2, 0, 1))
    sh["w_out"] = f(inp["rg_w_out"][0])
    sh["w_qkv"] = f(inp["da_w_qkv"][0])
    sh["dalam"] = f(np.broadcast_to(np.asarray(inp["da_lambda"][0])[None], (128, 4, 64)))
    sh["subg"] = f(np.asarray(inp["da_subln_g"][0]).reshape(128, 1))
    sh["w_o"] = f(inp["da_w_o"][0])
    sh["rw"] = f(np.asarray(inp["router_w"]).reshape(8, 128, 32).transpose(1, 0, 2))
    sh["rb"] = f(np.broadcast_to(np.asarray(inp["router_bias"])[None], (128, 32)))
    sh["wg"] = f(inp["moe_w_gate"])
    sh["wu"] = f(inp["moe_w_up"])
    sh["wd"] = f(inp["moe_w_down"])
    t = np.arange(NX)
    row = (t // 64).astype(np.float32)
    col = (t % 64).astype(np.float32)
    inv = (1.0 / (10000.0 ** (np.arange(16, dtype=np.float32) / 16))).astype(np.float32)
    ang = np.stack([row, col], -1)[:, :, None] * inv
    ang = np.broadcast_to(ang[:, :, None, :], (NX, 2, 2, 16)).reshape(NX, 64).astype(np.float32)
    cos = np.ones((128, NT), np.float32)
    sin = np.zeros((128, NT), np.float32)
    if rev:
        ang = ang[::-1]
    cos[:, :NX] = np.tile(np.cos(ang).T, (2, 1))
    sin[:, :NX] = np.tile(np.sin(ang).T, (2, 1))
    sh["pcol"] = np.arange(128, dtype=np.float32).reshape(128, 1)
    sh["blkoff"] = np.ascontiguousarray(np.broadcast_to((128.0 * np.arange(100, dtype=np.float32))[None], (128, 100)))
    rm = np.zeros((128, 128), np.float32)
    for m in range(128):
        if (m % 32) < 16:
            rm[m + 16, m] = -1.0
        else:
            rm[m - 16, m] = 1.0
    sh["rmat"] = rm
    sh["cos"] = cos
    sh["sin"] = sin
    return sh


def _prep_core(inp, b, rev):
    f = lambda a: np.ascontiguousarray(np.asarray(a, dtype=np.float32))
    x = np.asarray(inp["x"][b])
    ctx = np.asarray(inp["ctx"][b])
    if rev:
        x = x[::-1]
        ctx = ctx[::-1]
    tok = np.concatenate([x, ctx], axis=0)
    d = {"xc": f(tok.T.reshape(8, 128, NT))}
    cond = np.stack([np.asarray(inp["c"][b]), np.asarray(inp["c_ctx"])], -1)
    d["cond"] = f(cond.reshape(8, 128, 2).transpose(1, 0, 2))
    return d


def kernel(**inputs):
    nc = build(DEBUG)
    shs = [_prep_shared(inputs, False), _prep_shared(inputs, True)]
    in_maps = []
    for core in range(8):
        b, rev = core // 2, core % 2
        m = dict(shs[rev])
        m.update(_prep_core(inputs, b, bool(rev)))
        in_maps.append(m)
    res = run_bass_kernel_spmd(nc, in_maps, core_ids=list(range(8)))
    out = np.empty((4, NX, 1024), np.float32)
    for core in range(8):
        b, rev = core // 2, core % 2
        o = np.asarray(res.results[core]["outT"]).reshape(1024, NQ).T
        if rev:
            out[b, NX - NQ:] = o[::-1]
        else:
            out[b, :NQ] = o
    return out
```
